# Optimizing a Trainium2 kernel written in Bass

```python
import math
import jax, jax.numpy as jnp
from jax import lax
import numpy as np

D_MODEL = 2048
BATCH = 8
SEQ = 2048
DEPTH = 1

MLA_HEADS = 8
QK_NOPE_DIM = 128
QK_ROPE_DIM = 64
QK_HEAD_DIM = QK_NOPE_DIM + QK_ROPE_DIM
V_HEAD_DIM = 128
Q_LORA_RANK = 512
KV_LORA_RANK = 256
ROPE_THETA = 10000.0
ATTN_BLOCK = 128
MLA_WIDTH = MLA_HEADS * V_HEAD_DIM

SSD_HEADS = 16
SSD_HEAD_DIM = 64
SSD_D_INNER = SSD_HEADS * SSD_HEAD_DIM
SSD_GROUPS = 2
SSD_STATE = 128
SSD_CONV = 4
SSD_CHUNK = 128
SSD_CONV_DIM = SSD_D_INNER + 2 * SSD_GROUPS * SSD_STATE

MIX_WIDTH = MLA_WIDTH + SSD_D_INNER

IN_SPLITS = (
    Q_LORA_RANK,
    Q_LORA_RANK + KV_LORA_RANK,
    Q_LORA_RANK + KV_LORA_RANK + QK_ROPE_DIM,
    Q_LORA_RANK + KV_LORA_RANK + QK_ROPE_DIM + SSD_D_INNER,
    Q_LORA_RANK + KV_LORA_RANK + QK_ROPE_DIM + SSD_D_INNER + SSD_CONV_DIM,
)
IN_COLS = Q_LORA_RANK + KV_LORA_RANK + QK_ROPE_DIM + SSD_D_INNER + SSD_CONV_DIM + SSD_HEADS

PEER_HEADS = 8
PEER_KEYS = 128
PEER_EXPERTS = PEER_KEYS * PEER_KEYS
PEER_TOPK = 16
PEER_KEY_DIM = 256
PEER_HALF = PEER_KEY_DIM // 2
PEER_TOKEN_BLOCK = 128

N_MOD = 6
EPS = 1e-6

kernel_name = "hybrid_mla_ssd_peer_adaln_block"


def rms_norm(x, gain=None):
    xf = x.astype(jnp.float32)
    y = xf * lax.rsqrt(jnp.mean(xf * xf, axis=-1, keepdims=True) + EPS)
    if gain is not None:
        y = y * gain.astype(jnp.float32)
    return y.astype(x.dtype)


def rope(x, positions):
    r = x.shape[-1]
    half = r // 2
    inv_freq = 1.0 / (ROPE_THETA ** (jnp.arange(half, dtype=jnp.float32) * (2.0 / r)))
    ang = positions.astype(jnp.float32)[:, :, None, None] * inv_freq
    cos, sin = jnp.cos(ang), jnp.sin(ang)
    xf = x.astype(jnp.float32)
    x1, x2 = xf[..., :half], xf[..., half:]
    return jnp.concatenate([x1 * cos - x2 * sin, x2 * cos + x1 * sin], axis=-1).astype(x.dtype)


def mla_attention(c_q, c_kv, k_rope, positions, q_a_norm, w_uq, kv_a_norm, w_ukv, q_norm, k_norm):
    b, s, _ = c_q.shape
    q = (rms_norm(c_q, q_a_norm) @ w_uq).reshape(b, s, MLA_HEADS, QK_HEAD_DIM)
    kv = (rms_norm(c_kv, kv_a_norm) @ w_ukv).reshape(b, s, MLA_HEADS, QK_NOPE_DIM + V_HEAD_DIM)
    k_nope, v = kv[..., :QK_NOPE_DIM], kv[..., QK_NOPE_DIM:]
    k_pe = jnp.broadcast_to(k_rope[:, :, None, :], (b, s, MLA_HEADS, QK_ROPE_DIM))
    k = jnp.concatenate([k_nope, k_pe], axis=-1)
    q = rms_norm(q, q_norm)
    k = rms_norm(k, k_norm)
    q = jnp.concatenate([q[..., :QK_NOPE_DIM], rope(q[..., QK_NOPE_DIM:], positions)], axis=-1)
    k = jnp.concatenate([k[..., :QK_NOPE_DIM], rope(k[..., QK_NOPE_DIM:], positions)], axis=-1)
    q = q.transpose(0, 2, 1, 3)
    k = k.transpose(0, 2, 1, 3)
    v = v.transpose(0, 2, 1, 3)
    scale = QK_HEAD_DIM ** -0.5
    outs = []
    for i in range(s // ATTN_BLOCK):
        lo = i * ATTN_BLOCK
        hi = lo + ATTN_BLOCK
        qb = q[:, :, lo:hi]
        kb = k[:, :, :hi]
        vb = v[:, :, :hi]
        sc = jnp.einsum('bhqd,bhkd->bhqk', qb, kb).astype(jnp.float32) * scale
        mask = jnp.arange(hi)[None, :] <= (lo + jnp.arange(ATTN_BLOCK))[:, None]
        sc = jnp.where(mask, sc, -jnp.inf)
        p = jax.nn.softmax(sc, axis=-1).astype(v.dtype)
        outs.append(jnp.einsum('bhqk,bhkd->bqhd', p, vb))
    o = jnp.concatenate(outs, axis=1)
    return o.reshape(b, s, MLA_WIDTH)


def causal_depthwise_conv(u, w, bias):
    ch = u.shape[-1]
    y = lax.conv_general_dilated(
        u, w[:, None, :].astype(u.dtype), window_strides=(1,), padding=[(SSD_CONV - 1, 0)],
        dimension_numbers=('NWC', 'WIO', 'NWC'), feature_group_count=ch)
    return y + bias


def segsum(a):
    n = a.shape[-1]
    cs = jnp.cumsum(a, axis=-1)
    diff = cs[..., :, None] - cs[..., None, :]
    mask = jnp.tril(jnp.ones((n, n), dtype=bool))
    return jnp.where(mask, diff, -jnp.inf)


def ssd_chunked(x, dt, a, bmat, cmat):
    b, s, h, p = x.shape
    n = bmat.shape[-1]
    nc = s // SSD_CHUNK
    xd = (x.astype(jnp.float32) * dt[..., None]).reshape(b, nc, SSD_CHUNK, h, p)
    a_dt = (dt * a).reshape(b, nc, SSD_CHUNK, h).transpose(0, 3, 1, 2)
    bc = bmat.astype(jnp.float32).reshape(b, nc, SSD_CHUNK, h, n)
    cc = cmat.astype(jnp.float32).reshape(b, nc, SSD_CHUNK, h, n)
    a_cs = jnp.cumsum(a_dt, axis=-1)
    decay = jnp.exp(segsum(a_dt))
    cb = jnp.einsum('bclhn,bcshn->bhcls', cc, bc) * decay
    y_diag = jnp.einsum('bhcls,bcshp->bclhp', cb, xd)
    decay_states = jnp.exp(a_cs[..., -1:] - a_cs)
    states = jnp.einsum('bclhn,bhcl,bclhp->bchpn', bc, decay_states, xd)
    chunk_decay = jnp.exp(a_cs[..., -1])

    def step(prev, inp):
        st, dec = inp
        return prev * dec[..., None, None] + st, prev

    init = jnp.zeros((b, h, p, n), jnp.float32)
    _, prev_states = lax.scan(step, init, (states.transpose(1, 0, 2, 3, 4), chunk_decay.transpose(2, 0, 1)))
    prev_states = prev_states.transpose(1, 0, 2, 3, 4)
    y_off = jnp.einsum('bclhn,bchpn,bhcl->bclhp', cc, prev_states, jnp.exp(a_cs))
    return (y_diag + y_off).reshape(b, s, h, p)


def ssd_mixer(z, xbc, dt_raw, conv_w, conv_b, dt_bias, a_log, d_skip, ssd_norm):
    b, s, _ = z.shape
    xbc = jax.nn.silu(causal_depthwise_conv(xbc, conv_w, conv_b))
    gn = SSD_GROUPS * SSD_STATE
    xs = xbc[..., :SSD_D_INNER].reshape(b, s, SSD_HEADS, SSD_HEAD_DIM)
    bm = xbc[..., SSD_D_INNER:SSD_D_INNER + gn].reshape(b, s, SSD_GROUPS, SSD_STATE)
    cm = xbc[..., SSD_D_INNER + gn:].reshape(b, s, SSD_GROUPS, SSD_STATE)
    rep = SSD_HEADS // SSD_GROUPS
    bm = jnp.repeat(bm, rep, axis=2)
    cm = jnp.repeat(cm, rep, axis=2)
    dt = jax.nn.softplus(dt_raw.astype(jnp.float32) + dt_bias.astype(jnp.float32))
    a = -jnp.exp(a_log.astype(jnp.float32))
    y = ssd_chunked(xs, dt, a, bm, cm) + xs.astype(jnp.float32) * d_skip.astype(jnp.float32)[:, None]
    y = y.reshape(b, s, SSD_D_INNER) * jax.nn.silu(z.astype(jnp.float32))
    gs = SSD_D_INNER // SSD_GROUPS
    y = rms_norm(y.reshape(b, s, SSD_GROUPS, gs), ssd_norm.reshape(SSD_GROUPS, gs))
    return y.reshape(b, s, SSD_D_INNER).astype(z.dtype)


def peer_ffn(h, w_query, sub_keys, u_experts, v_experts):
    b, s, d = h.shape
    t = b * s
    ht = h.reshape(t, d)
    q = (ht @ w_query).reshape(t, PEER_HEADS, 2, PEER_HALF)
    sc = jnp.einsum('thjd,hjkd->thjk', q, sub_keys).astype(jnp.float32)
    top_s, top_i = lax.top_k(sc, PEER_TOPK)
    cand_s = top_s[:, :, 0, :, None] + top_s[:, :, 1, None, :]
    cand_i = top_i[:, :, 0, :, None] * PEER_KEYS + top_i[:, :, 1, None, :]
    kk = PEER_TOPK * PEER_TOPK
    best_s, best_pos = lax.top_k(cand_s.reshape(t, PEER_HEADS, kk), PEER_TOPK)
    expert_idx = jnp.take_along_axis(cand_i.reshape(t, PEER_HEADS, kk), best_pos, axis=-1)
    gates = jax.nn.softmax(best_s, axis=-1)

    def block(args):
        hb, ib, gb = args
        u = jnp.take(u_experts, ib, axis=0)
        pre = jnp.einsum('thkd,td->thk', u, hb).astype(jnp.float32)
        act = (jax.nn.gelu(pre, approximate=False) * gb).astype(hb.dtype)
        vsel = jnp.take(v_experts, ib, axis=0)
        return jnp.einsum('thk,thkd->td', act, vsel)

    nb = t // PEER_TOKEN_BLOCK
    out = lax.map(block, (ht.reshape(nb, PEER_TOKEN_BLOCK, d),
                          expert_idx.reshape(nb, PEER_TOKEN_BLOCK, PEER_HEADS, PEER_TOPK),
                          gates.reshape(nb, PEER_TOKEN_BLOCK, PEER_HEADS, PEER_TOPK)))
    return out.reshape(b, s, d)


def setup_inputs(seed: int = 0) -> dict:
    key = jax.random.key(seed)
    ks = jax.random.split(key, 24)
    f32 = jnp.float32
    L = DEPTH

    def nrm(k, shape, scale):
        return jax.random.normal(k, shape, f32) * scale

    def gain(k, shape):
        return 1.0 + 0.02 * jax.random.normal(k, shape, f32)

    x = nrm(ks[0], (BATCH, SEQ, D_MODEL), 1.0)
    c = nrm(ks[1], (BATCH, D_MODEL), 1.0)
    offs = jax.random.randint(ks[2], (BATCH, 1), 0, 1024, dtype=jnp.int32)
    positions = jnp.arange(SEQ, dtype=jnp.int32)[None, :] + offs
    w_ada = nrm(ks[3], (L, D_MODEL, N_MOD * D_MODEL), 0.5 * D_MODEL ** -0.5)
    b_ada = nrm(ks[4], (L, N_MOD * D_MODEL), 0.02)
    w_in = nrm(ks[5], (L, D_MODEL, IN_COLS), D_MODEL ** -0.5)
    q_a_norm = gain(ks[6], (L, Q_LORA_RANK))
    w_uq = nrm(ks[7], (L, Q_LORA_RANK, MLA_HEADS * QK_HEAD_DIM), Q_LORA_RANK ** -0.5)
    kv_a_norm = gain(ks[8], (L, KV_LORA_RANK))
    w_ukv = nrm(ks[9], (L, KV_LORA_RANK, MLA_HEADS * (QK_NOPE_DIM + V_HEAD_DIM)), KV_LORA_RANK ** -0.5)
    q_norm = gain(ks[10], (L, QK_HEAD_DIM))
    k_norm = gain(ks[11], (L, QK_HEAD_DIM))
    attn_out_norm = gain(ks[12], (L, MLA_WIDTH))
    conv_w = nrm(ks[13], (L, SSD_CONV, SSD_CONV_DIM), SSD_CONV ** -0.5)
    conv_b = nrm(ks[14], (L, SSD_CONV_DIM), 0.02)
    dt0 = jnp.exp(jax.random.uniform(ks[15], (L, SSD_HEADS), f32, math.log(1e-3), math.log(1e-1)))
    dt_bias = dt0 + jnp.log(-jnp.expm1(-dt0))
    a_log = jnp.log(jax.random.uniform(ks[16], (L, SSD_HEADS), f32, 1.0, 16.0))
    d_skip = gain(ks[17], (L, SSD_HEADS))
    ssd_norm = gain(ks[18], (L, SSD_D_INNER))
    w_out = nrm(ks[19], (L, MIX_WIDTH, D_MODEL), MIX_WIDTH ** -0.5)
    w_query = nrm(ks[20], (L, D_MODEL, PEER_HEADS * PEER_KEY_DIM), D_MODEL ** -0.5)
    sub_keys = nrm(ks[21], (L, PEER_HEADS, 2, PEER_KEYS, PEER_HALF), PEER_HALF ** -0.5)
    u_experts = nrm(ks[22], (L, PEER_EXPERTS, D_MODEL), D_MODEL ** -0.5)
    v_experts = nrm(ks[23], (L, PEER_EXPERTS, D_MODEL), 0.5)
    return {"x": x, "c": c, "positions": positions, "w_ada": w_ada, "b_ada": b_ada, "w_in": w_in,
            "q_a_norm": q_a_norm, "w_uq": w_uq, "kv_a_norm": kv_a_norm, "w_ukv": w_ukv,
            "q_norm": q_norm, "k_norm": k_norm, "attn_out_norm": attn_out_norm,
            "conv_w": conv_w, "conv_b": conv_b, "dt_bias": dt_bias, "a_log": a_log,
            "d_skip": d_skip, "ssd_norm": ssd_norm, "w_out": w_out, "w_query": w_query,
            "sub_keys": sub_keys, "u_experts": u_experts, "v_experts": v_experts}


def reference(x, c, positions, w_ada, b_ada, w_in, q_a_norm, w_uq, kv_a_norm, w_ukv, q_norm, k_norm,
              attn_out_norm, conv_w, conv_b, dt_bias, a_log, d_skip, ssd_norm, w_out, w_query,
              sub_keys, u_experts, v_experts):
    c_act = jax.nn.silu(c)
    for l in range(DEPTH):
        mod = (c_act @ w_ada[l] + b_ada[l])[:, None, :]
        sh1, sc1, g1, sh2, sc2, g2 = jnp.split(mod, N_MOD, axis=-1)
        h = rms_norm(x) * (1.0 + sc1) + sh1
        proj = h @ w_in[l]
        c_q, c_kv, k_rope, z, xbc, dt_raw = jnp.split(proj, IN_SPLITS, axis=-1)
        attn = mla_attention(c_q, c_kv, k_rope, positions, q_a_norm[l], w_uq[l], kv_a_norm[l], w_ukv[l],
                             q_norm[l], k_norm[l])
        attn = rms_norm(attn, attn_out_norm[l])
        ssm = ssd_mixer(z, xbc, dt_raw, conv_w[l], conv_b[l], dt_bias[l], a_log[l], d_skip[l], ssd_norm[l])
        mix = jnp.concatenate([attn, ssm], axis=-1) @ w_out[l]
        x = x + g1 * mix
        h2 = rms_norm(x) * (1.0 + sc2) + sh2
        x = x + g2 * peer_ffn(h2, w_query[l], sub_keys[l], u_experts[l], v_experts[l])
    return x
```

```python
from contextlib import ExitStack
import os
import numpy as np
import concourse.bass as bass
import concourse.mybir as mybir
from concourse.bass_utils import run_bass_kernel_spmd

F32 = mybir.dt.float32
BF16 = mybir.dt.bfloat16
I32 = mybir.dt.int32
AF = mybir.ActivationFunctionType
ALU = mybir.AluOpType
AX = mybir.AxisListType

D = 2048
S = 2048
NB = 16
EPS = 1e-6
ENGS = ("pe", "act", "dve", "pool", "sp")
SEM_EPOCH = 20000


class Op:
    __slots__ = ("eng", "fn", "reads", "writes", "dma", "semkey", "deps", "waits", "signal", "idx", "barrier")

    def __init__(self, eng, fn, reads, writes, dma, semkey):
        self.eng, self.fn = eng, fn
        self.reads, self.writes = tuple(reads), tuple(writes)
        self.dma, self.semkey = dma, semkey
        self.deps, self.waits, self.signal = (), [], None
        self.barrier = False


class Prog:
    def __init__(self, nc):
        self.nc = nc
        self.ops = []
        self.self_sync = ("dve", "act", "pool")

    def op(self, eng, fn, reads=(), writes=()):
        xr = [k for k in reads if isinstance(k, str) and k[:2] in ("pf", "pt") and k not in writes]
        writes = tuple(writes) + tuple(xr)
        o = Op(eng, fn, reads, writes, False, None)
        self.ops.append(o)
        return o

    def dma(self, eng, fn, reads=(), writes=(), semkey=None):
        if semkey is None:
            semkey = ("dma",) + (tuple(writes) if writes else tuple(reads))
        o = Op(eng, fn, reads, writes, True, semkey)
        self.ops.append(o)
        return o

    def barrier(self, fn):
        o = Op("act", fn, (), (), False, None)
        o.barrier = True
        self.ops.append(o)
        return o

    def _analyze(self):
        last_write, readers = {}, {}
        last_on = {}
        dmas_since = []
        last_barrier = None
        for i, o in enumerate(self.ops):
            o.idx = i
            deps = set()
            if o.barrier:
                deps.update(last_on.values())
                deps.update(dmas_since)
                dmas_since = []
                if last_barrier is not None:
                    deps.add(last_barrier)
                last_barrier = i
            elif last_barrier is not None:
                deps.add(last_barrier)
            last_on[o.eng] = i
            if o.dma:
                dmas_since.append(i)
            for t in o.reads:
                if t in last_write:
                    deps.add(last_write[t])
            for t in o.writes:
                if t in last_write:
                    deps.add(last_write[t])
                deps.update(readers.get(t, ()))
            deps.discard(i)
            o.deps = deps
            for t in o.reads:
                readers.setdefault(t, []).append(i)
            for t in o.writes:
                last_write[t] = i
                readers[t] = []
        need = set()
        for o in self.ops:
            for d in o.deps:
                od = self.ops[d]
                if od.dma or od.eng != o.eng or o.eng in self.self_sync:
                    need.add(d)
        cnt = {e: 0 for e in ENGS}
        dcnt = {}
        self.semkeys = []
        for o in self.ops:
            if o.dma:
                k = dcnt.get(o.semkey, 0) + 1
                dcnt[o.semkey] = k
                if k == 1:
                    self.semkeys.append(o.semkey)
                o.signal = (("d", o.semkey), 16 * k, 16)
            elif o.idx in need:
                cnt[o.eng] += 1
                ep, v = divmod(cnt[o.eng] - 1, SEM_EPOCH)
                o.signal = (("e", o.eng, ep), v + 1, 1)
        self.nepochs = {e: (cnt[e] + SEM_EPOCH - 1) // SEM_EPOCH for e in ENGS}
        for o in self.ops:
            w = {}
            for d in o.deps:
                od = self.ops[d]
                if od.dma or od.eng != o.eng or o.eng in self.self_sync:
                    s, v, _ = od.signal
                    if w.get(s, 0) < v:
                        w[s] = v
            o.waits = sorted(w.items(), key=lambda kv: str(kv[0]))

    def emit(self):
        nc = self.nc
        self._analyze()
        with ExitStack() as es:
            sems = {}
            n = 0
            for e in ENGS:
                for ep in range(self.nepochs[e]):
                    sems[("e", e, ep)] = es.enter_context(nc.semaphore(f"s_{e}{ep}"))
                    n += 1
            for k in self.semkeys:
                sems[("d", k)] = es.enter_context(nc.semaphore(f"d{n}"))
                n += 1
            self.nsems = n
            by_eng = {e: [o for o in self.ops if o.eng == e] for e in ENGS}

            def run(engine, name):
                waited = {}
                for o in by_eng[name]:
                    for s, v in o.waits:
                        if waited.get(s, 0) < v:
                            engine.wait_ge(sems[s], v)
                            waited[s] = v
                    ins = o.fn(engine)
                    if o.signal is not None:
                        s, v, inc = o.signal
                        ins.then_inc(sems[s], inc)

            with nc.Block() as block:
                @block.tensor
                def _(e):
                    run(e, "pe")

                @block.scalar
                def _(e):
                    run(e, "act")

                @block.vector
                def _(e):
                    run(e, "dve")

                @block.gpsimd
                def _(e):
                    run(e, "pool")

                @block.sync
                def _(e):
                    run(e, "sp")


def bcast(ap, shape):
    return ap.to_broadcast(list(shape))


def build(stop_after=None, debug=False, nblk=NB, ngroups=2):
    nc = bass.Bass("TRN2", target_bir_lowering=False)
    P = Prog(nc)

    def din(name, shape, dt=F32):
        return nc.dram_tensor(name, list(shape), dt, kind="ExternalInput").ap()

    x_d = din("x", [S, D]); c_d = din("c", [D]); pos_d = din("pos", [S, 1], I32)
    w_ada = din("w_ada", [D, 6 * D]); b_ada = din("b_ada", [1, 6 * D]); w_in = din("w_in", [D, 3408])
    q_a_norm = din("q_a_norm", [512]); w_uq = din("w_uq", [512, 1536]); kv_a_norm = din("kv_a_norm", [256])
    w_ukv = din("w_ukv", [256, 2048]); q_norm = din("q_norm", [192]); k_norm = din("k_norm", [192])
    attn_out_norm = din("attn_out_norm", [1024]); conv_w = din("conv_w", [4, 1536]); conv_b = din("conv_b", [1536])
    dt_bias = din("dt_bias", [16]); a_log = din("a_log", [16]); d_skip = din("d_skip", [16])
    ssd_norm = din("ssd_norm", [1024]); w_out = din("w_out", [D, D]); w_query = din("w_query", [D, D])
    sub_keys = din("sub_keys", [16, 128, 128])
    if stop_after in (None, "pdbg", "pdbg2"):
        u_exp = din("u_experts", [16384, D]); v_exp = din("v_experts", [16384, D])
    ident_d = din("ident", [128, 128]); tri_d = din("tri", [128, 128]); negm_d = din("negmask", [128, 128])
    invf_d = din("invfreq", [128, 32])
    out_d = nc.dram_tensor("out", [S, D], F32, kind="ExternalOutput").ap()
    mod_d = nc.dram_tensor("mod_scr", [6 * D], F32, kind="Internal").ap()
    x1_d = nc.dram_tensor("x1_scr", [S, D], F32, kind=("ExternalOutput" if stop_after == "x1" else "Internal")).ap()
    dbg = {}
    conv_pending = []
    if stop_after in (None, "pdbg", "pdbg2"):
        u_bf = nc.dram_tensor("u_bf", [16384, D], BF16, kind="Internal").ap()
        v_bf = nc.dram_tensor("v_bf", [16384, D], BF16, kind="Internal").ap()
        for r0 in range(0, 16384, 1024):
            conv_pending.append((u_bf, u_exp, r0, "utab"))
            conv_pending.append((v_bf, v_exp, r0, "vtab"))

    def emit_conv(n):
        for _ in range(min(n, len(conv_pending))):
            dst, src, r0, key = conv_pending.pop(0)
            P.dma("pool", lambda e, dst=dst, src=src, r0=r0: e.dma_start(out=dst[r0:r0 + 1024, :], in_=src[r0:r0 + 1024, :]),
                  writes=[key], semkey=("conv", key))

    def dbg_out(nm, shp, dt=F32):
        dbg[nm] = nc.dram_tensor(nm, list(shp), dt, kind="ExternalOutput").ap()
        return dbg[nm]

    def finish(keys):
        P.op("sp", lambda e: None, reads=keys)
        P.emit()
        return nc

    top = ExitStack()

    def sb(es, name, shape, dt=F32):
        return es.enter_context(nc.sbuf_tensor(name, list(shape), dt))

    pf = [top.enter_context(nc.psum_tensor(f"pf{i}", [128, 512], F32)) for i in range(6)]
    pt = [top.enter_context(nc.psum_tensor(f"pt{i}", [128, 1024], BF16)) for i in range(2)]

    ident_f = sb(top, "ident_f", [128, 128]); ident_b = sb(top, "ident_b", [128, 128], BF16)
    tri_f = sb(top, "tri_f", [128, 128]); tri_b = sb(top, "tri_b", [128, 128], BF16)
    ones_b = sb(top, "ones_b", [128, 128], BF16); negm = sb(top, "negm", [128, 128])
    invf = sb(top, "invf", [128, 32]); bar = sb(top, "bar", [128, 2])
    BAR = lambda: P.barrier(lambda e: e.copy(out=bar[:, 0:1], in_=bar[:, 1:2]))
    modT = sb(top, "modT", [128, 96]); scp1 = sb(top, "scp1", [128, 16]); scp2 = sb(top, "scp2", [128, 16])

    P.dma("sp", lambda e: e.dma_start(out=ident_f[:], in_=ident_d), writes=["ident_f"])
    P.dma("sp", lambda e: e.dma_start(out=tri_f[:], in_=tri_d), writes=["tri_f"])
    P.dma("sp", lambda e: e.dma_start(out=negm[:], in_=negm_d), writes=["negm"])
    P.dma("sp", lambda e: e.dma_start(out=invf[:], in_=invf_d), writes=["invf"])
    P.op("dve", lambda e: e.tensor_copy(out=ident_b[:], in_=ident_f[:]), reads=["ident_f"], writes=["ident_b"])
    P.op("dve", lambda e: e.tensor_copy(out=tri_b[:], in_=tri_f[:]), reads=["tri_f"], writes=["tri_b"])
    P.op("dve", lambda e: e.memset(ones_b[:], 1.0), writes=["ones_b"])
    P.op("act", lambda e: e.activation(out=bar[:], in_=invf[:, 0:2], func=AF.Copy), reads=["invf"], writes=["bar"])

    with ExitStack() as sa:
        cT = sb(sa, "cT", [128, 16]); cab = sb(sa, "cab", [128, 16], BF16)
        brow = sb(sa, "brow", [1, 6 * D]); modrow = sb(sa, "modrow", [1, 6 * D])
        wa = [sb(sa, f"wa{i}", [128, 16, 512], BF16) for i in range(2)]
        with nc.allow_non_contiguous_dma(reason="tiny transposed loads"):
            P.dma("sp", lambda e: e.dma_start(out=cT[:], in_=c_d.rearrange("(k p) -> p k", p=128), allow_slow_non_contiguous=True), writes=["cT"])
        P.dma("sp", lambda e: e.dma_start(out=brow[:], in_=b_ada), writes=["brow"])
        P.op("act", lambda e: e.activation(out=cab[:], in_=cT[:], func=AF.Silu), reads=["cT"], writes=["cab"])
        for j in range(24):
            w = wa[j % 2]
            P.dma("pool", lambda e, w=w, j=j: e.dma_start(
                out=w[:], in_=w_ada[:, j * 512:(j + 1) * 512].rearrange("(k p) n -> p k n", p=128)),
                writes=[f"wa{j % 2}"])

            def mmA(e, w=w):
                for k in range(16):
                    r = e.matmul(pf[0][0:1, :], lhsT=cab[:, k:k + 1], rhs=w[:, k, :], start=(k == 0), stop=(k == 15))
                return r
            P.op("pe", mmA, reads=["cab", f"wa{j % 2}"], writes=["pf0"])
            P.op("dve", lambda e, j=j: e.tensor_tensor(out=modrow[0:1, j * 512:(j + 1) * 512], in0=pf[0][0:1, :],
                                                       in1=brow[0:1, j * 512:(j + 1) * 512], op=ALU.add),
                 reads=["pf0", "brow"], writes=["modrow"])
        P.dma("sp", lambda e: e.dma_start(out=mod_d.rearrange("(o n) -> o n", o=1), in_=modrow[:]),
              reads=["modrow"], writes=["mod_d"])
        with nc.allow_non_contiguous_dma(reason="tiny transposed loads"):
            P.dma("sp", lambda e: e.dma_start(out=modT[:], in_=mod_d.rearrange("(j p) -> p j", p=128), allow_slow_non_contiguous=True),
                  reads=["mod_d"], writes=["modT"])
        P.op("dve", lambda e: e.tensor_scalar(out=scp1[:], in0=modT[:, 16:32], scalar1=1.0, scalar2=None, op0=ALU.add),
             reads=["modT"], writes=["scp1"])
        P.op("dve", lambda e: e.tensor_scalar(out=scp2[:], in0=modT[:, 64:80], scalar1=1.0, scalar2=None, op0=ALU.add),
             reads=["modT"], writes=["scp2"])
        if stop_after == "A":
            o1 = dbg_out("d_mod", [1, 6 * D]); o2 = dbg_out("d_modT", [128, 96])
            P.dma("sp", lambda e: e.dma_start(out=o1, in_=modrow[:]), reads=["modrow"], writes=["o1"])
            P.dma("sp", lambda e: e.dma_start(out=o2, in_=modT[:]), reads=["modT"], writes=["o2"])
            return finish(["o1", "o2"])
    BAR()
    sh1 = modT[:, 0:16]
    sh2 = modT[:, 48:64]

    xt = [sb(top, f"xt{i}", [128, D]) for i in range(1)]
    junk = sb(top, "junk", [128, D], BF16)
    xn = sb(top, "xn", [128, D], BF16)
    st4 = sb(top, "st4", [128, 4])
    hT = sb(top, "hT", [128, 16, 128], BF16)
    cnt = {"x": 0}
    scat = ExitStack()
    catT = sb(scat, "catT", [128, 16, S], BF16)

    def rstd_of(ss_ap, n, out_ap, rkeys, wkey, tmp_ap, tmpkey):
        P.op("act", lambda e: e.activation(out=tmp_ap, in_=ss_ap, func=AF.Sqrt, scale=1.0 / n, bias=EPS),
             reads=rkeys, writes=[tmpkey])
        P.op("dve", lambda e: e.reciprocal(out=out_ap, in_=tmp_ap), reads=[tmpkey], writes=[wkey])

    def make_hT(src_d, i, scp, sh, keep_tokmajor=None):
        s = cnt["x"] % 1
        cnt["x"] += 1
        t = xt[s]
        P.dma("sp", lambda e: e.dma_start(out=t[:], in_=src_d[i * 128:(i + 1) * 128, :]), writes=[f"xt{s}"])
        P.op("act", lambda e: e.activation(out=junk[:], in_=t[:], func=AF.Square, accum_out=st4[:, 0:1]),
             reads=[f"xt{s}"], writes=["junk", "st_ss"])
        rstd_of(st4[:, 0:1], D, st4[:, 2:3], ["st_ss"], "st_r", st4[:, 1:2], "st_t")
        P.op("dve", lambda e: e.tensor_scalar(out=xn[:], in0=t[:], scalar1=st4[:, 2:3], scalar2=None, op0=ALU.mult),
             reads=[f"xt{s}", "st_r"], writes=["xn"])
        HTM = os.environ.get("HT_MODE", "")
        if HTM == "xn":
            o_ = dbg_out("d_xn", [128, D], BF16)
            P.dma("sp", lambda e, o_=o_: e.dma_start(out=o_, in_=xn[:]), reads=["xn"], writes=["d_xn"])
            raise StopIteration
        for half in range(2):
            def trs(e, half=half):
                for k in range(8):
                    kk = half * 8 + k
                    r = e.transpose(pt[half][:, k * 128:(k + 1) * 128], xn[:, kk * 128:(kk + 1) * 128], ident_b[:])
                return r
            P.op("pe", trs, reads=["xn", "ident_b"], writes=[f"pt{half}"])
            for k in range(8):
                kk = half * 8 + k
                if (k % 2 == 0 and HTM != "act") or HTM == "dve":
                    P.op("dve", lambda e, kk=kk, k=k, half=half: e.tensor_scalar(
                        out=hT[:, kk, :], in0=pt[half][:, k * 128:(k + 1) * 128], scalar1=scp[:, kk:kk + 1],
                        scalar2=sh[:, kk:kk + 1], op0=ALU.mult, op1=ALU.add),
                        reads=[f"pt{half}", "scp1", "scp2", "modT"], writes=[f"hT{kk}"])
                else:
                    P.op("act", lambda e, kk=kk, k=k, half=half: e.activation(
                        out=hT[:, kk, :], in_=pt[half][:, k * 128:(k + 1) * 128], func=AF.Identity,
                        scale=scp[:, kk:kk + 1], bias=sh[:, kk:kk + 1]),
                        reads=[f"pt{half}", "scp1", "scp2", "modT"], writes=[f"hT{kk}"])
        return s

    hT_keys = [f"hT{k}" for k in range(16)]

    def load_w(es, name, src_ap, kchunks, ncols, eng="pool"):
        t = sb(es, name, [128, kchunks, ncols], BF16)
        step = max(1, 4096 // ncols)
        for k0 in range(0, kchunks, step):
            k1 = min(kchunks, k0 + step)
            P.dma(eng, lambda e, k0=k0, k1=k1: e.dma_start(
                out=t[:, k0:k1, :], in_=src_ap[k0 * 128:k1 * 128, :].rearrange("(k p) n -> p k n", p=128)),
                writes=[f"{name}_{k0}"], semkey=("w", name, k0))
        return t, [f"{name}_{k0}" for k0 in range(0, kchunks, step)]

    def row_tile(es, name, src_1d, n):
        t = sb(es, name, [128, n])
        P.dma("sp", lambda e: e.dma_start(out=t[:], in_=src_1d.partition_broadcast(128)), writes=[name])
        return t

    with ExitStack() as s1:
        win_a, win_a_k = load_w(s1, "win_a", w_in[:, 0:832], 16, 832)
        wuq, wuq_k = load_w(s1, "wuq", w_uq, 4, 1536)
        wukv, wukv_k = load_w(s1, "wukv", w_ukv, 2, 2048)
        qag = row_tile(s1, "qag", q_a_norm, 512); kvag = row_tile(s1, "kvag", kv_a_norm, 256)
        qg = row_tile(s1, "qg", q_norm, 192); kg = row_tile(s1, "kg", k_norm, 192)
        KTn = sb(s1, "KTn", [128, 4, S], BF16); KTr = sb(s1, "KTr", [128, 2, S], BF16)
        Vc = sb(s1, "Vc", [128, NB, 512], BF16)
        cqn = sb(s1, "cqn", [128, 512], BF16); ckvn = sb(s1, "ckvn", [128, 256], BF16)
        cqnT = sb(s1, "cqnT", [128, 4, 128], BF16); ckvnT = sb(s1, "ckvnT", [128, 2, 128], BF16)
        kr = sb(s1, "kr", [128, 64]); krr = sb(s1, "krr", [128, 64])
        qraw = sb(s1, "qraw", [128, 4, 192]); kvraw = sb(s1, "kvraw", [128, 4, 256])
        sq = sb(s1, "sq", [128, 1024]); sm = sb(s1, "sm", [128, 32])
        posi = sb(s1, "posi", [128, 1], I32); posf = sb(s1, "posf", [128, 1])
        ang = sb(s1, "ang", [128, 64]); angk = sb(s1, "angk", [128, 64]); angi = sb(s1, "angi", [128, 64], I32)
        cs = sb(s1, "cs", [128, 64])
        rt = sb(s1, "rt", [128, 4, 32, 4])
        qbn = sb(s1, "qbn", [128, 4, 128], BF16); qbr = sb(s1, "qbr", [128, 4, 64], BF16)
        kbn = sb(s1, "kbn", [128, 4, 128], BF16); kbr = sb(s1, "kbr", [128, 4, 64], BF16)
        QTn = sb(s1, "QTn", [128, 4, 128], BF16); QTr = sb(s1, "QTr", [128, 2, 128], BF16)
        PT = [sb(s1, f"PT{i}", [128, 512], BF16) for i in range(2)]
        rden = sb(s1, "rden", [128, 128])

        def rope(src3, dst3, nh, rk, wk):
            cosb = bcast(cs[:, 0:32].unsqueeze(1), [128, nh, 32])
            sinb = bcast(cs[:, 32:64].unsqueeze(1), [128, nh, 32])
            x1, x2 = src3[:, :, 0:32], src3[:, :, 32:64]
            t = [rt[:, 0:nh, :, j] for j in range(4)]
            P.op("dve", lambda e: e.tensor_tensor(out=t[0], in0=x1, in1=cosb, op=ALU.mult), reads=rk + ["cs"], writes=["rt0"])
            P.op("dve", lambda e: e.tensor_tensor(out=t[1], in0=x2, in1=sinb, op=ALU.mult), reads=rk + ["cs"], writes=["rt1"])
            P.op("dve", lambda e: e.tensor_tensor(out=t[2], in0=x2, in1=cosb, op=ALU.mult), reads=rk + ["cs"], writes=["rt2"])
            P.op("dve", lambda e: e.tensor_tensor(out=t[3], in0=x1, in1=sinb, op=ALU.mult), reads=rk + ["cs"], writes=["rt3"])
            P.op("dve", lambda e: e.tensor_tensor(out=dst3[:, :, 0:32], in0=t[0], in1=t[1], op=ALU.subtract),
                 reads=["rt0", "rt1"], writes=[wk + "a"])
            P.op("dve", lambda e: e.tensor_tensor(out=dst3[:, :, 32:64], in0=t[2], in1=t[3], op=ALU.add),
                 reads=["rt2", "rt3"], writes=[wk + "b"])

        if stop_after == "mw":
            o_ = dbg_out("d_qag", [128, 512], F32); o2_ = dbg_out("d_wuq", [128, 4, 1536], BF16)
            P.dma("sp", lambda e: e.dma_start(out=o_, in_=qag[:]), reads=["qag"], writes=["d_qag"])
            P.dma("sp", lambda e: e.dma_start(out=o2_, in_=wuq[:]), reads=wuq_k, writes=["d_wuq"])
            return finish(["d_qag", "d_wuq"])
        for G in range(ngroups):
            for i in range(nblk):
                try:
                    make_hT(x_d, i, scp1, sh1)
                except StopIteration:
                    return finish(["d_xn"])
                emit_conv(2)
                if stop_after == "m0":
                    o_ = dbg_out("d_hT", [128, 16, 128], BF16)
                    P.dma("sp", lambda e, o_=o_: e.dma_start(out=o_, in_=hT[:]), reads=hT_keys, writes=["d_hT"])
                    return finish(["d_hT"])
                P.dma("sp", lambda e, i=i: e.dma_start(out=posi[:], in_=pos_d[i * 128:(i + 1) * 128, :]), writes=["posi"])
                P.op("dve", lambda e: e.tensor_copy(out=posf[:], in_=posi[:]), reads=["posi"], writes=["posf"])
                P.op("dve", lambda e: e.tensor_scalar(out=ang[:, 0:32], in0=invf[:], scalar1=posf[:, 0:1], scalar2=None, op0=ALU.mult),
                     reads=["posf", "invf"], writes=["ang0"])
                P.op("dve", lambda e: e.tensor_scalar(out=ang[:, 32:64], in0=ang[:, 0:32], scalar1=float(np.pi / 2), scalar2=None, op0=ALU.add),
                     reads=["ang0"], writes=["ang1"])
                P.op("dve", lambda e: e.tensor_scalar(out=angk[:], in0=ang[:], scalar1=float(1.0 / (2 * np.pi)), scalar2=None, op0=ALU.mult),
                     reads=["ang0", "ang1"], writes=["angk"])
                P.op("dve", lambda e: e.tensor_copy(out=angi[:], in_=angk[:]), reads=["angk"], writes=["angi"])
                P.op("dve", lambda e: e.tensor_copy(out=angk[:], in_=angi[:]), reads=["angi"], writes=["angk2"])
                P.op("dve", lambda e: e.scalar_tensor_tensor(out=ang[:], in0=angk[:], scalar=float(-2 * np.pi), in1=ang[:],
                                                             op0=ALU.mult, op1=ALU.add), reads=["angk2", "ang0", "ang1"], writes=["angr"])
                P.op("act", lambda e: e.activation(out=cs[:, 0:32], in_=ang[:, 32:64], func=AF.Sin), reads=["angr"], writes=["cs_c"])
                P.op("act", lambda e: e.activation(out=cs[:, 32:64], in_=ang[:, 0:32], func=AF.Sin), reads=["angr", "cs_c"], writes=["cs"])

                def mmP(e):
                    for k in range(16):
                        e.matmul(pf[0][:, :], lhsT=hT[:, k, :], rhs=win_a[:, k, 0:512], start=(k == 0), stop=(k == 15))
                    for k in range(16):
                        r = e.matmul(pf[1][:, 0:320], lhsT=hT[:, k, :], rhs=win_a[:, k, 512:832], start=(k == 0), stop=(k == 15))
                    return r
                P.op("pe", mmP, reads=hT_keys + win_a_k, writes=["pf0", "pf1"])
                P.op("act", lambda e: e.activation(out=sq[:, 0:512], in_=pf[0][:, :], func=AF.Square, accum_out=sm[:, 0:1]),
                     reads=["pf0"], writes=["sq", "sm0"])
                rstd_of(sm[:, 0:1], 512, sm[:, 2:3], ["sm0"], "sm2", sm[:, 1:2], "sm1")
                P.op("dve", lambda e: e.scalar_tensor_tensor(out=cqn[:], in0=pf[0][:, :], scalar=sm[:, 2:3], in1=qag[:],
                                                             op0=ALU.mult, op1=ALU.mult), reads=["pf0", "sm2", "qag"], writes=["cqn"])
                P.op("act", lambda e: e.activation(out=sq[:, 0:256], in_=pf[1][:, 0:256], func=AF.Square, accum_out=sm[:, 3:4]),
                     reads=["pf1", "sm0"], writes=["sq", "sm3"])
                rstd_of(sm[:, 3:4], 256, sm[:, 5:6], ["sm3"], "sm5", sm[:, 4:5], "sm4")
                P.op("dve", lambda e: e.scalar_tensor_tensor(out=ckvn[:], in0=pf[1][:, 0:256], scalar=sm[:, 5:6], in1=kvag[:],
                                                             op0=ALU.mult, op1=ALU.mult), reads=["pf1", "sm5", "kvag"], writes=["ckvn"])
                P.op("act", lambda e: e.copy(out=kr[:], in_=pf[1][:, 256:320]), reads=["pf1"], writes=["kr"])
                P.op("act", lambda e: e.activation(out=sq[:, 0:64], in_=kr[:], func=AF.Square, accum_out=sm[:, 6:7]),
                     reads=["kr", "sm3"], writes=["sq", "sm6"])
                def trC(e):
                    for c in range(4):
                        e.transpose(pt[0][:, c * 128:(c + 1) * 128], cqn[:, c * 128:(c + 1) * 128], ident_b[:])
                    for c in range(2):
                        r = e.transpose(pt[0][:, (4 + c) * 128:(5 + c) * 128], ckvn[:, c * 128:(c + 1) * 128], ident_b[:])
                    return r
                P.op("pe", trC, reads=["cqn", "ckvn", "ident_b"], writes=["pt0"])
                P.op("dve", lambda e: e.tensor_copy(out=cqnT[:].rearrange("p c t -> p (c t)"), in_=pt[0][:, 0:512]),
                     reads=["pt0"], writes=["cqnT"])
                P.op("act", lambda e: e.copy(out=ckvnT[:].rearrange("p c t -> p (c t)"), in_=pt[0][:, 512:768]),
                     reads=["pt0"], writes=["ckvnT"])

                if stop_after == "m1":
                    keys = []
                    o_ = dbg_out("d_cqnT", [128, 4, 128], BF16)
                    P.dma("sp", lambda e, o_=o_: e.dma_start(out=o_, in_=cqnT[:]), reads=["cqnT"], writes=["d_cqnT"])
                    keys.append("d_cqnT")
                    o_ = dbg_out("d_cs", [128, 64], F32)
                    P.dma("sp", lambda e, o_=o_: e.dma_start(out=o_, in_=cs[:]), reads=["cs"], writes=["d_cs"])
                    keys.append("d_cs")
                    o_ = dbg_out("d_kr", [128, 64], F32)
                    P.dma("sp", lambda e, o_=o_: e.dma_start(out=o_, in_=kr[:]), reads=["kr"], writes=["d_kr"])
                    keys.append("d_kr")
                    return finish(keys)
                def mmQ(e, G=G):
                    for c in range(4):
                        e.matmul(pf[2][:, :], lhsT=cqnT[:, c, :], rhs=wuq[:, c, G * 768:G * 768 + 512], start=(c == 0), stop=(c == 3))
                    for c in range(4):
                        r = e.matmul(pf[3][:, 0:256], lhsT=cqnT[:, c, :], rhs=wuq[:, c, G * 768 + 512:G * 768 + 768], start=(c == 0), stop=(c == 3))
                    return r
                P.op("pe", mmQ, reads=["cqnT"] + wuq_k, writes=["pf2", "pf3"])
                qflat = qraw[:].rearrange("p h d -> p (h d)")
                P.op("act", lambda e: e.copy(out=qflat[:, 0:512], in_=pf[2][:, :]), reads=["pf2"], writes=["qraw0"])
                P.op("dve", lambda e: e.tensor_copy(out=qflat[:, 512:768], in_=pf[3][:, 0:256]), reads=["pf3"], writes=["qraw1"])
                P.op("act", lambda e: e.activation(out=sq[:, 0:768], in_=qflat, func=AF.Square),
                     reads=["qraw0", "qraw1", "sm6"], writes=["sq"])
                P.op("dve", lambda e: e.tensor_reduce(out=sm[:, 8:12], in_=sq[:, 0:768].rearrange("p (h d) -> p h d", h=4),
                                                      axis=AX.X, op=ALU.add), reads=["sq"], writes=["sm8"])
                rstd_of(sm[:, 8:12], 192, sm[:, 16:20], ["sm8"], "sm16", sm[:, 12:16], "sm12")
                P.op("dve", lambda e: e.tensor_tensor(out=qraw[:], in0=qraw[:], in1=bcast(sm[:, 16:20].unsqueeze(2), [128, 4, 192]), op=ALU.mult),
                     reads=["qraw0", "qraw1", "sm16"], writes=["qraw2"])
                P.op("dve", lambda e: e.tensor_tensor(out=qraw[:], in0=qraw[:], in1=bcast(qg[:].unsqueeze(1), [128, 4, 192]), op=ALU.mult),
                     reads=["qraw2", "qg"], writes=["qraw3"])
                P.op("act", lambda e: e.copy(out=qbn[:], in_=qraw[:, :, 0:128]), reads=["qraw3"], writes=["qbn"])
                rope(qraw[:, :, 128:192], qbr[:], 4, ["qraw3"], "qbr")

                def trQ(e):
                    for hh in range(4):
                        e.transpose(pt[1][:, hh * 128:(hh + 1) * 128], qbn[:, hh, :], ident_b[:])
                    qr2 = qbr[:].rearrange("p (a b) d -> p a (b d)", b=2)
                    for pp in range(2):
                        r = e.transpose(pt[1][:, (4 + pp) * 128:(5 + pp) * 128], qr2[:, pp, :], ident_b[:])
                    return r
                P.op("pe", trQ, reads=["qbn", "qbra", "qbrb", "ident_b"], writes=["pt1"])
                P.op("dve", lambda e: e.tensor_copy(out=QTn[:].rearrange("p c t -> p (c t)"), in_=pt[1][:, 0:512]), reads=["pt1"], writes=["QTn"])
                P.op("act", lambda e: e.copy(out=QTr[:].rearrange("p c t -> p (c t)"), in_=pt[1][:, 512:768]), reads=["pt1"], writes=["QTr"])

                if stop_after == "m2":
                    keys = []
                    o_ = dbg_out("d_QTn", [128, 4, 128], BF16)
                    P.dma("sp", lambda e, o_=o_: e.dma_start(out=o_, in_=QTn[:]), reads=["QTn"], writes=["d_QTn"])
                    keys.append("d_QTn")
                    o_ = dbg_out("d_QTr", [128, 2, 128], BF16)
                    P.dma("sp", lambda e, o_=o_: e.dma_start(out=o_, in_=QTr[:]), reads=["QTr"], writes=["d_QTr"])
                    keys.append("d_QTr")
                    return finish(keys)
                def mmK(e, G=G):
                    for c in range(2):
                        e.matmul(pf[2][:, :], lhsT=ckvnT[:, c, :], rhs=wukv[:, c, G * 1024:G * 1024 + 512], start=(c == 0), stop=(c == 1))
                    for c in range(2):
                        r = e.matmul(pf[3][:, :], lhsT=ckvnT[:, c, :], rhs=wukv[:, c, G * 1024 + 512:G * 1024 + 1024], start=(c == 0), stop=(c == 1))
                    return r
                P.op("pe", mmK, reads=["ckvnT"] + wukv_k, writes=["pf2", "pf3"])
                kvflat = kvraw[:].rearrange("p h d -> p (h d)")
                P.op("act", lambda e: e.copy(out=kvflat[:, 0:512], in_=pf[2][:, :]), reads=["pf2"], writes=["kvraw0"])
                P.op("dve", lambda e: e.tensor_copy(out=kvflat[:, 512:1024], in_=pf[3][:, :]), reads=["pf3"], writes=["kvraw1"])
                P.op("act", lambda e, i=i: e.copy(out=Vc[:, i, :].rearrange("p (h d) -> p h d", h=4), in_=kvraw[:, :, 128:256]),
                     reads=["kvraw0", "kvraw1"], writes=[f"Vc{i}"])
                P.op("act", lambda e: e.activation(out=sq[:, 0:512].rearrange("p (h d) -> p h d", h=4), in_=kvraw[:, :, 0:128], func=AF.Square),
                     reads=["kvraw0", "kvraw1", "sm8"], writes=["sq"])
                P.op("dve", lambda e: e.tensor_reduce(out=sm[:, 20:24], in_=sq[:, 0:512].rearrange("p (h d) -> p h d", h=4),
                                                      axis=AX.X, op=ALU.add), reads=["sq"], writes=["sm20"])
                P.op("dve", lambda e: e.tensor_scalar(out=sm[:, 20:24], in0=sm[:, 20:24], scalar1=sm[:, 6:7], scalar2=None, op0=ALU.add),
                     reads=["sm20", "sm6"], writes=["sm20b"])
                rstd_of(sm[:, 20:24], 192, sm[:, 28:32], ["sm20b"], "sm28", sm[:, 24:28], "sm24")
                P.op("dve", lambda e: e.tensor_tensor(out=kvraw[:, :, 0:128], in0=kvraw[:, :, 0:128],
                                                      in1=bcast(sm[:, 28:32].unsqueeze(2), [128, 4, 128]), op=ALU.mult),
                     reads=["kvraw0", "kvraw1", "sm28"], writes=["kvraw2"])
                P.op("dve", lambda e: e.tensor_tensor(out=kbn[:], in0=kvraw[:, :, 0:128], in1=bcast(kg[:, 0:128].unsqueeze(1), [128, 4, 128]), op=ALU.mult),
                     reads=["kvraw2", "kg"], writes=["kbn"])
                P.op("dve", lambda e: e.tensor_tensor(out=kr[:], in0=kr[:], in1=kg[:, 128:192], op=ALU.mult), reads=["kr", "kg", "sm6"], writes=["krg"])
                rope(kr[:].unsqueeze(1), krr[:].unsqueeze(1), 1, ["krg"], "krr")
                P.op("dve", lambda e: e.tensor_tensor(out=kbr[:], in0=bcast(krr[:].unsqueeze(1), [128, 4, 64]),
                                                      in1=bcast(sm[:, 28:32].unsqueeze(2), [128, 4, 64]), op=ALU.mult),
                     reads=["krra", "krrb", "sm28"], writes=["kbr"])

                def trK(e):
                    for hh in range(4):
                        e.transpose(pt[0][:, hh * 128:(hh + 1) * 128], kbn[:, hh, :], ident_b[:])
                    kr2 = kbr[:].rearrange("p (a b) d -> p a (b d)", b=2)
                    for pp in range(2):
                        r = e.transpose(pt[0][:, (4 + pp) * 128:(5 + pp) * 128], kr2[:, pp, :], ident_b[:])
                    return r
                P.op("pe", trK, reads=["kbn", "kbr", "ident_b"], writes=["pt0"])
                P.op("dve", lambda e, i=i: e.tensor_copy(out=KTn[:, :, i * 128:(i + 1) * 128],
                                                         in_=pt[0][:, 0:512].rearrange("p (c t) -> p c t", c=4)), reads=["pt0"], writes=[f"KTn{i}"])
                P.op("act", lambda e, i=i: e.copy(out=KTr[:, :, i * 128:(i + 1) * 128],
                                                  in_=pt[0][:, 512:768].rearrange("p (c t) -> p c t", c=2)), reads=["pt0"], writes=[f"KTr{i}"])

                if stop_after == "m3":
                    keys = []
                    o_ = dbg_out("d_KTn", [128, 4, 128], BF16)
                    P.dma("sp", lambda e, o_=o_: e.dma_start(out=o_, in_=KTn[:, :, 0:128]), reads=["KTn0"], writes=["d_KTn"])
                    keys.append("d_KTn")
                    o_ = dbg_out("d_KTr", [128, 2, 128], BF16)
                    P.dma("sp", lambda e, o_=o_: e.dma_start(out=o_, in_=KTr[:, :, 0:128]), reads=["KTr0"], writes=["d_KTr"])
                    keys.append("d_KTr")
                    return finish(keys)
                for hh in range(4):
                    h = G * 4 + hh
                    pp, hf = hh // 2, hh % 2
                    ngrp = (i + 4) // 4
                    for g in range(ngrp):
                        j0, j1 = g * 4, min(i + 1, g * 4 + 4)
                        sbank = 4 + (g % 2)
                        pbuf = PT[g % 2]

                        def mmS(e, j0=j0, j1=j1, sbank=sbank, hh=hh, pp=pp, hf=hf):
                            for j in range(j0, j1):
                                o = pf[sbank][:, (j - j0) * 128:(j - j0 + 1) * 128]
                                e.matmul(o, lhsT=KTn[:, hh, j * 128:(j + 1) * 128], rhs=QTn[:, hh, :], start=True, stop=False)
                                r = e.matmul(o, lhsT=KTr[hf * 64:(hf + 1) * 64, pp, j * 128:(j + 1) * 128],
                                             rhs=QTr[hf * 64:(hf + 1) * 64, pp, :], start=False, stop=True)
                            return r
                        P.op("pe", mmS, reads=["QTn", "QTr"] + [f"KTn{j}" for j in range(j0, j1)] + [f"KTr{j}" for j in range(j0, j1)],
                             writes=[f"pf{sbank}"])
                        w = (j1 - j0) * 128
                        P.op("act", lambda e, sbank=sbank, pbuf=pbuf, w=w: e.activation(out=pbuf[:, 0:w], in_=pf[sbank][:, 0:w], func=AF.Exp,
                                                                                     scale=float(192 ** -0.5)),
                             reads=[f"pf{sbank}"], writes=[f"PT{g % 2}"])
                        if j1 == i + 1:
                            dcol = (i - j0) * 128
                            P.op("dve", lambda e, pbuf=pbuf, dcol=dcol: e.tensor_tensor(out=pbuf[:, dcol:dcol + 128], in0=pbuf[:, dcol:dcol + 128],
                                                                                      in1=tri_b[:], op=ALU.mult),
                                 reads=[f"PT{g % 2}", "tri_b"], writes=[f"PT{g % 2}"])

                        def mmO(e, j0=j0, j1=j1, pbuf=pbuf, hh=hh, i=i):
                            for j in range(j0, j1):
                                pj = pbuf[:, (j - j0) * 128:(j - j0 + 1) * 128]
                                e.matmul(pf[2][:, 0:128], lhsT=Vc[:, j, hh * 128:(hh + 1) * 128], rhs=pj, start=(j == 0), stop=(j == i))
                                r = e.matmul(pf[3][:, 0:128], lhsT=ones_b[:], rhs=pj, start=(j == 0), stop=(j == i))
                            return r
                        P.op("pe", mmO, reads=[f"PT{g % 2}", "ones_b"] + [f"Vc{j}" for j in range(j0, j1)], writes=["pf2", "pf3"])
                    P.op("dve", lambda e: e.reciprocal(out=rden[:], in_=pf[3][:, 0:128]), reads=["pf3"], writes=["rden"])
                    P.op("dve", lambda e, h=h, i=i: e.tensor_tensor(out=catT[:, h, i * 128:(i + 1) * 128], in0=pf[2][:, 0:128], in1=rden[:], op=ALU.mult),
                         reads=["pf2", "rden"], writes=[f"cat{h}_{i}"])

    BAR()
    with ExitStack() as s1:
        aog = sb(s1, "aog", [128, 8])
        with nc.allow_non_contiguous_dma(reason="tiny transposed loads"):
            P.dma("sp", lambda e: e.dma_start(out=aog[:], in_=attn_out_norm.rearrange("(j p) -> p j", p=128), allow_slow_non_contiguous=True), writes=["aog"])
        sqb = sb(s1, "sqb", [128, 8, 512], BF16); rnb = sb(s1, "rnb", [128, 512]); rnb2 = sb(s1, "rnb2", [128, 512])
        for tb in range((nblk + 3) // 4):
            wd = min(512, nblk * 128 - tb * 512)
            cols = slice(tb * 512, tb * 512 + wd)
            blks = range(tb * 4, min(nblk, tb * 4 + 4))
            ck = [f"cat{h}_{i}" for h in range(8) for i in blks]
            P.op("act", lambda e, cols=cols, wd=wd: e.activation(out=sqb[:, :, 0:wd], in_=catT[:, 0:8, cols], func=AF.Square), reads=ck, writes=["sqb"])

            def mmN(e, wd=wd):
                for h in range(8):
                    r = e.matmul(pf[0][:, 0:wd], lhsT=ones_b[:], rhs=sqb[:, h, 0:wd], start=(h == 0), stop=(h == 7))
                return r
            P.op("pe", mmN, reads=["sqb", "ones_b"], writes=["pf0"])
            P.op("act", lambda e, wd=wd: e.activation(out=rnb[:, 0:wd], in_=pf[0][:, 0:wd], func=AF.Sqrt, scale=1.0 / 1024, bias=EPS), reads=["pf0"], writes=["rnb"])
            P.op("dve", lambda e, wd=wd: e.reciprocal(out=rnb2[:, 0:wd], in_=rnb[:, 0:wd]), reads=["rnb"], writes=["rnb2"])
            for h in range(8):
                P.op("dve", lambda e, h=h, cols=cols, wd=wd: e.scalar_tensor_tensor(out=catT[:, h, cols], in0=catT[:, h, cols], scalar=aog[:, h:h + 1],
                                                                                  in1=rnb2[:, 0:wd], op0=ALU.mult, op1=ALU.mult),
                     reads=["rnb2", "aog"] + [f"cat{h}_{i}" for i in blks],
                     writes=[f"cat{h}_{i}" for i in blks])
    BAR()
    if stop_after == "mla":
        o1 = dbg_out("d_catA", [128, 8, S], BF16)
        P.dma("sp", lambda e: e.dma_start(out=o1[:, :, 0:nblk * 128], in_=catT[:, 0:8, 0:nblk * 128]), reads=[f"cat{h}_{i}" for h in range(8) for i in range(nblk)], writes=["o1"])
        return finish(["o1"])
    with ExitStack() as s2:
        wxb = [sb(s2, f"wxb{q}", [128, 16, 128], BF16) for q in range(2)]
        wzb = sb(s2, "wzb", [128, 16, 512], BF16)
        win_d, win_d_k = load_w(s2, "win_d", w_in[:, 3392:3408], 16, 16)
        wcnt = {"n": 0}
        cw = sb(s2, "cw", [128, 12, 4]); cb = sb(s2, "cb", [128, 12])
        with nc.allow_non_contiguous_dma(reason="tiny transposed loads"):
            for k in range(4):
                P.dma("sp", lambda e, k=k: e.dma_start(out=cw[:, :, k], in_=conv_w[k, :].rearrange("(j p) -> p j", p=128), allow_slow_non_contiguous=True),
                      writes=[f"cw{k}"])
            P.dma("sp", lambda e: e.dma_start(out=cb[:], in_=conv_b.rearrange("(j p) -> p j", p=128), allow_slow_non_contiguous=True), writes=["cb"])
        cwk = [f"cw{k}" for k in range(4)]
        dtb = row_tile(s2, "dtb", dt_bias, 16); arow = row_tile(s2, "arow", a_log, 16)
        dsk = row_tile(s2, "dsk", d_skip, 16); sng = row_tile(s2, "sng", ssd_norm, 1024)
        P.op("act", lambda e: e.activation(out=arow[:], in_=arow[:], func=AF.Exp), reads=["arow"], writes=["arow_e"])
        P.op("dve", lambda e: e.tensor_scalar(out=arow[:], in0=arow[:], scalar1=-1.0, scalar2=None, op0=ALU.mult), reads=["arow_e"], writes=["arow_n"])
        xraw = sb(s2, "xraw", [128, 12, 131]); xacc = sb(s2, "xacc", [128, 12, 128])
        xcT = sb(s2, "xcT", [128, 12, 128], BF16)
        xs = sb(s2, "xs", [128, 16, 64]); xd = sb(s2, "xd", [128, 16, 64], BF16); xdd = sb(s2, "xdd", [128, 16, 64], BF16)
        zs = sb(s2, "zs", [128, 1024]); dts = sb(s2, "dts", [128, 16]); adt = sb(s2, "adt", [128, 16])
        ahi = sb(s2, "ahi", [128, 16], BF16); alo = sb(s2, "alo", [128, 16], BF16); ahf = sb(s2, "ahf", [128, 16])
        Rhi = sb(s2, "Rhi", [128, 16, 128], BF16); Rlo = sb(s2, "Rlo", [128, 16, 128], BF16)
        acs = sb(s2, "acs", [128, 16]); alast = sb(s2, "alast", [128, 16]); ea = sb(s2, "ea", [128, 16])
        dsd = sb(s2, "dsd", [128, 16]); cdec = sb(s2, "cdec", [128, 16])
        dif = sb(s2, "dif", [128, 4, 128]); Ed = sb(s2, "Ed", [128, 4, 128])
        MT = sb(s2, "MT", [128, 16, 128], BF16); CBT = sb(s2, "CBT", [128, 2, 128])
        Btok = sb(s2, "Btok", [128, 2, 128], BF16)
        Sst = sb(s2, "Sst", [128, 16, 64]); Sbf = sb(s2, "Sbf", [128, 16, 64], BF16)
        yv = sb(s2, "yv", [128, 16, 64]); ytmp = sb(s2, "ytmp", [128, 16, 64]); ybf = sb(s2, "ybf", [128, 1024], BF16)
        ysm = sb(s2, "ysm", [128, 8])
        P.op("dve", lambda e: e.memset(Sst[:], 0.0), writes=["Sst"])
        P.op("dve", lambda e: e.memset(Sbf[:], 0.0), writes=["Sbf"])
        P.op("dve", lambda e: e.memset(xraw[:, :, 0:3], 0.0), writes=["xhalo"])

        for i in range(nblk):
            make_hT(x_d, i, scp1, sh1)
            for j in range(12):
                bank = j % 2
                wq_ = wcnt["n"] % 2
                wcnt["n"] += 1
                wx_ = wxb[wq_]
                P.dma("pool", lambda e, j=j, wx_=wx_: e.dma_start(
                    out=wx_[:], in_=w_in[:, 1856 + j * 128:1856 + (j + 1) * 128].rearrange("(k p) n -> p k n", p=128)), writes=[f"wxb{wq_}"])

                def mmX(e, j=j, bank=bank, wx_=wx_):
                    for k in range(16):
                        r = e.matmul(pf[bank][:, 0:128], lhsT=wx_[:, k, :], rhs=hT[:, k, :], start=(k == 0), stop=(k == 15))
                    return r
                P.op("pe", mmX, reads=hT_keys + [f"wxb{wq_}"], writes=[f"pf{bank}"])
                P.op("act", lambda e, j=j, bank=bank: e.copy(out=xraw[:, j, 3:131], in_=pf[bank][:, 0:128]),
                     reads=[f"pf{bank}", "xhalo"], writes=[f"xraw{j}"])
            xrk = [f"xraw{j}" for j in range(12)]
            for j in range(12):
                eng = "dve"
                P.op(eng, lambda e, j=j: e.tensor_scalar(out=xacc[:, j, :], in0=xraw[:, j, 0:128], scalar1=cw[:, j, 0:1], scalar2=cb[:, j:j + 1],
                                                         op0=ALU.mult, op1=ALU.add), reads=[f"xraw{j}", "cb"] + cwk, writes=[f"xacc{j}"])
                for k in range(1, 4):
                    P.op(eng, lambda e, j=j, k=k: e.scalar_tensor_tensor(out=xacc[:, j, :], in0=xraw[:, j, k:k + 128], scalar=cw[:, j, k:k + 1],
                                                                         in1=xacc[:, j, :], op0=ALU.mult, op1=ALU.add),
                         reads=[f"xraw{j}", f"xacc{j}"] + cwk, writes=[f"xacc{j}"])
                P.op("act", lambda e, j=j: e.activation(out=xcT[:, j, :], in_=xacc[:, j, :], func=AF.Silu), reads=[f"xacc{j}"], writes=[f"xcT{j}"])
                P.op(eng, lambda e, j=j: e.tensor_copy(out=xraw[:, j, 0:3], in_=xraw[:, j, 128:131]), reads=[f"xraw{j}", f"xacc{j}"], writes=[f"xraw{j}", "xhalo"])
            for half in range(2):
                P.dma("pool", lambda e, half=half: e.dma_start(
                    out=wzb[:], in_=w_in[:, 832 + half * 512:832 + (half + 1) * 512].rearrange("(k p) n -> p k n", p=128)), writes=["wzb"])

                def mmZ(e, half=half):
                    for k in range(16):
                        r = e.matmul(pf[2 + half][:, :], lhsT=hT[:, k, :], rhs=wzb[:, k, :], start=(k == 0), stop=(k == 15))
                    return r
                P.op("pe", mmZ, reads=hT_keys + ["wzb"], writes=[f"pf{2 + half}"])

            def mmD(e):
                for k in range(16):
                    r = e.matmul(pf[4][:, 0:16], lhsT=hT[:, k, :], rhs=win_d[:, k, :], start=(k == 0), stop=(k == 15))
                return r
            P.op("pe", mmD, reads=hT_keys + win_d_k, writes=["pf4"])
            P.op("act", lambda e: e.activation(out=zs[:, 0:512], in_=pf[2][:, :], func=AF.Silu), reads=["pf2"], writes=["zs0"])
            P.op("act", lambda e: e.activation(out=zs[:, 512:1024], in_=pf[3][:, :], func=AF.Silu), reads=["pf3"], writes=["zs1"])
            P.op("dve", lambda e: e.tensor_tensor(out=dts[:], in0=pf[4][:, 0:16], in1=dtb[:], op=ALU.add), reads=["pf4", "dtb"], writes=["dts0"])
            P.op("act", lambda e: e.activation(out=dts[:], in_=dts[:], func=AF.Exp), reads=["dts0"], writes=["dts1"])
            P.op("act", lambda e: e.activation(out=dts[:], in_=dts[:], func=AF.Ln, bias=1.0, scale=1.0), reads=["dts1"], writes=["dts"])
            P.op("dve", lambda e: e.tensor_tensor(out=adt[:], in0=dts[:], in1=arow[:], op=ALU.mult), reads=["dts", "arow_n"], writes=["adt"])
            P.op("dve", lambda e: e.tensor_copy(out=ahi[:], in_=adt[:]), reads=["adt"], writes=["ahi"])
            P.op("dve", lambda e: e.tensor_copy(out=ahf[:], in_=ahi[:]), reads=["ahi"], writes=["ahf"])
            P.op("dve", lambda e: e.tensor_tensor(out=alo[:], in0=adt[:], in1=ahf[:], op=ALU.subtract), reads=["adt", "ahf"], writes=["alo"])
            trib = bcast(tri_b[:].unsqueeze(1), [128, 16, 128])
            P.op("dve", lambda e: e.tensor_tensor(out=Rhi[:], in0=trib, in1=bcast(ahi[:].unsqueeze(2), [128, 16, 128]), op=ALU.mult),
                 reads=["ahi", "tri_b"], writes=["Rhi"])
            P.op("pool", lambda e: e.tensor_tensor(out=Rlo[:], in0=trib, in1=bcast(alo[:].unsqueeze(2), [128, 16, 128]), op=ALU.mult),
                 reads=["alo", "tri_b"], writes=["Rlo"])
            def mmC(e):
                e.matmul(pf[4][:, 16:32], lhsT=tri_b[:], rhs=ahi[:], start=True, stop=False)
                return e.matmul(pf[4][:, 16:32], lhsT=tri_b[:], rhs=alo[:], start=False, stop=True)
            P.op("pe", mmC, reads=["tri_b", "ahi", "alo", "dts0"], writes=["pf4"])
            P.op("dve", lambda e: e.tensor_copy(out=acs[:], in_=pf[4][:, 16:32]), reads=["pf4"], writes=["acs"])
            P.op("act", lambda e: e.activation(out=ea[:], in_=acs[:], func=AF.Exp), reads=["acs"], writes=["ea"])
            def trX(e):
                for c in range(8):
                    r = e.transpose(pt[0][:, c * 128:(c + 1) * 128], xcT[:, c, :], ident_b[:])
                return r
            P.op("pe", trX, reads=[f"xcT{c}" for c in range(8)] + ["ident_b"], writes=["pt0"])
            P.op("act", lambda e: e.copy(out=xs[:].rearrange("p h d -> p (h d)"), in_=pt[0][:, :]), reads=["pt0"], writes=["xs"])
            P.op("dve", lambda e: e.tensor_tensor(out=xd[:], in0=xs[:], in1=bcast(dts[:].unsqueeze(2), [128, 16, 64]), op=ALU.mult),
                 reads=["xs", "dts"], writes=["xd"])
            def trB(e):
                for g in range(2):
                    r = e.transpose(pt[1][:, g * 128:(g + 1) * 128], xcT[:, 8 + g, :], ident_b[:])
                return r
            P.op("pe", trB, reads=["xcT8", "xcT9", "ident_b"], writes=["pt1"])
            P.op("act", lambda e: e.copy(out=Btok[:].rearrange("p g n -> p (g n)"), in_=pt[1][:, 0:256]), reads=["pt1"], writes=["Btok"])
            def mmCB(e):
                for g in range(2):
                    r = e.matmul(pf[5][:, g * 128:(g + 1) * 128], lhsT=xcT[:, 8 + g, :], rhs=xcT[:, 10 + g, :], start=True, stop=True)
                return r
            P.op("pe", mmCB, reads=["xcT8", "xcT9", "xcT10", "xcT11"], writes=["pf5"])
            P.op("act", lambda e: e.copy(out=CBT[:].rearrange("p g n -> p (g n)"), in_=pf[5][:, 0:256]), reads=["pf5"], writes=["CBT"])
            for q4 in range(4):
                bank = q4 % 2

                def mmBC(e, q4=q4, bank=bank):
                    e.matmul(pf[bank][:, :], lhsT=ones_b[:], rhs=Rhi[:, q4 * 4:(q4 + 1) * 4, :].rearrange("p h l -> p (h l)"), start=True, stop=False)
                    return e.matmul(pf[bank][:, :], lhsT=ones_b[:], rhs=Rlo[:, q4 * 4:(q4 + 1) * 4, :].rearrange("p h l -> p (h l)"), start=False, stop=True)
                P.op("pe", mmBC, reads=["ones_b", "Rhi", "Rlo"], writes=[f"pf{bank}"])
                pv = pf[bank][:, :].rearrange("p (h l) -> p h l", h=4)
                P.op("dve", lambda e, pv=pv, q4=q4: e.tensor_copy(out=alast[:, q4 * 4:(q4 + 1) * 4], in_=pv[:, :, 127]), reads=[f"pf{bank}"], writes=[f"alast{q4}"])
                P.op("dve", lambda e, pv=pv, q4=q4: e.tensor_tensor(out=dif[:], in0=pv, in1=bcast(acs[:, q4 * 4:(q4 + 1) * 4].unsqueeze(2), [128, 4, 128]),
                                                                   op=ALU.subtract), reads=[f"pf{bank}", "acs"], writes=["dif"])
                P.op("dve", lambda e: e.tensor_tensor(out=dif[:], in0=dif[:], in1=bcast(negm[:].unsqueeze(1), [128, 4, 128]), op=ALU.add),
                     reads=["dif", "negm"], writes=["dif2"])
                P.op("act", lambda e: e.activation(out=Ed[:], in_=dif[:], func=AF.Exp), reads=["dif2"], writes=["Ed"])
                P.op("dve", lambda e, q4=q4: e.tensor_tensor(out=MT[:, q4 * 4:(q4 + 1) * 4, :], in0=Ed[:],
                                                           in1=bcast(CBT[:, q4 // 2, :].unsqueeze(1), [128, 4, 128]), op=ALU.mult),
                     reads=["Ed", "CBT"], writes=[f"MT{q4}"])
            alk = [f"alast{q}" for q in range(4)]
            P.op("dve", lambda e: e.tensor_tensor(out=dsd[:], in0=alast[:], in1=acs[:], op=ALU.subtract), reads=alk + ["acs"], writes=["dsd0"])
            P.op("act", lambda e: e.activation(out=dsd[:], in_=dsd[:], func=AF.Exp), reads=["dsd0"], writes=["dsd"])
            P.op("act", lambda e: e.activation(out=cdec[:], in_=alast[:], func=AF.Exp), reads=alk, writes=["cdec"])
            P.op("dve", lambda e: e.tensor_tensor(out=xdd[:], in0=xd[:], in1=bcast(dsd[:].unsqueeze(2), [128, 16, 64]), op=ALU.mult),
                 reads=["xd", "dsd"], writes=["xdd"])
            def mmY(e):
                for hd in range(16):
                    r = e.matmul(pf[2 + hd // 8][:, (hd % 8) * 64:(hd % 8 + 1) * 64], lhsT=MT[:, hd, :], rhs=xd[:, hd, :], start=True, stop=True)
                return r
            P.op("pe", mmY, reads=[f"MT{q}" for q in range(4)] + ["xd"], writes=["pf2", "pf3"])

            def mmYo(e):
                for g in range(2):
                    r = e.matmul(pf[g][:, :], lhsT=xcT[:, 10 + g, :], rhs=Sbf[:, g * 8:(g + 1) * 8, :].rearrange("p h d -> p (h d)"), start=True, stop=True)
                return r
            P.op("pe", mmYo, reads=["xcT10", "xcT11", "Sbf"], writes=["pf0", "pf1"])
            for g in range(2):
                hs = slice(g * 8, (g + 1) * 8)
                P.op("dve", lambda e, g=g, hs=hs: e.tensor_tensor(out=ytmp[:, hs, :], in0=pf[g][:, :].rearrange("p (h d) -> p h d", h=8),
                                                                 in1=bcast(ea[:, hs].unsqueeze(2), [128, 8, 64]), op=ALU.mult),
                     reads=[f"pf{g}", "ea"], writes=[f"ytmp{g}"])
                P.op("dve", lambda e, g=g, hs=hs: e.tensor_tensor(out=yv[:, hs, :], in0=pf[2 + g][:, :].rearrange("p (h d) -> p h d", h=8),
                                                                 in1=ytmp[:, hs, :], op=ALU.add), reads=[f"pf{2 + g}", f"ytmp{g}"], writes=[f"yv{g}"])
            def mmSt(e):
                for g in range(2):
                    r = e.matmul(pf[g][:, :], lhsT=Btok[:, g, :], rhs=xdd[:, g * 8:(g + 1) * 8, :].rearrange("p h d -> p (h d)"), start=True, stop=True)
                return r
            P.op("pe", mmSt, reads=["Btok", "xdd"], writes=["pf0", "pf1"])
            P.op("dve", lambda e: e.tensor_tensor(out=Sst[:], in0=Sst[:], in1=bcast(cdec[:].unsqueeze(2), [128, 16, 64]), op=ALU.mult),
                 reads=["Sst", "cdec"], writes=["Sst"])
            for g in range(2):
                hs = slice(g * 8, (g + 1) * 8)
                P.op("dve", lambda e, g=g, hs=hs: e.tensor_tensor(out=Sst[:, hs, :], in0=pf[g][:, :].rearrange("p (h d) -> p h d", h=8),
                                                                 in1=Sst[:, hs, :], op=ALU.add), reads=[f"pf{g}", "Sst"], writes=["Sst"])
            P.op("act", lambda e: e.copy(out=Sbf[:], in_=Sst[:]), reads=["Sst"], writes=["Sbf"])
            P.op("dve", lambda e: e.tensor_tensor(out=ytmp[:], in0=xs[:], in1=bcast(dsk[:].unsqueeze(2), [128, 16, 64]), op=ALU.mult),
                 reads=["xs", "dsk", "yv0", "yv1"], writes=["ytmp0", "ytmp1"])
            P.op("dve", lambda e: e.tensor_tensor(out=yv[:], in0=yv[:], in1=ytmp[:], op=ALU.add), reads=["yv0", "yv1", "ytmp0", "ytmp1"], writes=["yv0", "yv1"])
            yflat = yv[:].rearrange("p h d -> p (h d)")
            P.op("dve", lambda e: e.tensor_tensor(out=yflat, in0=yflat, in1=zs[:], op=ALU.mult), reads=["yv0", "yv1", "zs0", "zs1"], writes=["yv0", "yv1"])
            tfl = ytmp[:].rearrange("p h d -> p (h d)")
            P.op("act", lambda e: e.activation(out=tfl, in_=yflat, func=AF.Square), reads=["yv0", "yv1"], writes=["ytmp0", "ytmp1"])
            P.op("dve", lambda e: e.tensor_reduce(out=ysm[:, 0:2], in_=tfl.rearrange("p (g d) -> p g d", g=2), axis=AX.X, op=ALU.add),
                 reads=["ytmp0", "ytmp1"], writes=["ysm0"])
            rstd_of(ysm[:, 0:2], 512, ysm[:, 4:6], ["ysm0"], "ysm4", ysm[:, 2:4], "ysm2")
            P.op("dve", lambda e: e.tensor_tensor(out=yflat.rearrange("p (g d) -> p g d", g=2), in0=yflat.rearrange("p (g d) -> p g d", g=2),
                                                  in1=bcast(ysm[:, 4:6].unsqueeze(2), [128, 2, 512]), op=ALU.mult),
                 reads=["yv0", "yv1", "ysm4"], writes=["yv0", "yv1"])
            P.op("dve", lambda e: e.tensor_tensor(out=ybf[:], in0=yflat, in1=sng[:], op=ALU.mult), reads=["yv0", "yv1", "sng"], writes=["ybf"])

            def trY(e):
                for c in range(8):
                    r = e.transpose(pt[1][:, c * 128:(c + 1) * 128], ybf[:, c * 128:(c + 1) * 128], ident_b[:])
                return r
            P.op("pe", trY, reads=["ybf", "ident_b"], writes=["pt1"])
            P.op("act", lambda e, i=i: e.copy(out=catT[:, 8:16, i * 128:(i + 1) * 128], in_=pt[1][:, :].rearrange("p (c t) -> p c t", c=8)),
                 reads=["pt1"], writes=[f"cat{h}_{i}" for h in range(8, 16)])

    BAR()
    if stop_after == "ssd":
        o1 = dbg_out("d_catS", [128, 8, S], BF16)
        P.dma("sp", lambda e: e.dma_start(out=o1[:, :, 0:nblk * 128], in_=catT[:, 8:16, 0:nblk * 128]), reads=[f"cat{h}_{i}" for h in range(8, 16) for i in range(nblk)], writes=["o1"])
        return finish(["o1"])
    with ExitStack() as s3:
        g1row = row_tile(s3, "g1row", mod_d[2 * D:3 * D], D)
        P.ops[-1].reads = ("mod_d",)
        wo, wo_k = load_w(s3, "wo", w_out, 16, D)
        x1t = sb(s3, "x1t", [128, D])
        for i in range(nblk):
            s = cnt["x"] % 1
            cnt["x"] += 1
            t = xt[s]
            P.dma("sp", lambda e, t=t, i=i: e.dma_start(out=t[:], in_=x_d[i * 128:(i + 1) * 128, :]), writes=[f"xt{s}"])
            for nb in range(4):
                def mmM(e, nb=nb, i=i):
                    for c in range(16):
                        r = e.matmul(pf[nb][:, :], lhsT=catT[:, c, i * 128:(i + 1) * 128], rhs=wo[:, c, nb * 512:(nb + 1) * 512], start=(c == 0), stop=(c == 15))
                    return r
                P.op("pe", mmM, reads=[f"cat{c}_{i}" for c in range(16)] + wo_k, writes=[f"pf{nb}"])
                cs_ = slice(nb * 512, (nb + 1) * 512)
                P.op("dve", lambda e, nb=nb, cs_=cs_: e.tensor_tensor(out=x1t[:, cs_], in0=pf[nb][:, :], in1=g1row[:, cs_], op=ALU.mult),
                     reads=[f"pf{nb}", "g1row"], writes=[f"x1t{nb}"])
                P.op("dve", lambda e, nb=nb, cs_=cs_, t=t: e.tensor_tensor(out=x1t[:, cs_], in0=x1t[:, cs_], in1=t[:, cs_], op=ALU.add),
                     reads=[f"x1t{nb}", f"xt{s}"], writes=[f"x1t{nb}"])
            P.dma("sp", lambda e, i=i: e.dma_start(out=x1_d[i * 128:(i + 1) * 128, :], in_=x1t[:]), reads=[f"x1t{nb}" for nb in range(4)],
                  writes=[f"x1d{i}"], semkey=("x1st",))
    BAR()
    if stop_after == "x1":
        return finish([f"x1d{i}" for i in range(nblk)])
    scat.close()
    BAR()

    emit_conv(len(conv_pending))
    with ExitStack() as s4:
        wq, wq_k = load_w(s4, "wq", w_query, 16, D)
        g2row = row_tile(s4, "g2row", mod_d[5 * D:6 * D], D); P.ops[-1].reads = ("mod_d",)
        sc2row = row_tile(s4, "sc2row", mod_d[4 * D:5 * D], D); P.ops[-1].reads = ("mod_d",)
        sh2row = row_tile(s4, "sh2row", mod_d[3 * D:4 * D], D); P.ops[-1].reads = ("mod_d",)
        P.op("dve", lambda e: e.tensor_scalar(out=sc2row[:], in0=sc2row[:], scalar1=1.0, scalar2=None, op0=ALU.add), reads=["sc2row"], writes=["sc2row"])
        skT = sb(s4, "skT", [128, 16, 128], BF16)
        qT = sb(s4, "qT", [128, 16, 128], BF16)
        xt2 = sb(s4, "xt2", [128, D]); h2b2 = sb(s4, "h2b2", [128, D], BF16)
        XT = [xt[0], xt2]; H2B = [xn, h2b2]
        sc = sb(s4, "sc", [128, 16, 128]); zp = sb(s4, "zp", [128, 128])
        tops = sb(s4, "tops", [128, 16, 16]); topi = sb(s4, "topi", [128, 16, 16])
        iot256 = sb(s4, "iot256", [128, 256]); iot = iot256[:, 0:128]
        topiu = sb(s4, "topiu", [128, 16, 16], mybir.dt.uint32); bposu = sb(s4, "bposu", [128, 8, 16], mybir.dt.uint32)
        bposf = sb(s4, "bposf", [128, 8, 16])
        cands = sb(s4, "cands", [128, 8, 16, 16]); candi = sb(s4, "candi", [128, 8, 16, 16]); zc = sb(s4, "zc", [128, 256])
        bests = sb(s4, "bests", [128, 8, 16]); besti = sb(s4, "besti", [128, 8, 16])
        bidx2 = [sb(s4, f"bidx{q}", [128, 128], I32) for q in range(2)]
        gsm = sb(s4, "gsm", [128, 32])
        gate2 = [sb(s4, f"gate{q}", [128, 8, 16]) for q in range(2)]
        pre2 = [sb(s4, f"pre{q}", [128, 128]) for q in range(2)]
        actv2 = [sb(s4, f"actv{q}", [128, 128]) for q in range(2)]
        scr2 = sb(s4, "scr2", [128, D], BF16)
        dg = [sb(s4, f"dg{q}", [128, 128], BF16) for q in range(2)]
        NG = 6
        gb = [sb(s4, f"gb{q}", [128, D], BF16) for q in range(NG)]
        acc = sb(s4, "acc", [128, D]); h2 = acc
        SCRK = ["scr2_0", "scr2_1", "scr2b_0", "scr2b_1"]
        skf = acc[:].rearrange("p (g d) -> p g d", g=16); skb = scr2[:].rearrange("p (g d) -> p g d", g=16)
        P.dma("sp", lambda e: e.dma_start(out=skf, in_=sub_keys.rearrange("g k d -> k g d")), writes=["acc"], semkey=("skf",))
        P.op("dve", lambda e: e.tensor_copy(out=skb, in_=skf), reads=["acc"], writes=SCRK)
        for half in range(2):
            def trSK(e, half=half):
                for g in range(8):
                    r = e.transpose(pt[half][:, g * 128:(g + 1) * 128], skb[:, half * 8 + g, :], ident_b[:])
                return r
            P.op("pe", trSK, reads=SCRK + ["ident_b"], writes=[f"pt{half}"])
            P.op("dve", lambda e, half=half: e.tensor_copy(out=skT[:, half * 8:(half + 1) * 8, :], in_=pt[half][:, :].rearrange("p (g k) -> p g k", g=8)),
                 reads=[f"pt{half}"], writes=[f"skT{half}"])
        def mmI(e):
            return e.matmul(pf[5][:, 0:128], lhsT=ones_b[:], rhs=tri_b[:], start=True, stop=True)
        P.op("pe", mmI, reads=["ones_b", "tri_b"], writes=["pf5"])
        P.op("dve", lambda e: e.tensor_scalar(out=iot256[:, 0:128], in0=pf[5][:, 0:128], scalar1=-1.0, scalar2=None, op0=ALU.add), reads=["pf5"], writes=["iotA"])
        P.op("dve", lambda e: e.tensor_scalar(out=iot256[:, 128:256], in0=pf[5][:, 0:128], scalar1=127.0, scalar2=None, op0=ALU.add), reads=["pf5", "iotA"], writes=["iot256"])
        gcount = {"n": 0}

        def prep_topk(i):
            p = i % 2
            t = XT[p]; hb = H2B[p]; bidx = bidx2[p]; gate = gate2[p]
            P.dma("sp", lambda e: e.dma_start(out=t[:], in_=x1_d[i * 128:(i + 1) * 128, :]), reads=[f"x1d{i}"], writes=[f"XT{p}"])
            P.op("act", lambda e: e.activation(out=junk[:], in_=t[:], func=AF.Square, accum_out=st4[:, 0:1]), reads=[f"XT{p}"], writes=["junk", "st_ss"])
            rstd_of(st4[:, 0:1], D, st4[:, 2:3], ["st_ss"], "st_r", st4[:, 1:2], "st_t")
            P.op("dve", lambda e: e.scalar_tensor_tensor(out=h2[:], in0=t[:], scalar=st4[:, 2:3], in1=sc2row[:], op0=ALU.mult, op1=ALU.mult),
                 reads=[f"XT{p}", "st_r", "sc2row"], writes=["acc"])
            P.op("dve", lambda e: e.tensor_tensor(out=hb[:], in0=h2[:], in1=sh2row[:], op=ALU.add), reads=["acc", "sh2row"], writes=[f"H2B{p}"])
            for half in range(2):
                def trs(e, half=half):
                    for k in range(8):
                        kk = half * 8 + k
                        r = e.transpose(pt[half][:, k * 128:(k + 1) * 128], hb[:, kk * 128:(kk + 1) * 128], ident_b[:])
                    return r
                P.op("pe", trs, reads=[f"H2B{p}", "ident_b"], writes=[f"pt{half}"])
                P.op("act", lambda e, half=half: e.copy(out=hT[:, half * 8:(half + 1) * 8, :], in_=pt[half][:, :].rearrange("p (c t) -> p c t", c=8)),
                     reads=[f"pt{half}"], writes=[f"hT{kk}" for kk in range(half * 8, half * 8 + 8)])
            for c4 in range(4):
                def mmQ2(e, c4=c4):
                    for cc in range(4):
                        ch = c4 * 4 + cc
                        for k in range(16):
                            r = e.matmul(pf[4][:, cc * 128:(cc + 1) * 128], lhsT=wq[:, k, ch * 128:(ch + 1) * 128], rhs=hT[:, k, :], start=(k == 0), stop=(k == 15))
                    return r
                P.op("pe", mmQ2, reads=hT_keys + wq_k, writes=["pf4"])
                P.op("act", lambda e, c4=c4: e.copy(out=qT[:, c4 * 4:(c4 + 1) * 4, :], in_=pf[4][:, :].rearrange("p (c t) -> p c t", c=4)),
                     reads=["pf4"], writes=[f"qT{c4}"])

                def mmSc(e, c4=c4):
                    for cc in range(4):
                        ch = c4 * 4 + cc
                        r = e.matmul(pf[5][:, cc * 128:(cc + 1) * 128], lhsT=qT[:, ch, :], rhs=skT[:, ch, :], start=True, stop=True)
                    return r
                P.op("pe", mmSc, reads=[f"qT{c4}", "skT0", "skT1"], writes=["pf5"])
                P.op("act", lambda e, c4=c4: e.copy(out=sc[:, c4 * 4:(c4 + 1) * 4, :], in_=pf[5][:, :].rearrange("p (c t) -> p c t", c=4)),
                     reads=["pf5"], writes=[f"sc{c4}"])
            for g in range(16):
                sk_ = f"sc{g // 4}"
                P.op("dve", lambda e, g=g: e.max(out=tops[:, g, 0:8], in_=sc[:, g, :]), reads=[sk_], writes=["tA"])
                P.op("dve", lambda e, g=g: e.max_index(out=topiu[:, g, 0:8], in_max=tops[:, g, 0:8], in_values=sc[:, g, :]), reads=[sk_, "tA"], writes=["tiA"])
                P.op("dve", lambda e, g=g: e.match_replace(out=zp[:], in_to_replace=tops[:, g, 0:8], in_values=sc[:, g, :], imm_value=-1e30),
                     reads=[sk_, "tA"], writes=["zp"])
                P.op("dve", lambda e, g=g: e.max(out=tops[:, g, 8:16], in_=zp[:]), reads=["zp"], writes=["tB"])
                P.op("dve", lambda e, g=g: e.max_index(out=topiu[:, g, 8:16], in_max=tops[:, g, 8:16], in_values=zp[:]), reads=["zp", "tB"], writes=["tiB"])
            P.op("dve", lambda e: e.tensor_copy(out=topi[:], in_=topiu[:]), reads=["tiA", "tiB"], writes=["topi"])
            t4 = tops[:].rearrange("p (h j) k -> p h j k", j=2)
            i4 = topi[:].rearrange("p (h j) k -> p h j k", j=2)
            for h in range(8):
                P.op("dve", lambda e, h=h: e.tensor_tensor(out=cands[:, h, :, :], in0=bcast(t4[:, h, 0, :].unsqueeze(2), [128, 16, 16]),
                                                           in1=bcast(t4[:, h, 1, :].unsqueeze(1), [128, 16, 16]), op=ALU.add),
                     reads=["tA", "tB"], writes=[f"cands{h}"])
                P.op("dve", lambda e, h=h: e.scalar_tensor_tensor(out=candi[:, h, :, :], in0=bcast(i4[:, h, 0, :].unsqueeze(2), [128, 16, 16]), scalar=128.0,
                                                                  in1=bcast(i4[:, h, 1, :].unsqueeze(1), [128, 16, 16]), op0=ALU.mult, op1=ALU.add),
                     reads=["topi"], writes=[f"candi{h}"])
            for h in range(8):
                cf = cands[:, h, :, :].rearrange("p a b -> p (a b)")
                P.op("dve", lambda e, h=h, cf=cf: e.max(out=bests[:, h, 0:8], in_=cf), reads=[f"cands{h}"], writes=["bA"])
                P.op("dve", lambda e, h=h, cf=cf: e.max_index(out=bposu[:, h, 0:8], in_max=bests[:, h, 0:8], in_values=cf), reads=[f"cands{h}", "bA"], writes=["bpA"])
                P.op("dve", lambda e, h=h, cf=cf: e.match_replace(out=zc[:], in_to_replace=bests[:, h, 0:8], in_values=cf, imm_value=-1e30),
                     reads=[f"cands{h}", "bA"], writes=["zc"])
                P.op("dve", lambda e, h=h: e.max(out=bests[:, h, 8:16], in_=zc[:]), reads=["zc"], writes=["bB"])
                P.op("dve", lambda e, h=h: e.max_index(out=bposu[:, h, 8:16], in_max=bests[:, h, 8:16], in_values=zc[:]), reads=["zc", "bB"], writes=["bpB"])
            P.op("dve", lambda e: e.tensor_copy(out=bposf[:], in_=bposu[:]), reads=["bpA", "bpB"], writes=["bposf"])
            for h in range(8):
                cif = candi[:, h, :, :].rearrange("p a b -> p (a b)")
                for k0 in (0, 8):
                    def idx2(e, h=h, cif=cif, k0=k0):
                        for k in range(k0, k0 + 8):
                            r = e.scalar_tensor_tensor(out=scr2[:, (k % 8) * 256:(k % 8 + 1) * 256], in0=iot256[:], scalar=bposf[:, h, k:k + 1], in1=cif,
                                                       op0=ALU.is_equal, op1=ALU.mult, accum_out=besti[:, h, k:k + 1])
                        return r
                    P.op("dve", idx2, reads=[f"candi{h}", "bposf", "iot256"], writes=SCRK + [f"besti{h}"])
            bik = [f"besti{h}" for h in range(8)]
            P.op("dve", lambda e: e.tensor_copy(out=bidx[:], in_=besti[:].rearrange("p h k -> p (h k)")), reads=bik, writes=[f"bidx{p}"])
            P.op("dve", lambda e: e.tensor_tensor(out=gate[:], in0=bests[:], in1=bcast(bests[:, :, 0:1], [128, 8, 16]), op=ALU.subtract),
                 reads=["bA", "bB"], writes=[f"gate{p}a"])
            P.op("act", lambda e: e.activation(out=gate[:], in_=gate[:], func=AF.Exp), reads=[f"gate{p}a"], writes=[f"gate{p}b"])
            P.op("dve", lambda e: e.tensor_reduce(out=gsm[:, 0:8], in_=gate[:], axis=AX.X, op=ALU.add), reads=[f"gate{p}b"], writes=["gsm0"])
            P.op("dve", lambda e: e.reciprocal(out=gsm[:, 8:16], in_=gsm[:, 0:8]), reads=["gsm0"], writes=["gsm8"])
            P.op("dve", lambda e: e.tensor_tensor(out=gate[:], in0=gate[:], in1=bcast(gsm[:, 8:16].unsqueeze(2), [128, 8, 16]), op=ALU.mult),
                 reads=[f"gate{p}b", "gsm8"], writes=[f"gate{p}"])

        def u_phase(i):
            p = i % 2
            hb = H2B[p]; bidx = bidx2[p]; pre = pre2[p]; actv = actv2[p]; gate = gate2[p]
            for hk in range(128):
                b_ = gcount["n"] % NG
                gcount["n"] += 1
                P.dma("pool", lambda e, b_=b_, hk=hk: e.indirect_dma_start(
                    out=gb[b_][:], out_offset=None, in_=u_bf, in_offset=bass.IndirectOffsetOnAxis(ap=bidx[:, hk:hk + 1], axis=0)),
                    reads=[f"bidx{p}", "utab"], writes=[f"gb{b_}"])
                P.op("dve", lambda e, b_=b_, hk=hk: e.scalar_tensor_tensor(out=scr2[:], in0=gb[b_][:], scalar=1.0, in1=hb[:], op0=ALU.mult, op1=ALU.mult,
                                                                         accum_out=pre[:, hk:hk + 1]),
                     reads=[f"gb{b_}", f"H2B{p}"], writes=SCRK + [f"pre{p}"])
            P.op("act", lambda e: e.activation(out=actv[:], in_=pre[:], func=AF.Gelu), reads=[f"pre{p}"], writes=[f"actv{p}a"])
            P.op("dve", lambda e: e.tensor_tensor(out=actv[:], in0=actv[:], in1=gate[:].rearrange("p h k -> p (h k)"), op=ALU.mult),
                 reads=[f"actv{p}a", f"gate{p}"], writes=[f"actv{p}"])

        def v_phase(i):
            p = i % 2
            bidx = bidx2[p]; actv = actv2[p]
            for hk in range(128):
                b_ = gcount["n"] % NG
                gcount["n"] += 1
                dq = hk % 2
                P.dma("pool", lambda e, b_=b_, hk=hk: e.indirect_dma_start(
                    out=gb[b_][:], out_offset=None, in_=v_bf, in_offset=bass.IndirectOffsetOnAxis(ap=bidx[:, hk:hk + 1], axis=0)),
                    reads=[f"bidx{p}", "vtab"], writes=[f"gb{b_}"])
                P.op("act", lambda e, dq=dq, hk=hk: e.activation(out=dg[dq][:], in_=ident_b[:], func=AF.Copy, scale=actv[:, hk:hk + 1]),
                     reads=[f"actv{p}", "ident_b"], writes=[f"dg{dq}"])

                def mmV(e, b_=b_, dq=dq, hk=hk):
                    for nb in range(4):
                        r = e.matmul(pf[nb][:, :], lhsT=dg[dq][:], rhs=gb[b_][:, nb * 512:(nb + 1) * 512], start=(hk == 0), stop=(hk == 127))
                    return r
                P.op("pe", mmV, reads=[f"dg{dq}", f"gb{b_}"], writes=["pf0", "pf1", "pf2", "pf3"])

        def final(i):
            p = i % 2
            t = XT[p]
            for nb in range(4):
                cs_ = slice(nb * 512, (nb + 1) * 512)
                P.op("dve", lambda e, nb=nb, cs_=cs_: e.tensor_tensor(out=acc[:, cs_], in0=pf[nb][:, :], in1=g2row[:, cs_], op=ALU.mult),
                     reads=[f"pf{nb}", "g2row"], writes=["acc"])
            P.op("dve", lambda e: e.tensor_tensor(out=acc[:], in0=acc[:], in1=t[:], op=ALU.add), reads=["acc", f"XT{p}"], writes=["acc"])
            P.dma("sp", lambda e: e.dma_start(out=out_d[i * 128:(i + 1) * 128, :], in_=acc[:]), reads=["acc"], writes=[f"out{i}", "acc"],
                  semkey=("outst",))

        if stop_after == "pdbg2":
            prep_topk(0)
            u_phase(0)
            v_phase(0)
            for nb in range(4):
                P.op("dve", lambda e, nb=nb: e.tensor_copy(out=acc[:, nb * 512:(nb + 1) * 512], in_=pf[nb][:, :]), reads=[f"pf{nb}"], writes=["acc"])
            o1 = dbg_out("d_acc", [128, D]); o2 = dbg_out("d_bidx", [128, 128], I32); o3 = dbg_out("d_actv", [128, 128]); o4 = dbg_out("d_dg", [128, 128], BF16)
            P.dma("sp", lambda e: e.dma_start(out=o1, in_=acc[:]), reads=["acc"], writes=["o1"])
            P.dma("sp", lambda e: e.dma_start(out=o2, in_=bidx2[0][:]), reads=["bidx0"], writes=["o2"])
            P.dma("sp", lambda e: e.dma_start(out=o3, in_=actv2[0][:]), reads=["actv0"], writes=["o3"])
            P.dma("sp", lambda e: e.dma_start(out=o4, in_=dg[1][:]), reads=["dg1"], writes=["o4"])
            return finish(["o1", "o2", "o3", "o4"])
        if stop_after == "pdbg":
            prep_topk(0)
            u_phase(0)
            o1 = dbg_out("d_pre", [128, 128]); o2 = dbg_out("d_bidx", [128, 128], I32); o3 = dbg_out("d_actv", [128, 128])
            o4 = dbg_out("d_ubf", [128, D], BF16); o5 = dbg_out("d_gate", [128, 128]); o6 = dbg_out("d_h2b", [128, D], BF16)
            P.dma("sp", lambda e: e.dma_start(out=o1, in_=pre2[0][:]), reads=["pre0"], writes=["o1"])
            P.dma("sp", lambda e: e.dma_start(out=o2, in_=bidx2[0][:]), reads=["bidx0"], writes=["o2"])
            P.dma("sp", lambda e: e.dma_start(out=o3, in_=actv2[0][:]), reads=["actv0"], writes=["o3"])
            P.dma("sp", lambda e: e.dma_start(out=o4, in_=u_bf[1024:1152, :]), reads=["utab"], writes=["o4"])
            P.dma("sp", lambda e: e.dma_start(out=o5, in_=gate2[0][:].rearrange("p h k -> p (h k)")), reads=["gate0"], writes=["o5"])
            P.dma("sp", lambda e: e.dma_start(out=o6, in_=H2B[0][:]), reads=["H2B0"], writes=["o6"])
            return finish(["o1", "o2", "o3", "o4", "o5", "o6"])
        for i in range(nblk):
            prep_topk(i)
            if i > 0:
                final(i - 1)
            u_phase(i)
            v_phase(i)
        final(nblk - 1)
        return finish([f"out{i}" for i in range(nblk)])


_CACHE = {}


def _consts():
    ident = np.eye(128, dtype=np.float32)
    k = np.arange(128)
    tri = (k[None, :] >= k[:, None]).astype(np.float32)
    negmask = np.where(k[None, :] >= k[:, None], 0.0, -30000.0).astype(np.float32)
    invf = (1.0 / (10000.0 ** (np.arange(32, dtype=np.float32) * (2.0 / 64)))).astype(np.float32)
    invf = np.broadcast_to(invf[None, :], (128, 32)).copy()
    return ident, tri, negmask, invf


def kernel(**inputs):
    if "nc" not in _CACHE:
        _CACHE["nc"] = build()
    nc = _CACHE["nc"]
    ident, tri, negmask, invf = _consts()
    f = lambda a: np.ascontiguousarray(np.asarray(a))
    shared = {
        "w_ada": f(inputs["w_ada"][0]), "b_ada": f(inputs["b_ada"][0]).reshape(1, -1), "w_in": f(inputs["w_in"][0]),
        "q_a_norm": f(inputs["q_a_norm"][0]), "w_uq": f(inputs["w_uq"][0]), "kv_a_norm": f(inputs["kv_a_norm"][0]),
        "w_ukv": f(inputs["w_ukv"][0]), "q_norm": f(inputs["q_norm"][0]), "k_norm": f(inputs["k_norm"][0]),
        "attn_out_norm": f(inputs["attn_out_norm"][0]), "conv_w": f(inputs["conv_w"][0]), "conv_b": f(inputs["conv_b"][0]),
        "dt_bias": f(inputs["dt_bias"][0]), "a_log": f(inputs["a_log"][0]), "d_skip": f(inputs["d_skip"][0]),
        "ssd_norm": f(inputs["ssd_norm"][0]), "w_out": f(inputs["w_out"][0]), "w_query": f(inputs["w_query"][0]),
        "sub_keys": f(inputs["sub_keys"][0]).reshape(16, 128, 128), "u_experts": f(inputs["u_experts"][0]),
        "v_experts": f(inputs["v_experts"][0]), "ident": ident, "tri": tri, "negmask": negmask, "invfreq": invf,
    }
    x = np.asarray(inputs["x"]); c = np.asarray(inputs["c"]); pos = np.asarray(inputs["positions"])
    in_maps = []
    for b in range(8):
        m = dict(shared)
        m["x"] = f(x[b]); m["c"] = f(c[b]); m["pos"] = f(pos[b]).reshape(S, 1).astype(np.int32)
        in_maps.append(m)
    res = run_bass_kernel_spmd(nc, in_maps, core_ids=list(range(8)))
    return np.stack([np.asarray(r["out"]) for r in res.results], axis=0).astype(np.float32)
```

```python
from contextlib import ExitStack
import os
import numpy as np
import concourse.bass as bass
import concourse.mybir as mybir
from concourse.bass_utils import run_bass_kernel_spmd

F32 = mybir.dt.float32
BF16 = mybir.dt.bfloat16
I32 = mybir.dt.int32
AF = mybir.ActivationFunctionType
ALU = mybir.AluOpType
AX = mybir.AxisListType

D = 2048
S = 2048
NB = 16
EPS = 1e-6
ENGS = ("pe", "act", "dve", "pool", "sp")
SEM_EPOCH = 20000


class Op:
    __slots__ = ("eng", "fn", "reads", "writes", "dma", "semkey", "deps", "waits", "signal", "idx", "barrier")

    def __init__(self, eng, fn, reads, writes, dma, semkey):
        self.eng, self.fn = eng, fn
        self.reads, self.writes = tuple(reads), tuple(writes)
        self.dma, self.semkey = dma, semkey
        self.deps, self.waits, self.signal = (), [], None
        self.barrier = False


class Prog:
    def __init__(self, nc):
        self.nc = nc
        self.ops = []
        self.self_sync = ("dve", "act", "pool")

    def op(self, eng, fn, reads=(), writes=()):
        xr = [k for k in reads if isinstance(k, str) and k[:2] in ("pf", "pt") and k not in writes]
        writes = tuple(writes) + tuple(xr)
        o = Op(eng, fn, reads, writes, False, None)
        self.ops.append(o)
        return o

    def dma(self, eng, fn, reads=(), writes=(), semkey=None):
        if semkey is None:
            semkey = ("dma",) + (tuple(writes) if writes else tuple(reads))
        o = Op(eng, fn, reads, writes, True, semkey)
        self.ops.append(o)
        return o

    def barrier(self, fn):
        o = Op("act", fn, (), (), False, None)
        o.barrier = True
        self.ops.append(o)
        return o

    def _analyze(self):
        last_write, readers = {}, {}
        last_on = {}
        dmas_since = []
        last_barrier = None
        for i, o in enumerate(self.ops):
            o.idx = i
            deps = set()
            if o.barrier:
                deps.update(last_on.values())
                deps.update(dmas_since)
                dmas_since = []
                if last_barrier is not None:
                    deps.add(last_barrier)
                last_barrier = i
            elif last_barrier is not None:
                deps.add(last_barrier)
            last_on[o.eng] = i
            if o.dma:
                dmas_since.append(i)
            for t in o.reads:
                if t in last_write:
                    deps.add(last_write[t])
            for t in o.writes:
                if t in last_write:
                    deps.add(last_write[t])
                deps.update(readers.get(t, ()))
            deps.discard(i)
            o.deps = deps
            for t in o.reads:
                readers.setdefault(t, []).append(i)
            for t in o.writes:
                last_write[t] = i
                readers[t] = []
        need = set()
        for o in self.ops:
            for d in o.deps:
                od = self.ops[d]
                if od.dma or od.eng != o.eng or o.eng in self.self_sync:
                    need.add(d)
        cnt = {e: 0 for e in ENGS}
        dcnt = {}
        self.semkeys = []
        for o in self.ops:
            if o.dma:
                k = dcnt.get(o.semkey, 0) + 1
                dcnt[o.semkey] = k
                if k == 1:
                    self.semkeys.append(o.semkey)
                o.signal = (("d", o.semkey), 16 * k, 16)
            elif o.idx in need:
                cnt[o.eng] += 1
                ep, v = divmod(cnt[o.eng] - 1, SEM_EPOCH)
                o.signal = (("e", o.eng, ep), v + 1, 1)
        self.nepochs = {e: (cnt[e] + SEM_EPOCH - 1) // SEM_EPOCH for e in ENGS}
        for o in self.ops:
            w = {}
            for d in o.deps:
                od = self.ops[d]
                if od.dma or od.eng != o.eng or o.eng in self.self_sync:
                    s, v, _ = od.signal
                    if w.get(s, 0) < v:
                        w[s] = v
            o.waits = sorted(w.items(), key=lambda kv: str(kv[0]))

    def emit(self):
        nc = self.nc
        self._analyze()
        with ExitStack() as es:
            sems = {}
            n = 0
            for e in ENGS:
                for ep in range(self.nepochs[e]):
                    sems[("e", e, ep)] = es.enter_context(nc.semaphore(f"s_{e}{ep}"))
                    n += 1
            for k in self.semkeys:
                sems[("d", k)] = es.enter_context(nc.semaphore(f"d{n}"))
                n += 1
            self.nsems = n
            by_eng = {e: [o for o in self.ops if o.eng == e] for e in ENGS}

            def run(engine, name):
                waited = {}
                for o in by_eng[name]:
                    for s, v in o.waits:
                        if waited.get(s, 0) < v:
                            engine.wait_ge(sems[s], v)
                            waited[s] = v
                    ins = o.fn(engine)
                    if o.signal is not None:
                        s, v, inc = o.signal
                        ins.then_inc(sems[s], inc)

            with nc.Block() as block:
                @block.tensor
                def _(e):
                    run(e, "pe")

                @block.scalar
                def _(e):
                    run(e, "act")

                @block.vector
                def _(e):
                    run(e, "dve")

                @block.gpsimd
                def _(e):
                    run(e, "pool")

                @block.sync
                def _(e):
                    run(e, "sp")


def bcast(ap, shape):
    return ap.to_broadcast(list(shape))


def build(stop_after=None, debug=False, nblk=NB, ngroups=2):
    nc = bass.Bass("TRN2", target_bir_lowering=False)
    P = Prog(nc)

    def din(name, shape, dt=F32):
        return nc.dram_tensor(name, list(shape), dt, kind="ExternalInput").ap()

    x_d = din("x", [S, D]); c_d = din("c", [D]); pos_d = din("pos", [S, 1], I32)
    w_ada = din("w_ada", [D, 6 * D]); b_ada = din("b_ada", [1, 6 * D]); w_in = din("w_in", [D, 3408])
    q_a_norm = din("q_a_norm", [512]); w_uq = din("w_uq", [512, 1536]); kv_a_norm = din("kv_a_norm", [256])
    w_ukv = din("w_ukv", [256, 2048]); q_norm = din("q_norm", [192]); k_norm = din("k_norm", [192])
    attn_out_norm = din("attn_out_norm", [1024]); conv_w = din("conv_w", [4, 1536]); conv_b = din("conv_b", [1536])
    dt_bias = din("dt_bias", [16]); a_log = din("a_log", [16]); d_skip = din("d_skip", [16])
    ssd_norm = din("ssd_norm", [1024]); w_out = din("w_out", [D, D]); w_query = din("w_query", [D, D])
    sub_keys = din("sub_keys", [16, 128, 128])
    if stop_after in (None, "pdbg", "pdbg2"):
        u_exp = din("u_experts", [16384, D]); v_exp = din("v_experts", [16384, D])
    ident_d = din("ident", [128, 128]); tri_d = din("tri", [128, 128]); negm_d = din("negmask", [128, 128])
    invf_d = din("invfreq", [128, 32])
    out_d = nc.dram_tensor("out", [S, D], F32, kind="ExternalOutput").ap()
    mod_d = nc.dram_tensor("mod_scr", [6 * D], F32, kind="Internal").ap()
    x1_d = nc.dram_tensor("x1_scr", [S, D], F32, kind=("ExternalOutput" if stop_after == "x1" else "Internal")).ap()
    dbg = {}
    conv_pending = []
    if stop_after in (None, "pdbg", "pdbg2"):
        u_bf = nc.dram_tensor("u_bf", [16384, D], BF16, kind="Internal").ap()
        v_bf = nc.dram_tensor("v_bf", [16384, D], BF16, kind="Internal").ap()
        for r0 in range(0, 16384, 1024):
            conv_pending.append((u_bf, u_exp, r0, "utab"))
            conv_pending.append((v_bf, v_exp, r0, "vtab"))

    def emit_conv(n):
        for _ in range(min(n, len(conv_pending))):
            dst, src, r0, key = conv_pending.pop(0)
            P.dma("pool", lambda e, dst=dst, src=src, r0=r0: e.dma_start(out=dst[r0:r0 + 1024, :], in_=src[r0:r0 + 1024, :]),
                  writes=[key], semkey=("conv", key))

    def dbg_out(nm, shp, dt=F32):
        dbg[nm] = nc.dram_tensor(nm, list(shp), dt, kind="ExternalOutput").ap()
        return dbg[nm]

    def finish(keys):
        P.op("sp", lambda e: None, reads=keys)
        P.emit()
        return nc

    top = ExitStack()

    def sb(es, name, shape, dt=F32):
        return es.enter_context(nc.sbuf_tensor(name, list(shape), dt))

    pf = [top.enter_context(nc.psum_tensor(f"pf{i}", [128, 512], F32)) for i in range(6)]
    pt = [top.enter_context(nc.psum_tensor(f"pt{i}", [128, 1024], BF16)) for i in range(2)]

    ident_f = sb(top, "ident_f", [128, 128]); ident_b = sb(top, "ident_b", [128, 128], BF16)
    tri_f = sb(top, "tri_f", [128, 128]); tri_b = sb(top, "tri_b", [128, 128], BF16)
    ones_b = sb(top, "ones_b", [128, 128], BF16); negm = sb(top, "negm", [128, 128])
    invf = sb(top, "invf", [128, 32]); bar = sb(top, "bar", [128, 2])
    BAR = lambda: P.barrier(lambda e: e.copy(out=bar[:, 0:1], in_=bar[:, 1:2]))
    modT = sb(top, "modT", [128, 96]); scp1 = sb(top, "scp1", [128, 16]); scp2 = sb(top, "scp2", [128, 16])

    P.dma("sp", lambda e: e.dma_start(out=ident_f[:], in_=ident_d), writes=["ident_f"])
    P.dma("sp", lambda e: e.dma_start(out=tri_f[:], in_=tri_d), writes=["tri_f"])
    P.dma("sp", lambda e: e.dma_start(out=negm[:], in_=negm_d), writes=["negm"])
    P.dma("sp", lambda e: e.dma_start(out=invf[:], in_=invf_d), writes=["invf"])
    P.op("dve", lambda e: e.tensor_copy(out=ident_b[:], in_=ident_f[:]), reads=["ident_f"], writes=["ident_b"])
    P.op("dve", lambda e: e.tensor_copy(out=tri_b[:], in_=tri_f[:]), reads=["tri_f"], writes=["tri_b"])
    P.op("dve", lambda e: e.memset(ones_b[:], 1.0), writes=["ones_b"])
    P.op("act", lambda e: e.activation(out=bar[:], in_=invf[:, 0:2], func=AF.Copy), reads=["invf"], writes=["bar"])

    with ExitStack() as sa:
        cT = sb(sa, "cT", [128, 16]); cab = sb(sa, "cab", [128, 16], BF16)
        brow = sb(sa, "brow", [1, 6 * D]); modrow = sb(sa, "modrow", [1, 6 * D])
        wa = [sb(sa, f"wa{i}", [128, 16, 512], BF16) for i in range(2)]
        with nc.allow_non_contiguous_dma(reason="tiny transposed loads"):
            P.dma("sp", lambda e: e.dma_start(out=cT[:], in_=c_d.rearrange("(k p) -> p k", p=128), allow_slow_non_contiguous=True), writes=["cT"])
        P.dma("sp", lambda e: e.dma_start(out=brow[:], in_=b_ada), writes=["brow"])
        P.op("act", lambda e: e.activation(out=cab[:], in_=cT[:], func=AF.Silu), reads=["cT"], writes=["cab"])
        for j in range(24):
            w = wa[j % 2]
            P.dma("pool", lambda e, w=w, j=j: e.dma_start(
                out=w[:], in_=w_ada[:, j * 512:(j + 1) * 512].rearrange("(k p) n -> p k n", p=128)),
                writes=[f"wa{j % 2}"])

            def mmA(e, w=w):
                for k in range(16):
                    r = e.matmul(pf[0][0:1, :], lhsT=cab[:, k:k + 1], rhs=w[:, k, :], start=(k == 0), stop=(k == 15))
                return r
            P.op("pe", mmA, reads=["cab", f"wa{j % 2}"], writes=["pf0"])
            P.op("dve", lambda e, j=j: e.tensor_tensor(out=modrow[0:1, j * 512:(j + 1) * 512], in0=pf[0][0:1, :],
                                                       in1=brow[0:1, j * 512:(j + 1) * 512], op=ALU.add),
                 reads=["pf0", "brow"], writes=["modrow"])
        P.dma("sp", lambda e: e.dma_start(out=mod_d.rearrange("(o n) -> o n", o=1), in_=modrow[:]),
              reads=["modrow"], writes=["mod_d"])
        with nc.allow_non_contiguous_dma(reason="tiny transposed loads"):
            P.dma("sp", lambda e: e.dma_start(out=modT[:], in_=mod_d.rearrange("(j p) -> p j", p=128), allow_slow_non_contiguous=True),
                  reads=["mod_d"], writes=["modT"])
        P.op("dve", lambda e: e.tensor_scalar(out=scp1[:], in0=modT[:, 16:32], scalar1=1.0, scalar2=None, op0=ALU.add),
             reads=["modT"], writes=["scp1"])
        P.op("dve", lambda e: e.tensor_scalar(out=scp2[:], in0=modT[:, 64:80], scalar1=1.0, scalar2=None, op0=ALU.add),
             reads=["modT"], writes=["scp2"])
        if stop_after == "A":
            o1 = dbg_out("d_mod", [1, 6 * D]); o2 = dbg_out("d_modT", [128, 96])
            P.dma("sp", lambda e: e.dma_start(out=o1, in_=modrow[:]), reads=["modrow"], writes=["o1"])
            P.dma("sp", lambda e: e.dma_start(out=o2, in_=modT[:]), reads=["modT"], writes=["o2"])
            return finish(["o1", "o2"])
    BAR()
    sh1 = modT[:, 0:16]
    sh2 = modT[:, 48:64]

    xt = [sb(top, f"xt{i}", [128, D]) for i in range(1)]
    junk = sb(top, "junk", [128, D], BF16)
    xn = sb(top, "xn", [128, D], BF16)
    st4 = sb(top, "st4", [128, 4])
    hT = sb(top, "hT", [128, 16, 128], BF16)
    cnt = {"x": 0}
    scat = ExitStack()
    catT = sb(scat, "catT", [128, 16, S], BF16)

    def rstd_of(ss_ap, n, out_ap, rkeys, wkey, tmp_ap, tmpkey):
        P.op("act", lambda e: e.activation(out=tmp_ap, in_=ss_ap, func=AF.Sqrt, scale=1.0 / n, bias=EPS),
             reads=rkeys, writes=[tmpkey])
        P.op("dve", lambda e: e.reciprocal(out=out_ap, in_=tmp_ap), reads=[tmpkey], writes=[wkey])

    def make_hT(src_d, i, scp, sh, keep_tokmajor=None):
        s = cnt["x"] % 1
        cnt["x"] += 1
        t = xt[s]
        P.dma("sp", lambda e: e.dma_start(out=t[:], in_=src_d[i * 128:(i + 1) * 128, :]), writes=[f"xt{s}"])
        P.op("act", lambda e: e.activation(out=junk[:], in_=t[:], func=AF.Square, accum_out=st4[:, 0:1]),
             reads=[f"xt{s}"], writes=["junk", "st_ss"])
        rstd_of(st4[:, 0:1], D, st4[:, 2:3], ["st_ss"], "st_r", st4[:, 1:2], "st_t")
        P.op("dve", lambda e: e.tensor_scalar(out=xn[:], in0=t[:], scalar1=st4[:, 2:3], scalar2=None, op0=ALU.mult),
             reads=[f"xt{s}", "st_r"], writes=["xn"])
        HTM = os.environ.get("HT_MODE", "")
        if HTM == "xn":
            o_ = dbg_out("d_xn", [128, D], BF16)
            P.dma("sp", lambda e, o_=o_: e.dma_start(out=o_, in_=xn[:]), reads=["xn"], writes=["d_xn"])
            raise StopIteration
        for half in range(2):
            def trs(e, half=half):
                for k in range(8):
                    kk = half * 8 + k
                    r = e.transpose(pt[half][:, k * 128:(k + 1) * 128], xn[:, kk * 128:(kk + 1) * 128], ident_b[:])
                return r
            P.op("pe", trs, reads=["xn", "ident_b"], writes=[f"pt{half}"])
            for k in range(8):
                kk = half * 8 + k
                if (k % 2 == 0 and HTM != "act") or HTM == "dve":
                    P.op("dve", lambda e, kk=kk, k=k, half=half: e.tensor_scalar(
                        out=hT[:, kk, :], in0=pt[half][:, k * 128:(k + 1) * 128], scalar1=scp[:, kk:kk + 1],
                        scalar2=sh[:, kk:kk + 1], op0=ALU.mult, op1=ALU.add),
                        reads=[f"pt{half}", "scp1", "scp2", "modT"], writes=[f"hT{kk}"])
                else:
                    P.op("act", lambda e, kk=kk, k=k, half=half: e.activation(
                        out=hT[:, kk, :], in_=pt[half][:, k * 128:(k + 1) * 128], func=AF.Identity,
                        scale=scp[:, kk:kk + 1], bias=sh[:, kk:kk + 1]),
                        reads=[f"pt{half}", "scp1", "scp2", "modT"], writes=[f"hT{kk}"])
        return s

    hT_keys = [f"hT{k}" for k in range(16)]

    def load_w(es, name, src_ap, kchunks, ncols, eng="pool"):
        t = sb(es, name, [128, kchunks, ncols], BF16)
        step = max(1, 4096 // ncols)
        for k0 in range(0, kchunks, step):
            k1 = min(kchunks, k0 + step)
            P.dma(eng, lambda e, k0=k0, k1=k1: e.dma_start(
                out=t[:, k0:k1, :], in_=src_ap[k0 * 128:k1 * 128, :].rearrange("(k p) n -> p k n", p=128)),
                writes=[f"{name}_{k0}"], semkey=("w", name, k0))
        return t, [f"{name}_{k0}" for k0 in range(0, kchunks, step)]

    def row_tile(es, name, src_1d, n):
        t = sb(es, name, [128, n])
        P.dma("sp", lambda e: e.dma_start(out=t[:], in_=src_1d.partition_broadcast(128)), writes=[name])
        return t

    with ExitStack() as s1:
        win_a, win_a_k = load_w(s1, "win_a", w_in[:, 0:832], 16, 832)
        wuq, wuq_k = load_w(s1, "wuq", w_uq, 4, 1536)
        wukv, wukv_k = load_w(s1, "wukv", w_ukv, 2, 2048)
        qag = row_tile(s1, "qag", q_a_norm, 512); kvag = row_tile(s1, "kvag", kv_a_norm, 256)
        qg = row_tile(s1, "qg", q_norm, 192); kg = row_tile(s1, "kg", k_norm, 192)
        KTn = sb(s1, "KTn", [128, 4, S], BF16); KTr = sb(s1, "KTr", [128, 2, S], BF16)
        Vc = sb(s1, "Vc", [128, NB, 512], BF16)
        cqn = sb(s1, "cqn", [128, 512], BF16); ckvn = sb(s1, "ckvn", [128, 256], BF16)
        cqnT = sb(s1, "cqnT", [128, 4, 128], BF16); ckvnT = sb(s1, "ckvnT", [128, 2, 128], BF16)
        kr = sb(s1, "kr", [128, 64]); krr = sb(s1, "krr", [128, 64])
        qraw = sb(s1, "qraw", [128, 4, 192]); kvraw = sb(s1, "kvraw", [128, 4, 256])
        sq = sb(s1, "sq", [128, 1024]); sm = sb(s1, "sm", [128, 32])
        posi = sb(s1, "posi", [128, 1], I32); posf = sb(s1, "posf", [128, 1])
        ang = sb(s1, "ang", [128, 64]); angk = sb(s1, "angk", [128, 64]); angi = sb(s1, "angi", [128, 64], I32)
        cs = sb(s1, "cs", [128, 64])
        rt = sb(s1, "rt", [128, 4, 32, 4])
        qbn = sb(s1, "qbn", [128, 4, 128], BF16); qbr = sb(s1, "qbr", [128, 4, 64], BF16)
        kbn = sb(s1, "kbn", [128, 4, 128], BF16); kbr = sb(s1, "kbr", [128, 4, 64], BF16)
        QTn = sb(s1, "QTn", [128, 4, 128], BF16); QTr = sb(s1, "QTr", [128, 2, 128], BF16)
        PT = [sb(s1, f"PT{i}", [128, 512], BF16) for i in range(2)]
        rden = sb(s1, "rden", [128, 128])

        def rope(src3, dst3, nh, rk, wk):
            cosb = bcast(cs[:, 0:32].unsqueeze(1), [128, nh, 32])
            sinb = bcast(cs[:, 32:64].unsqueeze(1), [128, nh, 32])
            x1, x2 = src3[:, :, 0:32], src3[:, :, 32:64]
            t = [rt[:, 0:nh, :, j] for j in range(4)]
            P.op("dve", lambda e: e.tensor_tensor(out=t[0], in0=x1, in1=cosb, op=ALU.mult), reads=rk + ["cs"], writes=["rt0"])
            P.op("dve", lambda e: e.tensor_tensor(out=t[1], in0=x2, in1=sinb, op=ALU.mult), reads=rk + ["cs"], writes=["rt1"])
            P.op("dve", lambda e: e.tensor_tensor(out=t[2], in0=x2, in1=cosb, op=ALU.mult), reads=rk + ["cs"], writes=["rt2"])
            P.op("dve", lambda e: e.tensor_tensor(out=t[3], in0=x1, in1=sinb, op=ALU.mult), reads=rk + ["cs"], writes=["rt3"])
            P.op("dve", lambda e: e.tensor_tensor(out=dst3[:, :, 0:32], in0=t[0], in1=t[1], op=ALU.subtract),
                 reads=["rt0", "rt1"], writes=[wk + "a"])
            P.op("dve", lambda e: e.tensor_tensor(out=dst3[:, :, 32:64], in0=t[2], in1=t[3], op=ALU.add),
                 reads=["rt2", "rt3"], writes=[wk + "b"])

        if stop_after == "mw":
            o_ = dbg_out("d_qag", [128, 512], F32); o2_ = dbg_out("d_wuq", [128, 4, 1536], BF16)
            P.dma("sp", lambda e: e.dma_start(out=o_, in_=qag[:]), reads=["qag"], writes=["d_qag"])
            P.dma("sp", lambda e: e.dma_start(out=o2_, in_=wuq[:]), reads=wuq_k, writes=["d_wuq"])
            return finish(["d_qag", "d_wuq"])
        for G in range(ngroups):
            for i in range(nblk):
                try:
                    make_hT(x_d, i, scp1, sh1)
                except StopIteration:
                    return finish(["d_xn"])
                emit_conv(2)
                if stop_after == "m0":
                    o_ = dbg_out("d_hT", [128, 16, 128], BF16)
                    P.dma("sp", lambda e, o_=o_: e.dma_start(out=o_, in_=hT[:]), reads=hT_keys, writes=["d_hT"])
                    return finish(["d_hT"])
                P.dma("sp", lambda e, i=i: e.dma_start(out=posi[:], in_=pos_d[i * 128:(i + 1) * 128, :]), writes=["posi"])
                P.op("dve", lambda e: e.tensor_copy(out=posf[:], in_=posi[:]), reads=["posi"], writes=["posf"])
                P.op("dve", lambda e: e.tensor_scalar(out=ang[:, 0:32], in0=invf[:], scalar1=posf[:, 0:1], scalar2=None, op0=ALU.mult),
                     reads=["posf", "invf"], writes=["ang0"])
                P.op("dve", lambda e: e.tensor_scalar(out=ang[:, 32:64], in0=ang[:, 0:32], scalar1=float(np.pi / 2), scalar2=None, op0=ALU.add),
                     reads=["ang0"], writes=["ang1"])
                P.op("dve", lambda e: e.tensor_scalar(out=angk[:], in0=ang[:], scalar1=float(1.0 / (2 * np.pi)), scalar2=None, op0=ALU.mult),
                     reads=["ang0", "ang1"], writes=["angk"])
                P.op("dve", lambda e: e.tensor_copy(out=angi[:], in_=angk[:]), reads=["angk"], writes=["angi"])
                P.op("dve", lambda e: e.tensor_copy(out=angk[:], in_=angi[:]), reads=["angi"], writes=["angk2"])
                P.op("dve", lambda e: e.scalar_tensor_tensor(out=ang[:], in0=angk[:], scalar=float(-2 * np.pi), in1=ang[:],
                                                             op0=ALU.mult, op1=ALU.add), reads=["angk2", "ang0", "ang1"], writes=["angr"])
                P.op("act", lambda e: e.activation(out=cs[:, 0:32], in_=ang[:, 32:64], func=AF.Sin), reads=["angr"], writes=["cs_c"])
                P.op("act", lambda e: e.activation(out=cs[:, 32:64], in_=ang[:, 0:32], func=AF.Sin), reads=["angr", "cs_c"], writes=["cs"])

                def mmP(e):
                    for k in range(16):
                        e.matmul(pf[0][:, :], lhsT=hT[:, k, :], rhs=win_a[:, k, 0:512], start=(k == 0), stop=(k == 15))
                    for k in range(16):
                        r = e.matmul(pf[1][:, 0:320], lhsT=hT[:, k, :], rhs=win_a[:, k, 512:832], start=(k == 0), stop=(k == 15))
                    return r
                P.op("pe", mmP, reads=hT_keys + win_a_k, writes=["pf0", "pf1"])
                P.op("act", lambda e: e.activation(out=sq[:, 0:512], in_=pf[0][:, :], func=AF.Square, accum_out=sm[:, 0:1]),
                     reads=["pf0"], writes=["sq", "sm0"])
                rstd_of(sm[:, 0:1], 512, sm[:, 2:3], ["sm0"], "sm2", sm[:, 1:2], "sm1")
                P.op("dve", lambda e: e.scalar_tensor_tensor(out=cqn[:], in0=pf[0][:, :], scalar=sm[:, 2:3], in1=qag[:],
                                                             op0=ALU.mult, op1=ALU.mult), reads=["pf0", "sm2", "qag"], writes=["cqn"])
                P.op("act", lambda e: e.activation(out=sq[:, 0:256], in_=pf[1][:, 0:256], func=AF.Square, accum_out=sm[:, 3:4]),
                     reads=["pf1", "sm0"], writes=["sq", "sm3"])
                rstd_of(sm[:, 3:4], 256, sm[:, 5:6], ["sm3"], "sm5", sm[:, 4:5], "sm4")
                P.op("dve", lambda e: e.scalar_tensor_tensor(out=ckvn[:], in0=pf[1][:, 0:256], scalar=sm[:, 5:6], in1=kvag[:],
                                                             op0=ALU.mult, op1=ALU.mult), reads=["pf1", "sm5", "kvag"], writes=["ckvn"])
                P.op("act", lambda e: e.copy(out=kr[:], in_=pf[1][:, 256:320]), reads=["pf1"], writes=["kr"])
                P.op("act", lambda e: e.activation(out=sq[:, 0:64], in_=kr[:], func=AF.Square, accum_out=sm[:, 6:7]),
                     reads=["kr", "sm3"], writes=["sq", "sm6"])
                def trC(e):
                    for c in range(4):
                        e.transpose(pt[0][:, c * 128:(c + 1) * 128], cqn[:, c * 128:(c + 1) * 128], ident_b[:])
                    for c in range(2):
                        r = e.transpose(pt[0][:, (4 + c) * 128:(5 + c) * 128], ckvn[:, c * 128:(c + 1) * 128], ident_b[:])
                    return r
                P.op("pe", trC, reads=["cqn", "ckvn", "ident_b"], writes=["pt0"])
                P.op("dve", lambda e: e.tensor_copy(out=cqnT[:].rearrange("p c t -> p (c t)"), in_=pt[0][:, 0:512]),
                     reads=["pt0"], writes=["cqnT"])
                P.op("act", lambda e: e.copy(out=ckvnT[:].rearrange("p c t -> p (c t)"), in_=pt[0][:, 512:768]),
                     reads=["pt0"], writes=["ckvnT"])

                if stop_after == "m1":
                    keys = []
                    o_ = dbg_out("d_cqnT", [128, 4, 128], BF16)
                    P.dma("sp", lambda e, o_=o_: e.dma_start(out=o_, in_=cqnT[:]), reads=["cqnT"], writes=["d_cqnT"])
                    keys.append("d_cqnT")
                    o_ = dbg_out("d_cs", [128, 64], F32)
                    P.dma("sp", lambda e, o_=o_: e.dma_start(out=o_, in_=cs[:]), reads=["cs"], writes=["d_cs"])
                    keys.append("d_cs")
                    o_ = dbg_out("d_kr", [128, 64], F32)
                    P.dma("sp", lambda e, o_=o_: e.dma_start(out=o_, in_=kr[:]), reads=["kr"], writes=["d_kr"])
                    keys.append("d_kr")
                    return finish(keys)
                def mmQ(e, G=G):
                    for c in range(4):
                        e.matmul(pf[2][:, :], lhsT=cqnT[:, c, :], rhs=wuq[:, c, G * 768:G * 768 + 512], start=(c == 0), stop=(c == 3))
                    for c in range(4):
                        r = e.matmul(pf[3][:, 0:256], lhsT=cqnT[:, c, :], rhs=wuq[:, c, G * 768 + 512:G * 768 + 768], start=(c == 0), stop=(c == 3))
                    return r
                P.op("pe", mmQ, reads=["cqnT"] + wuq_k, writes=["pf2", "pf3"])
                qflat = qraw[:].rearrange("p h d -> p (h d)")
                P.op("act", lambda e: e.copy(out=qflat[:, 0:512], in_=pf[2][:, :]), reads=["pf2"], writes=["qraw0"])
                P.op("dve", lambda e: e.tensor_copy(out=qflat[:, 512:768], in_=pf[3][:, 0:256]), reads=["pf3"], writes=["qraw1"])
                P.op("act", lambda e: e.activation(out=sq[:, 0:768], in_=qflat, func=AF.Square),
                     reads=["qraw0", "qraw1", "sm6"], writes=["sq"])
                P.op("dve", lambda e: e.tensor_reduce(out=sm[:, 8:12], in_=sq[:, 0:768].rearrange("p (h d) -> p h d", h=4),
                                                      axis=AX.X, op=ALU.add), reads=["sq"], writes=["sm8"])
                rstd_of(sm[:, 8:12], 192, sm[:, 16:20], ["sm8"], "sm16", sm[:, 12:16], "sm12")
                P.op("dve", lambda e: e.tensor_tensor(out=qraw[:], in0=qraw[:], in1=bcast(sm[:, 16:20].unsqueeze(2), [128, 4, 192]), op=ALU.mult),
                     reads=["qraw0", "qraw1", "sm16"], writes=["qraw2"])
                P.op("dve", lambda e: e.tensor_tensor(out=qraw[:], in0=qraw[:], in1=bcast(qg[:].unsqueeze(1), [128, 4, 192]), op=ALU.mult),
                     reads=["qraw2", "qg"], writes=["qraw3"])
                P.op("act", lambda e: e.copy(out=qbn[:], in_=qraw[:, :, 0:128]), reads=["qraw3"], writes=["qbn"])
                rope(qraw[:, :, 128:192], qbr[:], 4, ["qraw3"], "qbr")

                def trQ(e):
                    for hh in range(4):
                        e.transpose(pt[1][:, hh * 128:(hh + 1) * 128], qbn[:, hh, :], ident_b[:])
                    qr2 = qbr[:].rearrange("p (a b) d -> p a (b d)", b=2)
                    for pp in range(2):
                        r = e.transpose(pt[1][:, (4 + pp) * 128:(5 + pp) * 128], qr2[:, pp, :], ident_b[:])
                    return r
                P.op("pe", trQ, reads=["qbn", "qbra", "qbrb", "ident_b"], writes=["pt1"])
                P.op("dve", lambda e: e.tensor_copy(out=QTn[:].rearrange("p c t -> p (c t)"), in_=pt[1][:, 0:512]), reads=["pt1"], writes=["QTn"])
                P.op("act", lambda e: e.copy(out=QTr[:].rearrange("p c t -> p (c t)"), in_=pt[1][:, 512:768]), reads=["pt1"], writes=["QTr"])

                if stop_after == "m2":
                    keys = []
                    o_ = dbg_out("d_QTn", [128, 4, 128], BF16)
                    P.dma("sp", lambda e, o_=o_: e.dma_start(out=o_, in_=QTn[:]), reads=["QTn"], writes=["d_QTn"])
                    keys.append("d_QTn")
                    o_ = dbg_out("d_QTr", [128, 2, 128], BF16)
                    P.dma("sp", lambda e, o_=o_: e.dma_start(out=o_, in_=QTr[:]), reads=["QTr"], writes=["d_QTr"])
                    keys.append("d_QTr")
                    return finish(keys)
                def mmK(e, G=G):
                    for c in range(2):
                        e.matmul(pf[2][:, :], lhsT=ckvnT[:, c, :], rhs=wukv[:, c, G * 1024:G * 1024 + 512], start=(c == 0), stop=(c == 1))
                    for c in range(2):
                        r = e.matmul(pf[3][:, :], lhsT=ckvnT[:, c, :], rhs=wukv[:, c, G * 1024 + 512:G * 1024 + 1024], start=(c == 0), stop=(c == 1))
                    return r
                P.op("pe", mmK, reads=["ckvnT"] + wukv_k, writes=["pf2", "pf3"])
                kvflat = kvraw[:].rearrange("p h d -> p (h d)")
                P.op("act", lambda e: e.copy(out=kvflat[:, 0:512], in_=pf[2][:, :]), reads=["pf2"], writes=["kvraw0"])
                P.op("dve", lambda e: e.tensor_copy(out=kvflat[:, 512:1024], in_=pf[3][:, :]), reads=["pf3"], writes=["kvraw1"])
                P.op("act", lambda e, i=i: e.copy(out=Vc[:, i, :].rearrange("p (h d) -> p h d", h=4), in_=kvraw[:, :, 128:256]),
                     reads=["kvraw0", "kvraw1"], writes=[f"Vc{i}"])
                P.op("act", lambda e: e.activation(out=sq[:, 0:512].rearrange("p (h d) -> p h d", h=4), in_=kvraw[:, :, 0:128], func=AF.Square),
                     reads=["kvraw0", "kvraw1", "sm8"], writes=["sq"])
                P.op("dve", lambda e: e.tensor_reduce(out=sm[:, 20:24], in_=sq[:, 0:512].rearrange("p (h d) -> p h d", h=4),
                                                      axis=AX.X, op=ALU.add), reads=["sq"], writes=["sm20"])
                P.op("dve", lambda e: e.tensor_scalar(out=sm[:, 20:24], in0=sm[:, 20:24], scalar1=sm[:, 6:7], scalar2=None, op0=ALU.add),
                     reads=["sm20", "sm6"], writes=["sm20b"])
                rstd_of(sm[:, 20:24], 192, sm[:, 28:32], ["sm20b"], "sm28", sm[:, 24:28], "sm24")
                P.op("dve", lambda e: e.tensor_tensor(out=kvraw[:, :, 0:128], in0=kvraw[:, :, 0:128],
                                                      in1=bcast(sm[:, 28:32].unsqueeze(2), [128, 4, 128]), op=ALU.mult),
                     reads=["kvraw0", "kvraw1", "sm28"], writes=["kvraw2"])
                P.op("dve", lambda e: e.tensor_tensor(out=kbn[:], in0=kvraw[:, :, 0:128], in1=bcast(kg[:, 0:128].unsqueeze(1), [128, 4, 128]), op=ALU.mult),
                     reads=["kvraw2", "kg"], writes=["kbn"])
                P.op("dve", lambda e: e.tensor_tensor(out=kr[:], in0=kr[:], in1=kg[:, 128:192], op=ALU.mult), reads=["kr", "kg", "sm6"], writes=["krg"])
                rope(kr[:].unsqueeze(1), krr[:].unsqueeze(1), 1, ["krg"], "krr")
                P.op("dve", lambda e: e.tensor_tensor(out=kbr[:], in0=bcast(krr[:].unsqueeze(1), [128, 4, 64]),
                                                      in1=bcast(sm[:, 28:32].unsqueeze(2), [128, 4, 64]), op=ALU.mult),
                     reads=["krra", "krrb", "sm28"], writes=["kbr"])

                def trK(e):
                    for hh in range(4):
                        e.transpose(pt[0][:, hh * 128:(hh + 1) * 128], kbn[:, hh, :], ident_b[:])
                    kr2 = kbr[:].rearrange("p (a b) d -> p a (b d)", b=2)
                    for pp in range(2):
                        r = e.transpose(pt[0][:, (4 + pp) * 128:(5 + pp) * 128], kr2[:, pp, :], ident_b[:])
                    return r
                P.op("pe", trK, reads=["kbn", "kbr", "ident_b"], writes=["pt0"])
                P.op("dve", lambda e, i=i: e.tensor_copy(out=KTn[:, :, i * 128:(i + 1) * 128],
                                                         in_=pt[0][:, 0:512].rearrange("p (c t) -> p c t", c=4)), reads=["pt0"], writes=[f"KTn{i}"])
                P.op("act", lambda e, i=i: e.copy(out=KTr[:, :, i * 128:(i + 1) * 128],
                                                  in_=pt[0][:, 512:768].rearrange("p (c t) -> p c t", c=2)), reads=["pt0"], writes=[f"KTr{i}"])

                if stop_after == "m3":
                    keys = []
                    o_ = dbg_out("d_KTn", [128, 4, 128], BF16)
                    P.dma("sp", lambda e, o_=o_: e.dma_start(out=o_, in_=KTn[:, :, 0:128]), reads=["KTn0"], writes=["d_KTn"])
                    keys.append("d_KTn")
                    o_ = dbg_out("d_KTr", [128, 2, 128], BF16)
                    P.dma("sp", lambda e, o_=o_: e.dma_start(out=o_, in_=KTr[:, :, 0:128]), reads=["KTr0"], writes=["d_KTr"])
                    keys.append("d_KTr")
                    return finish(keys)
                for hh in range(4):
                    h = G * 4 + hh
                    pp, hf = hh // 2, hh % 2
                    ngrp = (i + 4) // 4
                    for g in range(ngrp):
                        j0, j1 = g * 4, min(i + 1, g * 4 + 4)
                        sbank = 4 + (g % 2)
                        pbuf = PT[g % 2]

                        def mmS(e, j0=j0, j1=j1, sbank=sbank, hh=hh, pp=pp, hf=hf):
                            for j in range(j0, j1):
                                o = pf[sbank][:, (j - j0) * 128:(j - j0 + 1) * 128]
                                e.matmul(o, lhsT=KTn[:, hh, j * 128:(j + 1) * 128], rhs=QTn[:, hh, :], start=True, stop=False)
                                r = e.matmul(o, lhsT=KTr[hf * 64:(hf + 1) * 64, pp, j * 128:(j + 1) * 128],
                                             rhs=QTr[hf * 64:(hf + 1) * 64, pp, :], start=False, stop=True)
                            return r
                        P.op("pe", mmS, reads=["QTn", "QTr"] + [f"KTn{j}" for j in range(j0, j1)] + [f"KTr{j}" for j in range(j0, j1)],
                             writes=[f"pf{sbank}"])
                        w = (j1 - j0) * 128
                        P.op("act", lambda e, sbank=sbank, pbuf=pbuf, w=w: e.activation(out=pbuf[:, 0:w], in_=pf[sbank][:, 0:w], func=AF.Exp,
                                                                                     scale=float(192 ** -0.5)),
                             reads=[f"pf{sbank}"], writes=[f"PT{g % 2}"])
                        if j1 == i + 1:
                            dcol = (i - j0) * 128
                            P.op("dve", lambda e, pbuf=pbuf, dcol=dcol: e.tensor_tensor(out=pbuf[:, dcol:dcol + 128], in0=pbuf[:, dcol:dcol + 128],
                                                                                      in1=tri_b[:], op=ALU.mult),
                                 reads=[f"PT{g % 2}", "tri_b"], writes=[f"PT{g % 2}"])

                        def mmO(e, j0=j0, j1=j1, pbuf=pbuf, hh=hh, i=i):
                            for j in range(j0, j1):
                                pj = pbuf[:, (j - j0) * 128:(j - j0 + 1) * 128]
                                e.matmul(pf[2][:, 0:128], lhsT=Vc[:, j, hh * 128:(hh + 1) * 128], rhs=pj, start=(j == 0), stop=(j == i))
                                r = e.matmul(pf[3][:, 0:128], lhsT=ones_b[:], rhs=pj, start=(j == 0), stop=(j == i))
                            return r
                        P.op("pe", mmO, reads=[f"PT{g % 2}", "ones_b"] + [f"Vc{j}" for j in range(j0, j1)], writes=["pf2", "pf3"])
                    P.op("dve", lambda e: e.reciprocal(out=rden[:], in_=pf[3][:, 0:128]), reads=["pf3"], writes=["rden"])
                    P.op("dve", lambda e, h=h, i=i: e.tensor_tensor(out=catT[:, h, i * 128:(i + 1) * 128], in0=pf[2][:, 0:128], in1=rden[:], op=ALU.mult),
                         reads=["pf2", "rden"], writes=[f"cat{h}_{i}"])

    BAR()
    with ExitStack() as s1:
        aog = sb(s1, "aog", [128, 8])
        with nc.allow_non_contiguous_dma(reason="tiny transposed loads"):
            P.dma("sp", lambda e: e.dma_start(out=aog[:], in_=attn_out_norm.rearrange("(j p) -> p j", p=128), allow_slow_non_contiguous=True), writes=["aog"])
        sqb = sb(s1, "sqb", [128, 8, 512], BF16); rnb = sb(s1, "rnb", [128, 512]); rnb2 = sb(s1, "rnb2", [128, 512])
        for tb in range((nblk + 3) // 4):
            wd = min(512, nblk * 128 - tb * 512)
            cols = slice(tb * 512, tb * 512 + wd)
            blks = range(tb * 4, min(nblk, tb * 4 + 4))
            ck = [f"cat{h}_{i}" for h in range(8) for i in blks]
            P.op("act", lambda e, cols=cols, wd=wd: e.activation(out=sqb[:, :, 0:wd], in_=catT[:, 0:8, cols], func=AF.Square), reads=ck, writes=["sqb"])

            def mmN(e, wd=wd):
                for h in range(8):
                    r = e.matmul(pf[0][:, 0:wd], lhsT=ones_b[:], rhs=sqb[:, h, 0:wd], start=(h == 0), stop=(h == 7))
                return r
            P.op("pe", mmN, reads=["sqb", "ones_b"], writes=["pf0"])
            P.op("act", lambda e, wd=wd: e.activation(out=rnb[:, 0:wd], in_=pf[0][:, 0:wd], func=AF.Sqrt, scale=1.0 / 1024, bias=EPS), reads=["pf0"], writes=["rnb"])
            P.op("dve", lambda e, wd=wd: e.reciprocal(out=rnb2[:, 0:wd], in_=rnb[:, 0:wd]), reads=["rnb"], writes=["rnb2"])
            for h in range(8):
                P.op("dve", lambda e, h=h, cols=cols, wd=wd: e.scalar_tensor_tensor(out=catT[:, h, cols], in0=catT[:, h, cols], scalar=aog[:, h:h + 1],
                                                                                  in1=rnb2[:, 0:wd], op0=ALU.mult, op1=ALU.mult),
                     reads=["rnb2", "aog"] + [f"cat{h}_{i}" for i in blks],
                     writes=[f"cat{h}_{i}" for i in blks])
    BAR()
    if stop_after == "mla":
        o1 = dbg_out("d_catA", [128, 8, S], BF16)
        P.dma("sp", lambda e: e.dma_start(out=o1[:, :, 0:nblk * 128], in_=catT[:, 0:8, 0:nblk * 128]), reads=[f"cat{h}_{i}" for h in range(8) for i in range(nblk)], writes=["o1"])
        return finish(["o1"])
    with ExitStack() as s2:
        wxb = [sb(s2, f"wxb{q}", [128, 16, 128], BF16) for q in range(2)]
        wzb = sb(s2, "wzb", [128, 16, 512], BF16)
        win_d, win_d_k = load_w(s2, "win_d", w_in[:, 3392:3408], 16, 16)
        wcnt = {"n": 0}
        cw = sb(s2, "cw", [128, 12, 4]); cb = sb(s2, "cb", [128, 12])
        with nc.allow_non_contiguous_dma(reason="tiny transposed loads"):
            for k in range(4):
                P.dma("sp", lambda e, k=k: e.dma_start(out=cw[:, :, k], in_=conv_w[k, :].rearrange("(j p) -> p j", p=128), allow_slow_non_contiguous=True),
                      writes=[f"cw{k}"])
            P.dma("sp", lambda e: e.dma_start(out=cb[:], in_=conv_b.rearrange("(j p) -> p j", p=128), allow_slow_non_contiguous=True), writes=["cb"])
        cwk = [f"cw{k}" for k in range(4)]
        dtb = row_tile(s2, "dtb", dt_bias, 16); arow = row_tile(s2, "arow", a_log, 16)
        dsk = row_tile(s2, "dsk", d_skip, 16); sng = row_tile(s2, "sng", ssd_norm, 1024)
        P.op("act", lambda e: e.activation(out=arow[:], in_=arow[:], func=AF.Exp), reads=["arow"], writes=["arow_e"])
        P.op("dve", lambda e: e.tensor_scalar(out=arow[:], in0=arow[:], scalar1=-1.0, scalar2=None, op0=ALU.mult), reads=["arow_e"], writes=["arow_n"])
        xraw = sb(s2, "xraw", [128, 12, 131]); xacc = sb(s2, "xacc", [128, 12, 128])
        xcT = sb(s2, "xcT", [128, 12, 128], BF16)
        xs = sb(s2, "xs", [128, 16, 64]); xd = sb(s2, "xd", [128, 16, 64], BF16); xdd = sb(s2, "xdd", [128, 16, 64], BF16)
        zs = sb(s2, "zs", [128, 1024]); dts = sb(s2, "dts", [128, 16]); adt = sb(s2, "adt", [128, 16])
        ahi = sb(s2, "ahi", [128, 16], BF16); alo = sb(s2, "alo", [128, 16], BF16); ahf = sb(s2, "ahf", [128, 16])
        Rhi = sb(s2, "Rhi", [128, 16, 128], BF16); Rlo = sb(s2, "Rlo", [128, 16, 128], BF16)
        acs = sb(s2, "acs", [128, 16]); alast = sb(s2, "alast", [128, 16]); ea = sb(s2, "ea", [128, 16])
        dsd = sb(s2, "dsd", [128, 16]); cdec = sb(s2, "cdec", [128, 16])
        dif = sb(s2, "dif", [128, 4, 128]); Ed = sb(s2, "Ed", [128, 4, 128])
        MT = sb(s2, "MT", [128, 16, 128], BF16); CBT = sb(s2, "CBT", [128, 2, 128])
        Btok = sb(s2, "Btok", [128, 2, 128], BF16)
        Sst = sb(s2, "Sst", [128, 16, 64]); Sbf = sb(s2, "Sbf", [128, 16, 64], BF16)
        yv = sb(s2, "yv", [128, 16, 64]); ytmp = sb(s2, "ytmp", [128, 16, 64]); ybf = sb(s2, "ybf", [128, 1024], BF16)
        ysm = sb(s2, "ysm", [128, 8])
        P.op("dve", lambda e: e.memset(Sst[:], 0.0), writes=["Sst"])
        P.op("dve", lambda e: e.memset(Sbf[:], 0.0), writes=["Sbf"])
        P.op("dve", lambda e: e.memset(xraw[:, :, 0:3], 0.0), writes=["xhalo"])

        for i in range(nblk):
            make_hT(x_d, i, scp1, sh1)
            for j in range(12):
                bank = j % 2
                wq_ = wcnt["n"] % 2
                wcnt["n"] += 1
                wx_ = wxb[wq_]
                P.dma("pool", lambda e, j=j, wx_=wx_: e.dma_start(
                    out=wx_[:], in_=w_in[:, 1856 + j * 128:1856 + (j + 1) * 128].rearrange("(k p) n -> p k n", p=128)), writes=[f"wxb{wq_}"])

                def mmX(e, j=j, bank=bank, wx_=wx_):
                    for k in range(16):
                        r = e.matmul(pf[bank][:, 0:128], lhsT=wx_[:, k, :], rhs=hT[:, k, :], start=(k == 0), stop=(k == 15))
                    return r
                P.op("pe", mmX, reads=hT_keys + [f"wxb{wq_}"], writes=[f"pf{bank}"])
                P.op("act", lambda e, j=j, bank=bank: e.copy(out=xraw[:, j, 3:131], in_=pf[bank][:, 0:128]),
                     reads=[f"pf{bank}", "xhalo"], writes=[f"xraw{j}"])
            xrk = [f"xraw{j}" for j in range(12)]
            for j in range(12):
                eng = "dve"
                P.op(eng, lambda e, j=j: e.tensor_scalar(out=xacc[:, j, :], in0=xraw[:, j, 0:128], scalar1=cw[:, j, 0:1], scalar2=cb[:, j:j + 1],
                                                         op0=ALU.mult, op1=ALU.add), reads=[f"xraw{j}", "cb"] + cwk, writes=[f"xacc{j}"])
                for k in range(1, 4):
                    P.op(eng, lambda e, j=j, k=k: e.scalar_tensor_tensor(out=xacc[:, j, :], in0=xraw[:, j, k:k + 128], scalar=cw[:, j, k:k + 1],
                                                                         in1=xacc[:, j, :], op0=ALU.mult, op1=ALU.add),
                         reads=[f"xraw{j}", f"xacc{j}"] + cwk, writes=[f"xacc{j}"])
                P.op("act", lambda e, j=j: e.activation(out=xcT[:, j, :], in_=xacc[:, j, :], func=AF.Silu), reads=[f"xacc{j}"], writes=[f"xcT{j}"])
                P.op(eng, lambda e, j=j: e.tensor_copy(out=xraw[:, j, 0:3], in_=xraw[:, j, 128:131]), reads=[f"xraw{j}", f"xacc{j}"], writes=[f"xraw{j}", "xhalo"])
            for half in range(2):
                P.dma("pool", lambda e, half=half: e.dma_start(
                    out=wzb[:], in_=w_in[:, 832 + half * 512:832 + (half + 1) * 512].rearrange("(k p) n -> p k n", p=128)), writes=["wzb"])

                def mmZ(e, half=half):
                    for k in range(16):
                        r = e.matmul(pf[2 + half][:, :], lhsT=hT[:, k, :], rhs=wzb[:, k, :], start=(k == 0), stop=(k == 15))
                    return r
                P.op("pe", mmZ, reads=hT_keys + ["wzb"], writes=[f"pf{2 + half}"])

            def mmD(e):
                for k in range(16):
                    r = e.matmul(pf[4][:, 0:16], lhsT=hT[:, k, :], rhs=win_d[:, k, :], start=(k == 0), stop=(k == 15))
                return r
            P.op("pe", mmD, reads=hT_keys + win_d_k, writes=["pf4"])
            P.op("act", lambda e: e.activation(out=zs[:, 0:512], in_=pf[2][:, :], func=AF.Silu), reads=["pf2"], writes=["zs0"])
            P.op("act", lambda e: e.activation(out=zs[:, 512:1024], in_=pf[3][:, :], func=AF.Silu), reads=["pf3"], writes=["zs1"])
            P.op("dve", lambda e: e.tensor_tensor(out=dts[:], in0=pf[4][:, 0:16], in1=dtb[:], op=ALU.add), reads=["pf4", "dtb"], writes=["dts0"])
            P.op("act", lambda e: e.activation(out=dts[:], in_=dts[:], func=AF.Exp), reads=["dts0"], writes=["dts1"])
            P.op("act", lambda e: e.activation(out=dts[:], in_=dts[:], func=AF.Ln, bias=1.0, scale=1.0), reads=["dts1"], writes=["dts"])
            P.op("dve", lambda e: e.tensor_tensor(out=adt[:], in0=dts[:], in1=arow[:], op=ALU.mult), reads=["dts", "arow_n"], writes=["adt"])
            P.op("dve", lambda e: e.tensor_copy(out=ahi[:], in_=adt[:]), reads=["adt"], writes=["ahi"])
            P.op("dve", lambda e: e.tensor_copy(out=ahf[:], in_=ahi[:]), reads=["ahi"], writes=["ahf"])
            P.op("dve", lambda e: e.tensor_tensor(out=alo[:], in0=adt[:], in1=ahf[:], op=ALU.subtract), reads=["adt", "ahf"], writes=["alo"])
            trib = bcast(tri_b[:].unsqueeze(1), [128, 16, 128])
            P.op("dve", lambda e: e.tensor_tensor(out=Rhi[:], in0=trib, in1=bcast(ahi[:].unsqueeze(2), [128, 16, 128]), op=ALU.mult),
                 reads=["ahi", "tri_b"], writes=["Rhi"])
            P.op("pool", lambda e: e.tensor_tensor(out=Rlo[:], in0=trib, in1=bcast(alo[:].unsqueeze(2), [128, 16, 128]), op=ALU.mult),
                 reads=["alo", "tri_b"], writes=["Rlo"])
            def mmC(e):
                e.matmul(pf[4][:, 16:32], lhsT=tri_b[:], rhs=ahi[:], start=True, stop=False)
                return e.matmul(pf[4][:, 16:32], lhsT=tri_b[:], rhs=alo[:], start=False, stop=True)
            P.op("pe", mmC, reads=["tri_b", "ahi", "alo", "dts0"], writes=["pf4"])
            P.op("dve", lambda e: e.tensor_copy(out=acs[:], in_=pf[4][:, 16:32]), reads=["pf4"], writes=["acs"])
            P.op("act", lambda e: e.activation(out=ea[:], in_=acs[:], func=AF.Exp), reads=["acs"], writes=["ea"])
            def trX(e):
                for c in range(8):
                    r = e.transpose(pt[0][:, c * 128:(c + 1) * 128], xcT[:, c, :], ident_b[:])
                return r
            P.op("pe", trX, reads=[f"xcT{c}" for c in range(8)] + ["ident_b"], writes=["pt0"])
            P.op("act", lambda e: e.copy(out=xs[:].rearrange("p h d -> p (h d)"), in_=pt[0][:, :]), reads=["pt0"], writes=["xs"])
            P.op("dve", lambda e: e.tensor_tensor(out=xd[:], in0=xs[:], in1=bcast(dts[:].unsqueeze(2), [128, 16, 64]), op=ALU.mult),
                 reads=["xs", "dts"], writes=["xd"])
            def trB(e):
                for g in range(2):
                    r = e.transpose(pt[1][:, g * 128:(g + 1) * 128], xcT[:, 8 + g, :], ident_b[:])
                return r
            P.op("pe", trB, reads=["xcT8", "xcT9", "ident_b"], writes=["pt1"])
            P.op("act", lambda e: e.copy(out=Btok[:].rearrange("p g n -> p (g n)"), in_=pt[1][:, 0:256]), reads=["pt1"], writes=["Btok"])
            def mmCB(e):
                for g in range(2):
                    r = e.matmul(pf[5][:, g * 128:(g + 1) * 128], lhsT=xcT[:, 8 + g, :], rhs=xcT[:, 10 + g, :], start=True, stop=True)
                return r
            P.op("pe", mmCB, reads=["xcT8", "xcT9", "xcT10", "xcT11"], writes=["pf5"])
            P.op("act", lambda e: e.copy(out=CBT[:].rearrange("p g n -> p (g n)"), in_=pf[5][:, 0:256]), reads=["pf5"], writes=["CBT"])
            for q4 in range(4):
                bank = q4 % 2

                def mmBC(e, q4=q4, bank=bank):
                    e.matmul(pf[bank][:, :], lhsT=ones_b[:], rhs=Rhi[:, q4 * 4:(q4 + 1) * 4, :].rearrange("p h l -> p (h l)"), start=True, stop=False)
                    return e.matmul(pf[bank][:, :], lhsT=ones_b[:], rhs=Rlo[:, q4 * 4:(q4 + 1) * 4, :].rearrange("p h l -> p (h l)"), start=False, stop=True)
                P.op("pe", mmBC, reads=["ones_b", "Rhi", "Rlo"], writes=[f"pf{bank}"])
                pv = pf[bank][:, :].rearrange("p (h l) -> p h l", h=4)
                P.op("dve", lambda e, pv=pv, q4=q4: e.tensor_copy(out=alast[:, q4 * 4:(q4 + 1) * 4], in_=pv[:, :, 127]), reads=[f"pf{bank}"], writes=[f"alast{q4}"])
                P.op("dve", lambda e, pv=pv, q4=q4: e.tensor_tensor(out=dif[:], in0=pv, in1=bcast(acs[:, q4 * 4:(q4 + 1) * 4].unsqueeze(2), [128, 4, 128]),
                                                                   op=ALU.subtract), reads=[f"pf{bank}", "acs"], writes=["dif"])
                P.op("dve", lambda e: e.tensor_tensor(out=dif[:], in0=dif[:], in1=bcast(negm[:].unsqueeze(1), [128, 4, 128]), op=ALU.add),
                     reads=["dif", "negm"], writes=["dif2"])
                P.op("act", lambda e: e.activation(out=Ed[:], in_=dif[:], func=AF.Exp), reads=["dif2"], writes=["Ed"])
                P.op("dve", lambda e, q4=q4: e.tensor_tensor(out=MT[:, q4 * 4:(q4 + 1) * 4, :], in0=Ed[:],
                                                           in1=bcast(CBT[:, q4 // 2, :].unsqueeze(1), [128, 4, 128]), op=ALU.mult),
                     reads=["Ed", "CBT"], writes=[f"MT{q4}"])
            alk = [f"alast{q}" for q in range(4)]
            P.op("dve", lambda e: e.tensor_tensor(out=dsd[:], in0=alast[:], in1=acs[:], op=ALU.subtract), reads=alk + ["acs"], writes=["dsd0"])
            P.op("act", lambda e: e.activation(out=dsd[:], in_=dsd[:], func=AF.Exp), reads=["dsd0"], writes=["dsd"])
            P.op("act", lambda e: e.activation(out=cdec[:], in_=alast[:], func=AF.Exp), reads=alk, writes=["cdec"])
            P.op("dve", lambda e: e.tensor_tensor(out=xdd[:], in0=xd[:], in1=bcast(dsd[:].unsqueeze(2), [128, 16, 64]), op=ALU.mult),
                 reads=["xd", "dsd"], writes=["xdd"])
            def mmY(e):
                for hd in range(16):
                    r = e.matmul(pf[2 + hd // 8][:, (hd % 8) * 64:(hd % 8 + 1) * 64], lhsT=MT[:, hd, :], rhs=xd[:, hd, :], start=True, stop=True)
                return r
            P.op("pe", mmY, reads=[f"MT{q}" for q in range(4)] + ["xd"], writes=["pf2", "pf3"])

            def mmYo(e):
                for g in range(2):
                    r = e.matmul(pf[g][:, :], lhsT=xcT[:, 10 + g, :], rhs=Sbf[:, g * 8:(g + 1) * 8, :].rearrange("p h d -> p (h d)"), start=True, stop=True)
                return r
            P.op("pe", mmYo, reads=["xcT10", "xcT11", "Sbf"], writes=["pf0", "pf1"])
            for g in range(2):
                hs = slice(g * 8, (g + 1) * 8)
                P.op("dve", lambda e, g=g, hs=hs: e.tensor_tensor(out=ytmp[:, hs, :], in0=pf[g][:, :].rearrange("p (h d) -> p h d", h=8),
                                                                 in1=bcast(ea[:, hs].unsqueeze(2), [128, 8, 64]), op=ALU.mult),
                     reads=[f"pf{g}", "ea"], writes=[f"ytmp{g}"])
                P.op("dve", lambda e, g=g, hs=hs: e.tensor_tensor(out=yv[:, hs, :], in0=pf[2 + g][:, :].rearrange("p (h d) -> p h d", h=8),
                                                                 in1=ytmp[:, hs, :], op=ALU.add), reads=[f"pf{2 + g}", f"ytmp{g}"], writes=[f"yv{g}"])
            def mmSt(e):
                for g in range(2):
                    r = e.matmul(pf[g][:, :], lhsT=Btok[:, g, :], rhs=xdd[:, g * 8:(g + 1) * 8, :].rearrange("p h d -> p (h d)"), start=True, stop=True)
                return r
            P.op("pe", mmSt, reads=["Btok", "xdd"], writes=["pf0", "pf1"])
            P.op("dve", lambda e: e.tensor_tensor(out=Sst[:], in0=Sst[:], in1=bcast(cdec[:].unsqueeze(2), [128, 16, 64]), op=ALU.mult),
                 reads=["Sst", "cdec"], writes=["Sst"])
            for g in range(2):
                hs = slice(g * 8, (g + 1) * 8)
                P.op("dve", lambda e, g=g, hs=hs: e.tensor_tensor(out=Sst[:, hs, :], in0=pf[g][:, :].rearrange("p (h d) -> p h d", h=8),
                                                                 in1=Sst[:, hs, :], op=ALU.add), reads=[f"pf{g}", "Sst"], writes=["Sst"])
            P.op("act", lambda e: e.copy(out=Sbf[:], in_=Sst[:]), reads=["Sst"], writes=["Sbf"])
            P.op("dve", lambda e: e.tensor_tensor(out=ytmp[:], in0=xs[:], in1=bcast(dsk[:].unsqueeze(2), [128, 16, 64]), op=ALU.mult),
                 reads=["xs", "dsk", "yv0", "yv1"], writes=["ytmp0", "ytmp1"])
            P.op("dve", lambda e: e.tensor_tensor(out=yv[:], in0=yv[:], in1=ytmp[:], op=ALU.add), reads=["yv0", "yv1", "ytmp0", "ytmp1"], writes=["yv0", "yv1"])
            yflat = yv[:].rearrange("p h d -> p (h d)")
            P.op("dve", lambda e: e.tensor_tensor(out=yflat, in0=yflat, in1=zs[:], op=ALU.mult), reads=["yv0", "yv1", "zs0", "zs1"], writes=["yv0", "yv1"])
            tfl = ytmp[:].rearrange("p h d -> p (h d)")
            P.op("act", lambda e: e.activation(out=tfl, in_=yflat, func=AF.Square), reads=["yv0", "yv1"], writes=["ytmp0", "ytmp1"])
            P.op("dve", lambda e: e.tensor_reduce(out=ysm[:, 0:2], in_=tfl.rearrange("p (g d) -> p g d", g=2), axis=AX.X, op=ALU.add),
                 reads=["ytmp0", "ytmp1"], writes=["ysm0"])
            rstd_of(ysm[:, 0:2], 512, ysm[:, 4:6], ["ysm0"], "ysm4", ysm[:, 2:4], "ysm2")
            P.op("dve", lambda e: e.tensor_tensor(out=yflat.rearrange("p (g d) -> p g d", g=2), in0=yflat.rearrange("p (g d) -> p g d", g=2),
                                                  in1=bcast(ysm[:, 4:6].unsqueeze(2), [128, 2, 512]), op=ALU.mult),
                 reads=["yv0", "yv1", "ysm4"], writes=["yv0", "yv1"])
            P.op("dve", lambda e: e.tensor_tensor(out=ybf[:], in0=yflat, in1=sng[:], op=ALU.mult), reads=["yv0", "yv1", "sng"], writes=["ybf"])

            def trY(e):
                for c in range(8):
                    r = e.transpose(pt[1][:, c * 128:(c + 1) * 128], ybf[:, c * 128:(c + 1) * 128], ident_b[:])
                return r
            P.op("pe", trY, reads=["ybf", "ident_b"], writes=["pt1"])
            P.op("act", lambda e, i=i: e.copy(out=catT[:, 8:16, i * 128:(i + 1) * 128], in_=pt[1][:, :].rearrange("p (c t) -> p c t", c=8)),
                 reads=["pt1"], writes=[f"cat{h}_{i}" for h in range(8, 16)])

    BAR()
    if stop_after == "ssd":
        o1 = dbg_out("d_catS", [128, 8, S], BF16)
        P.dma("sp", lambda e: e.dma_start(out=o1[:, :, 0:nblk * 128], in_=catT[:, 8:16, 0:nblk * 128]), reads=[f"cat{h}_{i}" for h in range(8, 16) for i in range(nblk)], writes=["o1"])
        return finish(["o1"])
    with ExitStack() as s3:
        g1row = row_tile(s3, "g1row", mod_d[2 * D:3 * D], D)
        P.ops[-1].reads = ("mod_d",)
        wo, wo_k = load_w(s3, "wo", w_out, 16, D)
        x1t = sb(s3, "x1t", [128, D])
        for i in range(nblk):
            s = cnt["x"] % 1
            cnt["x"] += 1
            t = xt[s]
            P.dma("sp", lambda e, t=t, i=i: e.dma_start(out=t[:], in_=x_d[i * 128:(i + 1) * 128, :]), writes=[f"xt{s}"])
            for nb in range(4):
                def mmM(e, nb=nb, i=i):
                    for c in range(16):
                        r = e.matmul(pf[nb][:, :], lhsT=catT[:, c, i * 128:(i + 1) * 128], rhs=wo[:, c, nb * 512:(nb + 1) * 512], start=(c == 0), stop=(c == 15))
                    return r
                P.op("pe", mmM, reads=[f"cat{c}_{i}" for c in range(16)] + wo_k, writes=[f"pf{nb}"])
                cs_ = slice(nb * 512, (nb + 1) * 512)
                P.op("dve", lambda e, nb=nb, cs_=cs_: e.tensor_tensor(out=x1t[:, cs_], in0=pf[nb][:, :], in1=g1row[:, cs_], op=ALU.mult),
                     reads=[f"pf{nb}", "g1row"], writes=[f"x1t{nb}"])
                P.op("dve", lambda e, nb=nb, cs_=cs_, t=t: e.tensor_tensor(out=x1t[:, cs_], in0=x1t[:, cs_], in1=t[:, cs_], op=ALU.add),
                     reads=[f"x1t{nb}", f"xt{s}"], writes=[f"x1t{nb}"])
            P.dma("sp", lambda e, i=i: e.dma_start(out=x1_d[i * 128:(i + 1) * 128, :], in_=x1t[:]), reads=[f"x1t{nb}" for nb in range(4)],
                  writes=[f"x1d{i}"], semkey=("x1st",))
    BAR()
    if stop_after == "x1":
        return finish([f"x1d{i}" for i in range(nblk)])
    scat.close()
    BAR()

    emit_conv(len(conv_pending))
    with ExitStack() as s4:
        wq, wq_k = load_w(s4, "wq", w_query, 16, D)
        g2row = row_tile(s4, "g2row", mod_d[5 * D:6 * D], D); P.ops[-1].reads = ("mod_d",)
        sc2row = row_tile(s4, "sc2row", mod_d[4 * D:5 * D], D); P.ops[-1].reads = ("mod_d",)
        sh2row = row_tile(s4, "sh2row", mod_d[3 * D:4 * D], D); P.ops[-1].reads = ("mod_d",)
        P.op("dve", lambda e: e.tensor_scalar(out=sc2row[:], in0=sc2row[:], scalar1=1.0, scalar2=None, op0=ALU.add), reads=["sc2row"], writes=["sc2row"])
        skT = sb(s4, "skT", [128, 16, 128], BF16)
        qT = sb(s4, "qT", [128, 16, 128], BF16)
        h2b2 = sb(s4, "h2b2", [128, D], BF16)
        XT1 = xt[0]; H2B = [xn, h2b2]
        sc = sb(s4, "sc", [128, 16, 128]); zp = sb(s4, "zp", [128, 128])
        tops = sb(s4, "tops", [128, 16, 16]); topi = sb(s4, "topi", [128, 16, 16])
        iot256 = sb(s4, "iot256", [128, 256])
        topiu = sb(s4, "topiu", [128, 16, 16], mybir.dt.uint32); bposu = sb(s4, "bposu", [128, 16], mybir.dt.uint32)
        bposf = sb(s4, "bposf", [128, 16])
        cands1 = sb(s4, "cands1", [128, 16, 16]); candi1 = sb(s4, "candi1", [128, 16, 16]); zc = sb(s4, "zc", [128, 256])
        bests = sb(s4, "bests", [128, 8, 16]); besti = sb(s4, "besti", [128, 8, 16])
        bidx3 = [sb(s4, f"bidx{q}", [128, 128], I32) for q in range(3)]
        gsm = sb(s4, "gsm", [128, 32])
        gate2 = [sb(s4, f"gate{q}", [128, 8, 16]) for q in range(2)]
        pre2 = [sb(s4, f"pre{q}", [128, 128]) for q in range(2)]
        actv2 = [sb(s4, f"actv{q}", [128, 128]) for q in range(2)]
        scr2 = sb(s4, "scr2", [128, D], BF16)
        dg = [sb(s4, f"dg{q}", [128, 128], BF16) for q in range(2)]
        prod = [sb(s4, f"prod{q}", [128, D], BF16) for q in range(2)]
        xr = sb(s4, "xr", [128, 1024])
        NG = 8
        gb = [sb(s4, f"gb{q}", [128, D], BF16) for q in range(NG)]
        acc = sb(s4, "acc", [128, D]); h2 = acc
        SCRK = ["scr2_0", "scr2_1", "scr2b_0", "scr2b_1"]
        skf = acc[:].rearrange("p (g d) -> p g d", g=16); skb = scr2[:].rearrange("p (g d) -> p g d", g=16)
        P.dma("sp", lambda e: e.dma_start(out=skf, in_=sub_keys.rearrange("g k d -> k g d")), writes=["acc"], semkey=("skf",))
        P.op("dve", lambda e: e.tensor_copy(out=skb, in_=skf), reads=["acc"], writes=SCRK)
        for half in range(2):
            def trSK(e, half=half):
                for g in range(8):
                    r = e.transpose(pt[half][:, g * 128:(g + 1) * 128], skb[:, half * 8 + g, :], ident_b[:])
                return r
            P.op("pe", trSK, reads=SCRK + ["ident_b"], writes=[f"pt{half}"])
            P.op("dve", lambda e, half=half: e.tensor_copy(out=skT[:, half * 8:(half + 1) * 8, :], in_=pt[half][:, :].rearrange("p (g k) -> p g k", g=8)),
                 reads=[f"pt{half}"], writes=[f"skT{half}"])
        def mmI(e):
            return e.matmul(pf[5][:, 0:128], lhsT=ones_b[:], rhs=tri_b[:], start=True, stop=True)
        P.op("pe", mmI, reads=["ones_b", "tri_b"], writes=["pf5"])
        P.op("dve", lambda e: e.tensor_scalar(out=iot256[:, 0:128], in0=pf[5][:, 0:128], scalar1=-1.0, scalar2=None, op0=ALU.add), reads=["pf5"], writes=["iotA"])
        P.op("dve", lambda e: e.tensor_scalar(out=iot256[:, 128:256], in0=pf[5][:, 0:128], scalar1=127.0, scalar2=None, op0=ALU.add), reads=["pf5", "iotA"], writes=["iot256"])
        gcount = {"n": 0, "pr": 0}

        def prep_topk(i):
            p = i % 2
            t = XT1; hb = H2B[p]; bidx = bidx3[i % 3]; gate = gate2[p]
            P.dma("sp", lambda e: e.dma_start(out=t[:], in_=x1_d[i * 128:(i + 1) * 128, :]), reads=[f"x1d{i}"], writes=["XT"])
            P.op("act", lambda e: e.activation(out=junk[:], in_=t[:], func=AF.Square, accum_out=st4[:, 0:1]), reads=["XT"], writes=["junk", "st_ss"])
            rstd_of(st4[:, 0:1], D, st4[:, 2:3], ["st_ss"], "st_r", st4[:, 1:2], "st_t")
            P.op("dve", lambda e: e.scalar_tensor_tensor(out=h2[:], in0=t[:], scalar=st4[:, 2:3], in1=sc2row[:], op0=ALU.mult, op1=ALU.mult),
                 reads=["XT", "st_r", "sc2row"], writes=["acc"])
            P.op("dve", lambda e: e.tensor_tensor(out=hb[:], in0=h2[:], in1=sh2row[:], op=ALU.add), reads=["acc", "sh2row"], writes=[f"H2B{p}"])
            for half in range(2):
                def trs(e, half=half):
                    for k in range(8):
                        kk = half * 8 + k
                        r = e.transpose(pt[half][:, k * 128:(k + 1) * 128], hb[:, kk * 128:(kk + 1) * 128], ident_b[:])
                    return r
                P.op("pe", trs, reads=[f"H2B{p}", "ident_b"], writes=[f"pt{half}"])
                P.op("act", lambda e, half=half: e.copy(out=hT[:, half * 8:(half + 1) * 8, :], in_=pt[half][:, :].rearrange("p (c t) -> p c t", c=8)),
                     reads=[f"pt{half}"], writes=[f"hT{kk}" for kk in range(half * 8, half * 8 + 8)])
            for c4 in range(4):
                def mmQ2(e, c4=c4):
                    for cc in range(4):
                        ch = c4 * 4 + cc
                        for k in range(16):
                            r = e.matmul(pf[4][:, cc * 128:(cc + 1) * 128], lhsT=wq[:, k, ch * 128:(ch + 1) * 128], rhs=hT[:, k, :], start=(k == 0), stop=(k == 15))
                    return r
                P.op("pe", mmQ2, reads=hT_keys + wq_k, writes=["pf4"])
                P.op("act", lambda e, c4=c4: e.copy(out=qT[:, c4 * 4:(c4 + 1) * 4, :], in_=pf[4][:, :].rearrange("p (c t) -> p c t", c=4)),
                     reads=["pf4"], writes=[f"qT{c4}"])

                def mmSc(e, c4=c4):
                    for cc in range(4):
                        ch = c4 * 4 + cc
                        r = e.matmul(pf[5][:, cc * 128:(cc + 1) * 128], lhsT=qT[:, ch, :], rhs=skT[:, ch, :], start=True, stop=True)
                    return r
                P.op("pe", mmSc, reads=[f"qT{c4}", "skT0", "skT1"], writes=["pf5"])
                P.op("act", lambda e, c4=c4: e.copy(out=sc[:, c4 * 4:(c4 + 1) * 4, :], in_=pf[5][:, :].rearrange("p (c t) -> p c t", c=4)),
                     reads=["pf5"], writes=[f"sc{c4}"])
            for g in range(16):
                sk_ = f"sc{g // 4}"
                P.op("dve", lambda e, g=g: e.max(out=tops[:, g, 0:8], in_=sc[:, g, :]), reads=[sk_], writes=["tA"])
                P.op("dve", lambda e, g=g: e.max_index(out=topiu[:, g, 0:8], in_max=tops[:, g, 0:8], in_values=sc[:, g, :]), reads=[sk_, "tA"], writes=["tiA"])
                P.op("dve", lambda e, g=g: e.match_replace(out=zp[:], in_to_replace=tops[:, g, 0:8], in_values=sc[:, g, :], imm_value=-1e30),
                     reads=[sk_, "tA"], writes=["zp"])
                P.op("dve", lambda e, g=g: e.max(out=tops[:, g, 8:16], in_=zp[:]), reads=["zp"], writes=["tB"])
                P.op("dve", lambda e, g=g: e.max_index(out=topiu[:, g, 8:16], in_max=tops[:, g, 8:16], in_values=zp[:]), reads=["zp", "tB"], writes=["tiB"])
            P.op("dve", lambda e: e.tensor_copy(out=topi[:], in_=topiu[:]), reads=["tiA", "tiB"], writes=["topi"])
            t4 = tops[:].rearrange("p (h j) k -> p h j k", j=2)
            i4 = topi[:].rearrange("p (h j) k -> p h j k", j=2)
            cf = cands1[:].rearrange("p a b -> p (a b)")
            cif = candi1[:].rearrange("p a b -> p (a b)")
            for h in range(8):
                P.op("dve", lambda e, h=h: e.tensor_tensor(out=cands1[:], in0=bcast(t4[:, h, 0, :].unsqueeze(2), [128, 16, 16]),
                                                           in1=bcast(t4[:, h, 1, :].unsqueeze(1), [128, 16, 16]), op=ALU.add),
                     reads=["tA", "tB"], writes=["cands"])
                P.op("dve", lambda e, h=h: e.scalar_tensor_tensor(out=candi1[:], in0=bcast(i4[:, h, 0, :].unsqueeze(2), [128, 16, 16]), scalar=128.0,
                                                                  in1=bcast(i4[:, h, 1, :].unsqueeze(1), [128, 16, 16]), op0=ALU.mult, op1=ALU.add),
                     reads=["topi"], writes=["candi"])
                P.op("dve", lambda e, h=h: e.max(out=bests[:, h, 0:8], in_=cf), reads=["cands"], writes=["bA"])
                P.op("dve", lambda e, h=h: e.max_index(out=bposu[:, 0:8], in_max=bests[:, h, 0:8], in_values=cf), reads=["cands", "bA"], writes=["bpA"])
                P.op("dve", lambda e, h=h: e.match_replace(out=zc[:], in_to_replace=bests[:, h, 0:8], in_values=cf, imm_value=-1e30),
                     reads=["cands", "bA"], writes=["zc"])
                P.op("dve", lambda e, h=h: e.max(out=bests[:, h, 8:16], in_=zc[:]), reads=["zc"], writes=["bB"])
                P.op("dve", lambda e, h=h: e.max_index(out=bposu[:, 8:16], in_max=bests[:, h, 8:16], in_values=zc[:]), reads=["zc", "bB"], writes=["bpB"])
                P.op("dve", lambda e: e.tensor_copy(out=bposf[:], in_=bposu[:]), reads=["bpA", "bpB"], writes=["bposf"])
                for k0 in (0, 8):
                    def idx2(e, h=h, k0=k0):
                        for k in range(k0, k0 + 8):
                            r = e.scalar_tensor_tensor(out=scr2[:, (k % 8) * 256:(k % 8 + 1) * 256], in0=iot256[:], scalar=bposf[:, k:k + 1], in1=cif,
                                                       op0=ALU.is_equal, op1=ALU.mult, accum_out=besti[:, h, k:k + 1])
                        return r
                    P.op("dve", idx2, reads=["candi", "bposf", "iot256"], writes=SCRK + [f"besti{h}"])
            bik = [f"besti{h}" for h in range(8)]
            P.op("dve", lambda e: e.tensor_copy(out=bidx[:], in_=besti[:].rearrange("p h k -> p (h k)")), reads=bik, writes=[f"bidx{i % 3}"])
            P.op("dve", lambda e: e.tensor_tensor(out=gate[:], in0=bests[:], in1=bcast(bests[:, :, 0:1], [128, 8, 16]), op=ALU.subtract),
                 reads=["bA", "bB"], writes=[f"gate{p}a"])
            P.op("act", lambda e: e.activation(out=gate[:], in_=gate[:], func=AF.Exp), reads=[f"gate{p}a"], writes=[f"gate{p}b"])
            P.op("dve", lambda e: e.tensor_reduce(out=gsm[:, 0:8], in_=gate[:], axis=AX.X, op=ALU.add), reads=[f"gate{p}b"], writes=["gsm0"])
            P.op("dve", lambda e: e.reciprocal(out=gsm[:, 8:16], in_=gsm[:, 0:8]), reads=["gsm0"], writes=["gsm8"])
            P.op("dve", lambda e: e.tensor_tensor(out=gate[:], in0=gate[:], in1=bcast(gsm[:, 8:16].unsqueeze(2), [128, 8, 16]), op=ALU.mult),
                 reads=[f"gate{p}b", "gsm8"], writes=[f"gate{p}"])

        def u_step(i, hk):
            p = i % 2
            hb = H2B[p]; bidx = bidx3[i % 3]; pre = pre2[p]
            b_ = gcount["n"] % NG
            gcount["n"] += 1
            q_ = gcount["pr"] % 2
            gcount["pr"] += 1
            P.dma("pool", lambda e: e.indirect_dma_start(
                out=gb[b_][:], out_offset=None, in_=u_bf, in_offset=bass.IndirectOffsetOnAxis(ap=bidx[:, hk:hk + 1], axis=0)),
                reads=[f"bidx{i % 3}", "utab"], writes=[f"gb{b_}"])
            P.op("dve", lambda e: e.tensor_tensor(out=prod[q_][:], in0=gb[b_][:], in1=hb[:], op=ALU.mult),
                 reads=[f"gb{b_}", f"H2B{p}"], writes=[f"prod{q_}"])
            P.op("act", lambda e: e.activation(out=prod[q_][:], in_=prod[q_][:], func=AF.Copy, accum_out=pre[:, hk:hk + 1]),
                 reads=[f"prod{q_}"], writes=[f"prod{q_}", f"pre{p}"])

        def u_end(i):
            p = i % 2
            pre = pre2[p]; actv = actv2[p]; gate = gate2[p]
            P.op("act", lambda e: e.activation(out=actv[:], in_=pre[:], func=AF.Gelu), reads=[f"pre{p}"], writes=[f"actv{p}a"])
            P.op("dve", lambda e: e.tensor_tensor(out=actv[:], in0=actv[:], in1=gate[:].rearrange("p h k -> p (h k)"), op=ALU.mult),
                 reads=[f"actv{p}a", f"gate{p}"], writes=[f"actv{p}"])

        def v_step(i, hk):
            p = i % 2
            bidx = bidx3[i % 3]; actv = actv2[p]
            b_ = gcount["n"] % NG
            gcount["n"] += 1
            dq = hk % 2
            P.dma("pool", lambda e: e.indirect_dma_start(
                out=gb[b_][:], out_offset=None, in_=v_bf, in_offset=bass.IndirectOffsetOnAxis(ap=bidx[:, hk:hk + 1], axis=0)),
                reads=[f"bidx{i % 3}", "vtab"], writes=[f"gb{b_}"])
            P.op("act", lambda e: e.activation(out=dg[dq][:], in_=ident_b[:], func=AF.Copy, scale=actv[:, hk:hk + 1]),
                 reads=[f"actv{p}", "ident_b"], writes=[f"dg{dq}"])

            def mmV(e):
                for nb in range(4):
                    r = e.matmul(pf[nb][:, :], lhsT=dg[dq][:], rhs=gb[b_][:, nb * 512:(nb + 1) * 512], start=(hk == 0), stop=(hk == 127))
                return r
            P.op("pe", mmV, reads=[f"dg{dq}", f"gb{b_}"], writes=["pf0", "pf1", "pf2", "pf3"])

        def final(i):
            for nb in range(4):
                cs_ = slice(nb * 512, (nb + 1) * 512)
                P.op("dve", lambda e, nb=nb, cs_=cs_: e.tensor_tensor(out=acc[:, cs_], in0=pf[nb][:, :], in1=g2row[:, cs_], op=ALU.mult),
                     reads=[f"pf{nb}", "g2row"], writes=["acc"])
            for hf in range(2):
                P.dma("sp", lambda e, hf=hf: e.dma_start(out=xr[:], in_=x1_d[i * 128:(i + 1) * 128, hf * 1024:(hf + 1) * 1024]), reads=[f"x1d{i}"], writes=["xr"])
                P.op("dve", lambda e, hf=hf: e.tensor_tensor(out=acc[:, hf * 1024:(hf + 1) * 1024], in0=acc[:, hf * 1024:(hf + 1) * 1024], in1=xr[:], op=ALU.add),
                     reads=["acc", "xr"], writes=["acc"])
            P.dma("sp", lambda e: e.dma_start(out=out_d[i * 128:(i + 1) * 128, :], in_=acc[:]), reads=["acc"], writes=[f"out{i}", "acc"],
                  semkey=("outst",))

        def capture(fn, *a):
            saved = P.ops
            P.ops = []
            fn(*a)
            got = P.ops
            P.ops = saved
            return got

        if stop_after in ("pdbg", "pdbg2"):
            return finish([])
        prep_topk(0)
        for i in range(nblk + 1):
            nxt = capture(prep_topk, i + 1) if i + 1 < nblk else []
            per = (len(nxt) + 127) // 128
            for hk in range(128):
                if i < nblk:
                    u_step(i, hk)
                if i >= 1:
                    v_step(i - 1, hk)
                P.ops.extend(nxt[hk * per:(hk + 1) * per])
            if i < nblk:
                u_end(i)
            if i >= 1:
                final(i - 1)
        return finish([f"out{i}" for i in range(nblk)])


_CACHE = {}


def _consts():
    ident = np.eye(128, dtype=np.float32)
    k = np.arange(128)
    tri = (k[None, :] >= k[:, None]).astype(np.float32)
    negmask = np.where(k[None, :] >= k[:, None], 0.0, -30000.0).astype(np.float32)
    invf = (1.0 / (10000.0 ** (np.arange(32, dtype=np.float32) * (2.0 / 64)))).astype(np.float32)
    invf = np.broadcast_to(invf[None, :], (128, 32)).copy()
    return ident, tri, negmask, invf


def kernel(**inputs):
    if "nc" not in _CACHE:
        _CACHE["nc"] = build()
    nc = _CACHE["nc"]
    ident, tri, negmask, invf = _consts()
    f = lambda a: np.ascontiguousarray(np.asarray(a))
    shared = {
        "w_ada": f(inputs["w_ada"][0]), "b_ada": f(inputs["b_ada"][0]).reshape(1, -1), "w_in": f(inputs["w_in"][0]),
        "q_a_norm": f(inputs["q_a_norm"][0]), "w_uq": f(inputs["w_uq"][0]), "kv_a_norm": f(inputs["kv_a_norm"][0]),
        "w_ukv": f(inputs["w_ukv"][0]), "q_norm": f(inputs["q_norm"][0]), "k_norm": f(inputs["k_norm"][0]),
        "attn_out_norm": f(inputs["attn_out_norm"][0]), "conv_w": f(inputs["conv_w"][0]), "conv_b": f(inputs["conv_b"][0]),
        "dt_bias": f(inputs["dt_bias"][0]), "a_log": f(inputs["a_log"][0]), "d_skip": f(inputs["d_skip"][0]),
        "ssd_norm": f(inputs["ssd_norm"][0]), "w_out": f(inputs["w_out"][0]), "w_query": f(inputs["w_query"][0]),
        "sub_keys": f(inputs["sub_keys"][0]).reshape(16, 128, 128), "u_experts": f(inputs["u_experts"][0]),
        "v_experts": f(inputs["v_experts"][0]), "ident": ident, "tri": tri, "negmask": negmask, "invfreq": invf,
    }
    x = np.asarray(inputs["x"]); c = np.asarray(inputs["c"]); pos = np.asarray(inputs["positions"])
    in_maps = []
    for b in range(8):
        m = dict(shared)
        m["x"] = f(x[b]); m["c"] = f(c[b]); m["pos"] = f(pos[b]).reshape(S, 1).astype(np.int32)
        in_maps.append(m)
    res = run_bass_kernel_spmd(nc, in_maps, core_ids=list(range(8)))
    return np.stack([np.asarray(r["out"]) for r in res.results], axis=0).astype(np.float32)
```

```python
from contextlib import ExitStack
import os
import numpy as np
import concourse.bass as bass
import concourse.mybir as mybir
from concourse.bass_utils import run_bass_kernel_spmd

F32 = mybir.dt.float32
BF16 = mybir.dt.bfloat16
I32 = mybir.dt.int32
AF = mybir.ActivationFunctionType
ALU = mybir.AluOpType
AX = mybir.AxisListType

D = 2048
S = 2048
NB = 16
EPS = 1e-6
ENGS = ("pe", "act", "dve", "pool", "sp")
SEM_EPOCH = 20000


class Op:
    __slots__ = ("eng", "fn", "reads", "writes", "dma", "semkey", "deps", "waits", "signal", "idx", "barrier")

    def __init__(self, eng, fn, reads, writes, dma, semkey):
        self.eng, self.fn = eng, fn
        self.reads, self.writes = tuple(reads), tuple(writes)
        self.dma, self.semkey = dma, semkey
        self.deps, self.waits, self.signal = (), [], None
        self.barrier = False


class Prog:
    def __init__(self, nc):
        self.nc = nc
        self.ops = []
        self.self_sync = ("dve", "act", "pool")

    def op(self, eng, fn, reads=(), writes=()):
        xr = [k for k in reads if isinstance(k, str) and k[:2] in ("pf", "pt") and k not in writes]
        writes = tuple(writes) + tuple(xr)
        o = Op(eng, fn, reads, writes, False, None)
        self.ops.append(o)
        return o

    def dma(self, eng, fn, reads=(), writes=(), semkey=None):
        if semkey is None:
            semkey = ("dma",) + (tuple(writes) if writes else tuple(reads))
        o = Op(eng, fn, reads, writes, True, semkey)
        self.ops.append(o)
        return o

    def barrier(self, fn):
        o = Op("act", fn, (), (), False, None)
        o.barrier = True
        self.ops.append(o)
        return o

    def _analyze(self):
        last_write, readers = {}, {}
        last_on = {}
        dmas_since = []
        last_barrier = None
        for i, o in enumerate(self.ops):
            o.idx = i
            deps = set()
            if o.barrier:
                deps.update(last_on.values())
                deps.update(dmas_since)
                dmas_since = []
                if last_barrier is not None:
                    deps.add(last_barrier)
                last_barrier = i
            elif last_barrier is not None:
                deps.add(last_barrier)
            last_on[o.eng] = i
            if o.dma:
                dmas_since.append(i)
            for t in o.reads:
                if t in last_write:
                    deps.add(last_write[t])
            for t in o.writes:
                if t in last_write:
                    deps.add(last_write[t])
                deps.update(readers.get(t, ()))
            deps.discard(i)
            o.deps = deps
            for t in o.reads:
                readers.setdefault(t, []).append(i)
            for t in o.writes:
                last_write[t] = i
                readers[t] = []
        need = set()
        for o in self.ops:
            for d in o.deps:
                od = self.ops[d]
                if od.dma or od.eng != o.eng or o.eng in self.self_sync:
                    need.add(d)
        cnt = {e: 0 for e in ENGS}
        dcnt = {}
        self.semkeys = []
        for o in self.ops:
            if o.dma:
                k = dcnt.get(o.semkey, 0) + 1
                dcnt[o.semkey] = k
                if k == 1:
                    self.semkeys.append(o.semkey)
                o.signal = (("d", o.semkey), 16 * k, 16)
            elif o.idx in need:
                cnt[o.eng] += 1
                ep, v = divmod(cnt[o.eng] - 1, SEM_EPOCH)
                o.signal = (("e", o.eng, ep), v + 1, 1)
        self.nepochs = {e: (cnt[e] + SEM_EPOCH - 1) // SEM_EPOCH for e in ENGS}
        for o in self.ops:
            w = {}
            for d in o.deps:
                od = self.ops[d]
                if od.dma or od.eng != o.eng or o.eng in self.self_sync:
                    s, v, _ = od.signal
                    if w.get(s, 0) < v:
                        w[s] = v
            o.waits = sorted(w.items(), key=lambda kv: str(kv[0]))

    def emit(self):
        nc = self.nc
        self._analyze()
        with ExitStack() as es:
            sems = {}
            n = 0
            for e in ENGS:
                for ep in range(self.nepochs[e]):
                    sems[("e", e, ep)] = es.enter_context(nc.semaphore(f"s_{e}{ep}"))
                    n += 1
            for k in self.semkeys:
                sems[("d", k)] = es.enter_context(nc.semaphore(f"d{n}"))
                n += 1
            self.nsems = n
            by_eng = {e: [o for o in self.ops if o.eng == e] for e in ENGS}

            def run(engine, name):
                waited = {}
                for o in by_eng[name]:
                    for s, v in o.waits:
                        if waited.get(s, 0) < v:
                            engine.wait_ge(sems[s], v)
                            waited[s] = v
                    ins = o.fn(engine)
                    if o.signal is not None:
                        s, v, inc = o.signal
                        ins.then_inc(sems[s], inc)

            with nc.Block() as block:
                @block.tensor
                def _(e):
                    run(e, "pe")

                @block.scalar
                def _(e):
                    run(e, "act")

                @block.vector
                def _(e):
                    run(e, "dve")

                @block.gpsimd
                def _(e):
                    run(e, "pool")

                @block.sync
                def _(e):
                    run(e, "sp")


def bcast(ap, shape):
    return ap.to_broadcast(list(shape))


def build(stop_after=None, debug=False, nblk=NB, ngroups=2):
    nc = bass.Bass("TRN2", target_bir_lowering=False)
    P = Prog(nc)

    def din(name, shape, dt=F32):
        return nc.dram_tensor(name, list(shape), dt, kind="ExternalInput").ap()

    x_d = din("x", [S, D]); c_d = din("c", [D]); pos_d = din("pos", [S, 1], I32)
    w_ada = din("w_ada", [D, 6 * D]); b_ada = din("b_ada", [1, 6 * D]); w_in = din("w_in", [D, 3408])
    q_a_norm = din("q_a_norm", [512]); w_uq = din("w_uq", [512, 1536]); kv_a_norm = din("kv_a_norm", [256])
    w_ukv = din("w_ukv", [256, 2048]); q_norm = din("q_norm", [192]); k_norm = din("k_norm", [192])
    attn_out_norm = din("attn_out_norm", [1024]); conv_w = din("conv_w", [4, 1536]); conv_b = din("conv_b", [1536])
    dt_bias = din("dt_bias", [16]); a_log = din("a_log", [16]); d_skip = din("d_skip", [16])
    ssd_norm = din("ssd_norm", [1024]); w_out = din("w_out", [D, D]); w_query = din("w_query", [D, D])
    sub_keys = din("sub_keys", [16, 128, 128])
    if stop_after in (None, "pdbg", "pdbg2"):
        u_exp = din("u_experts", [16384, D]); v_exp = din("v_experts", [16384, D])
    ident_d = din("ident", [128, 128]); tri_d = din("tri", [128, 128]); negm_d = din("negmask", [128, 128])
    invf_d = din("invfreq", [128, 32])
    out_d = nc.dram_tensor("out", [S, D], F32, kind="ExternalOutput").ap()
    mod_d = nc.dram_tensor("mod_scr", [6 * D], F32, kind="Internal").ap()
    x1_d = nc.dram_tensor("x1_scr", [S, D], F32, kind=("ExternalOutput" if stop_after == "x1" else "Internal")).ap()
    dbg = {}
    conv_pending = []
    if stop_after in (None, "pdbg", "pdbg2"):
        u_bf = nc.dram_tensor("u_bf", [16384, D], BF16, kind="Internal").ap()
        v_bf = nc.dram_tensor("v_bf", [16384, D], BF16, kind="Internal").ap()
        for r0 in range(0, 16384, 1024):
            conv_pending.append((u_bf, u_exp, r0, "utab"))
            conv_pending.append((v_bf, v_exp, r0, "vtab"))

    win_bf = nc.dram_tensor("win_bf", [D, 2560], BF16, kind="Internal").ap()
    for r0 in range(0, D, 512):
        conv_pending.insert(r0 // 512, (win_bf, w_in[:, 832:3392], r0, "wintab"))

    def emit_conv(n):
        for _ in range(min(n, len(conv_pending))):
            dst, src, r0, key = conv_pending.pop(0)
            nr = 512 if key == "wintab" else 1024
            P.dma("pool", lambda e, dst=dst, src=src, r0=r0, nr=nr: e.dma_start(out=dst[r0:r0 + nr, :], in_=src[r0:r0 + nr, :]),
                  writes=[key], semkey=("conv", key))

    def dbg_out(nm, shp, dt=F32):
        dbg[nm] = nc.dram_tensor(nm, list(shp), dt, kind="ExternalOutput").ap()
        return dbg[nm]

    def finish(keys):
        P.op("sp", lambda e: None, reads=keys)
        P.emit()
        return nc

    top = ExitStack()

    def sb(es, name, shape, dt=F32):
        return es.enter_context(nc.sbuf_tensor(name, list(shape), dt))

    pf = [top.enter_context(nc.psum_tensor(f"pf{i}", [128, 512], F32)) for i in range(6)]
    pt = [top.enter_context(nc.psum_tensor(f"pt{i}", [128, 1024], BF16)) for i in range(2)]

    ident_f = sb(top, "ident_f", [128, 128]); ident_b = sb(top, "ident_b", [128, 128], BF16)
    tri_f = sb(top, "tri_f", [128, 128]); tri_b = sb(top, "tri_b", [128, 128], BF16)
    ones_b = sb(top, "ones_b", [128, 128], BF16); negm = sb(top, "negm", [128, 128])
    invf = sb(top, "invf", [128, 32]); bar = sb(top, "bar", [128, 2])
    BAR = lambda: P.barrier(lambda e: e.copy(out=bar[:, 0:1], in_=bar[:, 1:2]))
    modT = sb(top, "modT", [128, 96]); scp1 = sb(top, "scp1", [128, 16]); scp2 = sb(top, "scp2", [128, 16])

    P.dma("sp", lambda e: e.dma_start(out=ident_f[:], in_=ident_d), writes=["ident_f"])
    P.dma("sp", lambda e: e.dma_start(out=tri_f[:], in_=tri_d), writes=["tri_f"])
    P.dma("sp", lambda e: e.dma_start(out=negm[:], in_=negm_d), writes=["negm"])
    P.dma("sp", lambda e: e.dma_start(out=invf[:], in_=invf_d), writes=["invf"])
    P.op("dve", lambda e: e.tensor_copy(out=ident_b[:], in_=ident_f[:]), reads=["ident_f"], writes=["ident_b"])
    P.op("dve", lambda e: e.tensor_copy(out=tri_b[:], in_=tri_f[:]), reads=["tri_f"], writes=["tri_b"])
    P.op("dve", lambda e: e.memset(ones_b[:], 1.0), writes=["ones_b"])
    P.op("act", lambda e: e.activation(out=bar[:], in_=invf[:, 0:2], func=AF.Copy), reads=["invf"], writes=["bar"])

    with ExitStack() as sa:
        cT = sb(sa, "cT", [128, 16]); cab = sb(sa, "cab", [128, 16], BF16)
        brow = sb(sa, "brow", [1, 6 * D]); modrow = sb(sa, "modrow", [1, 6 * D])
        wa = [sb(sa, f"wa{i}", [128, 16, 512], BF16) for i in range(2)]
        waf = [sb(sa, f"waf{i}", [128, 16, 512], F32) for i in range(2)]
        with nc.allow_non_contiguous_dma(reason="tiny transposed loads"):
            P.dma("sp", lambda e: e.dma_start(out=cT[:], in_=c_d.rearrange("(k p) -> p k", p=128), allow_slow_non_contiguous=True), writes=["cT"])
        P.dma("sp", lambda e: e.dma_start(out=brow[:], in_=b_ada), writes=["brow"])
        P.op("act", lambda e: e.activation(out=cab[:], in_=cT[:], func=AF.Silu), reads=["cT"], writes=["cab"])
        for j in range(24):
            w = wa[j % 2]
            wf = waf[j % 2]
            P.dma("sp", lambda e, wf=wf, j=j: e.dma_start(
                out=wf[:], in_=w_ada[:, j * 512:(j + 1) * 512].rearrange("(k p) n -> p k n", p=128)),
                writes=[f"waf{j % 2}"])
            P.op("dve", lambda e, w=w, wf=wf: e.tensor_copy(out=w[:, 0:8, :], in_=wf[:, 0:8, :]), reads=[f"waf{j % 2}"], writes=[f"wa{j % 2}a"])
            P.op("act", lambda e, w=w, wf=wf: e.copy(out=w[:, 8:16, :], in_=wf[:, 8:16, :]), reads=[f"waf{j % 2}"], writes=[f"wa{j % 2}b"])

            def mmA(e, w=w):
                for k in range(16):
                    r = e.matmul(pf[0][0:1, :], lhsT=cab[:, k:k + 1], rhs=w[:, k, :], start=(k == 0), stop=(k == 15))
                return r
            P.op("pe", mmA, reads=["cab", f"wa{j % 2}a", f"wa{j % 2}b"], writes=["pf0"])
            P.op("dve", lambda e, j=j: e.tensor_tensor(out=modrow[0:1, j * 512:(j + 1) * 512], in0=pf[0][0:1, :],
                                                       in1=brow[0:1, j * 512:(j + 1) * 512], op=ALU.add),
                 reads=["pf0", "brow"], writes=["modrow"])
        P.dma("sp", lambda e: e.dma_start(out=mod_d.rearrange("(o n) -> o n", o=1), in_=modrow[:]),
              reads=["modrow"], writes=["mod_d"])
        with nc.allow_non_contiguous_dma(reason="tiny transposed loads"):
            P.dma("sp", lambda e: e.dma_start(out=modT[:], in_=mod_d.rearrange("(j p) -> p j", p=128), allow_slow_non_contiguous=True),
                  reads=["mod_d"], writes=["modT"])
        P.op("dve", lambda e: e.tensor_scalar(out=scp1[:], in0=modT[:, 16:32], scalar1=1.0, scalar2=None, op0=ALU.add),
             reads=["modT"], writes=["scp1"])
        P.op("dve", lambda e: e.tensor_scalar(out=scp2[:], in0=modT[:, 64:80], scalar1=1.0, scalar2=None, op0=ALU.add),
             reads=["modT"], writes=["scp2"])
        if stop_after == "A":
            o1 = dbg_out("d_mod", [1, 6 * D]); o2 = dbg_out("d_modT", [128, 96])
            P.dma("sp", lambda e: e.dma_start(out=o1, in_=modrow[:]), reads=["modrow"], writes=["o1"])
            P.dma("sp", lambda e: e.dma_start(out=o2, in_=modT[:]), reads=["modT"], writes=["o2"])
            return finish(["o1", "o2"])
    BAR()
    emit_conv(4)
    sh1 = modT[:, 0:16]
    sh2 = modT[:, 48:64]

    xt = [sb(top, f"xt{i}", [128, D]) for i in range(1)]
    junk = sb(top, "junk", [128, D], BF16)
    xn = sb(top, "xn", [128, D], BF16)
    st4 = sb(top, "st4", [128, 4])
    hT = sb(top, "hT", [128, 16, 128], BF16)
    cnt = {"x": 0}
    scat = ExitStack()
    catT = sb(scat, "catT", [128, 16, S], BF16)

    def rstd_of(ss_ap, n, out_ap, rkeys, wkey, tmp_ap, tmpkey):
        P.op("act", lambda e: e.activation(out=tmp_ap, in_=ss_ap, func=AF.Sqrt, scale=1.0 / n, bias=EPS),
             reads=rkeys, writes=[tmpkey])
        P.op("dve", lambda e: e.reciprocal(out=out_ap, in_=tmp_ap), reads=[tmpkey], writes=[wkey])

    def make_hT(src_d, i, scp, sh, keep_tokmajor=None):
        s = cnt["x"] % 1
        cnt["x"] += 1
        t = xt[s]
        P.dma("sp", lambda e: e.dma_start(out=t[:], in_=src_d[i * 128:(i + 1) * 128, :]), writes=[f"xt{s}"])
        P.op("act", lambda e: e.activation(out=junk[:], in_=t[:], func=AF.Square, accum_out=st4[:, 0:1]),
             reads=[f"xt{s}"], writes=["junk", "st_ss"])
        rstd_of(st4[:, 0:1], D, st4[:, 2:3], ["st_ss"], "st_r", st4[:, 1:2], "st_t")
        P.op("dve", lambda e: e.tensor_scalar(out=xn[:], in0=t[:], scalar1=st4[:, 2:3], scalar2=None, op0=ALU.mult),
             reads=[f"xt{s}", "st_r"], writes=["xn"])
        HTM = os.environ.get("HT_MODE", "")
        if HTM == "xn":
            o_ = dbg_out("d_xn", [128, D], BF16)
            P.dma("sp", lambda e, o_=o_: e.dma_start(out=o_, in_=xn[:]), reads=["xn"], writes=["d_xn"])
            raise StopIteration
        for half in range(2):
            def trs(e, half=half):
                for k in range(8):
                    kk = half * 8 + k
                    r = e.transpose(pt[half][:, k * 128:(k + 1) * 128], xn[:, kk * 128:(kk + 1) * 128], ident_b[:])
                return r
            P.op("pe", trs, reads=["xn", "ident_b"], writes=[f"pt{half}"])
            for k in range(8):
                kk = half * 8 + k
                if (k % 2 == 0 and HTM != "act") or HTM == "dve":
                    P.op("dve", lambda e, kk=kk, k=k, half=half: e.tensor_scalar(
                        out=hT[:, kk, :], in0=pt[half][:, k * 128:(k + 1) * 128], scalar1=scp[:, kk:kk + 1],
                        scalar2=sh[:, kk:kk + 1], op0=ALU.mult, op1=ALU.add),
                        reads=[f"pt{half}", "scp1", "scp2", "modT"], writes=[f"hT{kk}"])
                else:
                    P.op("act", lambda e, kk=kk, k=k, half=half: e.activation(
                        out=hT[:, kk, :], in_=pt[half][:, k * 128:(k + 1) * 128], func=AF.Identity,
                        scale=scp[:, kk:kk + 1], bias=sh[:, kk:kk + 1]),
                        reads=[f"pt{half}", "scp1", "scp2", "modT"], writes=[f"hT{kk}"])
        return s

    hT_keys = [f"hT{k}" for k in range(16)]

    def load_w(es, name, src_ap, kchunks, ncols, eng="pool"):
        t = sb(es, name, [128, kchunks, ncols], BF16)
        step = max(1, 4096 // ncols)
        for k0 in range(0, kchunks, step):
            k1 = min(kchunks, k0 + step)
            P.dma(eng, lambda e, k0=k0, k1=k1: e.dma_start(
                out=t[:, k0:k1, :], in_=src_ap[k0 * 128:k1 * 128, :].rearrange("(k p) n -> p k n", p=128)),
                writes=[f"{name}_{k0}"], semkey=("w", name, k0))
        return t, [f"{name}_{k0}" for k0 in range(0, kchunks, step)]

    def row_tile(es, name, src_1d, n):
        t = sb(es, name, [128, n])
        P.dma("sp", lambda e: e.dma_start(out=t[:], in_=src_1d.partition_broadcast(128)), writes=[name])
        return t

    with ExitStack() as s1:
        win_a, win_a_k = load_w(s1, "win_a", w_in[:, 0:832], 16, 832)
        wuq, wuq_k = load_w(s1, "wuq", w_uq, 4, 1536)
        wukv, wukv_k = load_w(s1, "wukv", w_ukv, 2, 2048)
        qag = row_tile(s1, "qag", q_a_norm, 512); kvag = row_tile(s1, "kvag", kv_a_norm, 256)
        qg = row_tile(s1, "qg", q_norm, 192); kg = row_tile(s1, "kg", k_norm, 192)
        KTn = sb(s1, "KTn", [128, 4, S], BF16); KTr = sb(s1, "KTr", [128, 2, S], BF16)
        Vc = sb(s1, "Vc", [128, NB, 512], BF16)
        cqn = sb(s1, "cqn", [128, 512], BF16); ckvn = sb(s1, "ckvn", [128, 256], BF16)
        cqnT = sb(s1, "cqnT", [128, 4, 128], BF16); ckvnT = sb(s1, "ckvnT", [128, 2, 128], BF16)
        kr = sb(s1, "kr", [128, 64]); krr = sb(s1, "krr", [128, 64])
        qraw = sb(s1, "qraw", [128, 4, 192]); kvraw = sb(s1, "kvraw", [128, 4, 256])
        sq = sb(s1, "sq", [128, 1024]); sm = sb(s1, "sm", [128, 32])
        posi = sb(s1, "posi", [128, 1], I32); posf = sb(s1, "posf", [128, 1])
        ang = sb(s1, "ang", [128, 64]); angk = sb(s1, "angk", [128, 64]); angi = sb(s1, "angi", [128, 64], I32)
        cs = sb(s1, "cs", [128, 64])
        rt = sb(s1, "rt", [128, 4, 32, 4])
        qbn = sb(s1, "qbn", [128, 4, 128], BF16); qbr = sb(s1, "qbr", [128, 4, 64], BF16)
        kbn = sb(s1, "kbn", [128, 4, 128], BF16); kbr = sb(s1, "kbr", [128, 4, 64], BF16)
        QTn = sb(s1, "QTn", [128, 4, 128], BF16); QTr = sb(s1, "QTr", [128, 2, 128], BF16)
        PT = [sb(s1, f"PT{i}", [128, 512], BF16) for i in range(2)]
        rden = sb(s1, "rden", [128, 128])

        def rope(src3, dst3, nh, rk, wk):
            cosb = bcast(cs[:, 0:32].unsqueeze(1), [128, nh, 32])
            sinb = bcast(cs[:, 32:64].unsqueeze(1), [128, nh, 32])
            x1, x2 = src3[:, :, 0:32], src3[:, :, 32:64]
            t = [rt[:, 0:nh, :, j] for j in range(4)]
            P.op("dve", lambda e: e.tensor_tensor(out=t[0], in0=x1, in1=cosb, op=ALU.mult), reads=rk + ["cs"], writes=["rt0"])
            P.op("dve", lambda e: e.tensor_tensor(out=t[1], in0=x2, in1=sinb, op=ALU.mult), reads=rk + ["cs"], writes=["rt1"])
            P.op("dve", lambda e: e.tensor_tensor(out=t[2], in0=x2, in1=cosb, op=ALU.mult), reads=rk + ["cs"], writes=["rt2"])
            P.op("dve", lambda e: e.tensor_tensor(out=t[3], in0=x1, in1=sinb, op=ALU.mult), reads=rk + ["cs"], writes=["rt3"])
            P.op("dve", lambda e: e.tensor_tensor(out=dst3[:, :, 0:32], in0=t[0], in1=t[1], op=ALU.subtract),
                 reads=["rt0", "rt1"], writes=[wk + "a"])
            P.op("dve", lambda e: e.tensor_tensor(out=dst3[:, :, 32:64], in0=t[2], in1=t[3], op=ALU.add),
                 reads=["rt2", "rt3"], writes=[wk + "b"])

        if stop_after == "mw":
            o_ = dbg_out("d_qag", [128, 512], F32); o2_ = dbg_out("d_wuq", [128, 4, 1536], BF16)
            P.dma("sp", lambda e: e.dma_start(out=o_, in_=qag[:]), reads=["qag"], writes=["d_qag"])
            P.dma("sp", lambda e: e.dma_start(out=o2_, in_=wuq[:]), reads=wuq_k, writes=["d_wuq"])
            return finish(["d_qag", "d_wuq"])
        for G in range(ngroups):
            for i in range(nblk):
                try:
                    make_hT(x_d, i, scp1, sh1)
                except StopIteration:
                    return finish(["d_xn"])
                emit_conv(2)
                if stop_after == "m0":
                    o_ = dbg_out("d_hT", [128, 16, 128], BF16)
                    P.dma("sp", lambda e, o_=o_: e.dma_start(out=o_, in_=hT[:]), reads=hT_keys, writes=["d_hT"])
                    return finish(["d_hT"])
                P.dma("sp", lambda e, i=i: e.dma_start(out=posi[:], in_=pos_d[i * 128:(i + 1) * 128, :]), writes=["posi"])
                P.op("dve", lambda e: e.tensor_copy(out=posf[:], in_=posi[:]), reads=["posi"], writes=["posf"])
                P.op("dve", lambda e: e.tensor_scalar(out=ang[:, 0:32], in0=invf[:], scalar1=posf[:, 0:1], scalar2=None, op0=ALU.mult),
                     reads=["posf", "invf"], writes=["ang0"])
                P.op("dve", lambda e: e.tensor_scalar(out=ang[:, 32:64], in0=ang[:, 0:32], scalar1=float(np.pi / 2), scalar2=None, op0=ALU.add),
                     reads=["ang0"], writes=["ang1"])
                P.op("dve", lambda e: e.tensor_scalar(out=angk[:], in0=ang[:], scalar1=float(1.0 / (2 * np.pi)), scalar2=None, op0=ALU.mult),
                     reads=["ang0", "ang1"], writes=["angk"])
                P.op("dve", lambda e: e.tensor_copy(out=angi[:], in_=angk[:]), reads=["angk"], writes=["angi"])
                P.op("dve", lambda e: e.tensor_copy(out=angk[:], in_=angi[:]), reads=["angi"], writes=["angk2"])
                P.op("dve", lambda e: e.scalar_tensor_tensor(out=ang[:], in0=angk[:], scalar=float(-2 * np.pi), in1=ang[:],
                                                             op0=ALU.mult, op1=ALU.add), reads=["angk2", "ang0", "ang1"], writes=["angr"])
                P.op("act", lambda e: e.activation(out=cs[:, 0:32], in_=ang[:, 32:64], func=AF.Sin), reads=["angr"], writes=["cs_c"])
                P.op("act", lambda e: e.activation(out=cs[:, 32:64], in_=ang[:, 0:32], func=AF.Sin), reads=["angr", "cs_c"], writes=["cs"])

                def mmP(e):
                    for k in range(16):
                        e.matmul(pf[0][:, :], lhsT=hT[:, k, :], rhs=win_a[:, k, 0:512], start=(k == 0), stop=(k == 15))
                    for k in range(16):
                        r = e.matmul(pf[1][:, 0:320], lhsT=hT[:, k, :], rhs=win_a[:, k, 512:832], start=(k == 0), stop=(k == 15))
                    return r
                P.op("pe", mmP, reads=hT_keys + win_a_k, writes=["pf0", "pf1"])
                P.op("act", lambda e: e.activation(out=sq[:, 0:512], in_=pf[0][:, :], func=AF.Square, accum_out=sm[:, 0:1]),
                     reads=["pf0"], writes=["sq", "sm0"])
                rstd_of(sm[:, 0:1], 512, sm[:, 2:3], ["sm0"], "sm2", sm[:, 1:2], "sm1")
                P.op("dve", lambda e: e.scalar_tensor_tensor(out=cqn[:], in0=pf[0][:, :], scalar=sm[:, 2:3], in1=qag[:],
                                                             op0=ALU.mult, op1=ALU.mult), reads=["pf0", "sm2", "qag"], writes=["cqn"])
                P.op("act", lambda e: e.activation(out=sq[:, 0:256], in_=pf[1][:, 0:256], func=AF.Square, accum_out=sm[:, 3:4]),
                     reads=["pf1", "sm0"], writes=["sq", "sm3"])
                rstd_of(sm[:, 3:4], 256, sm[:, 5:6], ["sm3"], "sm5", sm[:, 4:5], "sm4")
                P.op("dve", lambda e: e.scalar_tensor_tensor(out=ckvn[:], in0=pf[1][:, 0:256], scalar=sm[:, 5:6], in1=kvag[:],
                                                             op0=ALU.mult, op1=ALU.mult), reads=["pf1", "sm5", "kvag"], writes=["ckvn"])
                P.op("act", lambda e: e.copy(out=kr[:], in_=pf[1][:, 256:320]), reads=["pf1"], writes=["kr"])
                P.op("act", lambda e: e.activation(out=sq[:, 0:64], in_=kr[:], func=AF.Square, accum_out=sm[:, 6:7]),
                     reads=["kr", "sm3"], writes=["sq", "sm6"])
                def trC(e):
                    for c in range(4):
                        e.transpose(pt[0][:, c * 128:(c + 1) * 128], cqn[:, c * 128:(c + 1) * 128], ident_b[:])
                    for c in range(2):
                        r = e.transpose(pt[0][:, (4 + c) * 128:(5 + c) * 128], ckvn[:, c * 128:(c + 1) * 128], ident_b[:])
                    return r
                P.op("pe", trC, reads=["cqn", "ckvn", "ident_b"], writes=["pt0"])
                P.op("dve", lambda e: e.tensor_copy(out=cqnT[:].rearrange("p c t -> p (c t)"), in_=pt[0][:, 0:512]),
                     reads=["pt0"], writes=["cqnT"])
                P.op("act", lambda e: e.copy(out=ckvnT[:].rearrange("p c t -> p (c t)"), in_=pt[0][:, 512:768]),
                     reads=["pt0"], writes=["ckvnT"])

                if stop_after == "m1":
                    keys = []
                    o_ = dbg_out("d_cqnT", [128, 4, 128], BF16)
                    P.dma("sp", lambda e, o_=o_: e.dma_start(out=o_, in_=cqnT[:]), reads=["cqnT"], writes=["d_cqnT"])
                    keys.append("d_cqnT")
                    o_ = dbg_out("d_cs", [128, 64], F32)
                    P.dma("sp", lambda e, o_=o_: e.dma_start(out=o_, in_=cs[:]), reads=["cs"], writes=["d_cs"])
                    keys.append("d_cs")
                    o_ = dbg_out("d_kr", [128, 64], F32)
                    P.dma("sp", lambda e, o_=o_: e.dma_start(out=o_, in_=kr[:]), reads=["kr"], writes=["d_kr"])
                    keys.append("d_kr")
                    return finish(keys)
                def mmQ(e, G=G):
                    for c in range(4):
                        e.matmul(pf[2][:, :], lhsT=cqnT[:, c, :], rhs=wuq[:, c, G * 768:G * 768 + 512], start=(c == 0), stop=(c == 3))
                    for c in range(4):
                        r = e.matmul(pf[3][:, 0:256], lhsT=cqnT[:, c, :], rhs=wuq[:, c, G * 768 + 512:G * 768 + 768], start=(c == 0), stop=(c == 3))
                    return r
                P.op("pe", mmQ, reads=["cqnT"] + wuq_k, writes=["pf2", "pf3"])
                qflat = qraw[:].rearrange("p h d -> p (h d)")
                P.op("act", lambda e: e.copy(out=qflat[:, 0:512], in_=pf[2][:, :]), reads=["pf2"], writes=["qraw0"])
                P.op("dve", lambda e: e.tensor_copy(out=qflat[:, 512:768], in_=pf[3][:, 0:256]), reads=["pf3"], writes=["qraw1"])
                P.op("act", lambda e: e.activation(out=sq[:, 0:768], in_=qflat, func=AF.Square),
                     reads=["qraw0", "qraw1", "sm6"], writes=["sq"])
                P.op("dve", lambda e: e.tensor_reduce(out=sm[:, 8:12], in_=sq[:, 0:768].rearrange("p (h d) -> p h d", h=4),
                                                      axis=AX.X, op=ALU.add), reads=["sq"], writes=["sm8"])
                rstd_of(sm[:, 8:12], 192, sm[:, 16:20], ["sm8"], "sm16", sm[:, 12:16], "sm12")
                P.op("dve", lambda e: e.tensor_tensor(out=qraw[:], in0=qraw[:], in1=bcast(sm[:, 16:20].unsqueeze(2), [128, 4, 192]), op=ALU.mult),
                     reads=["qraw0", "qraw1", "sm16"], writes=["qraw2"])
                P.op("dve", lambda e: e.tensor_tensor(out=qraw[:], in0=qraw[:], in1=bcast(qg[:].unsqueeze(1), [128, 4, 192]), op=ALU.mult),
                     reads=["qraw2", "qg"], writes=["qraw3"])
                P.op("act", lambda e: e.copy(out=qbn[:], in_=qraw[:, :, 0:128]), reads=["qraw3"], writes=["qbn"])
                rope(qraw[:, :, 128:192], qbr[:], 4, ["qraw3"], "qbr")

                def trQ(e):
                    for hh in range(4):
                        e.transpose(pt[1][:, hh * 128:(hh + 1) * 128], qbn[:, hh, :], ident_b[:])
                    qr2 = qbr[:].rearrange("p (a b) d -> p a (b d)", b=2)
                    for pp in range(2):
                        r = e.transpose(pt[1][:, (4 + pp) * 128:(5 + pp) * 128], qr2[:, pp, :], ident_b[:])
                    return r
                P.op("pe", trQ, reads=["qbn", "qbra", "qbrb", "ident_b"], writes=["pt1"])
                P.op("dve", lambda e: e.tensor_copy(out=QTn[:].rearrange("p c t -> p (c t)"), in_=pt[1][:, 0:512]), reads=["pt1"], writes=["QTn"])
                P.op("act", lambda e: e.copy(out=QTr[:].rearrange("p c t -> p (c t)"), in_=pt[1][:, 512:768]), reads=["pt1"], writes=["QTr"])

                if stop_after == "m2":
                    keys = []
                    o_ = dbg_out("d_QTn", [128, 4, 128], BF16)
                    P.dma("sp", lambda e, o_=o_: e.dma_start(out=o_, in_=QTn[:]), reads=["QTn"], writes=["d_QTn"])
                    keys.append("d_QTn")
                    o_ = dbg_out("d_QTr", [128, 2, 128], BF16)
                    P.dma("sp", lambda e, o_=o_: e.dma_start(out=o_, in_=QTr[:]), reads=["QTr"], writes=["d_QTr"])
                    keys.append("d_QTr")
                    return finish(keys)
                def mmK(e, G=G):
                    for c in range(2):
                        e.matmul(pf[2][:, :], lhsT=ckvnT[:, c, :], rhs=wukv[:, c, G * 1024:G * 1024 + 512], start=(c == 0), stop=(c == 1))
                    for c in range(2):
                        r = e.matmul(pf[3][:, :], lhsT=ckvnT[:, c, :], rhs=wukv[:, c, G * 1024 + 512:G * 1024 + 1024], start=(c == 0), stop=(c == 1))
                    return r
                P.op("pe", mmK, reads=["ckvnT"] + wukv_k, writes=["pf2", "pf3"])
                kvflat = kvraw[:].rearrange("p h d -> p (h d)")
                P.op("act", lambda e: e.copy(out=kvflat[:, 0:512], in_=pf[2][:, :]), reads=["pf2"], writes=["kvraw0"])
                P.op("dve", lambda e: e.tensor_copy(out=kvflat[:, 512:1024], in_=pf[3][:, :]), reads=["pf3"], writes=["kvraw1"])
                P.op("act", lambda e, i=i: e.copy(out=Vc[:, i, :].rearrange("p (h d) -> p h d", h=4), in_=kvraw[:, :, 128:256]),
                     reads=["kvraw0", "kvraw1"], writes=[f"Vc{i}"])
                P.op("act", lambda e: e.activation(out=sq[:, 0:512].rearrange("p (h d) -> p h d", h=4), in_=kvraw[:, :, 0:128], func=AF.Square),
                     reads=["kvraw0", "kvraw1", "sm8"], writes=["sq"])
                P.op("dve", lambda e: e.tensor_reduce(out=sm[:, 20:24], in_=sq[:, 0:512].rearrange("p (h d) -> p h d", h=4),
                                                      axis=AX.X, op=ALU.add), reads=["sq"], writes=["sm20"])
                P.op("dve", lambda e: e.tensor_scalar(out=sm[:, 20:24], in0=sm[:, 20:24], scalar1=sm[:, 6:7], scalar2=None, op0=ALU.add),
                     reads=["sm20", "sm6"], writes=["sm20b"])
                rstd_of(sm[:, 20:24], 192, sm[:, 28:32], ["sm20b"], "sm28", sm[:, 24:28], "sm24")
                P.op("dve", lambda e: e.tensor_tensor(out=kvraw[:, :, 0:128], in0=kvraw[:, :, 0:128],
                                                      in1=bcast(sm[:, 28:32].unsqueeze(2), [128, 4, 128]), op=ALU.mult),
                     reads=["kvraw0", "kvraw1", "sm28"], writes=["kvraw2"])
                P.op("dve", lambda e: e.tensor_tensor(out=kbn[:], in0=kvraw[:, :, 0:128], in1=bcast(kg[:, 0:128].unsqueeze(1), [128, 4, 128]), op=ALU.mult),
                     reads=["kvraw2", "kg"], writes=["kbn"])
                P.op("dve", lambda e: e.tensor_tensor(out=kr[:], in0=kr[:], in1=kg[:, 128:192], op=ALU.mult), reads=["kr", "kg", "sm6"], writes=["krg"])
                rope(kr[:].unsqueeze(1), krr[:].unsqueeze(1), 1, ["krg"], "krr")
                P.op("dve", lambda e: e.tensor_tensor(out=kbr[:], in0=bcast(krr[:].unsqueeze(1), [128, 4, 64]),
                                                      in1=bcast(sm[:, 28:32].unsqueeze(2), [128, 4, 64]), op=ALU.mult),
                     reads=["krra", "krrb", "sm28"], writes=["kbr"])

                def trK(e):
                    for hh in range(4):
                        e.transpose(pt[0][:, hh * 128:(hh + 1) * 128], kbn[:, hh, :], ident_b[:])
                    kr2 = kbr[:].rearrange("p (a b) d -> p a (b d)", b=2)
                    for pp in range(2):
                        r = e.transpose(pt[0][:, (4 + pp) * 128:(5 + pp) * 128], kr2[:, pp, :], ident_b[:])
                    return r
                P.op("pe", trK, reads=["kbn", "kbr", "ident_b"], writes=["pt0"])
                P.op("dve", lambda e, i=i: e.tensor_copy(out=KTn[:, :, i * 128:(i + 1) * 128],
                                                         in_=pt[0][:, 0:512].rearrange("p (c t) -> p c t", c=4)), reads=["pt0"], writes=[f"KTn{i}"])
                P.op("act", lambda e, i=i: e.copy(out=KTr[:, :, i * 128:(i + 1) * 128],
                                                  in_=pt[0][:, 512:768].rearrange("p (c t) -> p c t", c=2)), reads=["pt0"], writes=[f"KTr{i}"])

                if stop_after == "m3":
                    keys = []
                    o_ = dbg_out("d_KTn", [128, 4, 128], BF16)
                    P.dma("sp", lambda e, o_=o_: e.dma_start(out=o_, in_=KTn[:, :, 0:128]), reads=["KTn0"], writes=["d_KTn"])
                    keys.append("d_KTn")
                    o_ = dbg_out("d_KTr", [128, 2, 128], BF16)
                    P.dma("sp", lambda e, o_=o_: e.dma_start(out=o_, in_=KTr[:, :, 0:128]), reads=["KTr0"], writes=["d_KTr"])
                    keys.append("d_KTr")
                    return finish(keys)
                for hh in range(4):
                    h = G * 4 + hh
                    pp, hf = hh // 2, hh % 2
                    ngrp = (i + 4) // 4
                    for g in range(ngrp):
                        j0, j1 = g * 4, min(i + 1, g * 4 + 4)
                        sbank = 4 + (g % 2)
                        pbuf = PT[g % 2]

                        def mmS(e, j0=j0, j1=j1, sbank=sbank, hh=hh, pp=pp, hf=hf):
                            for j in range(j0, j1):
                                o = pf[sbank][:, (j - j0) * 128:(j - j0 + 1) * 128]
                                e.matmul(o, lhsT=KTn[:, hh, j * 128:(j + 1) * 128], rhs=QTn[:, hh, :], start=True, stop=False)
                                r = e.matmul(o, lhsT=KTr[hf * 64:(hf + 1) * 64, pp, j * 128:(j + 1) * 128],
                                             rhs=QTr[hf * 64:(hf + 1) * 64, pp, :], start=False, stop=True)
                            return r
                        P.op("pe", mmS, reads=["QTn", "QTr"] + [f"KTn{j}" for j in range(j0, j1)] + [f"KTr{j}" for j in range(j0, j1)],
                             writes=[f"pf{sbank}"])
                        w = (j1 - j0) * 128
                        P.op("act", lambda e, sbank=sbank, pbuf=pbuf, w=w: e.activation(out=pbuf[:, 0:w], in_=pf[sbank][:, 0:w], func=AF.Exp,
                                                                                     scale=float(192 ** -0.5)),
                             reads=[f"pf{sbank}"], writes=[f"PT{g % 2}"])
                        if j1 == i + 1:
                            dcol = (i - j0) * 128
                            P.op("dve", lambda e, pbuf=pbuf, dcol=dcol: e.tensor_tensor(out=pbuf[:, dcol:dcol + 128], in0=pbuf[:, dcol:dcol + 128],
                                                                                      in1=tri_b[:], op=ALU.mult),
                                 reads=[f"PT{g % 2}", "tri_b"], writes=[f"PT{g % 2}"])

                        def mmO(e, j0=j0, j1=j1, pbuf=pbuf, hh=hh, i=i):
                            for j in range(j0, j1):
                                pj = pbuf[:, (j - j0) * 128:(j - j0 + 1) * 128]
                                e.matmul(pf[2][:, 0:128], lhsT=Vc[:, j, hh * 128:(hh + 1) * 128], rhs=pj, start=(j == 0), stop=(j == i))
                                r = e.matmul(pf[3][:, 0:128], lhsT=ones_b[:], rhs=pj, start=(j == 0), stop=(j == i))
                            return r
                        P.op("pe", mmO, reads=[f"PT{g % 2}", "ones_b"] + [f"Vc{j}" for j in range(j0, j1)], writes=["pf2", "pf3"])
                    P.op("dve", lambda e: e.reciprocal(out=rden[:], in_=pf[3][:, 0:128]), reads=["pf3"], writes=["rden"])
                    P.op("dve", lambda e, h=h, i=i: e.tensor_tensor(out=catT[:, h, i * 128:(i + 1) * 128], in0=pf[2][:, 0:128], in1=rden[:], op=ALU.mult),
                         reads=["pf2", "rden"], writes=[f"cat{h}_{i}"])

    BAR()
    with ExitStack() as s1:
        aog = sb(s1, "aog", [128, 8])
        with nc.allow_non_contiguous_dma(reason="tiny transposed loads"):
            P.dma("sp", lambda e: e.dma_start(out=aog[:], in_=attn_out_norm.rearrange("(j p) -> p j", p=128), allow_slow_non_contiguous=True), writes=["aog"])
        sqb = sb(s1, "sqb", [128, 8, 512], BF16); rnb = sb(s1, "rnb", [128, 512]); rnb2 = sb(s1, "rnb2", [128, 512])
        for tb in range((nblk + 3) // 4):
            wd = min(512, nblk * 128 - tb * 512)
            cols = slice(tb * 512, tb * 512 + wd)
            blks = range(tb * 4, min(nblk, tb * 4 + 4))
            ck = [f"cat{h}_{i}" for h in range(8) for i in blks]
            P.op("act", lambda e, cols=cols, wd=wd: e.activation(out=sqb[:, :, 0:wd], in_=catT[:, 0:8, cols], func=AF.Square), reads=ck, writes=["sqb"])

            def mmN(e, wd=wd):
                for h in range(8):
                    r = e.matmul(pf[0][:, 0:wd], lhsT=ones_b[:], rhs=sqb[:, h, 0:wd], start=(h == 0), stop=(h == 7))
                return r
            P.op("pe", mmN, reads=["sqb", "ones_b"], writes=["pf0"])
            P.op("act", lambda e, wd=wd: e.activation(out=rnb[:, 0:wd], in_=pf[0][:, 0:wd], func=AF.Sqrt, scale=1.0 / 1024, bias=EPS), reads=["pf0"], writes=["rnb"])
            P.op("dve", lambda e, wd=wd: e.reciprocal(out=rnb2[:, 0:wd], in_=rnb[:, 0:wd]), reads=["rnb"], writes=["rnb2"])
            for h in range(8):
                P.op("dve", lambda e, h=h, cols=cols, wd=wd: e.scalar_tensor_tensor(out=catT[:, h, cols], in0=catT[:, h, cols], scalar=aog[:, h:h + 1],
                                                                                  in1=rnb2[:, 0:wd], op0=ALU.mult, op1=ALU.mult),
                     reads=["rnb2", "aog"] + [f"cat{h}_{i}" for i in blks],
                     writes=[f"cat{h}_{i}" for i in blks])
    BAR()
    if stop_after == "mla":
        o1 = dbg_out("d_catA", [128, 8, S], BF16)
        P.dma("sp", lambda e: e.dma_start(out=o1[:, :, 0:nblk * 128], in_=catT[:, 0:8, 0:nblk * 128]), reads=[f"cat{h}_{i}" for h in range(8) for i in range(nblk)], writes=["o1"])
        return finish(["o1"])
    with ExitStack() as s2:
        NWX = 4
        wxb = [sb(s2, f"wxb{q}", [128, 16, 128], BF16) for q in range(NWX)]
        wzb2 = [sb(s2, f"wzb{q}", [128, 16, 512], BF16) for q in range(2)]
        win_d, win_d_k = load_w(s2, "win_d", w_in[:, 3392:3408], 16, 16)
        wcnt = {"n": 0}
        cw = sb(s2, "cw", [128, 12, 4]); cb = sb(s2, "cb", [128, 12])
        with nc.allow_non_contiguous_dma(reason="tiny transposed loads"):
            for k in range(4):
                P.dma("sp", lambda e, k=k: e.dma_start(out=cw[:, :, k], in_=conv_w[k, :].rearrange("(j p) -> p j", p=128), allow_slow_non_contiguous=True),
                      writes=[f"cw{k}"])
            P.dma("sp", lambda e: e.dma_start(out=cb[:], in_=conv_b.rearrange("(j p) -> p j", p=128), allow_slow_non_contiguous=True), writes=["cb"])
        cwk = [f"cw{k}" for k in range(4)]
        dtb = row_tile(s2, "dtb", dt_bias, 16); arow = row_tile(s2, "arow", a_log, 16)
        dsk = row_tile(s2, "dsk", d_skip, 16); sng = row_tile(s2, "sng", ssd_norm, 1024)
        P.op("act", lambda e: e.activation(out=arow[:], in_=arow[:], func=AF.Exp), reads=["arow"], writes=["arow_e"])
        P.op("dve", lambda e: e.tensor_scalar(out=arow[:], in0=arow[:], scalar1=-1.0, scalar2=None, op0=ALU.mult), reads=["arow_e"], writes=["arow_n"])
        xraw = sb(s2, "xraw", [128, 12, 131]); xacc = sb(s2, "xacc", [128, 12, 128])
        xcT = sb(s2, "xcT", [128, 12, 128], BF16)
        xs = sb(s2, "xs", [128, 16, 64]); xd = sb(s2, "xd", [128, 16, 64], BF16); xdd = sb(s2, "xdd", [128, 16, 64], BF16)
        zs = sb(s2, "zs", [128, 1024]); dts = sb(s2, "dts", [128, 16]); adt = sb(s2, "adt", [128, 16])
        ahi = sb(s2, "ahi", [128, 16], BF16); alo = sb(s2, "alo", [128, 16], BF16); ahf = sb(s2, "ahf", [128, 16])
        Rhi = sb(s2, "Rhi", [128, 16, 128], BF16); Rlo = sb(s2, "Rlo", [128, 16, 128], BF16)
        acs = sb(s2, "acs", [128, 16]); alast = sb(s2, "alast", [128, 16]); ea = sb(s2, "ea", [128, 16])
        dsd = sb(s2, "dsd", [128, 16]); cdec = sb(s2, "cdec", [128, 16])
        dif = sb(s2, "dif", [128, 4, 128]); Ed = sb(s2, "Ed", [128, 4, 128])
        MT = sb(s2, "MT", [128, 16, 128], BF16); CBT = sb(s2, "CBT", [128, 2, 128])
        Btok = sb(s2, "Btok", [128, 2, 128], BF16)
        Sst = sb(s2, "Sst", [128, 16, 64]); Sbf = sb(s2, "Sbf", [128, 16, 64], BF16)
        yv = sb(s2, "yv", [128, 16, 64]); ytmp = sb(s2, "ytmp", [128, 16, 64]); ybf = sb(s2, "ybf", [128, 1024], BF16)
        ysm = sb(s2, "ysm", [128, 8])
        P.op("dve", lambda e: e.memset(Sst[:], 0.0), writes=["Sst"])
        P.op("dve", lambda e: e.memset(Sbf[:], 0.0), writes=["Sbf"])
        P.op("dve", lambda e: e.memset(xraw[:, :, 0:3], 0.0), writes=["xhalo"])

        for i in range(nblk):
            make_hT(x_d, i, scp1, sh1)
            for j in range(12):
                bank = j % 2
                wq_ = wcnt["n"] % NWX
                wcnt["n"] += 1
                wx_ = wxb[wq_]
                P.dma("sp", lambda e, j=j, wx_=wx_: e.dma_start(
                    out=wx_[:], in_=win_bf[:, 1024 + j * 128:1024 + (j + 1) * 128].rearrange("(k p) n -> p k n", p=128)),
                    reads=["wintab"], writes=[f"wxb{wq_}"])

                def mmX(e, j=j, bank=bank, wx_=wx_):
                    for k in range(16):
                        r = e.matmul(pf[bank][:, 0:128], lhsT=wx_[:, k, :], rhs=hT[:, k, :], start=(k == 0), stop=(k == 15))
                    return r
                P.op("pe", mmX, reads=hT_keys + [f"wxb{wq_}"], writes=[f"pf{bank}"])
                P.op("act", lambda e, j=j, bank=bank: e.copy(out=xraw[:, j, 3:131], in_=pf[bank][:, 0:128]),
                     reads=[f"pf{bank}", "xhalo"], writes=[f"xraw{j}"])
            xrk = [f"xraw{j}" for j in range(12)]
            for j in range(12):
                eng = "dve"
                P.op(eng, lambda e, j=j: e.tensor_scalar(out=xacc[:, j, :], in0=xraw[:, j, 0:128], scalar1=cw[:, j, 0:1], scalar2=cb[:, j:j + 1],
                                                         op0=ALU.mult, op1=ALU.add), reads=[f"xraw{j}", "cb"] + cwk, writes=[f"xacc{j}"])
                for k in range(1, 4):
                    P.op(eng, lambda e, j=j, k=k: e.scalar_tensor_tensor(out=xacc[:, j, :], in0=xraw[:, j, k:k + 128], scalar=cw[:, j, k:k + 1],
                                                                         in1=xacc[:, j, :], op0=ALU.mult, op1=ALU.add),
                         reads=[f"xraw{j}", f"xacc{j}"] + cwk, writes=[f"xacc{j}"])
                P.op("act", lambda e, j=j: e.activation(out=xcT[:, j, :], in_=xacc[:, j, :], func=AF.Silu), reads=[f"xacc{j}"], writes=[f"xcT{j}"])
                P.op(eng, lambda e, j=j: e.tensor_copy(out=xraw[:, j, 0:3], in_=xraw[:, j, 128:131]), reads=[f"xraw{j}", f"xacc{j}"], writes=[f"xraw{j}", "xhalo"])
            for half in range(2):
                wzb = wzb2[half]
                P.dma("sp", lambda e, half=half, wzb=wzb: e.dma_start(
                    out=wzb[:], in_=win_bf[:, half * 512:(half + 1) * 512].rearrange("(k p) n -> p k n", p=128)),
                    reads=["wintab"], writes=[f"wzb{half}"])

                def mmZ(e, half=half, wzb=wzb):
                    for k in range(16):
                        r = e.matmul(pf[2 + half][:, :], lhsT=hT[:, k, :], rhs=wzb[:, k, :], start=(k == 0), stop=(k == 15))
                    return r
                P.op("pe", mmZ, reads=hT_keys + [f"wzb{half}"], writes=[f"pf{2 + half}"])

            def mmD(e):
                for k in range(16):
                    r = e.matmul(pf[4][:, 0:16], lhsT=hT[:, k, :], rhs=win_d[:, k, :], start=(k == 0), stop=(k == 15))
                return r
            P.op("pe", mmD, reads=hT_keys + win_d_k, writes=["pf4"])
            P.op("act", lambda e: e.activation(out=zs[:, 0:512], in_=pf[2][:, :], func=AF.Silu), reads=["pf2"], writes=["zs0"])
            P.op("act", lambda e: e.activation(out=zs[:, 512:1024], in_=pf[3][:, :], func=AF.Silu), reads=["pf3"], writes=["zs1"])
            P.op("dve", lambda e: e.tensor_tensor(out=dts[:], in0=pf[4][:, 0:16], in1=dtb[:], op=ALU.add), reads=["pf4", "dtb"], writes=["dts0"])
            P.op("act", lambda e: e.activation(out=dts[:], in_=dts[:], func=AF.Exp), reads=["dts0"], writes=["dts1"])
            P.op("act", lambda e: e.activation(out=dts[:], in_=dts[:], func=AF.Ln, bias=1.0, scale=1.0), reads=["dts1"], writes=["dts"])
            P.op("dve", lambda e: e.tensor_tensor(out=adt[:], in0=dts[:], in1=arow[:], op=ALU.mult), reads=["dts", "arow_n"], writes=["adt"])
            P.op("dve", lambda e: e.tensor_copy(out=ahi[:], in_=adt[:]), reads=["adt"], writes=["ahi"])
            P.op("dve", lambda e: e.tensor_copy(out=ahf[:], in_=ahi[:]), reads=["ahi"], writes=["ahf"])
            P.op("dve", lambda e: e.tensor_tensor(out=alo[:], in0=adt[:], in1=ahf[:], op=ALU.subtract), reads=["adt", "ahf"], writes=["alo"])
            trib = bcast(tri_b[:].unsqueeze(1), [128, 16, 128])
            P.op("dve", lambda e: e.tensor_tensor(out=Rhi[:], in0=trib, in1=bcast(ahi[:].unsqueeze(2), [128, 16, 128]), op=ALU.mult),
                 reads=["ahi", "tri_b"], writes=["Rhi"])
            P.op("pool", lambda e: e.tensor_tensor(out=Rlo[:], in0=trib, in1=bcast(alo[:].unsqueeze(2), [128, 16, 128]), op=ALU.mult),
                 reads=["alo", "tri_b"], writes=["Rlo"])
            def mmC(e):
                e.matmul(pf[4][:, 16:32], lhsT=tri_b[:], rhs=ahi[:], start=True, stop=False)
                return e.matmul(pf[4][:, 16:32], lhsT=tri_b[:], rhs=alo[:], start=False, stop=True)
            P.op("pe", mmC, reads=["tri_b", "ahi", "alo", "dts0"], writes=["pf4"])
            P.op("dve", lambda e: e.tensor_copy(out=acs[:], in_=pf[4][:, 16:32]), reads=["pf4"], writes=["acs"])
            P.op("act", lambda e: e.activation(out=ea[:], in_=acs[:], func=AF.Exp), reads=["acs"], writes=["ea"])
            def trX(e):
                for c in range(8):
                    r = e.transpose(pt[0][:, c * 128:(c + 1) * 128], xcT[:, c, :], ident_b[:])
                return r
            P.op("pe", trX, reads=[f"xcT{c}" for c in range(8)] + ["ident_b"], writes=["pt0"])
            P.op("act", lambda e: e.copy(out=xs[:].rearrange("p h d -> p (h d)"), in_=pt[0][:, :]), reads=["pt0"], writes=["xs"])
            P.op("dve", lambda e: e.tensor_tensor(out=xd[:], in0=xs[:], in1=bcast(dts[:].unsqueeze(2), [128, 16, 64]), op=ALU.mult),
                 reads=["xs", "dts"], writes=["xd"])
            def trB(e):
                for g in range(2):
                    r = e.transpose(pt[1][:, g * 128:(g + 1) * 128], xcT[:, 8 + g, :], ident_b[:])
                return r
            P.op("pe", trB, reads=["xcT8", "xcT9", "ident_b"], writes=["pt1"])
            P.op("act", lambda e: e.copy(out=Btok[:].rearrange("p g n -> p (g n)"), in_=pt[1][:, 0:256]), reads=["pt1"], writes=["Btok"])
            def mmCB(e):
                for g in range(2):
                    r = e.matmul(pf[5][:, g * 128:(g + 1) * 128], lhsT=xcT[:, 8 + g, :], rhs=xcT[:, 10 + g, :], start=True, stop=True)
                return r
            P.op("pe", mmCB, reads=["xcT8", "xcT9", "xcT10", "xcT11"], writes=["pf5"])
            P.op("act", lambda e: e.copy(out=CBT[:].rearrange("p g n -> p (g n)"), in_=pf[5][:, 0:256]), reads=["pf5"], writes=["CBT"])
            for q4 in range(4):
                bank = q4 % 2

                def mmBC(e, q4=q4, bank=bank):
                    e.matmul(pf[bank][:, :], lhsT=ones_b[:], rhs=Rhi[:, q4 * 4:(q4 + 1) * 4, :].rearrange("p h l -> p (h l)"), start=True, stop=False)
                    return e.matmul(pf[bank][:, :], lhsT=ones_b[:], rhs=Rlo[:, q4 * 4:(q4 + 1) * 4, :].rearrange("p h l -> p (h l)"), start=False, stop=True)
                P.op("pe", mmBC, reads=["ones_b", "Rhi", "Rlo"], writes=[f"pf{bank}"])
                pv = pf[bank][:, :].rearrange("p (h l) -> p h l", h=4)
                P.op("dve", lambda e, pv=pv, q4=q4: e.tensor_copy(out=alast[:, q4 * 4:(q4 + 1) * 4], in_=pv[:, :, 127]), reads=[f"pf{bank}"], writes=[f"alast{q4}"])
                P.op("dve", lambda e, pv=pv, q4=q4: e.tensor_tensor(out=dif[:], in0=pv, in1=bcast(acs[:, q4 * 4:(q4 + 1) * 4].unsqueeze(2), [128, 4, 128]),
                                                                   op=ALU.subtract), reads=[f"pf{bank}", "acs"], writes=["dif"])
                P.op("dve", lambda e: e.tensor_tensor(out=dif[:], in0=dif[:], in1=bcast(negm[:].unsqueeze(1), [128, 4, 128]), op=ALU.add),
                     reads=["dif", "negm"], writes=["dif2"])
                P.op("act", lambda e: e.activation(out=Ed[:], in_=dif[:], func=AF.Exp), reads=["dif2"], writes=["Ed"])
                P.op("dve", lambda e, q4=q4: e.tensor_tensor(out=MT[:, q4 * 4:(q4 + 1) * 4, :], in0=Ed[:],
                                                           in1=bcast(CBT[:, q4 // 2, :].unsqueeze(1), [128, 4, 128]), op=ALU.mult),
                     reads=["Ed", "CBT"], writes=[f"MT{q4}"])
            alk = [f"alast{q}" for q in range(4)]
            P.op("dve", lambda e: e.tensor_tensor(out=dsd[:], in0=alast[:], in1=acs[:], op=ALU.subtract), reads=alk + ["acs"], writes=["dsd0"])
            P.op("act", lambda e: e.activation(out=dsd[:], in_=dsd[:], func=AF.Exp), reads=["dsd0"], writes=["dsd"])
            P.op("act", lambda e: e.activation(out=cdec[:], in_=alast[:], func=AF.Exp), reads=alk, writes=["cdec"])
            P.op("dve", lambda e: e.tensor_tensor(out=xdd[:], in0=xd[:], in1=bcast(dsd[:].unsqueeze(2), [128, 16, 64]), op=ALU.mult),
                 reads=["xd", "dsd"], writes=["xdd"])
            def mmY(e):
                for hd in range(16):
                    r = e.matmul(pf[2 + hd // 8][:, (hd % 8) * 64:(hd % 8 + 1) * 64], lhsT=MT[:, hd, :], rhs=xd[:, hd, :], start=True, stop=True)
                return r
            P.op("pe", mmY, reads=[f"MT{q}" for q in range(4)] + ["xd"], writes=["pf2", "pf3"])

            def mmYo(e):
                for g in range(2):
                    r = e.matmul(pf[g][:, :], lhsT=xcT[:, 10 + g, :], rhs=Sbf[:, g * 8:(g + 1) * 8, :].rearrange("p h d -> p (h d)"), start=True, stop=True)
                return r
            P.op("pe", mmYo, reads=["xcT10", "xcT11", "Sbf"], writes=["pf0", "pf1"])
            for g in range(2):
                hs = slice(g * 8, (g + 1) * 8)
                P.op("dve", lambda e, g=g, hs=hs: e.tensor_tensor(out=ytmp[:, hs, :], in0=pf[g][:, :].rearrange("p (h d) -> p h d", h=8),
                                                                 in1=bcast(ea[:, hs].unsqueeze(2), [128, 8, 64]), op=ALU.mult),
                     reads=[f"pf{g}", "ea"], writes=[f"ytmp{g}"])
                P.op("dve", lambda e, g=g, hs=hs: e.tensor_tensor(out=yv[:, hs, :], in0=pf[2 + g][:, :].rearrange("p (h d) -> p h d", h=8),
                                                                 in1=ytmp[:, hs, :], op=ALU.add), reads=[f"pf{2 + g}", f"ytmp{g}"], writes=[f"yv{g}"])
            def mmSt(e):
                for g in range(2):
                    r = e.matmul(pf[g][:, :], lhsT=Btok[:, g, :], rhs=xdd[:, g * 8:(g + 1) * 8, :].rearrange("p h d -> p (h d)"), start=True, stop=True)
                return r
            P.op("pe", mmSt, reads=["Btok", "xdd"], writes=["pf0", "pf1"])
            P.op("dve", lambda e: e.tensor_tensor(out=Sst[:], in0=Sst[:], in1=bcast(cdec[:].unsqueeze(2), [128, 16, 64]), op=ALU.mult),
                 reads=["Sst", "cdec"], writes=["Sst"])
            for g in range(2):
                hs = slice(g * 8, (g + 1) * 8)
                P.op("dve", lambda e, g=g, hs=hs: e.tensor_tensor(out=Sst[:, hs, :], in0=pf[g][:, :].rearrange("p (h d) -> p h d", h=8),
                                                                 in1=Sst[:, hs, :], op=ALU.add), reads=[f"pf{g}", "Sst"], writes=["Sst"])
            P.op("act", lambda e: e.copy(out=Sbf[:], in_=Sst[:]), reads=["Sst"], writes=["Sbf"])
            P.op("dve", lambda e: e.tensor_tensor(out=ytmp[:], in0=xs[:], in1=bcast(dsk[:].unsqueeze(2), [128, 16, 64]), op=ALU.mult),
                 reads=["xs", "dsk", "yv0", "yv1"], writes=["ytmp0", "ytmp1"])
            P.op("dve", lambda e: e.tensor_tensor(out=yv[:], in0=yv[:], in1=ytmp[:], op=ALU.add), reads=["yv0", "yv1", "ytmp0", "ytmp1"], writes=["yv0", "yv1"])
            yflat = yv[:].rearrange("p h d -> p (h d)")
            P.op("dve", lambda e: e.tensor_tensor(out=yflat, in0=yflat, in1=zs[:], op=ALU.mult), reads=["yv0", "yv1", "zs0", "zs1"], writes=["yv0", "yv1"])
            tfl = ytmp[:].rearrange("p h d -> p (h d)")
            P.op("act", lambda e: e.activation(out=tfl, in_=yflat, func=AF.Square), reads=["yv0", "yv1"], writes=["ytmp0", "ytmp1"])
            P.op("dve", lambda e: e.tensor_reduce(out=ysm[:, 0:2], in_=tfl.rearrange("p (g d) -> p g d", g=2), axis=AX.X, op=ALU.add),
                 reads=["ytmp0", "ytmp1"], writes=["ysm0"])
            rstd_of(ysm[:, 0:2], 512, ysm[:, 4:6], ["ysm0"], "ysm4", ysm[:, 2:4], "ysm2")
            P.op("dve", lambda e: e.tensor_tensor(out=yflat.rearrange("p (g d) -> p g d", g=2), in0=yflat.rearrange("p (g d) -> p g d", g=2),
                                                  in1=bcast(ysm[:, 4:6].unsqueeze(2), [128, 2, 512]), op=ALU.mult),
                 reads=["yv0", "yv1", "ysm4"], writes=["yv0", "yv1"])
            P.op("dve", lambda e: e.tensor_tensor(out=ybf[:], in0=yflat, in1=sng[:], op=ALU.mult), reads=["yv0", "yv1", "sng"], writes=["ybf"])

            def trY(e):
                for c in range(8):
                    r = e.transpose(pt[1][:, c * 128:(c + 1) * 128], ybf[:, c * 128:(c + 1) * 128], ident_b[:])
                return r
            P.op("pe", trY, reads=["ybf", "ident_b"], writes=["pt1"])
            P.op("act", lambda e, i=i: e.copy(out=catT[:, 8:16, i * 128:(i + 1) * 128], in_=pt[1][:, :].rearrange("p (c t) -> p c t", c=8)),
                 reads=["pt1"], writes=[f"cat{h}_{i}" for h in range(8, 16)])

    BAR()
    if stop_after == "ssd":
        o1 = dbg_out("d_catS", [128, 8, S], BF16)
        P.dma("sp", lambda e: e.dma_start(out=o1[:, :, 0:nblk * 128], in_=catT[:, 8:16, 0:nblk * 128]), reads=[f"cat{h}_{i}" for h in range(8, 16) for i in range(nblk)], writes=["o1"])
        return finish(["o1"])
    with ExitStack() as s3:
        g1row = row_tile(s3, "g1row", mod_d[2 * D:3 * D], D)
        P.ops[-1].reads = ("mod_d",)
        wo, wo_k = load_w(s3, "wo", w_out, 16, D)
        x1t = sb(s3, "x1t", [128, D])
        for i in range(nblk):
            s = cnt["x"] % 1
            cnt["x"] += 1
            t = xt[s]
            P.dma("sp", lambda e, t=t, i=i: e.dma_start(out=t[:], in_=x_d[i * 128:(i + 1) * 128, :]), writes=[f"xt{s}"])
            for nb in range(4):
                def mmM(e, nb=nb, i=i):
                    for c in range(16):
                        r = e.matmul(pf[nb][:, :], lhsT=catT[:, c, i * 128:(i + 1) * 128], rhs=wo[:, c, nb * 512:(nb + 1) * 512], start=(c == 0), stop=(c == 15))
                    return r
                P.op("pe", mmM, reads=[f"cat{c}_{i}" for c in range(16)] + wo_k, writes=[f"pf{nb}"])
                cs_ = slice(nb * 512, (nb + 1) * 512)
                P.op("dve", lambda e, nb=nb, cs_=cs_: e.tensor_tensor(out=x1t[:, cs_], in0=pf[nb][:, :], in1=g1row[:, cs_], op=ALU.mult),
                     reads=[f"pf{nb}", "g1row"], writes=[f"x1t{nb}"])
                P.op("dve", lambda e, nb=nb, cs_=cs_, t=t: e.tensor_tensor(out=x1t[:, cs_], in0=x1t[:, cs_], in1=t[:, cs_], op=ALU.add),
                     reads=[f"x1t{nb}", f"xt{s}"], writes=[f"x1t{nb}"])
            P.dma("sp", lambda e, i=i: e.dma_start(out=x1_d[i * 128:(i + 1) * 128, :], in_=x1t[:]), reads=[f"x1t{nb}" for nb in range(4)],
                  writes=[f"x1d{i}"], semkey=("x1st",))
    BAR()
    if stop_after == "x1":
        return finish([f"x1d{i}" for i in range(nblk)])
    scat.close()
    BAR()

    emit_conv(len(conv_pending))
    with ExitStack() as s4:
        wq, wq_k = load_w(s4, "wq", w_query, 16, D)
        g2row = row_tile(s4, "g2row", mod_d[5 * D:6 * D], D); P.ops[-1].reads = ("mod_d",)
        sc2row = row_tile(s4, "sc2row", mod_d[4 * D:5 * D], D); P.ops[-1].reads = ("mod_d",)
        sh2row = row_tile(s4, "sh2row", mod_d[3 * D:4 * D], D); P.ops[-1].reads = ("mod_d",)
        P.op("dve", lambda e: e.tensor_scalar(out=sc2row[:], in0=sc2row[:], scalar1=1.0, scalar2=None, op0=ALU.add), reads=["sc2row"], writes=["sc2row"])
        skT = sb(s4, "skT", [128, 16, 128], BF16)
        qT = sb(s4, "qT", [128, 16, 128], BF16)
        h2b2 = sb(s4, "h2b2", [128, D], BF16)
        XT1 = xt[0]; H2B = [xn, h2b2]
        sc = sb(s4, "sc", [128, 16, 128]); zp = sb(s4, "zp", [128, 128])
        tops = sb(s4, "tops", [128, 16, 16]); topi = sb(s4, "topi", [128, 16, 16])
        iot256 = sb(s4, "iot256", [128, 256])
        topiu = sb(s4, "topiu", [128, 16, 16], mybir.dt.uint32); bposu = sb(s4, "bposu", [128, 16], mybir.dt.uint32)
        bposf = sb(s4, "bposf", [128, 16])
        cands1 = sb(s4, "cands1", [128, 16, 16]); candi1 = sb(s4, "candi1", [128, 16, 16]); zc = sb(s4, "zc", [128, 256])
        bests = sb(s4, "bests", [128, 8, 16]); besti = sb(s4, "besti", [128, 8, 16])
        bidx3 = [sb(s4, f"bidx{q}", [128, 128], I32) for q in range(3)]
        gsm = sb(s4, "gsm", [128, 32])
        gate2 = [sb(s4, f"gate{q}", [128, 8, 16]) for q in range(2)]
        pre2 = [sb(s4, f"pre{q}", [128, 128]) for q in range(2)]
        actv2 = [sb(s4, f"actv{q}", [128, 128]) for q in range(2)]
        scr2 = sb(s4, "scr2", [128, D], BF16)
        dg = [sb(s4, f"dg{q}", [128, 128], BF16) for q in range(2)]
        prod = [sb(s4, f"prod{q}", [128, D], BF16) for q in range(2)]
        xr = sb(s4, "xr", [128, 1024])
        NG = 8
        gb = [sb(s4, f"gb{q}", [128, D], BF16) for q in range(NG)]
        acc = sb(s4, "acc", [128, D]); h2 = acc
        SCRK = ["scr2_0", "scr2_1", "scr2b_0", "scr2b_1"]
        skf = acc[:].rearrange("p (g d) -> p g d", g=16); skb = scr2[:].rearrange("p (g d) -> p g d", g=16)
        P.dma("sp", lambda e: e.dma_start(out=skf, in_=sub_keys.rearrange("g k d -> k g d")), writes=["acc"], semkey=("skf",))
        P.op("dve", lambda e: e.tensor_copy(out=skb, in_=skf), reads=["acc"], writes=SCRK)
        for half in range(2):
            def trSK(e, half=half):
                for g in range(8):
                    r = e.transpose(pt[half][:, g * 128:(g + 1) * 128], skb[:, half * 8 + g, :], ident_b[:])
                return r
            P.op("pe", trSK, reads=SCRK + ["ident_b"], writes=[f"pt{half}"])
            P.op("dve", lambda e, half=half: e.tensor_copy(out=skT[:, half * 8:(half + 1) * 8, :], in_=pt[half][:, :].rearrange("p (g k) -> p g k", g=8)),
                 reads=[f"pt{half}"], writes=[f"skT{half}"])
        def mmI(e):
            return e.matmul(pf[5][:, 0:128], lhsT=ones_b[:], rhs=tri_b[:], start=True, stop=True)
        P.op("pe", mmI, reads=["ones_b", "tri_b"], writes=["pf5"])
        P.op("dve", lambda e: e.tensor_scalar(out=iot256[:, 0:128], in0=pf[5][:, 0:128], scalar1=-1.0, scalar2=None, op0=ALU.add), reads=["pf5"], writes=["iotA"])
        P.op("dve", lambda e: e.tensor_scalar(out=iot256[:, 128:256], in0=pf[5][:, 0:128], scalar1=127.0, scalar2=None, op0=ALU.add), reads=["pf5", "iotA"], writes=["iot256"])
        gcount = {"n": 0, "pr": 0}

        def prep_topk(i):
            p = i % 2
            t = XT1; hb = H2B[p]; bidx = bidx3[i % 3]; gate = gate2[p]
            P.dma("sp", lambda e: e.dma_start(out=t[:], in_=x1_d[i * 128:(i + 1) * 128, :]), reads=[f"x1d{i}"], writes=["XT"])
            P.op("act", lambda e: e.activation(out=junk[:], in_=t[:], func=AF.Square, accum_out=st4[:, 0:1]), reads=["XT"], writes=["junk", "st_ss"])
            rstd_of(st4[:, 0:1], D, st4[:, 2:3], ["st_ss"], "st_r", st4[:, 1:2], "st_t")
            P.op("dve", lambda e: e.scalar_tensor_tensor(out=h2[:], in0=t[:], scalar=st4[:, 2:3], in1=sc2row[:], op0=ALU.mult, op1=ALU.mult),
                 reads=["XT", "st_r", "sc2row"], writes=["acc"])
            P.op("dve", lambda e: e.tensor_tensor(out=hb[:], in0=h2[:], in1=sh2row[:], op=ALU.add), reads=["acc", "sh2row"], writes=[f"H2B{p}"])
            for half in range(2):
                def trs(e, half=half):
                    for k in range(8):
                        kk = half * 8 + k
                        r = e.transpose(pt[half][:, k * 128:(k + 1) * 128], hb[:, kk * 128:(kk + 1) * 128], ident_b[:])
                    return r
                P.op("pe", trs, reads=[f"H2B{p}", "ident_b"], writes=[f"pt{half}"])
                P.op("act", lambda e, half=half: e.copy(out=hT[:, half * 8:(half + 1) * 8, :], in_=pt[half][:, :].rearrange("p (c t) -> p c t", c=8)),
                     reads=[f"pt{half}"], writes=[f"hT{kk}" for kk in range(half * 8, half * 8 + 8)])
            for c4 in range(4):
                def mmQ2(e, c4=c4):
                    for cc in range(4):
                        ch = c4 * 4 + cc
                        for k in range(16):
                            r = e.matmul(pf[4][:, cc * 128:(cc + 1) * 128], lhsT=wq[:, k, ch * 128:(ch + 1) * 128], rhs=hT[:, k, :], start=(k == 0), stop=(k == 15))
                    return r
                P.op("pe", mmQ2, reads=hT_keys + wq_k, writes=["pf4"])
                P.op("act", lambda e, c4=c4: e.copy(out=qT[:, c4 * 4:(c4 + 1) * 4, :], in_=pf[4][:, :].rearrange("p (c t) -> p c t", c=4)),
                     reads=["pf4"], writes=[f"qT{c4}"])

                def mmSc(e, c4=c4):
                    for cc in range(4):
                        ch = c4 * 4 + cc
                        r = e.matmul(pf[5][:, cc * 128:(cc + 1) * 128], lhsT=qT[:, ch, :], rhs=skT[:, ch, :], start=True, stop=True)
                    return r
                P.op("pe", mmSc, reads=[f"qT{c4}", "skT0", "skT1"], writes=["pf5"])
                P.op("act", lambda e, c4=c4: e.copy(out=sc[:, c4 * 4:(c4 + 1) * 4, :], in_=pf[5][:, :].rearrange("p (c t) -> p c t", c=4)),
                     reads=["pf5"], writes=[f"sc{c4}"])
            for g in range(16):
                sk_ = f"sc{g // 4}"
                P.op("dve", lambda e, g=g: e.max(out=tops[:, g, 0:8], in_=sc[:, g, :]), reads=[sk_], writes=["tA"])
                P.op("dve", lambda e, g=g: e.max_index(out=topiu[:, g, 0:8], in_max=tops[:, g, 0:8], in_values=sc[:, g, :]), reads=[sk_, "tA"], writes=["tiA"])
                P.op("dve", lambda e, g=g: e.match_replace(out=zp[:], in_to_replace=tops[:, g, 0:8], in_values=sc[:, g, :], imm_value=-1e30),
                     reads=[sk_, "tA"], writes=["zp"])
                P.op("dve", lambda e, g=g: e.max(out=tops[:, g, 8:16], in_=zp[:]), reads=["zp"], writes=["tB"])
                P.op("dve", lambda e, g=g: e.max_index(out=topiu[:, g, 8:16], in_max=tops[:, g, 8:16], in_values=zp[:]), reads=["zp", "tB"], writes=["tiB"])
            P.op("dve", lambda e: e.tensor_copy(out=topi[:], in_=topiu[:]), reads=["tiA", "tiB"], writes=["topi"])
            t4 = tops[:].rearrange("p (h j) k -> p h j k", j=2)
            i4 = topi[:].rearrange("p (h j) k -> p h j k", j=2)
            cf = cands1[:].rearrange("p a b -> p (a b)")
            cif = candi1[:].rearrange("p a b -> p (a b)")
            for h in range(8):
                P.op("dve", lambda e, h=h: e.tensor_tensor(out=cands1[:], in0=bcast(t4[:, h, 0, :].unsqueeze(2), [128, 16, 16]),
                                                           in1=bcast(t4[:, h, 1, :].unsqueeze(1), [128, 16, 16]), op=ALU.add),
                     reads=["tA", "tB"], writes=["cands"])
                P.op("dve", lambda e, h=h: e.scalar_tensor_tensor(out=candi1[:], in0=bcast(i4[:, h, 0, :].unsqueeze(2), [128, 16, 16]), scalar=128.0,
                                                                  in1=bcast(i4[:, h, 1, :].unsqueeze(1), [128, 16, 16]), op0=ALU.mult, op1=ALU.add),
                     reads=["topi"], writes=["candi"])
                P.op("dve", lambda e, h=h: e.max(out=bests[:, h, 0:8], in_=cf), reads=["cands"], writes=["bA"])
                P.op("dve", lambda e, h=h: e.max_index(out=bposu[:, 0:8], in_max=bests[:, h, 0:8], in_values=cf), reads=["cands", "bA"], writes=["bpA"])
                P.op("dve", lambda e, h=h: e.match_replace(out=zc[:], in_to_replace=bests[:, h, 0:8], in_values=cf, imm_value=-1e30),
                     reads=["cands", "bA"], writes=["zc"])
                P.op("dve", lambda e, h=h: e.max(out=bests[:, h, 8:16], in_=zc[:]), reads=["zc"], writes=["bB"])
                P.op("dve", lambda e, h=h: e.max_index(out=bposu[:, 8:16], in_max=bests[:, h, 8:16], in_values=zc[:]), reads=["zc", "bB"], writes=["bpB"])
                P.op("dve", lambda e: e.tensor_copy(out=bposf[:], in_=bposu[:]), reads=["bpA", "bpB"], writes=["bposf"])
                for k0 in (0, 8):
                    def idx2(e, h=h, k0=k0):
                        for k in range(k0, k0 + 8):
                            r = e.scalar_tensor_tensor(out=scr2[:, (k % 8) * 256:(k % 8 + 1) * 256], in0=iot256[:], scalar=bposf[:, k:k + 1], in1=cif,
                                                       op0=ALU.is_equal, op1=ALU.mult, accum_out=besti[:, h, k:k + 1])
                        return r
                    P.op("dve", idx2, reads=["candi", "bposf", "iot256"], writes=SCRK + [f"besti{h}"])
            bik = [f"besti{h}" for h in range(8)]
            P.op("dve", lambda e: e.tensor_copy(out=bidx[:], in_=besti[:].rearrange("p h k -> p (h k)")), reads=bik, writes=[f"bidx{i % 3}"])
            P.op("dve", lambda e: e.tensor_tensor(out=gate[:], in0=bests[:], in1=bcast(bests[:, :, 0:1], [128, 8, 16]), op=ALU.subtract),
                 reads=["bA", "bB"], writes=[f"gate{p}a"])
            P.op("act", lambda e: e.activation(out=gate[:], in_=gate[:], func=AF.Exp), reads=[f"gate{p}a"], writes=[f"gate{p}b"])
            P.op("dve", lambda e: e.tensor_reduce(out=gsm[:, 0:8], in_=gate[:], axis=AX.X, op=ALU.add), reads=[f"gate{p}b"], writes=["gsm0"])
            P.op("dve", lambda e: e.reciprocal(out=gsm[:, 8:16], in_=gsm[:, 0:8]), reads=["gsm0"], writes=["gsm8"])
            P.op("dve", lambda e: e.tensor_tensor(out=gate[:], in0=gate[:], in1=bcast(gsm[:, 8:16].unsqueeze(2), [128, 8, 16]), op=ALU.mult),
                 reads=[f"gate{p}b", "gsm8"], writes=[f"gate{p}"])

        def u_step(i, hk):
            p = i % 2
            hb = H2B[p]; bidx = bidx3[i % 3]; pre = pre2[p]
            b_ = gcount["n"] % NG
            gcount["n"] += 1
            q_ = gcount["pr"] % 2
            gcount["pr"] += 1
            P.dma("pool", lambda e: e.indirect_dma_start(
                out=gb[b_][:], out_offset=None, in_=u_bf, in_offset=bass.IndirectOffsetOnAxis(ap=bidx[:, hk:hk + 1], axis=0)),
                reads=[f"bidx{i % 3}", "utab"], writes=[f"gb{b_}"])
            P.op("dve", lambda e: e.tensor_tensor(out=prod[q_][:], in0=gb[b_][:], in1=hb[:], op=ALU.mult),
                 reads=[f"gb{b_}", f"H2B{p}"], writes=[f"prod{q_}"])
            P.op("act", lambda e: e.activation(out=prod[q_][:], in_=prod[q_][:], func=AF.Copy, accum_out=pre[:, hk:hk + 1]),
                 reads=[f"prod{q_}"], writes=[f"prod{q_}", f"pre{p}"])

        def u_end(i):
            p = i % 2
            pre = pre2[p]; actv = actv2[p]; gate = gate2[p]
            P.op("act", lambda e: e.activation(out=actv[:], in_=pre[:], func=AF.Gelu), reads=[f"pre{p}"], writes=[f"actv{p}a"])
            P.op("dve", lambda e: e.tensor_tensor(out=actv[:], in0=actv[:], in1=gate[:].rearrange("p h k -> p (h k)"), op=ALU.mult),
                 reads=[f"actv{p}a", f"gate{p}"], writes=[f"actv{p}"])

        def v_step(i, hk):
            p = i % 2
            bidx = bidx3[i % 3]; actv = actv2[p]
            b_ = gcount["n"] % NG
            gcount["n"] += 1
            dq = hk % 2
            P.dma("pool", lambda e: e.indirect_dma_start(
                out=gb[b_][:], out_offset=None, in_=v_bf, in_offset=bass.IndirectOffsetOnAxis(ap=bidx[:, hk:hk + 1], axis=0)),
                reads=[f"bidx{i % 3}", "vtab"], writes=[f"gb{b_}"])
            P.op("act", lambda e: e.activation(out=dg[dq][:], in_=ident_b[:], func=AF.Copy, scale=actv[:, hk:hk + 1]),
                 reads=[f"actv{p}", "ident_b"], writes=[f"dg{dq}"])

            def mmV(e):
                for nb in range(4):
                    r = e.matmul(pf[nb][:, :], lhsT=dg[dq][:], rhs=gb[b_][:, nb * 512:(nb + 1) * 512], start=(hk == 0), stop=(hk == 127))
                return r
            P.op("pe", mmV, reads=[f"dg{dq}", f"gb{b_}"], writes=["pf0", "pf1", "pf2", "pf3"])

        def final(i):
            for nb in range(4):
                cs_ = slice(nb * 512, (nb + 1) * 512)
                P.op("dve", lambda e, nb=nb, cs_=cs_: e.tensor_tensor(out=acc[:, cs_], in0=pf[nb][:, :], in1=g2row[:, cs_], op=ALU.mult),
                     reads=[f"pf{nb}", "g2row"], writes=["acc"])
            for hf in range(2):
                P.dma("sp", lambda e, hf=hf: e.dma_start(out=xr[:], in_=x1_d[i * 128:(i + 1) * 128, hf * 1024:(hf + 1) * 1024]), reads=[f"x1d{i}"], writes=["xr"])
                P.op("dve", lambda e, hf=hf: e.tensor_tensor(out=acc[:, hf * 1024:(hf + 1) * 1024], in0=acc[:, hf * 1024:(hf + 1) * 1024], in1=xr[:], op=ALU.add),
                     reads=["acc", "xr"], writes=["acc"])
            P.dma("sp", lambda e: e.dma_start(out=out_d[i * 128:(i + 1) * 128, :], in_=acc[:]), reads=["acc"], writes=[f"out{i}", "acc"],
                  semkey=("outst",))

        def capture(fn, *a):
            saved = P.ops
            P.ops = []
            fn(*a)
            got = P.ops
            P.ops = saved
            return got

        if stop_after in ("pdbg", "pdbg2"):
            return finish([])
        prep_topk(0)
        for i in range(nblk + 1):
            nxt = capture(prep_topk, i + 1) if i + 1 < nblk else []
            per = (len(nxt) + 127) // 128
            for hk in range(128):
                if i < nblk:
                    u_step(i, hk)
                if i >= 1:
                    v_step(i - 1, hk)
                P.ops.extend(nxt[hk * per:(hk + 1) * per])
            if i < nblk:
                u_end(i)
            if i >= 1:
                final(i - 1)
        return finish([f"out{i}" for i in range(nblk)])


_CACHE = {}


def _consts():
    ident = np.eye(128, dtype=np.float32)
    k = np.arange(128)
    tri = (k[None, :] >= k[:, None]).astype(np.float32)
    negmask = np.where(k[None, :] >= k[:, None], 0.0, -30000.0).astype(np.float32)
    invf = (1.0 / (10000.0 ** (np.arange(32, dtype=np.float32) * (2.0 / 64)))).astype(np.float32)
    invf = np.broadcast_to(invf[None, :], (128, 32)).copy()
    return ident, tri, negmask, invf


def kernel(**inputs):
    if "nc" not in _CACHE:
        _CACHE["nc"] = build()
    nc = _CACHE["nc"]
    ident, tri, negmask, invf = _consts()
    f = lambda a: np.ascontiguousarray(np.asarray(a))
    shared = {
        "w_ada": f(inputs["w_ada"][0]), "b_ada": f(inputs["b_ada"][0]).reshape(1, -1), "w_in": f(inputs["w_in"][0]),
        "q_a_norm": f(inputs["q_a_norm"][0]), "w_uq": f(inputs["w_uq"][0]), "kv_a_norm": f(inputs["kv_a_norm"][0]),
        "w_ukv": f(inputs["w_ukv"][0]), "q_norm": f(inputs["q_norm"][0]), "k_norm": f(inputs["k_norm"][0]),
        "attn_out_norm": f(inputs["attn_out_norm"][0]), "conv_w": f(inputs["conv_w"][0]), "conv_b": f(inputs["conv_b"][0]),
        "dt_bias": f(inputs["dt_bias"][0]), "a_log": f(inputs["a_log"][0]), "d_skip": f(inputs["d_skip"][0]),
        "ssd_norm": f(inputs["ssd_norm"][0]), "w_out": f(inputs["w_out"][0]), "w_query": f(inputs["w_query"][0]),
        "sub_keys": f(inputs["sub_keys"][0]).reshape(16, 128, 128), "u_experts": f(inputs["u_experts"][0]),
        "v_experts": f(inputs["v_experts"][0]), "ident": ident, "tri": tri, "negmask": negmask, "invfreq": invf,
    }
    x = np.asarray(inputs["x"]); c = np.asarray(inputs["c"]); pos = np.asarray(inputs["positions"])
    in_maps = []
    for b in range(8):
        m = dict(shared)
        m["x"] = f(x[b]); m["c"] = f(c[b]); m["pos"] = f(pos[b]).reshape(S, 1).astype(np.int32)
        in_maps.append(m)
    res = run_bass_kernel_spmd(nc, in_maps, core_ids=list(range(8)))
    return np.stack([np.asarray(r["out"]) for r in res.results], axis=0).astype(np.float32)
```

```python
from contextlib import ExitStack
import os
import numpy as np
import concourse.bass as bass
import concourse.mybir as mybir
from concourse.bass_utils import run_bass_kernel_spmd

F32 = mybir.dt.float32
BF16 = mybir.dt.bfloat16
I32 = mybir.dt.int32
AF = mybir.ActivationFunctionType
ALU = mybir.AluOpType
AX = mybir.AxisListType

D = 2048
S = 2048
NB = 16
EPS = 1e-6
ENGS = ("pe", "act", "dve", "pool", "sp")
SEM_EPOCH = 20000


class Op:
    __slots__ = ("eng", "fn", "reads", "writes", "dma", "semkey", "deps", "waits", "signal", "idx", "barrier")

    def __init__(self, eng, fn, reads, writes, dma, semkey):
        self.eng, self.fn = eng, fn
        self.reads, self.writes = tuple(reads), tuple(writes)
        self.dma, self.semkey = dma, semkey
        self.deps, self.waits, self.signal = (), [], None
        self.barrier = False


class Prog:
    def __init__(self, nc):
        self.nc = nc
        self.ops = []
        self.self_sync = ("dve", "act", "pool")

    def op(self, eng, fn, reads=(), writes=()):
        xr = [k for k in reads if isinstance(k, str) and k[:2] in ("pf", "pt") and k not in writes]
        writes = tuple(writes) + tuple(xr)
        o = Op(eng, fn, reads, writes, False, None)
        self.ops.append(o)
        return o

    def dma(self, eng, fn, reads=(), writes=(), semkey=None):
        if semkey is None:
            semkey = ("dma",) + (tuple(writes) if writes else tuple(reads))
        o = Op(eng, fn, reads, writes, True, semkey)
        self.ops.append(o)
        return o

    def capture(self, fn, *a):
        saved = self.ops
        self.ops = []
        fn(*a)
        got = self.ops
        self.ops = saved
        return got

    @staticmethod
    def merge(a, b):
        out, ia, ib = [], 0, 0
        na, nb = len(a), len(b)
        while ia < na or ib < nb:
            if ib >= nb or (ia < na and ia * nb <= ib * na):
                out.append(a[ia]); ia += 1
            else:
                out.append(b[ib]); ib += 1
        return out

    def barrier(self, fn):
        o = Op("act", fn, (), (), False, None)
        o.barrier = True
        self.ops.append(o)
        return o

    def _analyze(self):
        last_write, readers = {}, {}
        last_on = {}
        dmas_since = []
        last_barrier = None
        for i, o in enumerate(self.ops):
            o.idx = i
            deps = set()
            if o.barrier:
                deps.update(last_on.values())
                deps.update(dmas_since)
                dmas_since = []
                if last_barrier is not None:
                    deps.add(last_barrier)
                last_barrier = i
            elif last_barrier is not None:
                deps.add(last_barrier)
            last_on[o.eng] = i
            if o.dma:
                dmas_since.append(i)
            for t in o.reads:
                if t in last_write:
                    deps.add(last_write[t])
            for t in o.writes:
                if t in last_write:
                    deps.add(last_write[t])
                deps.update(readers.get(t, ()))
            deps.discard(i)
            o.deps = deps
            for t in o.reads:
                readers.setdefault(t, []).append(i)
            for t in o.writes:
                last_write[t] = i
                readers[t] = []
        need = set()
        for o in self.ops:
            for d in o.deps:
                od = self.ops[d]
                if od.dma or od.eng != o.eng or o.eng in self.self_sync:
                    need.add(d)
        cnt = {e: 0 for e in ENGS}
        dcnt = {}
        self.semkeys = []
        for o in self.ops:
            if o.dma:
                k = dcnt.get(o.semkey, 0) + 1
                dcnt[o.semkey] = k
                if k == 1:
                    self.semkeys.append(o.semkey)
                o.signal = (("d", o.semkey), 16 * k, 16)
            elif o.idx in need:
                cnt[o.eng] += 1
                ep, v = divmod(cnt[o.eng] - 1, SEM_EPOCH)
                o.signal = (("e", o.eng, ep), v + 1, 1)
        self.nepochs = {e: (cnt[e] + SEM_EPOCH - 1) // SEM_EPOCH for e in ENGS}
        for o in self.ops:
            w = {}
            for d in o.deps:
                od = self.ops[d]
                if od.dma or od.eng != o.eng or o.eng in self.self_sync:
                    s, v, _ = od.signal
                    if w.get(s, 0) < v:
                        w[s] = v
            o.waits = sorted(w.items(), key=lambda kv: str(kv[0]))

    def emit(self):
        nc = self.nc
        self._analyze()
        with ExitStack() as es:
            sems = {}
            n = 0
            for e in ENGS:
                for ep in range(self.nepochs[e]):
                    sems[("e", e, ep)] = es.enter_context(nc.semaphore(f"s_{e}{ep}"))
                    n += 1
            for k in self.semkeys:
                sems[("d", k)] = es.enter_context(nc.semaphore(f"d{n}"))
                n += 1
            self.nsems = n
            by_eng = {e: [o for o in self.ops if o.eng == e] for e in ENGS}

            def run(engine, name):
                waited = {}
                for o in by_eng[name]:
                    for s, v in o.waits:
                        if waited.get(s, 0) < v:
                            engine.wait_ge(sems[s], v)
                            waited[s] = v
                    ins = o.fn(engine)
                    if o.signal is not None:
                        s, v, inc = o.signal
                        ins.then_inc(sems[s], inc)

            with nc.Block() as block:
                @block.tensor
                def _(e):
                    run(e, "pe")

                @block.scalar
                def _(e):
                    run(e, "act")

                @block.vector
                def _(e):
                    run(e, "dve")

                @block.gpsimd
                def _(e):
                    run(e, "pool")

                @block.sync
                def _(e):
                    run(e, "sp")


def bcast(ap, shape):
    return ap.to_broadcast(list(shape))


def build(stop_after=None, debug=False, nblk=NB, ngroups=2):
    nc = bass.Bass("TRN2", target_bir_lowering=False)
    P = Prog(nc)

    def din(name, shape, dt=F32):
        return nc.dram_tensor(name, list(shape), dt, kind="ExternalInput").ap()

    x_d = din("x", [S, D]); c_d = din("c", [D]); pos_d = din("pos", [S, 1], I32)
    w_ada = din("w_ada", [D, 6 * D]); b_ada = din("b_ada", [1, 6 * D]); w_in = din("w_in", [D, 3408])
    q_a_norm = din("q_a_norm", [512]); w_uq = din("w_uq", [512, 1536]); kv_a_norm = din("kv_a_norm", [256])
    w_ukv = din("w_ukv", [256, 2048]); q_norm = din("q_norm", [192]); k_norm = din("k_norm", [192])
    attn_out_norm = din("attn_out_norm", [1024]); conv_w = din("conv_w", [4, 1536]); conv_b = din("conv_b", [1536])
    dt_bias = din("dt_bias", [16]); a_log = din("a_log", [16]); d_skip = din("d_skip", [16])
    ssd_norm = din("ssd_norm", [1024]); w_out = din("w_out", [D, D]); w_query = din("w_query", [D, D])
    sub_keys = din("sub_keys", [16, 128, 128])
    if stop_after in (None, "pdbg", "pdbg2"):
        u_exp = din("u_experts", [16384, D]); v_exp = din("v_experts", [16384, D])
    ident_d = din("ident", [128, 128]); tri_d = din("tri", [128, 128]); negm_d = din("negmask", [128, 128])
    invf_d = din("invfreq", [128, 32])
    out_d = nc.dram_tensor("out", [S, D], F32, kind="ExternalOutput").ap()
    mod_d = nc.dram_tensor("mod_scr", [6 * D], F32, kind="Internal").ap()
    x1_d = nc.dram_tensor("x1_scr", [S, D], F32, kind=("ExternalOutput" if stop_after == "x1" else "Internal")).ap()
    dbg = {}
    conv_pending = []
    if stop_after in (None, "pdbg", "pdbg2"):
        u_bf = nc.dram_tensor("u_bf", [16384, D], BF16, kind="Internal").ap()
        v_bf = nc.dram_tensor("v_bf", [16384, D], BF16, kind="Internal").ap()
        for r0 in range(0, 16384, 1024):
            conv_pending.append((u_bf, u_exp, r0, "utab"))
            conv_pending.append((v_bf, v_exp, r0, "vtab"))

    win_bf = nc.dram_tensor("win_bf", [D, 2560], BF16, kind="Internal").ap()
    for r0 in range(0, D, 512):
        conv_pending.insert(r0 // 512, (win_bf, w_in[:, 832:3392], r0, "wintab"))

    def emit_conv(n):
        for _ in range(min(n, len(conv_pending))):
            dst, src, r0, key = conv_pending.pop(0)
            nr = 512 if key == "wintab" else 1024
            P.dma("pool", lambda e, dst=dst, src=src, r0=r0, nr=nr: e.dma_start(out=dst[r0:r0 + nr, :], in_=src[r0:r0 + nr, :]),
                  writes=[key], semkey=("conv", key))

    def dbg_out(nm, shp, dt=F32):
        dbg[nm] = nc.dram_tensor(nm, list(shp), dt, kind="ExternalOutput").ap()
        return dbg[nm]

    def finish(keys):
        P.op("sp", lambda e: None, reads=keys)
        P.emit()
        return nc

    top = ExitStack()

    def sb(es, name, shape, dt=F32):
        return es.enter_context(nc.sbuf_tensor(name, list(shape), dt))

    pf = [top.enter_context(nc.psum_tensor(f"pf{i}", [128, 512], F32)) for i in range(6)]
    pt = [top.enter_context(nc.psum_tensor(f"pt{i}", [128, 1024], BF16)) for i in range(2)]

    ident_f = sb(top, "ident_f", [128, 128]); ident_b = sb(top, "ident_b", [128, 128], BF16)
    tri_f = sb(top, "tri_f", [128, 128]); tri_b = sb(top, "tri_b", [128, 128], BF16)
    ones_b = sb(top, "ones_b", [128, 128], BF16); negm = sb(top, "negm", [128, 128])
    invf = sb(top, "invf", [128, 32]); bar = sb(top, "bar", [128, 2])
    BAR = lambda: P.barrier(lambda e: e.copy(out=bar[:, 0:1], in_=bar[:, 1:2]))
    modT = sb(top, "modT", [128, 96]); scp1 = sb(top, "scp1", [128, 16]); scp2 = sb(top, "scp2", [128, 16])

    P.dma("sp", lambda e: e.dma_start(out=ident_f[:], in_=ident_d), writes=["ident_f"])
    P.dma("sp", lambda e: e.dma_start(out=tri_f[:], in_=tri_d), writes=["tri_f"])
    P.dma("sp", lambda e: e.dma_start(out=negm[:], in_=negm_d), writes=["negm"])
    P.dma("sp", lambda e: e.dma_start(out=invf[:], in_=invf_d), writes=["invf"])
    P.op("dve", lambda e: e.tensor_copy(out=ident_b[:], in_=ident_f[:]), reads=["ident_f"], writes=["ident_b"])
    P.op("dve", lambda e: e.tensor_copy(out=tri_b[:], in_=tri_f[:]), reads=["tri_f"], writes=["tri_b"])
    P.op("dve", lambda e: e.memset(ones_b[:], 1.0), writes=["ones_b"])
    P.op("act", lambda e: e.activation(out=bar[:], in_=invf[:, 0:2], func=AF.Copy), reads=["invf"], writes=["bar"])

    with ExitStack() as sa:
        cT = sb(sa, "cT", [128, 16]); cab = sb(sa, "cab", [128, 16], BF16)
        brow = sb(sa, "brow", [1, 6 * D]); modrow = sb(sa, "modrow", [1, 6 * D])
        wa = [sb(sa, f"wa{i}", [128, 16, 512], BF16) for i in range(2)]
        waf = [sb(sa, f"waf{i}", [128, 16, 512], F32) for i in range(2)]
        with nc.allow_non_contiguous_dma(reason="tiny transposed loads"):
            P.dma("sp", lambda e: e.dma_start(out=cT[:], in_=c_d.rearrange("(k p) -> p k", p=128), allow_slow_non_contiguous=True), writes=["cT"])
        P.dma("sp", lambda e: e.dma_start(out=brow[:], in_=b_ada), writes=["brow"])
        P.op("act", lambda e: e.activation(out=cab[:], in_=cT[:], func=AF.Silu), reads=["cT"], writes=["cab"])
        for j in range(24):
            w = wa[j % 2]
            wf = waf[j % 2]
            P.dma("sp", lambda e, wf=wf, j=j: e.dma_start(
                out=wf[:], in_=w_ada[:, j * 512:(j + 1) * 512].rearrange("(k p) n -> p k n", p=128)),
                writes=[f"waf{j % 2}"])
            P.op("dve", lambda e, w=w, wf=wf: e.tensor_copy(out=w[:, 0:8, :], in_=wf[:, 0:8, :]), reads=[f"waf{j % 2}"], writes=[f"wa{j % 2}a"])
            P.op("act", lambda e, w=w, wf=wf: e.copy(out=w[:, 8:16, :], in_=wf[:, 8:16, :]), reads=[f"waf{j % 2}"], writes=[f"wa{j % 2}b"])

            def mmA(e, w=w):
                for k in range(16):
                    r = e.matmul(pf[0][0:1, :], lhsT=cab[:, k:k + 1], rhs=w[:, k, :], start=(k == 0), stop=(k == 15))
                return r
            P.op("pe", mmA, reads=["cab", f"wa{j % 2}a", f"wa{j % 2}b"], writes=["pf0"])
            P.op("dve", lambda e, j=j: e.tensor_tensor(out=modrow[0:1, j * 512:(j + 1) * 512], in0=pf[0][0:1, :],
                                                       in1=brow[0:1, j * 512:(j + 1) * 512], op=ALU.add),
                 reads=["pf0", "brow"], writes=["modrow"])
        P.dma("sp", lambda e: e.dma_start(out=mod_d.rearrange("(o n) -> o n", o=1), in_=modrow[:]),
              reads=["modrow"], writes=["mod_d"])
        with nc.allow_non_contiguous_dma(reason="tiny transposed loads"):
            P.dma("sp", lambda e: e.dma_start(out=modT[:], in_=mod_d.rearrange("(j p) -> p j", p=128), allow_slow_non_contiguous=True),
                  reads=["mod_d"], writes=["modT"])
        P.op("dve", lambda e: e.tensor_scalar(out=scp1[:], in0=modT[:, 16:32], scalar1=1.0, scalar2=None, op0=ALU.add),
             reads=["modT"], writes=["scp1"])
        P.op("dve", lambda e: e.tensor_scalar(out=scp2[:], in0=modT[:, 64:80], scalar1=1.0, scalar2=None, op0=ALU.add),
             reads=["modT"], writes=["scp2"])
        if stop_after == "A":
            o1 = dbg_out("d_mod", [1, 6 * D]); o2 = dbg_out("d_modT", [128, 96])
            P.dma("sp", lambda e: e.dma_start(out=o1, in_=modrow[:]), reads=["modrow"], writes=["o1"])
            P.dma("sp", lambda e: e.dma_start(out=o2, in_=modT[:]), reads=["modT"], writes=["o2"])
            return finish(["o1", "o2"])
    BAR()
    emit_conv(4)
    sh1 = modT[:, 0:16]
    sh2 = modT[:, 48:64]

    xt = [sb(top, f"xt{i}", [128, D]) for i in range(1)]
    junk = sb(top, "junk", [128, D], BF16)
    xn = sb(top, "xn", [128, D], BF16)
    st4 = sb(top, "st4", [128, 4])
    hT = sb(top, "hT", [128, 16, 128], BF16)
    cnt = {"x": 0}
    scat = ExitStack()
    catT = sb(scat, "catT", [128, 16, S], BF16)

    def rstd_of(ss_ap, n, out_ap, rkeys, wkey, tmp_ap, tmpkey):
        P.op("act", lambda e: e.activation(out=tmp_ap, in_=ss_ap, func=AF.Sqrt, scale=1.0 / n, bias=EPS),
             reads=rkeys, writes=[tmpkey])
        P.op("dve", lambda e: e.reciprocal(out=out_ap, in_=tmp_ap), reads=[tmpkey], writes=[wkey])

    def make_hT(src_d, i, scp, sh, keep_tokmajor=None):
        s = cnt["x"] % 1
        cnt["x"] += 1
        t = xt[s]
        P.dma("sp", lambda e: e.dma_start(out=t[:], in_=src_d[i * 128:(i + 1) * 128, :]), writes=[f"xt{s}"])
        P.op("act", lambda e: e.activation(out=junk[:], in_=t[:], func=AF.Square, accum_out=st4[:, 0:1]),
             reads=[f"xt{s}"], writes=["junk", "st_ss"])
        rstd_of(st4[:, 0:1], D, st4[:, 2:3], ["st_ss"], "st_r", st4[:, 1:2], "st_t")
        P.op("dve", lambda e: e.tensor_scalar(out=xn[:], in0=t[:], scalar1=st4[:, 2:3], scalar2=None, op0=ALU.mult),
             reads=[f"xt{s}", "st_r"], writes=["xn"])
        HTM = os.environ.get("HT_MODE", "")
        if HTM == "xn":
            o_ = dbg_out("d_xn", [128, D], BF16)
            P.dma("sp", lambda e, o_=o_: e.dma_start(out=o_, in_=xn[:]), reads=["xn"], writes=["d_xn"])
            raise StopIteration
        for half in range(2):
            def trs(e, half=half):
                for k in range(8):
                    kk = half * 8 + k
                    r = e.transpose(pt[half][:, k * 128:(k + 1) * 128], xn[:, kk * 128:(kk + 1) * 128], ident_b[:])
                return r
            P.op("pe", trs, reads=["xn", "ident_b"], writes=[f"pt{half}"])
            for k in range(8):
                kk = half * 8 + k
                if (k % 2 == 0 and HTM != "act") or HTM == "dve":
                    P.op("dve", lambda e, kk=kk, k=k, half=half: e.tensor_scalar(
                        out=hT[:, kk, :], in0=pt[half][:, k * 128:(k + 1) * 128], scalar1=scp[:, kk:kk + 1],
                        scalar2=sh[:, kk:kk + 1], op0=ALU.mult, op1=ALU.add),
                        reads=[f"pt{half}", "scp1", "scp2", "modT"], writes=[f"hT{kk}"])
                else:
                    P.op("act", lambda e, kk=kk, k=k, half=half: e.activation(
                        out=hT[:, kk, :], in_=pt[half][:, k * 128:(k + 1) * 128], func=AF.Identity,
                        scale=scp[:, kk:kk + 1], bias=sh[:, kk:kk + 1]),
                        reads=[f"pt{half}", "scp1", "scp2", "modT"], writes=[f"hT{kk}"])
        return s

    hT_keys = [f"hT{k}" for k in range(16)]

    def load_w(es, name, src_ap, kchunks, ncols, eng="pool"):
        t = sb(es, name, [128, kchunks, ncols], BF16)
        step = max(1, 4096 // ncols)
        for k0 in range(0, kchunks, step):
            k1 = min(kchunks, k0 + step)
            P.dma(eng, lambda e, k0=k0, k1=k1: e.dma_start(
                out=t[:, k0:k1, :], in_=src_ap[k0 * 128:k1 * 128, :].rearrange("(k p) n -> p k n", p=128)),
                writes=[f"{name}_{k0}"], semkey=("w", name, k0))
        return t, [f"{name}_{k0}" for k0 in range(0, kchunks, step)]

    def row_tile(es, name, src_1d, n):
        t = sb(es, name, [128, n])
        P.dma("sp", lambda e: e.dma_start(out=t[:], in_=src_1d.partition_broadcast(128)), writes=[name])
        return t

    with ExitStack() as s1:
        win_a, win_a_k = load_w(s1, "win_a", w_in[:, 0:832], 16, 832)
        wuq, wuq_k = load_w(s1, "wuq", w_uq, 4, 1536)
        wukv, wukv_k = load_w(s1, "wukv", w_ukv, 2, 2048)
        qag = row_tile(s1, "qag", q_a_norm, 512); kvag = row_tile(s1, "kvag", kv_a_norm, 256)
        qg = row_tile(s1, "qg", q_norm, 192); kg = row_tile(s1, "kg", k_norm, 192)
        KTn = sb(s1, "KTn", [128, 4, S], BF16); KTr = sb(s1, "KTr", [128, 2, S], BF16)
        Vc = sb(s1, "Vc", [128, NB, 512], BF16)
        cqn = sb(s1, "cqn", [128, 512], BF16); ckvn = sb(s1, "ckvn", [128, 256], BF16)
        cqnT = sb(s1, "cqnT", [128, 4, 128], BF16); ckvnT = sb(s1, "ckvnT", [128, 2, 128], BF16)
        kr = sb(s1, "kr", [128, 64]); krr = sb(s1, "krr", [128, 64])
        qraw = sb(s1, "qraw", [128, 4, 192]); kvraw = sb(s1, "kvraw", [128, 4, 256])
        sq = sb(s1, "sq", [128, 1024]); sm = sb(s1, "sm", [128, 32])
        posi = sb(s1, "posi", [128, 1], I32); posf = sb(s1, "posf", [128, 1])
        ang = sb(s1, "ang", [128, 64]); angk = sb(s1, "angk", [128, 64]); angi = sb(s1, "angi", [128, 64], I32)
        cs = sb(s1, "cs", [128, 64])
        rt = sb(s1, "rt", [128, 4, 32, 4])
        qbn = sb(s1, "qbn", [128, 4, 128], BF16); qbr = sb(s1, "qbr", [128, 4, 64], BF16)
        kbn = sb(s1, "kbn", [128, 4, 128], BF16); kbr = sb(s1, "kbr", [128, 4, 64], BF16)
        QTn = sb(s1, "QTn", [128, 4, 128], BF16); QTr = sb(s1, "QTr", [128, 2, 128], BF16)
        QTnB = sb(s1, "QTnB", [128, 4, 128], BF16); QTrB = sb(s1, "QTrB", [128, 2, 128], BF16)
        QTn2 = [QTn, QTnB]; QTr2 = [QTr, QTrB]
        PT = [sb(s1, f"PT{i}", [128, 512], BF16) for i in range(2)]
        rden = sb(s1, "rden", [128, 128])

        def rope(src3, dst3, nh, rk, wk):
            cosb = bcast(cs[:, 0:32].unsqueeze(1), [128, nh, 32])
            sinb = bcast(cs[:, 32:64].unsqueeze(1), [128, nh, 32])
            x1, x2 = src3[:, :, 0:32], src3[:, :, 32:64]
            t = [rt[:, 0:nh, :, j] for j in range(4)]
            P.op("dve", lambda e: e.tensor_tensor(out=t[0], in0=x1, in1=cosb, op=ALU.mult), reads=rk + ["cs"], writes=["rt0"])
            P.op("dve", lambda e: e.tensor_tensor(out=t[1], in0=x2, in1=sinb, op=ALU.mult), reads=rk + ["cs"], writes=["rt1"])
            P.op("dve", lambda e: e.tensor_tensor(out=t[2], in0=x2, in1=cosb, op=ALU.mult), reads=rk + ["cs"], writes=["rt2"])
            P.op("dve", lambda e: e.tensor_tensor(out=t[3], in0=x1, in1=sinb, op=ALU.mult), reads=rk + ["cs"], writes=["rt3"])
            P.op("dve", lambda e: e.tensor_tensor(out=dst3[:, :, 0:32], in0=t[0], in1=t[1], op=ALU.subtract),
                 reads=["rt0", "rt1"], writes=[wk + "a"])
            P.op("dve", lambda e: e.tensor_tensor(out=dst3[:, :, 32:64], in0=t[2], in1=t[3], op=ALU.add),
                 reads=["rt2", "rt3"], writes=[wk + "b"])

        if stop_after == "mw":
            o_ = dbg_out("d_qag", [128, 512], F32); o2_ = dbg_out("d_wuq", [128, 4, 1536], BF16)
            P.dma("sp", lambda e: e.dma_start(out=o_, in_=qag[:]), reads=["qag"], writes=["d_qag"])
            P.dma("sp", lambda e: e.dma_start(out=o2_, in_=wuq[:]), reads=wuq_k, writes=["d_wuq"])
            return finish(["d_qag", "d_wuq"])
        def mla_front(G, i):
            if True:
                qp = (G * nblk + i) % 2
                QTn = QTn2[qp]; QTr = QTr2[qp]
                make_hT(x_d, i, scp1, sh1)
                emit_conv(2)
                P.dma("sp", lambda e, i=i: e.dma_start(out=posi[:], in_=pos_d[i * 128:(i + 1) * 128, :]), writes=["posi"])
                P.op("dve", lambda e: e.tensor_copy(out=posf[:], in_=posi[:]), reads=["posi"], writes=["posf"])
                P.op("dve", lambda e: e.tensor_scalar(out=ang[:, 0:32], in0=invf[:], scalar1=posf[:, 0:1], scalar2=None, op0=ALU.mult),
                     reads=["posf", "invf"], writes=["ang0"])
                P.op("dve", lambda e: e.tensor_scalar(out=ang[:, 32:64], in0=ang[:, 0:32], scalar1=float(np.pi / 2), scalar2=None, op0=ALU.add),
                     reads=["ang0"], writes=["ang1"])
                P.op("dve", lambda e: e.tensor_scalar(out=angk[:], in0=ang[:], scalar1=float(1.0 / (2 * np.pi)), scalar2=None, op0=ALU.mult),
                     reads=["ang0", "ang1"], writes=["angk"])
                P.op("dve", lambda e: e.tensor_copy(out=angi[:], in_=angk[:]), reads=["angk"], writes=["angi"])
                P.op("dve", lambda e: e.tensor_copy(out=angk[:], in_=angi[:]), reads=["angi"], writes=["angk2"])
                P.op("dve", lambda e: e.scalar_tensor_tensor(out=ang[:], in0=angk[:], scalar=float(-2 * np.pi), in1=ang[:],
                                                             op0=ALU.mult, op1=ALU.add), reads=["angk2", "ang0", "ang1"], writes=["angr"])
                P.op("act", lambda e: e.activation(out=cs[:, 0:32], in_=ang[:, 32:64], func=AF.Sin), reads=["angr"], writes=["cs_c"])
                P.op("act", lambda e: e.activation(out=cs[:, 32:64], in_=ang[:, 0:32], func=AF.Sin), reads=["angr", "cs_c"], writes=["cs"])

                def mmP(e):
                    for k in range(16):
                        e.matmul(pf[0][:, :], lhsT=hT[:, k, :], rhs=win_a[:, k, 0:512], start=(k == 0), stop=(k == 15))
                    for k in range(16):
                        r = e.matmul(pf[1][:, 0:320], lhsT=hT[:, k, :], rhs=win_a[:, k, 512:832], start=(k == 0), stop=(k == 15))
                    return r
                P.op("pe", mmP, reads=hT_keys + win_a_k, writes=["pf0", "pf1"])
                P.op("act", lambda e: e.activation(out=sq[:, 0:512], in_=pf[0][:, :], func=AF.Square, accum_out=sm[:, 0:1]),
                     reads=["pf0"], writes=["sq", "sm0"])
                rstd_of(sm[:, 0:1], 512, sm[:, 2:3], ["sm0"], "sm2", sm[:, 1:2], "sm1")
                P.op("dve", lambda e: e.scalar_tensor_tensor(out=cqn[:], in0=pf[0][:, :], scalar=sm[:, 2:3], in1=qag[:],
                                                             op0=ALU.mult, op1=ALU.mult), reads=["pf0", "sm2", "qag"], writes=["cqn"])
                P.op("act", lambda e: e.activation(out=sq[:, 0:256], in_=pf[1][:, 0:256], func=AF.Square, accum_out=sm[:, 3:4]),
                     reads=["pf1", "sm0"], writes=["sq", "sm3"])
                rstd_of(sm[:, 3:4], 256, sm[:, 5:6], ["sm3"], "sm5", sm[:, 4:5], "sm4")
                P.op("dve", lambda e: e.scalar_tensor_tensor(out=ckvn[:], in0=pf[1][:, 0:256], scalar=sm[:, 5:6], in1=kvag[:],
                                                             op0=ALU.mult, op1=ALU.mult), reads=["pf1", "sm5", "kvag"], writes=["ckvn"])
                P.op("act", lambda e: e.copy(out=kr[:], in_=pf[1][:, 256:320]), reads=["pf1"], writes=["kr"])
                P.op("act", lambda e: e.activation(out=sq[:, 0:64], in_=kr[:], func=AF.Square, accum_out=sm[:, 6:7]),
                     reads=["kr", "sm3"], writes=["sq", "sm6"])
                def trC(e):
                    for c in range(4):
                        e.transpose(pt[0][:, c * 128:(c + 1) * 128], cqn[:, c * 128:(c + 1) * 128], ident_b[:])
                    for c in range(2):
                        r = e.transpose(pt[0][:, (4 + c) * 128:(5 + c) * 128], ckvn[:, c * 128:(c + 1) * 128], ident_b[:])
                    return r
                P.op("pe", trC, reads=["cqn", "ckvn", "ident_b"], writes=["pt0"])
                P.op("dve", lambda e: e.tensor_copy(out=cqnT[:].rearrange("p c t -> p (c t)"), in_=pt[0][:, 0:512]),
                     reads=["pt0"], writes=["cqnT"])
                P.op("act", lambda e: e.copy(out=ckvnT[:].rearrange("p c t -> p (c t)"), in_=pt[0][:, 512:768]),
                     reads=["pt0"], writes=["ckvnT"])

                def mmQ(e, G=G):
                    for c in range(4):
                        e.matmul(pf[0][:, :], lhsT=cqnT[:, c, :], rhs=wuq[:, c, G * 768:G * 768 + 512], start=(c == 0), stop=(c == 3))
                    for c in range(4):
                        r = e.matmul(pf[1][:, 0:256], lhsT=cqnT[:, c, :], rhs=wuq[:, c, G * 768 + 512:G * 768 + 768], start=(c == 0), stop=(c == 3))
                    return r
                P.op("pe", mmQ, reads=["cqnT"] + wuq_k, writes=["pf0", "pf1"])
                qflat = qraw[:].rearrange("p h d -> p (h d)")
                P.op("act", lambda e: e.copy(out=qflat[:, 0:512], in_=pf[0][:, :]), reads=["pf0"], writes=["qraw0"])
                P.op("dve", lambda e: e.tensor_copy(out=qflat[:, 512:768], in_=pf[1][:, 0:256]), reads=["pf1"], writes=["qraw1"])
                P.op("act", lambda e: e.activation(out=sq[:, 0:768], in_=qflat, func=AF.Square),
                     reads=["qraw0", "qraw1", "sm6"], writes=["sq"])
                P.op("dve", lambda e: e.tensor_reduce(out=sm[:, 8:12], in_=sq[:, 0:768].rearrange("p (h d) -> p h d", h=4),
                                                      axis=AX.X, op=ALU.add), reads=["sq"], writes=["sm8"])
                rstd_of(sm[:, 8:12], 192, sm[:, 16:20], ["sm8"], "sm16", sm[:, 12:16], "sm12")
                P.op("dve", lambda e: e.tensor_tensor(out=qraw[:], in0=qraw[:], in1=bcast(sm[:, 16:20].unsqueeze(2), [128, 4, 192]), op=ALU.mult),
                     reads=["qraw0", "qraw1", "sm16"], writes=["qraw2"])
                P.op("dve", lambda e: e.tensor_tensor(out=qraw[:], in0=qraw[:], in1=bcast(qg[:].unsqueeze(1), [128, 4, 192]), op=ALU.mult),
                     reads=["qraw2", "qg"], writes=["qraw3"])
                P.op("act", lambda e: e.copy(out=qbn[:], in_=qraw[:, :, 0:128]), reads=["qraw3"], writes=["qbn"])
                rope(qraw[:, :, 128:192], qbr[:], 4, ["qraw3"], "qbr")

                def trQ(e):
                    for hh in range(4):
                        e.transpose(pt[1][:, hh * 128:(hh + 1) * 128], qbn[:, hh, :], ident_b[:])
                    qr2 = qbr[:].rearrange("p (a b) d -> p a (b d)", b=2)
                    for pp in range(2):
                        r = e.transpose(pt[1][:, (4 + pp) * 128:(5 + pp) * 128], qr2[:, pp, :], ident_b[:])
                    return r
                P.op("pe", trQ, reads=["qbn", "qbra", "qbrb", "ident_b"], writes=["pt1"])
                P.op("dve", lambda e: e.tensor_copy(out=QTn[:].rearrange("p c t -> p (c t)"), in_=pt[1][:, 0:512]), reads=["pt1"], writes=[f"QTn{qp}"])
                P.op("act", lambda e: e.copy(out=QTr[:].rearrange("p c t -> p (c t)"), in_=pt[1][:, 512:768]), reads=["pt1"], writes=[f"QTr{qp}"])

                def mmK(e, G=G):
                    for c in range(2):
                        e.matmul(pf[0][:, :], lhsT=ckvnT[:, c, :], rhs=wukv[:, c, G * 1024:G * 1024 + 512], start=(c == 0), stop=(c == 1))
                    for c in range(2):
                        r = e.matmul(pf[1][:, :], lhsT=ckvnT[:, c, :], rhs=wukv[:, c, G * 1024 + 512:G * 1024 + 1024], start=(c == 0), stop=(c == 1))
                    return r
                P.op("pe", mmK, reads=["ckvnT"] + wukv_k, writes=["pf0", "pf1"])
                kvflat = kvraw[:].rearrange("p h d -> p (h d)")
                P.op("act", lambda e: e.copy(out=kvflat[:, 0:512], in_=pf[0][:, :]), reads=["pf0"], writes=["kvraw0"])
                P.op("dve", lambda e: e.tensor_copy(out=kvflat[:, 512:1024], in_=pf[1][:, :]), reads=["pf1"], writes=["kvraw1"])
                P.op("act", lambda e, i=i: e.copy(out=Vc[:, i, :].rearrange("p (h d) -> p h d", h=4), in_=kvraw[:, :, 128:256]),
                     reads=["kvraw0", "kvraw1"], writes=[f"Vc{i}"])
                P.op("act", lambda e: e.activation(out=sq[:, 0:512].rearrange("p (h d) -> p h d", h=4), in_=kvraw[:, :, 0:128], func=AF.Square),
                     reads=["kvraw0", "kvraw1", "sm8"], writes=["sq"])
                P.op("dve", lambda e: e.tensor_reduce(out=sm[:, 20:24], in_=sq[:, 0:512].rearrange("p (h d) -> p h d", h=4),
                                                      axis=AX.X, op=ALU.add), reads=["sq"], writes=["sm20"])
                P.op("dve", lambda e: e.tensor_scalar(out=sm[:, 20:24], in0=sm[:, 20:24], scalar1=sm[:, 6:7], scalar2=None, op0=ALU.add),
                     reads=["sm20", "sm6"], writes=["sm20b"])
                rstd_of(sm[:, 20:24], 192, sm[:, 28:32], ["sm20b"], "sm28", sm[:, 24:28], "sm24")
                P.op("dve", lambda e: e.tensor_tensor(out=kvraw[:, :, 0:128], in0=kvraw[:, :, 0:128],
                                                      in1=bcast(sm[:, 28:32].unsqueeze(2), [128, 4, 128]), op=ALU.mult),
                     reads=["kvraw0", "kvraw1", "sm28"], writes=["kvraw2"])
                P.op("dve", lambda e: e.tensor_tensor(out=kbn[:], in0=kvraw[:, :, 0:128], in1=bcast(kg[:, 0:128].unsqueeze(1), [128, 4, 128]), op=ALU.mult),
                     reads=["kvraw2", "kg"], writes=["kbn"])
                P.op("dve", lambda e: e.tensor_tensor(out=kr[:], in0=kr[:], in1=kg[:, 128:192], op=ALU.mult), reads=["kr", "kg", "sm6"], writes=["krg"])
                rope(kr[:].unsqueeze(1), krr[:].unsqueeze(1), 1, ["krg"], "krr")
                P.op("dve", lambda e: e.tensor_tensor(out=kbr[:], in0=bcast(krr[:].unsqueeze(1), [128, 4, 64]),
                                                      in1=bcast(sm[:, 28:32].unsqueeze(2), [128, 4, 64]), op=ALU.mult),
                     reads=["krra", "krrb", "sm28"], writes=["kbr"])

                def trK(e):
                    for hh in range(4):
                        e.transpose(pt[0][:, hh * 128:(hh + 1) * 128], kbn[:, hh, :], ident_b[:])
                    kr2 = kbr[:].rearrange("p (a b) d -> p a (b d)", b=2)
                    for pp in range(2):
                        r = e.transpose(pt[0][:, (4 + pp) * 128:(5 + pp) * 128], kr2[:, pp, :], ident_b[:])
                    return r
                P.op("pe", trK, reads=["kbn", "kbr", "ident_b"], writes=["pt0"])
                P.op("dve", lambda e, i=i: e.tensor_copy(out=KTn[:, :, i * 128:(i + 1) * 128],
                                                         in_=pt[0][:, 0:512].rearrange("p (c t) -> p c t", c=4)), reads=["pt0"], writes=[f"KTn{i}"])
                P.op("act", lambda e, i=i: e.copy(out=KTr[:, :, i * 128:(i + 1) * 128],
                                                  in_=pt[0][:, 512:768].rearrange("p (c t) -> p c t", c=2)), reads=["pt0"], writes=[f"KTr{i}"])

        def mla_back(G, i):
            if True:
                qp = (G * nblk + i) % 2
                QTn = QTn2[qp]; QTr = QTr2[qp]
                for hh in range(4):
                    h = G * 4 + hh
                    pp, hf = hh // 2, hh % 2
                    ngrp = (i + 4) // 4
                    for g in range(ngrp):
                        j0, j1 = g * 4, min(i + 1, g * 4 + 4)
                        sbank = 4 + (g % 2)
                        pbuf = PT[g % 2]

                        def mmS(e, j0=j0, j1=j1, sbank=sbank, hh=hh, pp=pp, hf=hf):
                            for j in range(j0, j1):
                                o = pf[sbank][:, (j - j0) * 128:(j - j0 + 1) * 128]
                                e.matmul(o, lhsT=KTn[:, hh, j * 128:(j + 1) * 128], rhs=QTn[:, hh, :], start=True, stop=False)
                                r = e.matmul(o, lhsT=KTr[hf * 64:(hf + 1) * 64, pp, j * 128:(j + 1) * 128],
                                             rhs=QTr[hf * 64:(hf + 1) * 64, pp, :], start=False, stop=True)
                            return r
                        P.op("pe", mmS, reads=[f"QTn{qp}", f"QTr{qp}"] + [f"KTn{j}" for j in range(j0, j1)] + [f"KTr{j}" for j in range(j0, j1)],
                             writes=[f"pf{sbank}"])
                        w = (j1 - j0) * 128
                        P.op("act", lambda e, sbank=sbank, pbuf=pbuf, w=w: e.activation(out=pbuf[:, 0:w], in_=pf[sbank][:, 0:w], func=AF.Exp,
                                                                                     scale=float(192 ** -0.5)),
                             reads=[f"pf{sbank}"], writes=[f"PT{g % 2}"])
                        if j1 == i + 1:
                            dcol = (i - j0) * 128
                            P.op("dve", lambda e, pbuf=pbuf, dcol=dcol: e.tensor_tensor(out=pbuf[:, dcol:dcol + 128], in0=pbuf[:, dcol:dcol + 128],
                                                                                      in1=tri_b[:], op=ALU.mult),
                                 reads=[f"PT{g % 2}", "tri_b"], writes=[f"PT{g % 2}"])

                        def mmO(e, j0=j0, j1=j1, pbuf=pbuf, hh=hh, i=i):
                            for j in range(j0, j1):
                                pj = pbuf[:, (j - j0) * 128:(j - j0 + 1) * 128]
                                e.matmul(pf[2][:, 0:128], lhsT=Vc[:, j, hh * 128:(hh + 1) * 128], rhs=pj, start=(j == 0), stop=(j == i))
                                r = e.matmul(pf[3][:, 0:128], lhsT=ones_b[:], rhs=pj, start=(j == 0), stop=(j == i))
                            return r
                        P.op("pe", mmO, reads=[f"PT{g % 2}", "ones_b"] + [f"Vc{j}" for j in range(j0, j1)], writes=["pf2", "pf3"])
                    P.op("dve", lambda e: e.reciprocal(out=rden[:], in_=pf[3][:, 0:128]), reads=["pf3"], writes=["rden"])
                    P.op("dve", lambda e, h=h, i=i: e.tensor_tensor(out=catT[:, h, i * 128:(i + 1) * 128], in0=pf[2][:, 0:128], in1=rden[:], op=ALU.mult),
                         reads=["pf2", "rden"], writes=[f"cat{h}_{i}"])

        for G in range(ngroups):
            mla_front(G, 0)
            for i in range(nblk):
                A_ = P.capture(mla_back, G, i)
                B_ = P.capture(mla_front, G, i + 1) if i + 1 < nblk else []
                P.ops.extend(P.merge(A_, B_))
    BAR()
    with ExitStack() as s1:
        aog = sb(s1, "aog", [128, 8])
        with nc.allow_non_contiguous_dma(reason="tiny transposed loads"):
            P.dma("sp", lambda e: e.dma_start(out=aog[:], in_=attn_out_norm.rearrange("(j p) -> p j", p=128), allow_slow_non_contiguous=True), writes=["aog"])
        sqb = sb(s1, "sqb", [128, 8, 512], BF16); rnb = sb(s1, "rnb", [128, 512]); rnb2 = sb(s1, "rnb2", [128, 512])
        for tb in range((nblk + 3) // 4):
            wd = min(512, nblk * 128 - tb * 512)
            cols = slice(tb * 512, tb * 512 + wd)
            blks = range(tb * 4, min(nblk, tb * 4 + 4))
            ck = [f"cat{h}_{i}" for h in range(8) for i in blks]
            P.op("act", lambda e, cols=cols, wd=wd: e.activation(out=sqb[:, :, 0:wd], in_=catT[:, 0:8, cols], func=AF.Square), reads=ck, writes=["sqb"])

            def mmN(e, wd=wd):
                for h in range(8):
                    r = e.matmul(pf[0][:, 0:wd], lhsT=ones_b[:], rhs=sqb[:, h, 0:wd], start=(h == 0), stop=(h == 7))
                return r
            P.op("pe", mmN, reads=["sqb", "ones_b"], writes=["pf0"])
            P.op("act", lambda e, wd=wd: e.activation(out=rnb[:, 0:wd], in_=pf[0][:, 0:wd], func=AF.Sqrt, scale=1.0 / 1024, bias=EPS), reads=["pf0"], writes=["rnb"])
            P.op("dve", lambda e, wd=wd: e.reciprocal(out=rnb2[:, 0:wd], in_=rnb[:, 0:wd]), reads=["rnb"], writes=["rnb2"])
            for h in range(8):
                P.op("dve", lambda e, h=h, cols=cols, wd=wd: e.scalar_tensor_tensor(out=catT[:, h, cols], in0=catT[:, h, cols], scalar=aog[:, h:h + 1],
                                                                                  in1=rnb2[:, 0:wd], op0=ALU.mult, op1=ALU.mult),
                     reads=["rnb2", "aog"] + [f"cat{h}_{i}" for i in blks],
                     writes=[f"cat{h}_{i}" for i in blks])
    BAR()
    if stop_after == "mla":
        o1 = dbg_out("d_catA", [128, 8, S], BF16)
        P.dma("sp", lambda e: e.dma_start(out=o1[:, :, 0:nblk * 128], in_=catT[:, 0:8, 0:nblk * 128]), reads=[f"cat{h}_{i}" for h in range(8) for i in range(nblk)], writes=["o1"])
        return finish(["o1"])
    with ExitStack() as s2:
        NWX = 3
        wxb = [sb(s2, f"wxb{q}", [128, 16, 128], BF16) for q in range(NWX)]
        wzb2 = [sb(s2, f"wzb{q}", [128, 16, 512], BF16) for q in range(2)]
        win_d, win_d_k = load_w(s2, "win_d", w_in[:, 3392:3408], 16, 16)
        wcnt = {"n": 0}
        cw = sb(s2, "cw", [128, 12, 4]); cb = sb(s2, "cb", [128, 12])
        with nc.allow_non_contiguous_dma(reason="tiny transposed loads"):
            for k in range(4):
                P.dma("sp", lambda e, k=k: e.dma_start(out=cw[:, :, k], in_=conv_w[k, :].rearrange("(j p) -> p j", p=128), allow_slow_non_contiguous=True),
                      writes=[f"cw{k}"])
            P.dma("sp", lambda e: e.dma_start(out=cb[:], in_=conv_b.rearrange("(j p) -> p j", p=128), allow_slow_non_contiguous=True), writes=["cb"])
        cwk = [f"cw{k}" for k in range(4)]
        dtb = row_tile(s2, "dtb", dt_bias, 16); arow = row_tile(s2, "arow", a_log, 16)
        dsk = row_tile(s2, "dsk", d_skip, 16); sng = row_tile(s2, "sng", ssd_norm, 1024)
        P.op("act", lambda e: e.activation(out=arow[:], in_=arow[:], func=AF.Exp), reads=["arow"], writes=["arow_e"])
        P.op("dve", lambda e: e.tensor_scalar(out=arow[:], in0=arow[:], scalar1=-1.0, scalar2=None, op0=ALU.mult), reads=["arow_e"], writes=["arow_n"])
        xraw = sb(s2, "xraw", [128, 12, 131]); xacc = sb(s2, "xacc", [128, 12, 128])
        xcT = sb(s2, "xcT", [128, 12, 128], BF16)
        xs = sb(s2, "xs", [128, 16, 64]); xd = sb(s2, "xd", [128, 16, 64], BF16); xdd = sb(s2, "xdd", [128, 16, 64], BF16)
        zs = sb(s2, "zs", [128, 1024]); dts = sb(s2, "dts", [128, 16]); adt = sb(s2, "adt", [128, 16])
        ahi = sb(s2, "ahi", [128, 16], BF16); alo = sb(s2, "alo", [128, 16], BF16); ahf = sb(s2, "ahf", [128, 16])
        Rhi = sb(s2, "Rhi", [128, 16, 128], BF16); Rlo = sb(s2, "Rlo", [128, 16, 128], BF16)
        acs = sb(s2, "acs", [128, 16]); alast = sb(s2, "alast", [128, 16]); ea = sb(s2, "ea", [128, 16])
        dsd = sb(s2, "dsd", [128, 16]); cdec = sb(s2, "cdec", [128, 16])
        dif = sb(s2, "dif", [128, 4, 128]); Ed = sb(s2, "Ed", [128, 4, 128])
        MT = sb(s2, "MT", [128, 16, 128], BF16); CBT = sb(s2, "CBT", [128, 2, 128])
        Btok = sb(s2, "Btok", [128, 2, 128], BF16)
        Sst = sb(s2, "Sst", [128, 16, 64]); Sbf = sb(s2, "Sbf", [128, 16, 64], BF16)
        yv = sb(s2, "yv", [128, 16, 64]); ytmp = sb(s2, "ytmp", [128, 16, 64]); ybf = sb(s2, "ybf", [128, 1024], BF16)
        ysm = sb(s2, "ysm", [128, 8])
        P.op("dve", lambda e: e.memset(Sst[:], 0.0), writes=["Sst"])
        P.op("dve", lambda e: e.memset(Sbf[:], 0.0), writes=["Sbf"])
        xrawB = sb(s2, "xrawB", [128, 12, 131]); zsB = sb(s2, "zsB", [128, 1024]); dtsB = sb(s2, "dtsB", [128, 16])
        xraw2 = [xraw, xrawB]; zs2 = [zs, zsB]; dts2 = [dts, dtsB]
        P.op("dve", lambda e: e.memset(xraw[:, :, 0:3], 0.0), writes=["xhalo"])

        def ssd_front(i):
            p = i % 2
            xraw = xraw2[p]; xprev = xraw2[1 - p]; zs = zs2[p]; dts = dts2[p]
            make_hT(x_d, i, scp1, sh1)
            for j in range(12):
                bank = j % 2
                wq_ = wcnt["n"] % NWX
                wcnt["n"] += 1
                wx_ = wxb[wq_]
                P.dma("sp", lambda e, j=j, wx_=wx_: e.dma_start(
                    out=wx_[:], in_=win_bf[:, 1024 + j * 128:1024 + (j + 1) * 128].rearrange("(k p) n -> p k n", p=128)),
                    reads=["wintab"], writes=[f"wxb{wq_}"])

                def mmX(e, j=j, bank=bank, wx_=wx_):
                    for k in range(16):
                        r = e.matmul(pf[bank][:, 0:128], lhsT=wx_[:, k, :], rhs=hT[:, k, :], start=(k == 0), stop=(k == 15))
                    return r
                P.op("pe", mmX, reads=hT_keys + [f"wxb{wq_}"], writes=[f"pf{bank}"])
                P.op("act", lambda e, j=j, bank=bank: e.copy(out=xraw[:, j, 3:131], in_=pf[bank][:, 0:128]),
                     reads=[f"pf{bank}"], writes=[f"xraw{p}_{j}"])
                if i > 0:
                    P.op("act", lambda e, j=j: e.copy(out=xraw[:, j, 0:3], in_=xprev[:, j, 128:131]),
                         reads=[f"xraw{1 - p}_{j}", f"xraw{p}_{j}"], writes=[f"xraw{p}_{j}"])
            for half in range(2):
                wzb = wzb2[half]
                P.dma("sp", lambda e, half=half, wzb=wzb: e.dma_start(
                    out=wzb[:], in_=win_bf[:, half * 512:(half + 1) * 512].rearrange("(k p) n -> p k n", p=128)),
                    reads=["wintab"], writes=[f"wzb{half}"])

                def mmZ(e, half=half, wzb=wzb):
                    for k in range(16):
                        r = e.matmul(pf[half][:, :], lhsT=hT[:, k, :], rhs=wzb[:, k, :], start=(k == 0), stop=(k == 15))
                    return r
                P.op("pe", mmZ, reads=hT_keys + [f"wzb{half}"], writes=[f"pf{half}"])

            P.op("act", lambda e: e.activation(out=zs[:, 0:512], in_=pf[0][:, :], func=AF.Silu), reads=["pf0"], writes=[f"zs{p}_0"])
            P.op("act", lambda e: e.activation(out=zs[:, 512:1024], in_=pf[1][:, :], func=AF.Silu), reads=["pf1"], writes=[f"zs{p}_1"])

            def mmD(e):
                for k in range(16):
                    r = e.matmul(pf[0][:, 0:16], lhsT=hT[:, k, :], rhs=win_d[:, k, :], start=(k == 0), stop=(k == 15))
                return r
            P.op("pe", mmD, reads=hT_keys + win_d_k, writes=["pf0"])
            P.op("dve", lambda e: e.tensor_tensor(out=dts[:], in0=pf[0][:, 0:16], in1=dtb[:], op=ALU.add), reads=["pf0", "dtb"], writes=[f"dts{p}a"])
            P.op("act", lambda e: e.activation(out=dts[:], in_=dts[:], func=AF.Exp), reads=[f"dts{p}a"], writes=[f"dts{p}b"])
            P.op("act", lambda e: e.activation(out=dts[:], in_=dts[:], func=AF.Ln, bias=1.0, scale=1.0), reads=[f"dts{p}b"], writes=[f"dts{p}"])
        def ssd_back(i):
            p = i % 2
            xraw = xraw2[p]; zs = zs2[p]; dts = dts2[p]
            for j in range(12):
                eng = "dve"
                P.op(eng, lambda e, j=j: e.tensor_scalar(out=xacc[:, j, :], in0=xraw[:, j, 0:128], scalar1=cw[:, j, 0:1], scalar2=cb[:, j:j + 1],
                                                         op0=ALU.mult, op1=ALU.add), reads=[f"xraw{p}_{j}", "cb", "xhalo"] + cwk, writes=[f"xacc{j}"])
                for k in range(1, 4):
                    P.op(eng, lambda e, j=j, k=k: e.scalar_tensor_tensor(out=xacc[:, j, :], in0=xraw[:, j, k:k + 128], scalar=cw[:, j, k:k + 1],
                                                                         in1=xacc[:, j, :], op0=ALU.mult, op1=ALU.add),
                         reads=[f"xraw{p}_{j}", f"xacc{j}"] + cwk, writes=[f"xacc{j}"])
                P.op("act", lambda e, j=j: e.activation(out=xcT[:, j, :], in_=xacc[:, j, :], func=AF.Silu), reads=[f"xacc{j}"], writes=[f"xcT{j}"])
            P.op("dve", lambda e: e.tensor_tensor(out=adt[:], in0=dts[:], in1=arow[:], op=ALU.mult), reads=[f"dts{p}", "arow_n"], writes=["adt"])
            P.op("dve", lambda e: e.tensor_copy(out=ahi[:], in_=adt[:]), reads=["adt"], writes=["ahi"])
            P.op("dve", lambda e: e.tensor_copy(out=ahf[:], in_=ahi[:]), reads=["ahi"], writes=["ahf"])
            P.op("dve", lambda e: e.tensor_tensor(out=alo[:], in0=adt[:], in1=ahf[:], op=ALU.subtract), reads=["adt", "ahf"], writes=["alo"])
            trib = bcast(tri_b[:].unsqueeze(1), [128, 16, 128])
            P.op("dve", lambda e: e.tensor_tensor(out=Rhi[:], in0=trib, in1=bcast(ahi[:].unsqueeze(2), [128, 16, 128]), op=ALU.mult),
                 reads=["ahi", "tri_b"], writes=["Rhi"])
            P.op("pool", lambda e: e.tensor_tensor(out=Rlo[:], in0=trib, in1=bcast(alo[:].unsqueeze(2), [128, 16, 128]), op=ALU.mult),
                 reads=["alo", "tri_b"], writes=["Rlo"])
            def mmC(e):
                e.matmul(pf[4][:, 16:32], lhsT=tri_b[:], rhs=ahi[:], start=True, stop=False)
                return e.matmul(pf[4][:, 16:32], lhsT=tri_b[:], rhs=alo[:], start=False, stop=True)
            P.op("pe", mmC, reads=["tri_b", "ahi", "alo"], writes=["pf4"])
            P.op("dve", lambda e: e.tensor_copy(out=acs[:], in_=pf[4][:, 16:32]), reads=["pf4"], writes=["acs"])
            P.op("act", lambda e: e.activation(out=ea[:], in_=acs[:], func=AF.Exp), reads=["acs"], writes=["ea"])
            def trX(e):
                for c in range(8):
                    r = e.transpose(pt[0][:, c * 128:(c + 1) * 128], xcT[:, c, :], ident_b[:])
                return r
            P.op("pe", trX, reads=[f"xcT{c}" for c in range(8)] + ["ident_b"], writes=["pt0"])
            P.op("act", lambda e: e.copy(out=xs[:].rearrange("p h d -> p (h d)"), in_=pt[0][:, :]), reads=["pt0"], writes=["xs"])
            P.op("dve", lambda e: e.tensor_tensor(out=xd[:], in0=xs[:], in1=bcast(dts[:].unsqueeze(2), [128, 16, 64]), op=ALU.mult),
                 reads=["xs", f"dts{p}"], writes=["xd"])
            def trB(e):
                for g in range(2):
                    r = e.transpose(pt[1][:, g * 128:(g + 1) * 128], xcT[:, 8 + g, :], ident_b[:])
                return r
            P.op("pe", trB, reads=["xcT8", "xcT9", "ident_b"], writes=["pt1"])
            P.op("act", lambda e: e.copy(out=Btok[:].rearrange("p g n -> p (g n)"), in_=pt[1][:, 0:256]), reads=["pt1"], writes=["Btok"])
            def mmCB(e):
                for g in range(2):
                    r = e.matmul(pf[5][:, g * 128:(g + 1) * 128], lhsT=xcT[:, 8 + g, :], rhs=xcT[:, 10 + g, :], start=True, stop=True)
                return r
            P.op("pe", mmCB, reads=["xcT8", "xcT9", "xcT10", "xcT11"], writes=["pf5"])
            P.op("act", lambda e: e.copy(out=CBT[:].rearrange("p g n -> p (g n)"), in_=pf[5][:, 0:256]), reads=["pf5"], writes=["CBT"])
            for q4 in range(4):
                bank = 2 + q4 % 2

                def mmBC(e, q4=q4, bank=bank):
                    e.matmul(pf[bank][:, :], lhsT=ones_b[:], rhs=Rhi[:, q4 * 4:(q4 + 1) * 4, :].rearrange("p h l -> p (h l)"), start=True, stop=False)
                    return e.matmul(pf[bank][:, :], lhsT=ones_b[:], rhs=Rlo[:, q4 * 4:(q4 + 1) * 4, :].rearrange("p h l -> p (h l)"), start=False, stop=True)
                P.op("pe", mmBC, reads=["ones_b", "Rhi", "Rlo"], writes=[f"pf{bank}"])
                pv = pf[bank][:, :].rearrange("p (h l) -> p h l", h=4)
                P.op("dve", lambda e, pv=pv, q4=q4: e.tensor_copy(out=alast[:, q4 * 4:(q4 + 1) * 4], in_=pv[:, :, 127]), reads=[f"pf{bank}"], writes=[f"alast{q4}"])
                P.op("dve", lambda e, pv=pv, q4=q4: e.tensor_tensor(out=dif[:], in0=pv, in1=bcast(acs[:, q4 * 4:(q4 + 1) * 4].unsqueeze(2), [128, 4, 128]),
                                                                   op=ALU.subtract), reads=[f"pf{bank}", "acs"], writes=["dif"])
                P.op("dve", lambda e: e.tensor_tensor(out=dif[:], in0=dif[:], in1=bcast(negm[:].unsqueeze(1), [128, 4, 128]), op=ALU.add),
                     reads=["dif", "negm"], writes=["dif2"])
                P.op("act", lambda e: e.activation(out=Ed[:], in_=dif[:], func=AF.Exp), reads=["dif2"], writes=["Ed"])
                P.op("dve", lambda e, q4=q4: e.tensor_tensor(out=MT[:, q4 * 4:(q4 + 1) * 4, :], in0=Ed[:],
                                                           in1=bcast(CBT[:, q4 // 2, :].unsqueeze(1), [128, 4, 128]), op=ALU.mult),
                     reads=["Ed", "CBT"], writes=[f"MT{q4}"])
            alk = [f"alast{q}" for q in range(4)]
            P.op("dve", lambda e: e.tensor_tensor(out=dsd[:], in0=alast[:], in1=acs[:], op=ALU.subtract), reads=alk + ["acs"], writes=["dsd0"])
            P.op("act", lambda e: e.activation(out=dsd[:], in_=dsd[:], func=AF.Exp), reads=["dsd0"], writes=["dsd"])
            P.op("act", lambda e: e.activation(out=cdec[:], in_=alast[:], func=AF.Exp), reads=alk, writes=["cdec"])
            P.op("dve", lambda e: e.tensor_tensor(out=xdd[:], in0=xd[:], in1=bcast(dsd[:].unsqueeze(2), [128, 16, 64]), op=ALU.mult),
                 reads=["xd", "dsd"], writes=["xdd"])
            def mmY(e):
                for hd in range(16):
                    r = e.matmul(pf[2 + hd // 8][:, (hd % 8) * 64:(hd % 8 + 1) * 64], lhsT=MT[:, hd, :], rhs=xd[:, hd, :], start=True, stop=True)
                return r
            P.op("pe", mmY, reads=[f"MT{q}" for q in range(4)] + ["xd"], writes=["pf2", "pf3"])

            def mmYo(e):
                for g in range(2):
                    r = e.matmul(pf[4 + g][:, :], lhsT=xcT[:, 10 + g, :], rhs=Sbf[:, g * 8:(g + 1) * 8, :].rearrange("p h d -> p (h d)"), start=True, stop=True)
                return r
            P.op("pe", mmYo, reads=["xcT10", "xcT11", "Sbf"], writes=["pf4", "pf5"])
            for g in range(2):
                hs = slice(g * 8, (g + 1) * 8)
                P.op("dve", lambda e, g=g, hs=hs: e.tensor_tensor(out=ytmp[:, hs, :], in0=pf[4 + g][:, :].rearrange("p (h d) -> p h d", h=8),
                                                                 in1=bcast(ea[:, hs].unsqueeze(2), [128, 8, 64]), op=ALU.mult),
                     reads=[f"pf{4 + g}", "ea"], writes=[f"ytmp{g}"])
                P.op("dve", lambda e, g=g, hs=hs: e.tensor_tensor(out=yv[:, hs, :], in0=pf[2 + g][:, :].rearrange("p (h d) -> p h d", h=8),
                                                                 in1=ytmp[:, hs, :], op=ALU.add), reads=[f"pf{2 + g}", f"ytmp{g}"], writes=[f"yv{g}"])
            def mmSt(e):
                for g in range(2):
                    r = e.matmul(pf[4 + g][:, :], lhsT=Btok[:, g, :], rhs=xdd[:, g * 8:(g + 1) * 8, :].rearrange("p h d -> p (h d)"), start=True, stop=True)
                return r
            P.op("pe", mmSt, reads=["Btok", "xdd"], writes=["pf4", "pf5"])
            P.op("dve", lambda e: e.tensor_tensor(out=Sst[:], in0=Sst[:], in1=bcast(cdec[:].unsqueeze(2), [128, 16, 64]), op=ALU.mult),
                 reads=["Sst", "cdec"], writes=["Sst"])
            for g in range(2):
                hs = slice(g * 8, (g + 1) * 8)
                P.op("dve", lambda e, g=g, hs=hs: e.tensor_tensor(out=Sst[:, hs, :], in0=pf[4 + g][:, :].rearrange("p (h d) -> p h d", h=8),
                                                                 in1=Sst[:, hs, :], op=ALU.add), reads=[f"pf{4 + g}", "Sst"], writes=["Sst"])
            P.op("act", lambda e: e.copy(out=Sbf[:], in_=Sst[:]), reads=["Sst"], writes=["Sbf"])
            P.op("dve", lambda e: e.tensor_tensor(out=ytmp[:], in0=xs[:], in1=bcast(dsk[:].unsqueeze(2), [128, 16, 64]), op=ALU.mult),
                 reads=["xs", "dsk", "yv0", "yv1"], writes=["ytmp0", "ytmp1"])
            P.op("dve", lambda e: e.tensor_tensor(out=yv[:], in0=yv[:], in1=ytmp[:], op=ALU.add), reads=["yv0", "yv1", "ytmp0", "ytmp1"], writes=["yv0", "yv1"])
            yflat = yv[:].rearrange("p h d -> p (h d)")
            P.op("dve", lambda e: e.tensor_tensor(out=yflat, in0=yflat, in1=zs[:], op=ALU.mult), reads=["yv0", "yv1", f"zs{p}_0", f"zs{p}_1"], writes=["yv0", "yv1"])
            tfl = ytmp[:].rearrange("p h d -> p (h d)")
            P.op("act", lambda e: e.activation(out=tfl, in_=yflat, func=AF.Square), reads=["yv0", "yv1"], writes=["ytmp0", "ytmp1"])
            P.op("dve", lambda e: e.tensor_reduce(out=ysm[:, 0:2], in_=tfl.rearrange("p (g d) -> p g d", g=2), axis=AX.X, op=ALU.add),
                 reads=["ytmp0", "ytmp1"], writes=["ysm0"])
            rstd_of(ysm[:, 0:2], 512, ysm[:, 4:6], ["ysm0"], "ysm4", ysm[:, 2:4], "ysm2")
            P.op("dve", lambda e: e.tensor_tensor(out=yflat.rearrange("p (g d) -> p g d", g=2), in0=yflat.rearrange("p (g d) -> p g d", g=2),
                                                  in1=bcast(ysm[:, 4:6].unsqueeze(2), [128, 2, 512]), op=ALU.mult),
                 reads=["yv0", "yv1", "ysm4"], writes=["yv0", "yv1"])
            P.op("dve", lambda e: e.tensor_tensor(out=ybf[:], in0=yflat, in1=sng[:], op=ALU.mult), reads=["yv0", "yv1", "sng"], writes=["ybf"])

            def trY(e):
                for c in range(8):
                    r = e.transpose(pt[1][:, c * 128:(c + 1) * 128], ybf[:, c * 128:(c + 1) * 128], ident_b[:])
                return r
            P.op("pe", trY, reads=["ybf", "ident_b"], writes=["pt1"])
            P.op("act", lambda e, i=i: e.copy(out=catT[:, 8:16, i * 128:(i + 1) * 128], in_=pt[1][:, :].rearrange("p (c t) -> p c t", c=8)),
                 reads=["pt1"], writes=[f"cat{h}_{i}" for h in range(8, 16)])

        ssd_front(0)
        for i in range(nblk):
            A_ = P.capture(ssd_back, i)
            B_ = P.capture(ssd_front, i + 1) if i + 1 < nblk else []
            P.ops.extend(P.merge(A_, B_))
    BAR()
    if stop_after == "ssd":
        o1 = dbg_out("d_catS", [128, 8, S], BF16)
        P.dma("sp", lambda e: e.dma_start(out=o1[:, :, 0:nblk * 128], in_=catT[:, 8:16, 0:nblk * 128]), reads=[f"cat{h}_{i}" for h in range(8, 16) for i in range(nblk)], writes=["o1"])
        return finish(["o1"])
    with ExitStack() as s3:
        g1row = row_tile(s3, "g1row", mod_d[2 * D:3 * D], D)
        P.ops[-1].reads = ("mod_d",)
        wo, wo_k = load_w(s3, "wo", w_out, 16, D)
        x1t = sb(s3, "x1t", [128, D])
        for i in range(nblk):
            s = cnt["x"] % 1
            cnt["x"] += 1
            t = xt[s]
            P.dma("sp", lambda e, t=t, i=i: e.dma_start(out=t[:], in_=x_d[i * 128:(i + 1) * 128, :]), writes=[f"xt{s}"])
            for nb in range(4):
                def mmM(e, nb=nb, i=i):
                    for c in range(16):
                        r = e.matmul(pf[nb][:, :], lhsT=catT[:, c, i * 128:(i + 1) * 128], rhs=wo[:, c, nb * 512:(nb + 1) * 512], start=(c == 0), stop=(c == 15))
                    return r
                P.op("pe", mmM, reads=[f"cat{c}_{i}" for c in range(16)] + wo_k, writes=[f"pf{nb}"])
                cs_ = slice(nb * 512, (nb + 1) * 512)
                P.op("dve", lambda e, nb=nb, cs_=cs_: e.tensor_tensor(out=x1t[:, cs_], in0=pf[nb][:, :], in1=g1row[:, cs_], op=ALU.mult),
                     reads=[f"pf{nb}", "g1row"], writes=[f"x1t{nb}"])
                P.op("dve", lambda e, nb=nb, cs_=cs_, t=t: e.tensor_tensor(out=x1t[:, cs_], in0=x1t[:, cs_], in1=t[:, cs_], op=ALU.add),
                     reads=[f"x1t{nb}", f"xt{s}"], writes=[f"x1t{nb}"])
            P.dma("sp", lambda e, i=i: e.dma_start(out=x1_d[i * 128:(i + 1) * 128, :], in_=x1t[:]), reads=[f"x1t{nb}" for nb in range(4)],
                  writes=[f"x1d{i}"], semkey=("x1st",))
    BAR()
    if stop_after == "x1":
        return finish([f"x1d{i}" for i in range(nblk)])
    scat.close()
    BAR()

    emit_conv(len(conv_pending))
    with ExitStack() as s4:
        wq, wq_k = load_w(s4, "wq", w_query, 16, D)
        g2row = row_tile(s4, "g2row", mod_d[5 * D:6 * D], D); P.ops[-1].reads = ("mod_d",)
        sc2row = row_tile(s4, "sc2row", mod_d[4 * D:5 * D], D); P.ops[-1].reads = ("mod_d",)
        sh2row = row_tile(s4, "sh2row", mod_d[3 * D:4 * D], D); P.ops[-1].reads = ("mod_d",)
        P.op("dve", lambda e: e.tensor_scalar(out=sc2row[:], in0=sc2row[:], scalar1=1.0, scalar2=None, op0=ALU.add), reads=["sc2row"], writes=["sc2row"])
        skT = sb(s4, "skT", [128, 16, 128], BF16)
        qT = sb(s4, "qT", [128, 16, 128], BF16)
        h2b2 = sb(s4, "h2b2", [128, D], BF16)
        XT1 = xt[0]; H2B = [xn, h2b2]
        sc = sb(s4, "sc", [128, 16, 128]); zp = sb(s4, "zp", [128, 128])
        tops = sb(s4, "tops", [128, 16, 16]); topi = sb(s4, "topi", [128, 16, 16])
        iot256 = sb(s4, "iot256", [128, 256])
        topiu = sb(s4, "topiu", [128, 16, 16], mybir.dt.uint32); bposu = sb(s4, "bposu", [128, 16], mybir.dt.uint32)
        bposf = sb(s4, "bposf", [128, 16])
        cands1 = sb(s4, "cands1", [128, 16, 16]); candi1 = sb(s4, "candi1", [128, 16, 16]); zc = sb(s4, "zc", [128, 256])
        bests = sb(s4, "bests", [128, 8, 16]); besti = sb(s4, "besti", [128, 8, 16])
        bidx3 = [sb(s4, f"bidx{q}", [128, 128], I32) for q in range(3)]
        gsm = sb(s4, "gsm", [128, 32])
        gate2 = [sb(s4, f"gate{q}", [128, 8, 16]) for q in range(2)]
        pre2 = [sb(s4, f"pre{q}", [128, 128]) for q in range(2)]
        actv2 = [sb(s4, f"actv{q}", [128, 128]) for q in range(2)]
        scr2 = sb(s4, "scr2", [128, D], BF16)
        dg = [sb(s4, f"dg{q}", [128, 128], BF16) for q in range(2)]
        prod = [sb(s4, f"prod{q}", [128, D], BF16) for q in range(2)]
        xr = sb(s4, "xr", [128, 1024])
        NG = 8
        gb = [sb(s4, f"gb{q}", [128, D], BF16) for q in range(NG)]
        acc = sb(s4, "acc", [128, D]); h2 = acc
        SCRK = ["scr2_0", "scr2_1", "scr2b_0", "scr2b_1"]
        skf = acc[:].rearrange("p (g d) -> p g d", g=16); skb = scr2[:].rearrange("p (g d) -> p g d", g=16)
        P.dma("sp", lambda e: e.dma_start(out=skf, in_=sub_keys.rearrange("g k d -> k g d")), writes=["acc"], semkey=("skf",))
        P.op("dve", lambda e: e.tensor_copy(out=skb, in_=skf), reads=["acc"], writes=SCRK)
        for half in range(2):
            def trSK(e, half=half):
                for g in range(8):
                    r = e.transpose(pt[half][:, g * 128:(g + 1) * 128], skb[:, half * 8 + g, :], ident_b[:])
                return r
            P.op("pe", trSK, reads=SCRK + ["ident_b"], writes=[f"pt{half}"])
            P.op("dve", lambda e, half=half: e.tensor_copy(out=skT[:, half * 8:(half + 1) * 8, :], in_=pt[half][:, :].rearrange("p (g k) -> p g k", g=8)),
                 reads=[f"pt{half}"], writes=[f"skT{half}"])
        def mmI(e):
            return e.matmul(pf[5][:, 0:128], lhsT=ones_b[:], rhs=tri_b[:], start=True, stop=True)
        P.op("pe", mmI, reads=["ones_b", "tri_b"], writes=["pf5"])
        P.op("dve", lambda e: e.tensor_scalar(out=iot256[:, 0:128], in0=pf[5][:, 0:128], scalar1=-1.0, scalar2=None, op0=ALU.add), reads=["pf5"], writes=["iotA"])
        P.op("dve", lambda e: e.tensor_scalar(out=iot256[:, 128:256], in0=pf[5][:, 0:128], scalar1=127.0, scalar2=None, op0=ALU.add), reads=["pf5", "iotA"], writes=["iot256"])
        gcount = {"n": 0, "pr": 0}

        def prep_topk(i):
            p = i % 2
            t = XT1; hb = H2B[p]; bidx = bidx3[i % 3]; gate = gate2[p]
            P.dma("sp", lambda e: e.dma_start(out=t[:], in_=x1_d[i * 128:(i + 1) * 128, :]), reads=[f"x1d{i}"], writes=["XT"])
            P.op("act", lambda e: e.activation(out=junk[:], in_=t[:], func=AF.Square, accum_out=st4[:, 0:1]), reads=["XT"], writes=["junk", "st_ss"])
            rstd_of(st4[:, 0:1], D, st4[:, 2:3], ["st_ss"], "st_r", st4[:, 1:2], "st_t")
            P.op("dve", lambda e: e.scalar_tensor_tensor(out=h2[:], in0=t[:], scalar=st4[:, 2:3], in1=sc2row[:], op0=ALU.mult, op1=ALU.mult),
                 reads=["XT", "st_r", "sc2row"], writes=["acc"])
            P.op("dve", lambda e: e.tensor_tensor(out=hb[:], in0=h2[:], in1=sh2row[:], op=ALU.add), reads=["acc", "sh2row"], writes=[f"H2B{p}"])
            for half in range(2):
                def trs(e, half=half):
                    for k in range(8):
                        kk = half * 8 + k
                        r = e.transpose(pt[half][:, k * 128:(k + 1) * 128], hb[:, kk * 128:(kk + 1) * 128], ident_b[:])
                    return r
                P.op("pe", trs, reads=[f"H2B{p}", "ident_b"], writes=[f"pt{half}"])
                P.op("act", lambda e, half=half: e.copy(out=hT[:, half * 8:(half + 1) * 8, :], in_=pt[half][:, :].rearrange("p (c t) -> p c t", c=8)),
                     reads=[f"pt{half}"], writes=[f"hT{kk}" for kk in range(half * 8, half * 8 + 8)])
            for c4 in range(4):
                def mmQ2(e, c4=c4):
                    for cc in range(4):
                        ch = c4 * 4 + cc
                        for k in range(16):
                            r = e.matmul(pf[4][:, cc * 128:(cc + 1) * 128], lhsT=wq[:, k, ch * 128:(ch + 1) * 128], rhs=hT[:, k, :], start=(k == 0), stop=(k == 15))
                    return r
                P.op("pe", mmQ2, reads=hT_keys + wq_k, writes=["pf4"])
                P.op("act", lambda e, c4=c4: e.copy(out=qT[:, c4 * 4:(c4 + 1) * 4, :], in_=pf[4][:, :].rearrange("p (c t) -> p c t", c=4)),
                     reads=["pf4"], writes=[f"qT{c4}"])

                def mmSc(e, c4=c4):
                    for cc in range(4):
                        ch = c4 * 4 + cc
                        r = e.matmul(pf[5][:, cc * 128:(cc + 1) * 128], lhsT=qT[:, ch, :], rhs=skT[:, ch, :], start=True, stop=True)
                    return r
                P.op("pe", mmSc, reads=[f"qT{c4}", "skT0", "skT1"], writes=["pf5"])
                P.op("act", lambda e, c4=c4: e.copy(out=sc[:, c4 * 4:(c4 + 1) * 4, :], in_=pf[5][:, :].rearrange("p (c t) -> p c t", c=4)),
                     reads=["pf5"], writes=[f"sc{c4}"])
            for g in range(16):
                sk_ = f"sc{g // 4}"
                P.op("dve", lambda e, g=g: e.max(out=tops[:, g, 0:8], in_=sc[:, g, :]), reads=[sk_], writes=["tA"])
                P.op("dve", lambda e, g=g: e.max_index(out=topiu[:, g, 0:8], in_max=tops[:, g, 0:8], in_values=sc[:, g, :]), reads=[sk_, "tA"], writes=["tiA"])
                P.op("dve", lambda e, g=g: e.match_replace(out=zp[:], in_to_replace=tops[:, g, 0:8], in_values=sc[:, g, :], imm_value=-1e30),
                     reads=[sk_, "tA"], writes=["zp"])
                P.op("dve", lambda e, g=g: e.max(out=tops[:, g, 8:16], in_=zp[:]), reads=["zp"], writes=["tB"])
                P.op("dve", lambda e, g=g: e.max_index(out=topiu[:, g, 8:16], in_max=tops[:, g, 8:16], in_values=zp[:]), reads=["zp", "tB"], writes=["tiB"])
            P.op("dve", lambda e: e.tensor_copy(out=topi[:], in_=topiu[:]), reads=["tiA", "tiB"], writes=["topi"])
            t4 = tops[:].rearrange("p (h j) k -> p h j k", j=2)
            i4 = topi[:].rearrange("p (h j) k -> p h j k", j=2)
            cf = cands1[:].rearrange("p a b -> p (a b)")
            cif = candi1[:].rearrange("p a b -> p (a b)")
            for h in range(8):
                P.op("dve", lambda e, h=h: e.tensor_tensor(out=cands1[:], in0=bcast(t4[:, h, 0, :].unsqueeze(2), [128, 16, 16]),
                                                           in1=bcast(t4[:, h, 1, :].unsqueeze(1), [128, 16, 16]), op=ALU.add),
                     reads=["tA", "tB"], writes=["cands"])
                P.op("dve", lambda e, h=h: e.scalar_tensor_tensor(out=candi1[:], in0=bcast(i4[:, h, 0, :].unsqueeze(2), [128, 16, 16]), scalar=128.0,
                                                                  in1=bcast(i4[:, h, 1, :].unsqueeze(1), [128, 16, 16]), op0=ALU.mult, op1=ALU.add),
                     reads=["topi"], writes=["candi"])
                P.op("dve", lambda e, h=h: e.max(out=bests[:, h, 0:8], in_=cf), reads=["cands"], writes=["bA"])
                P.op("dve", lambda e, h=h: e.max_index(out=bposu[:, 0:8], in_max=bests[:, h, 0:8], in_values=cf), reads=["cands", "bA"], writes=["bpA"])
                P.op("dve", lambda e, h=h: e.match_replace(out=zc[:], in_to_replace=bests[:, h, 0:8], in_values=cf, imm_value=-1e30),
                     reads=["cands", "bA"], writes=["zc"])
                P.op("dve", lambda e, h=h: e.max(out=bests[:, h, 8:16], in_=zc[:]), reads=["zc"], writes=["bB"])
                P.op("dve", lambda e, h=h: e.max_index(out=bposu[:, 8:16], in_max=bests[:, h, 8:16], in_values=zc[:]), reads=["zc", "bB"], writes=["bpB"])
                P.op("dve", lambda e: e.tensor_copy(out=bposf[:], in_=bposu[:]), reads=["bpA", "bpB"], writes=["bposf"])
                for k0 in (0, 8):
                    def idx2(e, h=h, k0=k0):
                        for k in range(k0, k0 + 8):
                            r = e.scalar_tensor_tensor(out=scr2[:, (k % 8) * 256:(k % 8 + 1) * 256], in0=iot256[:], scalar=bposf[:, k:k + 1], in1=cif,
                                                       op0=ALU.is_equal, op1=ALU.mult, accum_out=besti[:, h, k:k + 1])
                        return r
                    P.op("dve", idx2, reads=["candi", "bposf", "iot256"], writes=SCRK + [f"besti{h}"])
            bik = [f"besti{h}" for h in range(8)]
            P.op("dve", lambda e: e.tensor_copy(out=bidx[:], in_=besti[:].rearrange("p h k -> p (h k)")), reads=bik, writes=[f"bidx{i % 3}"])
            P.op("dve", lambda e: e.tensor_tensor(out=gate[:], in0=bests[:], in1=bcast(bests[:, :, 0:1], [128, 8, 16]), op=ALU.subtract),
                 reads=["bA", "bB"], writes=[f"gate{p}a"])
            P.op("act", lambda e: e.activation(out=gate[:], in_=gate[:], func=AF.Exp), reads=[f"gate{p}a"], writes=[f"gate{p}b"])
            P.op("dve", lambda e: e.tensor_reduce(out=gsm[:, 0:8], in_=gate[:], axis=AX.X, op=ALU.add), reads=[f"gate{p}b"], writes=["gsm0"])
            P.op("dve", lambda e: e.reciprocal(out=gsm[:, 8:16], in_=gsm[:, 0:8]), reads=["gsm0"], writes=["gsm8"])
            P.op("dve", lambda e: e.tensor_tensor(out=gate[:], in0=gate[:], in1=bcast(gsm[:, 8:16].unsqueeze(2), [128, 8, 16]), op=ALU.mult),
                 reads=[f"gate{p}b", "gsm8"], writes=[f"gate{p}"])

        def u_step(i, hk):
            p = i % 2
            hb = H2B[p]; bidx = bidx3[i % 3]; pre = pre2[p]
            b_ = gcount["n"] % NG
            gcount["n"] += 1
            q_ = gcount["pr"] % 2
            gcount["pr"] += 1
            P.dma("pool", lambda e: e.indirect_dma_start(
                out=gb[b_][:], out_offset=None, in_=u_bf, in_offset=bass.IndirectOffsetOnAxis(ap=bidx[:, hk:hk + 1], axis=0)),
                reads=[f"bidx{i % 3}", "utab"], writes=[f"gb{b_}"])
            P.op("dve", lambda e: e.tensor_tensor(out=prod[q_][:], in0=gb[b_][:], in1=hb[:], op=ALU.mult),
                 reads=[f"gb{b_}", f"H2B{p}"], writes=[f"prod{q_}"])
            P.op("act", lambda e: e.activation(out=prod[q_][:], in_=prod[q_][:], func=AF.Copy, accum_out=pre[:, hk:hk + 1]),
                 reads=[f"prod{q_}"], writes=[f"prod{q_}", f"pre{p}"])

        def u_end(i):
            p = i % 2
            pre = pre2[p]; actv = actv2[p]; gate = gate2[p]
            P.op("act", lambda e: e.activation(out=actv[:], in_=pre[:], func=AF.Gelu), reads=[f"pre{p}"], writes=[f"actv{p}a"])
            P.op("dve", lambda e: e.tensor_tensor(out=actv[:], in0=actv[:], in1=gate[:].rearrange("p h k -> p (h k)"), op=ALU.mult),
                 reads=[f"actv{p}a", f"gate{p}"], writes=[f"actv{p}"])

        def v_step(i, hk):
            p = i % 2
            bidx = bidx3[i % 3]; actv = actv2[p]
            b_ = gcount["n"] % NG
            gcount["n"] += 1
            dq = hk % 2
            P.dma("pool", lambda e: e.indirect_dma_start(
                out=gb[b_][:], out_offset=None, in_=v_bf, in_offset=bass.IndirectOffsetOnAxis(ap=bidx[:, hk:hk + 1], axis=0)),
                reads=[f"bidx{i % 3}", "vtab"], writes=[f"gb{b_}"])
            P.op("act", lambda e: e.activation(out=dg[dq][:], in_=ident_b[:], func=AF.Copy, scale=actv[:, hk:hk + 1]),
                 reads=[f"actv{p}", "ident_b"], writes=[f"dg{dq}"])

            def mmV(e):
                for nb in range(4):
                    r = e.matmul(pf[nb][:, :], lhsT=dg[dq][:], rhs=gb[b_][:, nb * 512:(nb + 1) * 512], start=(hk == 0), stop=(hk == 127))
                return r
            P.op("pe", mmV, reads=[f"dg{dq}", f"gb{b_}"], writes=["pf0", "pf1", "pf2", "pf3"])

        def final(i):
            for nb in range(4):
                cs_ = slice(nb * 512, (nb + 1) * 512)
                P.op("dve", lambda e, nb=nb, cs_=cs_: e.tensor_tensor(out=acc[:, cs_], in0=pf[nb][:, :], in1=g2row[:, cs_], op=ALU.mult),
                     reads=[f"pf{nb}", "g2row"], writes=["acc"])
            for hf in range(2):
                P.dma("sp", lambda e, hf=hf: e.dma_start(out=xr[:], in_=x1_d[i * 128:(i + 1) * 128, hf * 1024:(hf + 1) * 1024]), reads=[f"x1d{i}"], writes=["xr"])
                P.op("dve", lambda e, hf=hf: e.tensor_tensor(out=acc[:, hf * 1024:(hf + 1) * 1024], in0=acc[:, hf * 1024:(hf + 1) * 1024], in1=xr[:], op=ALU.add),
                     reads=["acc", "xr"], writes=["acc"])
            P.dma("sp", lambda e: e.dma_start(out=out_d[i * 128:(i + 1) * 128, :], in_=acc[:]), reads=["acc"], writes=[f"out{i}", "acc"],
                  semkey=("outst",))

        def capture(fn, *a):
            saved = P.ops
            P.ops = []
            fn(*a)
            got = P.ops
            P.ops = saved
            return got

        if stop_after in ("pdbg", "pdbg2"):
            return finish([])
        prep_topk(0)
        for i in range(nblk + 1):
            nxt = capture(prep_topk, i + 1) if i + 1 < nblk else []
            per = (len(nxt) + 127) // 128
            for hk in range(128):
                if i < nblk:
                    u_step(i, hk)
                if i >= 1:
                    v_step(i - 1, hk)
                P.ops.extend(nxt[hk * per:(hk + 1) * per])
            if i < nblk:
                u_end(i)
            if i >= 1:
                final(i - 1)
        return finish([f"out{i}" for i in range(nblk)])


_CACHE = {}


def _consts():
    ident = np.eye(128, dtype=np.float32)
    k = np.arange(128)
    tri = (k[None, :] >= k[:, None]).astype(np.float32)
    negmask = np.where(k[None, :] >= k[:, None], 0.0, -30000.0).astype(np.float32)
    invf = (1.0 / (10000.0 ** (np.arange(32, dtype=np.float32) * (2.0 / 64)))).astype(np.float32)
    invf = np.broadcast_to(invf[None, :], (128, 32)).copy()
    return ident, tri, negmask, invf


def kernel(**inputs):
    if "nc" not in _CACHE:
        _CACHE["nc"] = build()
    nc = _CACHE["nc"]
    ident, tri, negmask, invf = _consts()
    f = lambda a: np.ascontiguousarray(np.asarray(a))
    shared = {
        "w_ada": f(inputs["w_ada"][0]), "b_ada": f(inputs["b_ada"][0]).reshape(1, -1), "w_in": f(inputs["w_in"][0]),
        "q_a_norm": f(inputs["q_a_norm"][0]), "w_uq": f(inputs["w_uq"][0]), "kv_a_norm": f(inputs["kv_a_norm"][0]),
        "w_ukv": f(inputs["w_ukv"][0]), "q_norm": f(inputs["q_norm"][0]), "k_norm": f(inputs["k_norm"][0]),
        "attn_out_norm": f(inputs["attn_out_norm"][0]), "conv_w": f(inputs["conv_w"][0]), "conv_b": f(inputs["conv_b"][0]),
        "dt_bias": f(inputs["dt_bias"][0]), "a_log": f(inputs["a_log"][0]), "d_skip": f(inputs["d_skip"][0]),
        "ssd_norm": f(inputs["ssd_norm"][0]), "w_out": f(inputs["w_out"][0]), "w_query": f(inputs["w_query"][0]),
        "sub_keys": f(inputs["sub_keys"][0]).reshape(16, 128, 128), "u_experts": f(inputs["u_experts"][0]),
        "v_experts": f(inputs["v_experts"][0]), "ident": ident, "tri": tri, "negmask": negmask, "invfreq": invf,
    }
    x = np.asarray(inputs["x"]); c = np.asarray(inputs["c"]); pos = np.asarray(inputs["positions"])
    in_maps = []
    for b in range(8):
        m = dict(shared)
        m["x"] = f(x[b]); m["c"] = f(c[b]); m["pos"] = f(pos[b]).reshape(S, 1).astype(np.int32)
        in_maps.append(m)
    res = run_bass_kernel_spmd(nc, in_maps, core_ids=list(range(8)))
    return np.stack([np.asarray(r["out"]) for r in res.results], axis=0).astype(np.float32)
```

```python
from contextlib import ExitStack
import os
import numpy as np
import concourse.bass as bass
import concourse.mybir as mybir
from concourse.bass_utils import run_bass_kernel_spmd

F32 = mybir.dt.float32
BF16 = mybir.dt.bfloat16
I32 = mybir.dt.int32
AF = mybir.ActivationFunctionType
ALU = mybir.AluOpType
AX = mybir.AxisListType

D = 2048
S = 2048
NB = 16
EPS = 1e-6
ENGS = ("pe", "act", "dve", "pool", "sp")
SEM_EPOCH = 20000


class Op:
    __slots__ = ("eng", "fn", "reads", "writes", "dma", "semkey", "deps", "waits", "signal", "idx", "barrier")

    def __init__(self, eng, fn, reads, writes, dma, semkey):
        self.eng, self.fn = eng, fn
        self.reads, self.writes = tuple(reads), tuple(writes)
        self.dma, self.semkey = dma, semkey
        self.deps, self.waits, self.signal = (), [], None
        self.barrier = False


class Prog:
    def __init__(self, nc):
        self.nc = nc
        self.ops = []
        self.self_sync = ("dve", "act", "pool")

    def op(self, eng, fn, reads=(), writes=()):
        xr = [k for k in reads if isinstance(k, str) and k[:2] in ("pf", "pt") and k not in writes]
        writes = tuple(writes) + tuple(xr)
        o = Op(eng, fn, reads, writes, False, None)
        self.ops.append(o)
        return o

    def dma(self, eng, fn, reads=(), writes=(), semkey=None):
        if semkey is None:
            semkey = ("dma",) + (tuple(writes) if writes else tuple(reads))
        o = Op(eng, fn, reads, writes, True, semkey)
        self.ops.append(o)
        return o

    def capture(self, fn, *a):
        saved = self.ops
        self.ops = []
        fn(*a)
        got = self.ops
        self.ops = saved
        return got

    @staticmethod
    def merge(a, b):
        out, ia, ib = [], 0, 0
        na, nb = len(a), len(b)
        while ia < na or ib < nb:
            if ib >= nb or (ia < na and ia * nb <= ib * na):
                out.append(a[ia]); ia += 1
            else:
                out.append(b[ib]); ib += 1
        return out

    def barrier(self, fn):
        o = Op("act", fn, (), (), False, None)
        o.barrier = True
        self.ops.append(o)
        return o

    def _analyze(self):
        last_write, readers = {}, {}
        last_on = {}
        dmas_since = []
        last_barrier = None
        for i, o in enumerate(self.ops):
            o.idx = i
            deps = set()
            if o.barrier:
                deps.update(last_on.values())
                deps.update(dmas_since)
                dmas_since = []
                if last_barrier is not None:
                    deps.add(last_barrier)
                last_barrier = i
            elif last_barrier is not None:
                deps.add(last_barrier)
            last_on[o.eng] = i
            if o.dma:
                dmas_since.append(i)
            for t in o.reads:
                if t in last_write:
                    deps.add(last_write[t])
            for t in o.writes:
                if t in last_write:
                    deps.add(last_write[t])
                deps.update(readers.get(t, ()))
            deps.discard(i)
            o.deps = deps
            for t in o.reads:
                readers.setdefault(t, []).append(i)
            for t in o.writes:
                last_write[t] = i
                readers[t] = []
        need = set()
        for o in self.ops:
            for d in o.deps:
                od = self.ops[d]
                if od.dma or od.eng != o.eng or o.eng in self.self_sync:
                    need.add(d)
        cnt = {e: 0 for e in ENGS}
        dcnt = {}
        self.semkeys = []
        for o in self.ops:
            if o.dma:
                k = dcnt.get(o.semkey, 0) + 1
                dcnt[o.semkey] = k
                if k == 1:
                    self.semkeys.append(o.semkey)
                o.signal = (("d", o.semkey), 16 * k, 16)
            elif o.idx in need:
                cnt[o.eng] += 1
                ep, v = divmod(cnt[o.eng] - 1, SEM_EPOCH)
                o.signal = (("e", o.eng, ep), v + 1, 1)
        self.nepochs = {e: (cnt[e] + SEM_EPOCH - 1) // SEM_EPOCH for e in ENGS}
        for o in self.ops:
            w = {}
            for d in o.deps:
                od = self.ops[d]
                if od.dma or od.eng != o.eng or o.eng in self.self_sync:
                    s, v, _ = od.signal
                    if w.get(s, 0) < v:
                        w[s] = v
            o.waits = sorted(w.items(), key=lambda kv: str(kv[0]))

    def emit(self):
        nc = self.nc
        self._analyze()
        with ExitStack() as es:
            sems = {}
            n = 0
            for e in ENGS:
                for ep in range(self.nepochs[e]):
                    sems[("e", e, ep)] = es.enter_context(nc.semaphore(f"s_{e}{ep}"))
                    n += 1
            for k in self.semkeys:
                sems[("d", k)] = es.enter_context(nc.semaphore(f"d{n}"))
                n += 1
            self.nsems = n
            by_eng = {e: [o for o in self.ops if o.eng == e] for e in ENGS}

            def run(engine, name):
                waited = {}
                for o in by_eng[name]:
                    for s, v in o.waits:
                        if waited.get(s, 0) < v:
                            engine.wait_ge(sems[s], v)
                            waited[s] = v
                    ins = o.fn(engine)
                    if o.signal is not None:
                        s, v, inc = o.signal
                        ins.then_inc(sems[s], inc)

            with nc.Block() as block:
                @block.tensor
                def _(e):
                    run(e, "pe")

                @block.scalar
                def _(e):
                    run(e, "act")

                @block.vector
                def _(e):
                    run(e, "dve")

                @block.gpsimd
                def _(e):
                    run(e, "pool")

                @block.sync
                def _(e):
                    run(e, "sp")


def bcast(ap, shape):
    return ap.to_broadcast(list(shape))


def build(stop_after=None, debug=False, nblk=NB, ngroups=2):
    nc = bass.Bass("TRN2", target_bir_lowering=False)
    P = Prog(nc)

    def din(name, shape, dt=F32):
        return nc.dram_tensor(name, list(shape), dt, kind="ExternalInput").ap()

    x_d = din("x", [S, D]); c_d = din("c", [D]); pos_d = din("pos", [S, 1], I32)
    w_ada = din("w_ada", [D, 6 * D]); b_ada = din("b_ada", [1, 6 * D]); w_in = din("w_in", [D, 3408])
    q_a_norm = din("q_a_norm", [512]); w_uq = din("w_uq", [512, 1536]); kv_a_norm = din("kv_a_norm", [256])
    w_ukv = din("w_ukv", [256, 2048]); q_norm = din("q_norm", [192]); k_norm = din("k_norm", [192])
    attn_out_norm = din("attn_out_norm", [1024]); conv_w = din("conv_w", [4, 1536]); conv_b = din("conv_b", [1536])
    dt_bias = din("dt_bias", [16]); a_log = din("a_log", [16]); d_skip = din("d_skip", [16])
    ssd_norm = din("ssd_norm", [1024]); w_out = din("w_out", [D, D]); w_query = din("w_query", [D, D])
    sub_keys = din("sub_keys", [16, 128, 128])
    if stop_after in (None, "pdbg", "pdbg2"):
        u_exp = din("u_experts", [16384, D]); v_exp = din("v_experts", [16384, D])
    ident_d = din("ident", [128, 128]); tri_d = din("tri", [128, 128]); negm_d = din("negmask", [128, 128])
    invf_d = din("invfreq", [128, 32])
    out_d = nc.dram_tensor("out", [S, D], F32, kind="ExternalOutput").ap()
    mod_d = nc.dram_tensor("mod_scr", [6 * D], F32, kind="Internal").ap()
    x1_d = nc.dram_tensor("x1_scr", [S, D], F32, kind=("ExternalOutput" if stop_after == "x1" else "Internal")).ap()
    dbg = {}
    conv_pending = []
    if stop_after in (None, "pdbg", "pdbg2"):
        u_bf = nc.dram_tensor("u_bf", [16384, D], BF16, kind="Internal").ap()
        v_bf = nc.dram_tensor("v_bf", [16384, D], BF16, kind="Internal").ap()
        for r0 in range(0, 16384, 1024):
            conv_pending.append((u_bf, u_exp, r0, "utab"))
            conv_pending.append((v_bf, v_exp, r0, "vtab"))

    win_bf = nc.dram_tensor("win_bf", [D, 2560], BF16, kind="Internal").ap()
    for r0 in range(0, D, 512):
        conv_pending.insert(r0 // 512, (win_bf, w_in[:, 832:3392], r0, "wintab"))

    def emit_conv(n):
        for _ in range(min(n, len(conv_pending))):
            dst, src, r0, key = conv_pending.pop(0)
            nr = 512 if key == "wintab" else 1024
            P.dma("pool", lambda e, dst=dst, src=src, r0=r0, nr=nr: e.dma_start(out=dst[r0:r0 + nr, :], in_=src[r0:r0 + nr, :]),
                  writes=[key], semkey=("conv", key))

    def dbg_out(nm, shp, dt=F32):
        dbg[nm] = nc.dram_tensor(nm, list(shp), dt, kind="ExternalOutput").ap()
        return dbg[nm]

    def finish(keys):
        P.op("sp", lambda e: None, reads=keys)
        P.emit()
        return nc

    top = ExitStack()

    def sb(es, name, shape, dt=F32):
        return es.enter_context(nc.sbuf_tensor(name, list(shape), dt))

    pf = [top.enter_context(nc.psum_tensor(f"pf{i}", [128, 512], F32)) for i in range(6)]
    pt = [top.enter_context(nc.psum_tensor(f"pt{i}", [128, 1024], BF16)) for i in range(2)]

    ident_f = sb(top, "ident_f", [128, 128]); ident_b = sb(top, "ident_b", [128, 128], BF16)
    tri_f = sb(top, "tri_f", [128, 128]); tri_b = sb(top, "tri_b", [128, 128], BF16)
    ones_b = sb(top, "ones_b", [128, 128], BF16); negm = sb(top, "negm", [128, 128])
    invf = sb(top, "invf", [128, 32]); bar = sb(top, "bar", [128, 2])
    BAR = lambda: P.barrier(lambda e: e.copy(out=bar[:, 0:1], in_=bar[:, 1:2]))
    modT = sb(top, "modT", [128, 96]); scp1 = sb(top, "scp1", [128, 16]); scp2 = sb(top, "scp2", [128, 16])

    P.dma("sp", lambda e: e.dma_start(out=ident_f[:], in_=ident_d), writes=["ident_f"])
    P.dma("sp", lambda e: e.dma_start(out=tri_f[:], in_=tri_d), writes=["tri_f"])
    P.dma("sp", lambda e: e.dma_start(out=negm[:], in_=negm_d), writes=["negm"])
    P.dma("sp", lambda e: e.dma_start(out=invf[:], in_=invf_d), writes=["invf"])
    P.op("dve", lambda e: e.tensor_copy(out=ident_b[:], in_=ident_f[:]), reads=["ident_f"], writes=["ident_b"])
    P.op("dve", lambda e: e.tensor_copy(out=tri_b[:], in_=tri_f[:]), reads=["tri_f"], writes=["tri_b"])
    P.op("dve", lambda e: e.memset(ones_b[:], 1.0), writes=["ones_b"])
    P.op("act", lambda e: e.activation(out=bar[:], in_=invf[:, 0:2], func=AF.Copy), reads=["invf"], writes=["bar"])

    with ExitStack() as sa:
        cT = sb(sa, "cT", [128, 16]); cab = sb(sa, "cab", [128, 16], BF16)
        brow = sb(sa, "brow", [1, 6 * D]); modrow = sb(sa, "modrow", [1, 6 * D])
        wa = [sb(sa, f"wa{i}", [128, 16, 512], BF16) for i in range(2)]
        waf = [sb(sa, f"waf{i}", [128, 16, 512], F32) for i in range(2)]
        with nc.allow_non_contiguous_dma(reason="tiny transposed loads"):
            P.dma("sp", lambda e: e.dma_start(out=cT[:], in_=c_d.rearrange("(k p) -> p k", p=128), allow_slow_non_contiguous=True), writes=["cT"])
        P.dma("sp", lambda e: e.dma_start(out=brow[:], in_=b_ada), writes=["brow"])
        P.op("act", lambda e: e.activation(out=cab[:], in_=cT[:], func=AF.Silu), reads=["cT"], writes=["cab"])
        for j in range(24):
            w = wa[j % 2]
            wf = waf[j % 2]
            P.dma("sp", lambda e, wf=wf, j=j: e.dma_start(
                out=wf[:], in_=w_ada[:, j * 512:(j + 1) * 512].rearrange("(k p) n -> p k n", p=128)),
                writes=[f"waf{j % 2}"])
            P.op("dve", lambda e, w=w, wf=wf: e.tensor_copy(out=w[:, 0:8, :], in_=wf[:, 0:8, :]), reads=[f"waf{j % 2}"], writes=[f"wa{j % 2}a"])
            P.op("act", lambda e, w=w, wf=wf: e.copy(out=w[:, 8:16, :], in_=wf[:, 8:16, :]), reads=[f"waf{j % 2}"], writes=[f"wa{j % 2}b"])

            def mmA(e, w=w):
                for k in range(16):
                    r = e.matmul(pf[0][0:1, :], lhsT=cab[:, k:k + 1], rhs=w[:, k, :], start=(k == 0), stop=(k == 15))
                return r
            P.op("pe", mmA, reads=["cab", f"wa{j % 2}a", f"wa{j % 2}b"], writes=["pf0"])
            P.op("dve", lambda e, j=j: e.tensor_tensor(out=modrow[0:1, j * 512:(j + 1) * 512], in0=pf[0][0:1, :],
                                                       in1=brow[0:1, j * 512:(j + 1) * 512], op=ALU.add),
                 reads=["pf0", "brow"], writes=["modrow"])
        P.dma("sp", lambda e: e.dma_start(out=mod_d.rearrange("(o n) -> o n", o=1), in_=modrow[:]),
              reads=["modrow"], writes=["mod_d"])
        with nc.allow_non_contiguous_dma(reason="tiny transposed loads"):
            P.dma("sp", lambda e: e.dma_start(out=modT[:], in_=mod_d.rearrange("(j p) -> p j", p=128), allow_slow_non_contiguous=True),
                  reads=["mod_d"], writes=["modT"])
        P.op("dve", lambda e: e.tensor_scalar(out=scp1[:], in0=modT[:, 16:32], scalar1=1.0, scalar2=None, op0=ALU.add),
             reads=["modT"], writes=["scp1"])
        P.op("dve", lambda e: e.tensor_scalar(out=scp2[:], in0=modT[:, 64:80], scalar1=1.0, scalar2=None, op0=ALU.add),
             reads=["modT"], writes=["scp2"])
        if stop_after == "A":
            o1 = dbg_out("d_mod", [1, 6 * D]); o2 = dbg_out("d_modT", [128, 96])
            P.dma("sp", lambda e: e.dma_start(out=o1, in_=modrow[:]), reads=["modrow"], writes=["o1"])
            P.dma("sp", lambda e: e.dma_start(out=o2, in_=modT[:]), reads=["modT"], writes=["o2"])
            return finish(["o1", "o2"])
    BAR()
    emit_conv(4)
    sh1 = modT[:, 0:16]
    sh2 = modT[:, 48:64]

    xt = [sb(top, f"xt{i}", [128, D]) for i in range(1)]
    junk = sb(top, "junk", [128, D], BF16)
    xn = sb(top, "xn", [128, D], BF16)
    st4 = sb(top, "st4", [128, 4])
    hT = sb(top, "hT", [128, 16, 128], BF16)
    cnt = {"x": 0}
    scat = ExitStack()
    catT = sb(scat, "catT", [128, 16, S], BF16)

    def rstd_of(ss_ap, n, out_ap, rkeys, wkey, tmp_ap, tmpkey):
        P.op("act", lambda e: e.activation(out=tmp_ap, in_=ss_ap, func=AF.Sqrt, scale=1.0 / n, bias=EPS),
             reads=rkeys, writes=[tmpkey])
        P.op("dve", lambda e: e.reciprocal(out=out_ap, in_=tmp_ap), reads=[tmpkey], writes=[wkey])

    def make_hT(src_d, i, scp, sh, keep_tokmajor=None):
        s = cnt["x"] % 1
        cnt["x"] += 1
        t = xt[s]
        P.dma("sp", lambda e: e.dma_start(out=t[:], in_=src_d[i * 128:(i + 1) * 128, :]), writes=[f"xt{s}"])
        P.op("act", lambda e: e.activation(out=junk[:], in_=t[:], func=AF.Square, accum_out=st4[:, 0:1]),
             reads=[f"xt{s}"], writes=["junk", "st_ss"])
        rstd_of(st4[:, 0:1], D, st4[:, 2:3], ["st_ss"], "st_r", st4[:, 1:2], "st_t")
        P.op("dve", lambda e: e.tensor_scalar(out=xn[:], in0=t[:], scalar1=st4[:, 2:3], scalar2=None, op0=ALU.mult),
             reads=[f"xt{s}", "st_r"], writes=["xn"])
        HTM = os.environ.get("HT_MODE", "")
        if HTM == "xn":
            o_ = dbg_out("d_xn", [128, D], BF16)
            P.dma("sp", lambda e, o_=o_: e.dma_start(out=o_, in_=xn[:]), reads=["xn"], writes=["d_xn"])
            raise StopIteration
        for half in range(2):
            def trs(e, half=half):
                for k in range(8):
                    kk = half * 8 + k
                    r = e.transpose(pt[half][:, k * 128:(k + 1) * 128], xn[:, kk * 128:(kk + 1) * 128], ident_b[:])
                return r
            P.op("pe", trs, reads=["xn", "ident_b"], writes=[f"pt{half}"])
            for k in range(8):
                kk = half * 8 + k
                if (k % 2 == 0 and HTM != "act") or HTM == "dve":
                    P.op("dve", lambda e, kk=kk, k=k, half=half: e.tensor_scalar(
                        out=hT[:, kk, :], in0=pt[half][:, k * 128:(k + 1) * 128], scalar1=scp[:, kk:kk + 1],
                        scalar2=sh[:, kk:kk + 1], op0=ALU.mult, op1=ALU.add),
                        reads=[f"pt{half}", "scp1", "scp2", "modT"], writes=[f"hT{kk}"])
                else:
                    P.op("act", lambda e, kk=kk, k=k, half=half: e.activation(
                        out=hT[:, kk, :], in_=pt[half][:, k * 128:(k + 1) * 128], func=AF.Identity,
                        scale=scp[:, kk:kk + 1], bias=sh[:, kk:kk + 1]),
                        reads=[f"pt{half}", "scp1", "scp2", "modT"], writes=[f"hT{kk}"])
        return s

    hT_keys = [f"hT{k}" for k in range(16)]

    def load_w(es, name, src_ap, kchunks, ncols, eng="pool"):
        t = sb(es, name, [128, kchunks, ncols], BF16)
        step = max(1, 4096 // ncols)
        for k0 in range(0, kchunks, step):
            k1 = min(kchunks, k0 + step)
            P.dma(eng, lambda e, k0=k0, k1=k1: e.dma_start(
                out=t[:, k0:k1, :], in_=src_ap[k0 * 128:k1 * 128, :].rearrange("(k p) n -> p k n", p=128)),
                writes=[f"{name}_{k0}"], semkey=("w", name, k0))
        return t, [f"{name}_{k0}" for k0 in range(0, kchunks, step)]

    def row_tile(es, name, src_1d, n):
        t = sb(es, name, [128, n])
        P.dma("sp", lambda e: e.dma_start(out=t[:], in_=src_1d.partition_broadcast(128)), writes=[name])
        return t

    with ExitStack() as s1:
        win_a, win_a_k = load_w(s1, "win_a", w_in[:, 0:832], 16, 832)
        wuq, wuq_k = load_w(s1, "wuq", w_uq, 4, 1536)
        wukv, wukv_k = load_w(s1, "wukv", w_ukv, 2, 2048)
        qag = row_tile(s1, "qag", q_a_norm, 512); kvag = row_tile(s1, "kvag", kv_a_norm, 256)
        qg = row_tile(s1, "qg", q_norm, 192); kg = row_tile(s1, "kg", k_norm, 192)
        KTn = sb(s1, "KTn", [128, 4, S], BF16); KTr = sb(s1, "KTr", [128, 2, S], BF16)
        Vc = sb(s1, "Vc", [128, NB, 512], BF16)
        cqn = sb(s1, "cqn", [128, 512], BF16); ckvn = sb(s1, "ckvn", [128, 256], BF16)
        cqnT = sb(s1, "cqnT", [128, 4, 128], BF16); ckvnT = sb(s1, "ckvnT", [128, 2, 128], BF16)
        kr = sb(s1, "kr", [128, 64]); krr = sb(s1, "krr", [128, 64])
        qraw = sb(s1, "qraw", [128, 4, 192]); kvraw = sb(s1, "kvraw", [128, 4, 256])
        sq = sb(s1, "sq", [128, 1024]); sm = sb(s1, "sm", [128, 32])
        posi = sb(s1, "posi", [128, 1], I32); posf = sb(s1, "posf", [128, 1])
        ang = sb(s1, "ang", [128, 64]); angk = sb(s1, "angk", [128, 64]); angi = sb(s1, "angi", [128, 64], I32)
        cs = sb(s1, "cs", [128, 64])
        rt = sb(s1, "rt", [128, 4, 32, 4])
        qbn = sb(s1, "qbn", [128, 4, 128], BF16); qbr = sb(s1, "qbr", [128, 4, 64], BF16)
        kbn = sb(s1, "kbn", [128, 4, 128], BF16); kbr = sb(s1, "kbr", [128, 4, 64], BF16)
        QTn = sb(s1, "QTn", [128, 4, 128], BF16); QTr = sb(s1, "QTr", [128, 2, 128], BF16)
        QTnB = sb(s1, "QTnB", [128, 4, 128], BF16); QTrB = sb(s1, "QTrB", [128, 2, 128], BF16)
        QTn2 = [QTn, QTnB]; QTr2 = [QTr, QTrB]
        PT = [sb(s1, f"PT{i}", [128, 512], BF16) for i in range(2)]
        rden = sb(s1, "rden", [128, 128])

        def rope(src3, dst3, nh, rk, wk):
            cosb = bcast(cs[:, 0:32].unsqueeze(1), [128, nh, 32])
            sinb = bcast(cs[:, 32:64].unsqueeze(1), [128, nh, 32])
            x1, x2 = src3[:, :, 0:32], src3[:, :, 32:64]
            t = [rt[:, 0:nh, :, j] for j in range(4)]
            P.op("dve", lambda e: e.tensor_tensor(out=t[0], in0=x1, in1=cosb, op=ALU.mult), reads=rk + ["cs"], writes=["rt0"])
            P.op("dve", lambda e: e.tensor_tensor(out=t[1], in0=x2, in1=sinb, op=ALU.mult), reads=rk + ["cs"], writes=["rt1"])
            P.op("dve", lambda e: e.tensor_tensor(out=t[2], in0=x2, in1=cosb, op=ALU.mult), reads=rk + ["cs"], writes=["rt2"])
            P.op("dve", lambda e: e.tensor_tensor(out=t[3], in0=x1, in1=sinb, op=ALU.mult), reads=rk + ["cs"], writes=["rt3"])
            P.op("dve", lambda e: e.tensor_tensor(out=dst3[:, :, 0:32], in0=t[0], in1=t[1], op=ALU.subtract),
                 reads=["rt0", "rt1"], writes=[wk + "a"])
            P.op("dve", lambda e: e.tensor_tensor(out=dst3[:, :, 32:64], in0=t[2], in1=t[3], op=ALU.add),
                 reads=["rt2", "rt3"], writes=[wk + "b"])

        if stop_after == "mw":
            o_ = dbg_out("d_qag", [128, 512], F32); o2_ = dbg_out("d_wuq", [128, 4, 1536], BF16)
            P.dma("sp", lambda e: e.dma_start(out=o_, in_=qag[:]), reads=["qag"], writes=["d_qag"])
            P.dma("sp", lambda e: e.dma_start(out=o2_, in_=wuq[:]), reads=wuq_k, writes=["d_wuq"])
            return finish(["d_qag", "d_wuq"])
        def mla_front(G, i):
            if True:
                qp = (G * nblk + i) % 2
                QTn = QTn2[qp]; QTr = QTr2[qp]
                make_hT(x_d, i, scp1, sh1)
                emit_conv(2)
                P.dma("sp", lambda e, i=i: e.dma_start(out=posi[:], in_=pos_d[i * 128:(i + 1) * 128, :]), writes=["posi"])
                P.op("dve", lambda e: e.tensor_copy(out=posf[:], in_=posi[:]), reads=["posi"], writes=["posf"])
                P.op("dve", lambda e: e.tensor_scalar(out=ang[:, 0:32], in0=invf[:], scalar1=posf[:, 0:1], scalar2=None, op0=ALU.mult),
                     reads=["posf", "invf"], writes=["ang0"])
                P.op("dve", lambda e: e.tensor_scalar(out=ang[:, 32:64], in0=ang[:, 0:32], scalar1=float(np.pi / 2), scalar2=None, op0=ALU.add),
                     reads=["ang0"], writes=["ang1"])
                P.op("dve", lambda e: e.tensor_scalar(out=angk[:], in0=ang[:], scalar1=float(1.0 / (2 * np.pi)), scalar2=None, op0=ALU.mult),
                     reads=["ang0", "ang1"], writes=["angk"])
                P.op("dve", lambda e: e.tensor_copy(out=angi[:], in_=angk[:]), reads=["angk"], writes=["angi"])
                P.op("dve", lambda e: e.tensor_copy(out=angk[:], in_=angi[:]), reads=["angi"], writes=["angk2"])
                P.op("dve", lambda e: e.scalar_tensor_tensor(out=ang[:], in0=angk[:], scalar=float(-2 * np.pi), in1=ang[:],
                                                             op0=ALU.mult, op1=ALU.add), reads=["angk2", "ang0", "ang1"], writes=["angr"])
                P.op("dve", lambda e: e.tensor_scalar(out=ang[:], in0=ang[:], scalar1=-3.1415925, scalar2=3.1415925, op0=ALU.max, op1=ALU.min),
                     reads=["angr"], writes=["angc"])
                P.op("act", lambda e: e.activation(out=cs[:, 0:32], in_=ang[:, 32:64], func=AF.Sin), reads=["angc"], writes=["cs_c"])
                P.op("act", lambda e: e.activation(out=cs[:, 32:64], in_=ang[:, 0:32], func=AF.Sin), reads=["angc", "cs_c"], writes=["cs"])

                def mmP(e):
                    for k in range(16):
                        e.matmul(pf[0][:, :], lhsT=hT[:, k, :], rhs=win_a[:, k, 0:512], start=(k == 0), stop=(k == 15))
                    for k in range(16):
                        r = e.matmul(pf[1][:, 0:320], lhsT=hT[:, k, :], rhs=win_a[:, k, 512:832], start=(k == 0), stop=(k == 15))
                    return r
                P.op("pe", mmP, reads=hT_keys + win_a_k, writes=["pf0", "pf1"])
                P.op("act", lambda e: e.activation(out=sq[:, 0:512], in_=pf[0][:, :], func=AF.Square, accum_out=sm[:, 0:1]),
                     reads=["pf0"], writes=["sq", "sm0"])
                rstd_of(sm[:, 0:1], 512, sm[:, 2:3], ["sm0"], "sm2", sm[:, 1:2], "sm1")
                P.op("dve", lambda e: e.scalar_tensor_tensor(out=cqn[:], in0=pf[0][:, :], scalar=sm[:, 2:3], in1=qag[:],
                                                             op0=ALU.mult, op1=ALU.mult), reads=["pf0", "sm2", "qag"], writes=["cqn"])
                P.op("act", lambda e: e.activation(out=sq[:, 0:256], in_=pf[1][:, 0:256], func=AF.Square, accum_out=sm[:, 3:4]),
                     reads=["pf1", "sm0"], writes=["sq", "sm3"])
                rstd_of(sm[:, 3:4], 256, sm[:, 5:6], ["sm3"], "sm5", sm[:, 4:5], "sm4")
                P.op("dve", lambda e: e.scalar_tensor_tensor(out=ckvn[:], in0=pf[1][:, 0:256], scalar=sm[:, 5:6], in1=kvag[:],
                                                             op0=ALU.mult, op1=ALU.mult), reads=["pf1", "sm5", "kvag"], writes=["ckvn"])
                P.op("act", lambda e: e.copy(out=kr[:], in_=pf[1][:, 256:320]), reads=["pf1"], writes=["kr"])
                P.op("act", lambda e: e.activation(out=sq[:, 0:64], in_=kr[:], func=AF.Square, accum_out=sm[:, 6:7]),
                     reads=["kr", "sm3"], writes=["sq", "sm6"])
                def trC(e):
                    for c in range(4):
                        e.transpose(pt[0][:, c * 128:(c + 1) * 128], cqn[:, c * 128:(c + 1) * 128], ident_b[:])
                    for c in range(2):
                        r = e.transpose(pt[0][:, (4 + c) * 128:(5 + c) * 128], ckvn[:, c * 128:(c + 1) * 128], ident_b[:])
                    return r
                P.op("pe", trC, reads=["cqn", "ckvn", "ident_b"], writes=["pt0"])
                P.op("dve", lambda e: e.tensor_copy(out=cqnT[:].rearrange("p c t -> p (c t)"), in_=pt[0][:, 0:512]),
                     reads=["pt0"], writes=["cqnT"])
                P.op("act", lambda e: e.copy(out=ckvnT[:].rearrange("p c t -> p (c t)"), in_=pt[0][:, 512:768]),
                     reads=["pt0"], writes=["ckvnT"])

                def mmQ(e, G=G):
                    for c in range(4):
                        e.matmul(pf[0][:, :], lhsT=cqnT[:, c, :], rhs=wuq[:, c, G * 768:G * 768 + 512], start=(c == 0), stop=(c == 3))
                    for c in range(4):
                        r = e.matmul(pf[1][:, 0:256], lhsT=cqnT[:, c, :], rhs=wuq[:, c, G * 768 + 512:G * 768 + 768], start=(c == 0), stop=(c == 3))
                    return r
                P.op("pe", mmQ, reads=["cqnT"] + wuq_k, writes=["pf0", "pf1"])
                qflat = qraw[:].rearrange("p h d -> p (h d)")
                P.op("act", lambda e: e.copy(out=qflat[:, 0:512], in_=pf[0][:, :]), reads=["pf0"], writes=["qraw0"])
                P.op("dve", lambda e: e.tensor_copy(out=qflat[:, 512:768], in_=pf[1][:, 0:256]), reads=["pf1"], writes=["qraw1"])
                P.op("act", lambda e: e.activation(out=sq[:, 0:768], in_=qflat, func=AF.Square),
                     reads=["qraw0", "qraw1", "sm6"], writes=["sq"])
                P.op("dve", lambda e: e.tensor_reduce(out=sm[:, 8:12], in_=sq[:, 0:768].rearrange("p (h d) -> p h d", h=4),
                                                      axis=AX.X, op=ALU.add), reads=["sq"], writes=["sm8"])
                rstd_of(sm[:, 8:12], 192, sm[:, 16:20], ["sm8"], "sm16", sm[:, 12:16], "sm12")
                P.op("dve", lambda e: e.tensor_tensor(out=qraw[:], in0=qraw[:], in1=bcast(sm[:, 16:20].unsqueeze(2), [128, 4, 192]), op=ALU.mult),
                     reads=["qraw0", "qraw1", "sm16"], writes=["qraw2"])
                P.op("dve", lambda e: e.tensor_tensor(out=qraw[:], in0=qraw[:], in1=bcast(qg[:].unsqueeze(1), [128, 4, 192]), op=ALU.mult),
                     reads=["qraw2", "qg"], writes=["qraw3"])
                P.op("act", lambda e: e.copy(out=qbn[:], in_=qraw[:, :, 0:128]), reads=["qraw3"], writes=["qbn"])
                rope(qraw[:, :, 128:192], qbr[:], 4, ["qraw3"], "qbr")

                def trQ(e):
                    for hh in range(4):
                        e.transpose(pt[1][:, hh * 128:(hh + 1) * 128], qbn[:, hh, :], ident_b[:])
                    qr2 = qbr[:].rearrange("p (a b) d -> p a (b d)", b=2)
                    for pp in range(2):
                        r = e.transpose(pt[1][:, (4 + pp) * 128:(5 + pp) * 128], qr2[:, pp, :], ident_b[:])
                    return r
                P.op("pe", trQ, reads=["qbn", "qbra", "qbrb", "ident_b"], writes=["pt1"])
                P.op("dve", lambda e: e.tensor_copy(out=QTn[:].rearrange("p c t -> p (c t)"), in_=pt[1][:, 0:512]), reads=["pt1"], writes=[f"QTn{qp}"])
                P.op("act", lambda e: e.copy(out=QTr[:].rearrange("p c t -> p (c t)"), in_=pt[1][:, 512:768]), reads=["pt1"], writes=[f"QTr{qp}"])

                def mmK(e, G=G):
                    for c in range(2):
                        e.matmul(pf[0][:, :], lhsT=ckvnT[:, c, :], rhs=wukv[:, c, G * 1024:G * 1024 + 512], start=(c == 0), stop=(c == 1))
                    for c in range(2):
                        r = e.matmul(pf[1][:, :], lhsT=ckvnT[:, c, :], rhs=wukv[:, c, G * 1024 + 512:G * 1024 + 1024], start=(c == 0), stop=(c == 1))
                    return r
                P.op("pe", mmK, reads=["ckvnT"] + wukv_k, writes=["pf0", "pf1"])
                kvflat = kvraw[:].rearrange("p h d -> p (h d)")
                P.op("act", lambda e: e.copy(out=kvflat[:, 0:512], in_=pf[0][:, :]), reads=["pf0"], writes=["kvraw0"])
                P.op("dve", lambda e: e.tensor_copy(out=kvflat[:, 512:1024], in_=pf[1][:, :]), reads=["pf1"], writes=["kvraw1"])
                P.op("act", lambda e, i=i: e.copy(out=Vc[:, i, :].rearrange("p (h d) -> p h d", h=4), in_=kvraw[:, :, 128:256]),
                     reads=["kvraw0", "kvraw1"], writes=[f"Vc{i}"])
                P.op("act", lambda e: e.activation(out=sq[:, 0:512].rearrange("p (h d) -> p h d", h=4), in_=kvraw[:, :, 0:128], func=AF.Square),
                     reads=["kvraw0", "kvraw1", "sm8"], writes=["sq"])
                P.op("dve", lambda e: e.tensor_reduce(out=sm[:, 20:24], in_=sq[:, 0:512].rearrange("p (h d) -> p h d", h=4),
                                                      axis=AX.X, op=ALU.add), reads=["sq"], writes=["sm20"])
                P.op("dve", lambda e: e.tensor_scalar(out=sm[:, 20:24], in0=sm[:, 20:24], scalar1=sm[:, 6:7], scalar2=None, op0=ALU.add),
                     reads=["sm20", "sm6"], writes=["sm20b"])
                rstd_of(sm[:, 20:24], 192, sm[:, 28:32], ["sm20b"], "sm28", sm[:, 24:28], "sm24")
                P.op("dve", lambda e: e.tensor_tensor(out=kvraw[:, :, 0:128], in0=kvraw[:, :, 0:128],
                                                      in1=bcast(sm[:, 28:32].unsqueeze(2), [128, 4, 128]), op=ALU.mult),
                     reads=["kvraw0", "kvraw1", "sm28"], writes=["kvraw2"])
                P.op("dve", lambda e: e.tensor_tensor(out=kbn[:], in0=kvraw[:, :, 0:128], in1=bcast(kg[:, 0:128].unsqueeze(1), [128, 4, 128]), op=ALU.mult),
                     reads=["kvraw2", "kg"], writes=["kbn"])
                P.op("dve", lambda e: e.tensor_tensor(out=kr[:], in0=kr[:], in1=kg[:, 128:192], op=ALU.mult), reads=["kr", "kg", "sm6"], writes=["krg"])
                rope(kr[:].unsqueeze(1), krr[:].unsqueeze(1), 1, ["krg"], "krr")
                P.op("dve", lambda e: e.tensor_tensor(out=kbr[:], in0=bcast(krr[:].unsqueeze(1), [128, 4, 64]),
                                                      in1=bcast(sm[:, 28:32].unsqueeze(2), [128, 4, 64]), op=ALU.mult),
                     reads=["krra", "krrb", "sm28"], writes=["kbr"])

                def trK(e):
                    for hh in range(4):
                        e.transpose(pt[0][:, hh * 128:(hh + 1) * 128], kbn[:, hh, :], ident_b[:])
                    kr2 = kbr[:].rearrange("p (a b) d -> p a (b d)", b=2)
                    for pp in range(2):
                        r = e.transpose(pt[0][:, (4 + pp) * 128:(5 + pp) * 128], kr2[:, pp, :], ident_b[:])
                    return r
                P.op("pe", trK, reads=["kbn", "kbr", "ident_b"], writes=["pt0"])
                P.op("dve", lambda e, i=i: e.tensor_copy(out=KTn[:, :, i * 128:(i + 1) * 128],
                                                         in_=pt[0][:, 0:512].rearrange("p (c t) -> p c t", c=4)), reads=["pt0"], writes=[f"KTn{i}"])
                P.op("act", lambda e, i=i: e.copy(out=KTr[:, :, i * 128:(i + 1) * 128],
                                                  in_=pt[0][:, 512:768].rearrange("p (c t) -> p c t", c=2)), reads=["pt0"], writes=[f"KTr{i}"])

        def mla_back(G, i):
            if True:
                qp = (G * nblk + i) % 2
                QTn = QTn2[qp]; QTr = QTr2[qp]
                for hh in range(4):
                    h = G * 4 + hh
                    pp, hf = hh // 2, hh % 2
                    ngrp = (i + 4) // 4
                    for g in range(ngrp):
                        j0, j1 = g * 4, min(i + 1, g * 4 + 4)
                        sbank = 4 + (g % 2)
                        pbuf = PT[g % 2]

                        def mmS(e, j0=j0, j1=j1, sbank=sbank, hh=hh, pp=pp, hf=hf):
                            for j in range(j0, j1):
                                o = pf[sbank][:, (j - j0) * 128:(j - j0 + 1) * 128]
                                e.matmul(o, lhsT=KTn[:, hh, j * 128:(j + 1) * 128], rhs=QTn[:, hh, :], start=True, stop=False)
                                r = e.matmul(o, lhsT=KTr[hf * 64:(hf + 1) * 64, pp, j * 128:(j + 1) * 128],
                                             rhs=QTr[hf * 64:(hf + 1) * 64, pp, :], start=False, stop=True)
                            return r
                        P.op("pe", mmS, reads=[f"QTn{qp}", f"QTr{qp}"] + [f"KTn{j}" for j in range(j0, j1)] + [f"KTr{j}" for j in range(j0, j1)],
                             writes=[f"pf{sbank}"])
                        w = (j1 - j0) * 128
                        P.op("act", lambda e, sbank=sbank, pbuf=pbuf, w=w: e.activation(out=pbuf[:, 0:w], in_=pf[sbank][:, 0:w], func=AF.Exp,
                                                                                     scale=float(192 ** -0.5)),
                             reads=[f"pf{sbank}"], writes=[f"PT{g % 2}"])
                        if j1 == i + 1:
                            dcol = (i - j0) * 128
                            P.op("dve", lambda e, pbuf=pbuf, dcol=dcol: e.tensor_tensor(out=pbuf[:, dcol:dcol + 128], in0=pbuf[:, dcol:dcol + 128],
                                                                                      in1=tri_b[:], op=ALU.mult),
                                 reads=[f"PT{g % 2}", "tri_b"], writes=[f"PT{g % 2}"])

                        def mmO(e, j0=j0, j1=j1, pbuf=pbuf, hh=hh, i=i):
                            for j in range(j0, j1):
                                pj = pbuf[:, (j - j0) * 128:(j - j0 + 1) * 128]
                                e.matmul(pf[2][:, 0:128], lhsT=Vc[:, j, hh * 128:(hh + 1) * 128], rhs=pj, start=(j == 0), stop=(j == i))
                                r = e.matmul(pf[3][:, 0:128], lhsT=ones_b[:], rhs=pj, start=(j == 0), stop=(j == i))
                            return r
                        P.op("pe", mmO, reads=[f"PT{g % 2}", "ones_b"] + [f"Vc{j}" for j in range(j0, j1)], writes=["pf2", "pf3"])
                    P.op("dve", lambda e: e.reciprocal(out=rden[:], in_=pf[3][:, 0:128]), reads=["pf3"], writes=["rden"])
                    P.op("dve", lambda e, h=h, i=i: e.tensor_tensor(out=catT[:, h, i * 128:(i + 1) * 128], in0=pf[2][:, 0:128], in1=rden[:], op=ALU.mult),
                         reads=["pf2", "rden"], writes=[f"cat{h}_{i}"])

        for G in range(ngroups):
            mla_front(G, 0)
            for i in range(nblk):
                A_ = P.capture(mla_back, G, i)
                B_ = P.capture(mla_front, G, i + 1) if i + 1 < nblk else []
                P.ops.extend(P.merge(A_, B_))
    BAR()
    with ExitStack() as s1:
        aog = sb(s1, "aog", [128, 8])
        with nc.allow_non_contiguous_dma(reason="tiny transposed loads"):
            P.dma("sp", lambda e: e.dma_start(out=aog[:], in_=attn_out_norm.rearrange("(j p) -> p j", p=128), allow_slow_non_contiguous=True), writes=["aog"])
        sqb = sb(s1, "sqb", [128, 8, 512], BF16); rnb = sb(s1, "rnb", [128, 512]); rnb2 = sb(s1, "rnb2", [128, 512])
        for tb in range((nblk + 3) // 4):
            wd = min(512, nblk * 128 - tb * 512)
            cols = slice(tb * 512, tb * 512 + wd)
            blks = range(tb * 4, min(nblk, tb * 4 + 4))
            ck = [f"cat{h}_{i}" for h in range(8) for i in blks]
            P.op("act", lambda e, cols=cols, wd=wd: e.activation(out=sqb[:, :, 0:wd], in_=catT[:, 0:8, cols], func=AF.Square), reads=ck, writes=["sqb"])

            def mmN(e, wd=wd):
                for h in range(8):
                    r = e.matmul(pf[0][:, 0:wd], lhsT=ones_b[:], rhs=sqb[:, h, 0:wd], start=(h == 0), stop=(h == 7))
                return r
            P.op("pe", mmN, reads=["sqb", "ones_b"], writes=["pf0"])
            P.op("act", lambda e, wd=wd: e.activation(out=rnb[:, 0:wd], in_=pf[0][:, 0:wd], func=AF.Sqrt, scale=1.0 / 1024, bias=EPS), reads=["pf0"], writes=["rnb"])
            P.op("dve", lambda e, wd=wd: e.reciprocal(out=rnb2[:, 0:wd], in_=rnb[:, 0:wd]), reads=["rnb"], writes=["rnb2"])
            for h in range(8):
                P.op("dve", lambda e, h=h, cols=cols, wd=wd: e.scalar_tensor_tensor(out=catT[:, h, cols], in0=catT[:, h, cols], scalar=aog[:, h:h + 1],
                                                                                  in1=rnb2[:, 0:wd], op0=ALU.mult, op1=ALU.mult),
                     reads=["rnb2", "aog"] + [f"cat{h}_{i}" for i in blks],
                     writes=[f"cat{h}_{i}" for i in blks])
    BAR()
    if stop_after == "mla":
        o1 = dbg_out("d_catA", [128, 8, S], BF16)
        P.dma("sp", lambda e: e.dma_start(out=o1[:, :, 0:nblk * 128], in_=catT[:, 0:8, 0:nblk * 128]), reads=[f"cat{h}_{i}" for h in range(8) for i in range(nblk)], writes=["o1"])
        return finish(["o1"])
    with ExitStack() as s2:
        NWX = 3
        wxb = [sb(s2, f"wxb{q}", [128, 16, 128], BF16) for q in range(NWX)]
        wzb2 = [sb(s2, f"wzb{q}", [128, 16, 512], BF16) for q in range(2)]
        win_d, win_d_k = load_w(s2, "win_d", w_in[:, 3392:3408], 16, 16)
        wcnt = {"n": 0}
        cw = sb(s2, "cw", [128, 12, 4]); cb = sb(s2, "cb", [128, 12])
        with nc.allow_non_contiguous_dma(reason="tiny transposed loads"):
            for k in range(4):
                P.dma("sp", lambda e, k=k: e.dma_start(out=cw[:, :, k], in_=conv_w[k, :].rearrange("(j p) -> p j", p=128), allow_slow_non_contiguous=True),
                      writes=[f"cw{k}"])
            P.dma("sp", lambda e: e.dma_start(out=cb[:], in_=conv_b.rearrange("(j p) -> p j", p=128), allow_slow_non_contiguous=True), writes=["cb"])
        cwk = [f"cw{k}" for k in range(4)]
        dtb = row_tile(s2, "dtb", dt_bias, 16); arow = row_tile(s2, "arow", a_log, 16)
        dsk = row_tile(s2, "dsk", d_skip, 16); sng = row_tile(s2, "sng", ssd_norm, 1024)
        P.op("act", lambda e: e.activation(out=arow[:], in_=arow[:], func=AF.Exp), reads=["arow"], writes=["arow_e"])
        P.op("dve", lambda e: e.tensor_scalar(out=arow[:], in0=arow[:], scalar1=-1.0, scalar2=None, op0=ALU.mult), reads=["arow_e"], writes=["arow_n"])
        xraw = sb(s2, "xraw", [128, 12, 131]); xacc = sb(s2, "xacc", [128, 12, 128])
        xcT = sb(s2, "xcT", [128, 12, 128], BF16)
        xs = sb(s2, "xs", [128, 16, 64]); xd = sb(s2, "xd", [128, 16, 64], BF16); xdd = sb(s2, "xdd", [128, 16, 64], BF16)
        zs = sb(s2, "zs", [128, 1024]); dts = sb(s2, "dts", [128, 16]); adt = sb(s2, "adt", [128, 16])
        ahi = sb(s2, "ahi", [128, 16], BF16); alo = sb(s2, "alo", [128, 16], BF16); ahf = sb(s2, "ahf", [128, 16])
        Rhi = sb(s2, "Rhi", [128, 16, 128], BF16); Rlo = sb(s2, "Rlo", [128, 16, 128], BF16)
        acs = sb(s2, "acs", [128, 16]); alast = sb(s2, "alast", [128, 16]); ea = sb(s2, "ea", [128, 16])
        dsd = sb(s2, "dsd", [128, 16]); cdec = sb(s2, "cdec", [128, 16])
        dif = sb(s2, "dif", [128, 4, 128]); Ed = sb(s2, "Ed", [128, 4, 128])
        MT = sb(s2, "MT", [128, 16, 128], BF16); CBT = sb(s2, "CBT", [128, 2, 128])
        Btok = sb(s2, "Btok", [128, 2, 128], BF16)
        Sst = sb(s2, "Sst", [128, 16, 64]); Sbf = sb(s2, "Sbf", [128, 16, 64], BF16)
        yv = sb(s2, "yv", [128, 16, 64]); ytmp = sb(s2, "ytmp", [128, 16, 64]); ybf = sb(s2, "ybf", [128, 1024], BF16)
        ysm = sb(s2, "ysm", [128, 8])
        P.op("dve", lambda e: e.memset(Sst[:], 0.0), writes=["Sst"])
        P.op("dve", lambda e: e.memset(Sbf[:], 0.0), writes=["Sbf"])
        xrawB = sb(s2, "xrawB", [128, 12, 131]); zsB = sb(s2, "zsB", [128, 1024]); dtsB = sb(s2, "dtsB", [128, 16])
        xraw2 = [xraw, xrawB]; zs2 = [zs, zsB]; dts2 = [dts, dtsB]
        P.op("dve", lambda e: e.memset(xraw[:, :, 0:3], 0.0), writes=["xhalo"])

        def ssd_front(i):
            p = i % 2
            xraw = xraw2[p]; xprev = xraw2[1 - p]; zs = zs2[p]; dts = dts2[p]
            make_hT(x_d, i, scp1, sh1)
            for j in range(12):
                bank = j % 2
                wq_ = wcnt["n"] % NWX
                wcnt["n"] += 1
                wx_ = wxb[wq_]
                P.dma("sp", lambda e, j=j, wx_=wx_: e.dma_start(
                    out=wx_[:], in_=win_bf[:, 1024 + j * 128:1024 + (j + 1) * 128].rearrange("(k p) n -> p k n", p=128)),
                    reads=["wintab"], writes=[f"wxb{wq_}"])

                def mmX(e, j=j, bank=bank, wx_=wx_):
                    for k in range(16):
                        r = e.matmul(pf[bank][:, 0:128], lhsT=wx_[:, k, :], rhs=hT[:, k, :], start=(k == 0), stop=(k == 15))
                    return r
                P.op("pe", mmX, reads=hT_keys + [f"wxb{wq_}"], writes=[f"pf{bank}"])
                P.op("act", lambda e, j=j, bank=bank: e.copy(out=xraw[:, j, 3:131], in_=pf[bank][:, 0:128]),
                     reads=[f"pf{bank}"], writes=[f"xraw{p}_{j}"])
                if i > 0:
                    P.op("act", lambda e, j=j: e.copy(out=xraw[:, j, 0:3], in_=xprev[:, j, 128:131]),
                         reads=[f"xraw{1 - p}_{j}", f"xraw{p}_{j}"], writes=[f"xraw{p}_{j}"])
            for half in range(2):
                wzb = wzb2[half]
                P.dma("sp", lambda e, half=half, wzb=wzb: e.dma_start(
                    out=wzb[:], in_=win_bf[:, half * 512:(half + 1) * 512].rearrange("(k p) n -> p k n", p=128)),
                    reads=["wintab"], writes=[f"wzb{half}"])

                def mmZ(e, half=half, wzb=wzb):
                    for k in range(16):
                        r = e.matmul(pf[half][:, :], lhsT=hT[:, k, :], rhs=wzb[:, k, :], start=(k == 0), stop=(k == 15))
                    return r
                P.op("pe", mmZ, reads=hT_keys + [f"wzb{half}"], writes=[f"pf{half}"])

            P.op("act", lambda e: e.activation(out=zs[:, 0:512], in_=pf[0][:, :], func=AF.Silu), reads=["pf0"], writes=[f"zs{p}_0"])
            P.op("act", lambda e: e.activation(out=zs[:, 512:1024], in_=pf[1][:, :], func=AF.Silu), reads=["pf1"], writes=[f"zs{p}_1"])

            def mmD(e):
                for k in range(16):
                    r = e.matmul(pf[0][:, 0:16], lhsT=hT[:, k, :], rhs=win_d[:, k, :], start=(k == 0), stop=(k == 15))
                return r
            P.op("pe", mmD, reads=hT_keys + win_d_k, writes=["pf0"])
            P.op("dve", lambda e: e.tensor_tensor(out=dts[:], in0=pf[0][:, 0:16], in1=dtb[:], op=ALU.add), reads=["pf0", "dtb"], writes=[f"dts{p}a"])
            P.op("act", lambda e: e.activation(out=dts[:], in_=dts[:], func=AF.Exp), reads=[f"dts{p}a"], writes=[f"dts{p}b"])
            P.op("act", lambda e: e.activation(out=dts[:], in_=dts[:], func=AF.Ln, bias=1.0, scale=1.0), reads=[f"dts{p}b"], writes=[f"dts{p}"])
        def ssd_back(i):
            p = i % 2
            xraw = xraw2[p]; zs = zs2[p]; dts = dts2[p]
            for j in range(12):
                eng = "dve"
                P.op(eng, lambda e, j=j: e.tensor_scalar(out=xacc[:, j, :], in0=xraw[:, j, 0:128], scalar1=cw[:, j, 0:1], scalar2=cb[:, j:j + 1],
                                                         op0=ALU.mult, op1=ALU.add), reads=[f"xraw{p}_{j}", "cb", "xhalo"] + cwk, writes=[f"xacc{j}"])
                for k in range(1, 4):
                    P.op(eng, lambda e, j=j, k=k: e.scalar_tensor_tensor(out=xacc[:, j, :], in0=xraw[:, j, k:k + 128], scalar=cw[:, j, k:k + 1],
                                                                         in1=xacc[:, j, :], op0=ALU.mult, op1=ALU.add),
                         reads=[f"xraw{p}_{j}", f"xacc{j}"] + cwk, writes=[f"xacc{j}"])
                P.op("act", lambda e, j=j: e.activation(out=xcT[:, j, :], in_=xacc[:, j, :], func=AF.Silu), reads=[f"xacc{j}"], writes=[f"xcT{j}"])
            P.op("dve", lambda e: e.tensor_tensor(out=adt[:], in0=dts[:], in1=arow[:], op=ALU.mult), reads=[f"dts{p}", "arow_n"], writes=["adt"])
            P.op("dve", lambda e: e.tensor_copy(out=ahi[:], in_=adt[:]), reads=["adt"], writes=["ahi"])
            P.op("dve", lambda e: e.tensor_copy(out=ahf[:], in_=ahi[:]), reads=["ahi"], writes=["ahf"])
            P.op("dve", lambda e: e.tensor_tensor(out=alo[:], in0=adt[:], in1=ahf[:], op=ALU.subtract), reads=["adt", "ahf"], writes=["alo"])
            trib = bcast(tri_b[:].unsqueeze(1), [128, 16, 128])
            P.op("dve", lambda e: e.tensor_tensor(out=Rhi[:], in0=trib, in1=bcast(ahi[:].unsqueeze(2), [128, 16, 128]), op=ALU.mult),
                 reads=["ahi", "tri_b"], writes=["Rhi"])
            P.op("pool", lambda e: e.tensor_tensor(out=Rlo[:], in0=trib, in1=bcast(alo[:].unsqueeze(2), [128, 16, 128]), op=ALU.mult),
                 reads=["alo", "tri_b"], writes=["Rlo"])
            def mmC(e):
                e.matmul(pf[4][:, 16:32], lhsT=tri_b[:], rhs=ahi[:], start=True, stop=False)
                return e.matmul(pf[4][:, 16:32], lhsT=tri_b[:], rhs=alo[:], start=False, stop=True)
            P.op("pe", mmC, reads=["tri_b", "ahi", "alo"], writes=["pf4"])
            P.op("dve", lambda e: e.tensor_copy(out=acs[:], in_=pf[4][:, 16:32]), reads=["pf4"], writes=["acs"])
            P.op("act", lambda e: e.activation(out=ea[:], in_=acs[:], func=AF.Exp), reads=["acs"], writes=["ea"])
            def trX(e):
                for c in range(8):
                    r = e.transpose(pt[0][:, c * 128:(c + 1) * 128], xcT[:, c, :], ident_b[:])
                return r
            P.op("pe", trX, reads=[f"xcT{c}" for c in range(8)] + ["ident_b"], writes=["pt0"])
            P.op("act", lambda e: e.copy(out=xs[:].rearrange("p h d -> p (h d)"), in_=pt[0][:, :]), reads=["pt0"], writes=["xs"])
            P.op("dve", lambda e: e.tensor_tensor(out=xd[:], in0=xs[:], in1=bcast(dts[:].unsqueeze(2), [128, 16, 64]), op=ALU.mult),
                 reads=["xs", f"dts{p}"], writes=["xd"])
            def trB(e):
                for g in range(2):
                    r = e.transpose(pt[1][:, g * 128:(g + 1) * 128], xcT[:, 8 + g, :], ident_b[:])
                return r
            P.op("pe", trB, reads=["xcT8", "xcT9", "ident_b"], writes=["pt1"])
            P.op("act", lambda e: e.copy(out=Btok[:].rearrange("p g n -> p (g n)"), in_=pt[1][:, 0:256]), reads=["pt1"], writes=["Btok"])
            def mmCB(e):
                for g in range(2):
                    r = e.matmul(pf[5][:, g * 128:(g + 1) * 128], lhsT=xcT[:, 8 + g, :], rhs=xcT[:, 10 + g, :], start=True, stop=True)
                return r
            P.op("pe", mmCB, reads=["xcT8", "xcT9", "xcT10", "xcT11"], writes=["pf5"])
            P.op("act", lambda e: e.copy(out=CBT[:].rearrange("p g n -> p (g n)"), in_=pf[5][:, 0:256]), reads=["pf5"], writes=["CBT"])
            for q4 in range(4):
                bank = 2 + q4 % 2

                def mmBC(e, q4=q4, bank=bank):
                    e.matmul(pf[bank][:, :], lhsT=ones_b[:], rhs=Rhi[:, q4 * 4:(q4 + 1) * 4, :].rearrange("p h l -> p (h l)"), start=True, stop=False)
                    return e.matmul(pf[bank][:, :], lhsT=ones_b[:], rhs=Rlo[:, q4 * 4:(q4 + 1) * 4, :].rearrange("p h l -> p (h l)"), start=False, stop=True)
                P.op("pe", mmBC, reads=["ones_b", "Rhi", "Rlo"], writes=[f"pf{bank}"])
                pv = pf[bank][:, :].rearrange("p (h l) -> p h l", h=4)
                P.op("dve", lambda e, pv=pv, q4=q4: e.tensor_copy(out=alast[:, q4 * 4:(q4 + 1) * 4], in_=pv[:, :, 127]), reads=[f"pf{bank}"], writes=[f"alast{q4}"])
                P.op("dve", lambda e, pv=pv, q4=q4: e.tensor_tensor(out=dif[:], in0=pv, in1=bcast(acs[:, q4 * 4:(q4 + 1) * 4].unsqueeze(2), [128, 4, 128]),
                                                                   op=ALU.subtract), reads=[f"pf{bank}", "acs"], writes=["dif"])
                P.op("dve", lambda e: e.tensor_tensor(out=dif[:], in0=dif[:], in1=bcast(negm[:].unsqueeze(1), [128, 4, 128]), op=ALU.add),
                     reads=["dif", "negm"], writes=["dif2"])
                P.op("act", lambda e: e.activation(out=Ed[:], in_=dif[:], func=AF.Exp), reads=["dif2"], writes=["Ed"])
                P.op("dve", lambda e, q4=q4: e.tensor_tensor(out=MT[:, q4 * 4:(q4 + 1) * 4, :], in0=Ed[:],
                                                           in1=bcast(CBT[:, q4 // 2, :].unsqueeze(1), [128, 4, 128]), op=ALU.mult),
                     reads=["Ed", "CBT"], writes=[f"MT{q4}"])
            alk = [f"alast{q}" for q in range(4)]
            P.op("dve", lambda e: e.tensor_tensor(out=dsd[:], in0=alast[:], in1=acs[:], op=ALU.subtract), reads=alk + ["acs"], writes=["dsd0"])
            P.op("act", lambda e: e.activation(out=dsd[:], in_=dsd[:], func=AF.Exp), reads=["dsd0"], writes=["dsd"])
            P.op("act", lambda e: e.activation(out=cdec[:], in_=alast[:], func=AF.Exp), reads=alk, writes=["cdec"])
            P.op("dve", lambda e: e.tensor_tensor(out=xdd[:], in0=xd[:], in1=bcast(dsd[:].unsqueeze(2), [128, 16, 64]), op=ALU.mult),
                 reads=["xd", "dsd"], writes=["xdd"])
            def mmY(e):
                for hd in range(16):
                    r = e.matmul(pf[2 + hd // 8][:, (hd % 8) * 64:(hd % 8 + 1) * 64], lhsT=MT[:, hd, :], rhs=xd[:, hd, :], start=True, stop=True)
                return r
            P.op("pe", mmY, reads=[f"MT{q}" for q in range(4)] + ["xd"], writes=["pf2", "pf3"])

            def mmYo(e):
                for g in range(2):
                    r = e.matmul(pf[4 + g][:, :], lhsT=xcT[:, 10 + g, :], rhs=Sbf[:, g * 8:(g + 1) * 8, :].rearrange("p h d -> p (h d)"), start=True, stop=True)
                return r
            P.op("pe", mmYo, reads=["xcT10", "xcT11", "Sbf"], writes=["pf4", "pf5"])
            for g in range(2):
                hs = slice(g * 8, (g + 1) * 8)
                P.op("dve", lambda e, g=g, hs=hs: e.tensor_tensor(out=ytmp[:, hs, :], in0=pf[4 + g][:, :].rearrange("p (h d) -> p h d", h=8),
                                                                 in1=bcast(ea[:, hs].unsqueeze(2), [128, 8, 64]), op=ALU.mult),
                     reads=[f"pf{4 + g}", "ea"], writes=[f"ytmp{g}"])
                P.op("dve", lambda e, g=g, hs=hs: e.tensor_tensor(out=yv[:, hs, :], in0=pf[2 + g][:, :].rearrange("p (h d) -> p h d", h=8),
                                                                 in1=ytmp[:, hs, :], op=ALU.add), reads=[f"pf{2 + g}", f"ytmp{g}"], writes=[f"yv{g}"])
            def mmSt(e):
                for g in range(2):
                    r = e.matmul(pf[4 + g][:, :], lhsT=Btok[:, g, :], rhs=xdd[:, g * 8:(g + 1) * 8, :].rearrange("p h d -> p (h d)"), start=True, stop=True)
                return r
            P.op("pe", mmSt, reads=["Btok", "xdd"], writes=["pf4", "pf5"])
            P.op("dve", lambda e: e.tensor_tensor(out=Sst[:], in0=Sst[:], in1=bcast(cdec[:].unsqueeze(2), [128, 16, 64]), op=ALU.mult),
                 reads=["Sst", "cdec"], writes=["Sst"])
            for g in range(2):
                hs = slice(g * 8, (g + 1) * 8)
                P.op("dve", lambda e, g=g, hs=hs: e.tensor_tensor(out=Sst[:, hs, :], in0=pf[4 + g][:, :].rearrange("p (h d) -> p h d", h=8),
                                                                 in1=Sst[:, hs, :], op=ALU.add), reads=[f"pf{4 + g}", "Sst"], writes=["Sst"])
            P.op("act", lambda e: e.copy(out=Sbf[:], in_=Sst[:]), reads=["Sst"], writes=["Sbf"])
            P.op("dve", lambda e: e.tensor_tensor(out=ytmp[:], in0=xs[:], in1=bcast(dsk[:].unsqueeze(2), [128, 16, 64]), op=ALU.mult),
                 reads=["xs", "dsk", "yv0", "yv1"], writes=["ytmp0", "ytmp1"])
            P.op("dve", lambda e: e.tensor_tensor(out=yv[:], in0=yv[:], in1=ytmp[:], op=ALU.add), reads=["yv0", "yv1", "ytmp0", "ytmp1"], writes=["yv0", "yv1"])
            yflat = yv[:].rearrange("p h d -> p (h d)")
            P.op("dve", lambda e: e.tensor_tensor(out=yflat, in0=yflat, in1=zs[:], op=ALU.mult), reads=["yv0", "yv1", f"zs{p}_0", f"zs{p}_1"], writes=["yv0", "yv1"])
            tfl = ytmp[:].rearrange("p h d -> p (h d)")
            P.op("act", lambda e: e.activation(out=tfl, in_=yflat, func=AF.Square), reads=["yv0", "yv1"], writes=["ytmp0", "ytmp1"])
            P.op("dve", lambda e: e.tensor_reduce(out=ysm[:, 0:2], in_=tfl.rearrange("p (g d) -> p g d", g=2), axis=AX.X, op=ALU.add),
                 reads=["ytmp0", "ytmp1"], writes=["ysm0"])
            rstd_of(ysm[:, 0:2], 512, ysm[:, 4:6], ["ysm0"], "ysm4", ysm[:, 2:4], "ysm2")
            P.op("dve", lambda e: e.tensor_tensor(out=yflat.rearrange("p (g d) -> p g d", g=2), in0=yflat.rearrange("p (g d) -> p g d", g=2),
                                                  in1=bcast(ysm[:, 4:6].unsqueeze(2), [128, 2, 512]), op=ALU.mult),
                 reads=["yv0", "yv1", "ysm4"], writes=["yv0", "yv1"])
            P.op("dve", lambda e: e.tensor_tensor(out=ybf[:], in0=yflat, in1=sng[:], op=ALU.mult), reads=["yv0", "yv1", "sng"], writes=["ybf"])

            def trY(e):
                for c in range(8):
                    r = e.transpose(pt[1][:, c * 128:(c + 1) * 128], ybf[:, c * 128:(c + 1) * 128], ident_b[:])
                return r
            P.op("pe", trY, reads=["ybf", "ident_b"], writes=["pt1"])
            P.op("act", lambda e, i=i: e.copy(out=catT[:, 8:16, i * 128:(i + 1) * 128], in_=pt[1][:, :].rearrange("p (c t) -> p c t", c=8)),
                 reads=["pt1"], writes=[f"cat{h}_{i}" for h in range(8, 16)])

        ssd_front(0)
        for i in range(nblk):
            A_ = P.capture(ssd_back, i)
            B_ = P.capture(ssd_front, i + 1) if i + 1 < nblk else []
            P.ops.extend(P.merge(A_, B_))
    BAR()
    if stop_after == "ssd":
        o1 = dbg_out("d_catS", [128, 8, S], BF16)
        P.dma("sp", lambda e: e.dma_start(out=o1[:, :, 0:nblk * 128], in_=catT[:, 8:16, 0:nblk * 128]), reads=[f"cat{h}_{i}" for h in range(8, 16) for i in range(nblk)], writes=["o1"])
        return finish(["o1"])
    with ExitStack() as s3:
        g1row = row_tile(s3, "g1row", mod_d[2 * D:3 * D], D)
        P.ops[-1].reads = ("mod_d",)
        wo, wo_k = load_w(s3, "wo", w_out, 16, D)
        x1t = sb(s3, "x1t", [128, D])
        for i in range(nblk):
            s = cnt["x"] % 1
            cnt["x"] += 1
            t = xt[s]
            P.dma("sp", lambda e, t=t, i=i: e.dma_start(out=t[:], in_=x_d[i * 128:(i + 1) * 128, :]), writes=[f"xt{s}"])
            for nb in range(4):
                def mmM(e, nb=nb, i=i):
                    for c in range(16):
                        r = e.matmul(pf[nb][:, :], lhsT=catT[:, c, i * 128:(i + 1) * 128], rhs=wo[:, c, nb * 512:(nb + 1) * 512], start=(c == 0), stop=(c == 15))
                    return r
                P.op("pe", mmM, reads=[f"cat{c}_{i}" for c in range(16)] + wo_k, writes=[f"pf{nb}"])
                cs_ = slice(nb * 512, (nb + 1) * 512)
                P.op("dve", lambda e, nb=nb, cs_=cs_: e.tensor_tensor(out=x1t[:, cs_], in0=pf[nb][:, :], in1=g1row[:, cs_], op=ALU.mult),
                     reads=[f"pf{nb}", "g1row"], writes=[f"x1t{nb}"])
                P.op("dve", lambda e, nb=nb, cs_=cs_, t=t: e.tensor_tensor(out=x1t[:, cs_], in0=x1t[:, cs_], in1=t[:, cs_], op=ALU.add),
                     reads=[f"x1t{nb}", f"xt{s}"], writes=[f"x1t{nb}"])
            P.dma("sp", lambda e, i=i: e.dma_start(out=x1_d[i * 128:(i + 1) * 128, :], in_=x1t[:]), reads=[f"x1t{nb}" for nb in range(4)],
                  writes=[f"x1d{i}"], semkey=("x1st",))
    BAR()
    if stop_after == "x1":
        return finish([f"x1d{i}" for i in range(nblk)])
    scat.close()
    BAR()

    emit_conv(len(conv_pending))
    with ExitStack() as s4:
        wq, wq_k = load_w(s4, "wq", w_query, 16, D)
        g2row = row_tile(s4, "g2row", mod_d[5 * D:6 * D], D); P.ops[-1].reads = ("mod_d",)
        sc2row = row_tile(s4, "sc2row", mod_d[4 * D:5 * D], D); P.ops[-1].reads = ("mod_d",)
        sh2row = row_tile(s4, "sh2row", mod_d[3 * D:4 * D], D); P.ops[-1].reads = ("mod_d",)
        P.op("dve", lambda e: e.tensor_scalar(out=sc2row[:], in0=sc2row[:], scalar1=1.0, scalar2=None, op0=ALU.add), reads=["sc2row"], writes=["sc2row"])
        skT = sb(s4, "skT", [128, 16, 128], BF16)
        qT = sb(s4, "qT", [128, 16, 128], BF16)
        h2b2 = sb(s4, "h2b2", [128, D], BF16)
        XT1 = xt[0]; H2B = [xn, h2b2]
        sc = sb(s4, "sc", [128, 16, 128]); zp = sb(s4, "zp", [128, 128])
        tops = sb(s4, "tops", [128, 16, 16]); topi = sb(s4, "topi", [128, 16, 16])
        iot256 = sb(s4, "iot256", [128, 256])
        topiu = sb(s4, "topiu", [128, 16, 16], mybir.dt.uint32); bposu = sb(s4, "bposu", [128, 16], mybir.dt.uint32)
        bposf = sb(s4, "bposf", [128, 16])
        cands1 = sb(s4, "cands1", [128, 16, 16]); candi1 = sb(s4, "candi1", [128, 16, 16]); zc = sb(s4, "zc", [128, 256])
        bests = sb(s4, "bests", [128, 8, 16]); besti = sb(s4, "besti", [128, 8, 16])
        bidx3 = [sb(s4, f"bidx{q}", [128, 128], I32) for q in range(3)]
        gsm = sb(s4, "gsm", [128, 32])
        gate2 = [sb(s4, f"gate{q}", [128, 8, 16]) for q in range(2)]
        pre2 = [sb(s4, f"pre{q}", [128, 128]) for q in range(2)]
        actv2 = [sb(s4, f"actv{q}", [128, 128]) for q in range(2)]
        scr2 = sb(s4, "scr2", [128, D], BF16)
        dg = [sb(s4, f"dg{q}", [128, 128], BF16) for q in range(2)]
        prod = [sb(s4, f"prod{q}", [128, D], BF16) for q in range(2)]
        xr = sb(s4, "xr", [128, 1024])
        NG = 8
        gb = [sb(s4, f"gb{q}", [128, D], BF16) for q in range(NG)]
        acc = sb(s4, "acc", [128, D]); h2 = acc
        SCRK = ["scr2_0", "scr2_1", "scr2b_0", "scr2b_1"]
        skf = acc[:].rearrange("p (g d) -> p g d", g=16); skb = scr2[:].rearrange("p (g d) -> p g d", g=16)
        P.dma("sp", lambda e: e.dma_start(out=skf, in_=sub_keys.rearrange("g k d -> k g d")), writes=["acc"], semkey=("skf",))
        P.op("dve", lambda e: e.tensor_copy(out=skb, in_=skf), reads=["acc"], writes=SCRK)
        for half in range(2):
            def trSK(e, half=half):
                for g in range(8):
                    r = e.transpose(pt[half][:, g * 128:(g + 1) * 128], skb[:, half * 8 + g, :], ident_b[:])
                return r
            P.op("pe", trSK, reads=SCRK + ["ident_b"], writes=[f"pt{half}"])
            P.op("dve", lambda e, half=half: e.tensor_copy(out=skT[:, half * 8:(half + 1) * 8, :], in_=pt[half][:, :].rearrange("p (g k) -> p g k", g=8)),
                 reads=[f"pt{half}"], writes=[f"skT{half}"])
        def mmI(e):
            return e.matmul(pf[5][:, 0:128], lhsT=ones_b[:], rhs=tri_b[:], start=True, stop=True)
        P.op("pe", mmI, reads=["ones_b", "tri_b"], writes=["pf5"])
        P.op("dve", lambda e: e.tensor_scalar(out=iot256[:, 0:128], in0=pf[5][:, 0:128], scalar1=-1.0, scalar2=None, op0=ALU.add), reads=["pf5"], writes=["iotA"])
        P.op("dve", lambda e: e.tensor_scalar(out=iot256[:, 128:256], in0=pf[5][:, 0:128], scalar1=127.0, scalar2=None, op0=ALU.add), reads=["pf5", "iotA"], writes=["iot256"])
        gcount = {"n": 0, "pr": 0}

        def prep_topk(i):
            p = i % 2
            t = XT1; hb = H2B[p]; bidx = bidx3[i % 3]; gate = gate2[p]
            P.dma("sp", lambda e: e.dma_start(out=t[:], in_=x1_d[i * 128:(i + 1) * 128, :]), reads=[f"x1d{i}"], writes=["XT"])
            P.op("act", lambda e: e.activation(out=junk[:], in_=t[:], func=AF.Square, accum_out=st4[:, 0:1]), reads=["XT"], writes=["junk", "st_ss"])
            rstd_of(st4[:, 0:1], D, st4[:, 2:3], ["st_ss"], "st_r", st4[:, 1:2], "st_t")
            P.op("dve", lambda e: e.scalar_tensor_tensor(out=h2[:], in0=t[:], scalar=st4[:, 2:3], in1=sc2row[:], op0=ALU.mult, op1=ALU.mult),
                 reads=["XT", "st_r", "sc2row"], writes=["acc"])
            P.op("dve", lambda e: e.tensor_tensor(out=hb[:], in0=h2[:], in1=sh2row[:], op=ALU.add), reads=["acc", "sh2row"], writes=[f"H2B{p}"])
            for half in range(2):
                def trs(e, half=half):
                    for k in range(8):
                        kk = half * 8 + k
                        r = e.transpose(pt[half][:, k * 128:(k + 1) * 128], hb[:, kk * 128:(kk + 1) * 128], ident_b[:])
                    return r
                P.op("pe", trs, reads=[f"H2B{p}", "ident_b"], writes=[f"pt{half}"])
                P.op("act", lambda e, half=half: e.copy(out=hT[:, half * 8:(half + 1) * 8, :], in_=pt[half][:, :].rearrange("p (c t) -> p c t", c=8)),
                     reads=[f"pt{half}"], writes=[f"hT{kk}" for kk in range(half * 8, half * 8 + 8)])
            for c4 in range(4):
                for cc in range(4):
                    def mmQ2(e, c4=c4, cc=cc):
                        ch = c4 * 4 + cc
                        for k in range(16):
                            r = e.matmul(pf[4][:, cc * 128:(cc + 1) * 128], lhsT=wq[:, k, ch * 128:(ch + 1) * 128], rhs=hT[:, k, :], start=(k == 0), stop=(k == 15))
                        return r
                    P.op("pe", mmQ2, reads=hT_keys + wq_k, writes=["pf4"])
                P.op("act", lambda e, c4=c4: e.copy(out=qT[:, c4 * 4:(c4 + 1) * 4, :], in_=pf[4][:, :].rearrange("p (c t) -> p c t", c=4)),
                     reads=["pf4"], writes=[f"qT{c4}"])

                def mmSc(e, c4=c4):
                    for cc in range(4):
                        ch = c4 * 4 + cc
                        r = e.matmul(pf[5][:, cc * 128:(cc + 1) * 128], lhsT=qT[:, ch, :], rhs=skT[:, ch, :], start=True, stop=True)
                    return r
                P.op("pe", mmSc, reads=[f"qT{c4}", "skT0", "skT1"], writes=["pf5"])
                P.op("act", lambda e, c4=c4: e.copy(out=sc[:, c4 * 4:(c4 + 1) * 4, :], in_=pf[5][:, :].rearrange("p (c t) -> p c t", c=4)),
                     reads=["pf5"], writes=[f"sc{c4}"])
            for g in range(16):
                sk_ = f"sc{g // 4}"
                P.op("dve", lambda e, g=g: e.max(out=tops[:, g, 0:8], in_=sc[:, g, :]), reads=[sk_], writes=["tA"])
                P.op("dve", lambda e, g=g: e.max_index(out=topiu[:, g, 0:8], in_max=tops[:, g, 0:8], in_values=sc[:, g, :]), reads=[sk_, "tA"], writes=["tiA"])
                P.op("dve", lambda e, g=g: e.match_replace(out=zp[:], in_to_replace=tops[:, g, 0:8], in_values=sc[:, g, :], imm_value=-1e30),
                     reads=[sk_, "tA"], writes=["zp"])
                P.op("dve", lambda e, g=g: e.max(out=tops[:, g, 8:16], in_=zp[:]), reads=["zp"], writes=["tB"])
                P.op("dve", lambda e, g=g: e.max_index(out=topiu[:, g, 8:16], in_max=tops[:, g, 8:16], in_values=zp[:]), reads=["zp", "tB"], writes=["tiB"])
            P.op("dve", lambda e: e.tensor_copy(out=topi[:], in_=topiu[:]), reads=["tiA", "tiB"], writes=["topi"])
            t4 = tops[:].rearrange("p (h j) k -> p h j k", j=2)
            i4 = topi[:].rearrange("p (h j) k -> p h j k", j=2)
            cf = cands1[:].rearrange("p a b -> p (a b)")
            cif = candi1[:].rearrange("p a b -> p (a b)")
            for h in range(8):
                P.op("dve", lambda e, h=h: e.tensor_tensor(out=cands1[:], in0=bcast(t4[:, h, 0, :].unsqueeze(2), [128, 16, 16]),
                                                           in1=bcast(t4[:, h, 1, :].unsqueeze(1), [128, 16, 16]), op=ALU.add),
                     reads=["tA", "tB"], writes=["cands"])
                P.op("dve", lambda e, h=h: e.scalar_tensor_tensor(out=candi1[:], in0=bcast(i4[:, h, 0, :].unsqueeze(2), [128, 16, 16]), scalar=128.0,
                                                                  in1=bcast(i4[:, h, 1, :].unsqueeze(1), [128, 16, 16]), op0=ALU.mult, op1=ALU.add),
                     reads=["topi"], writes=["candi"])
                P.op("dve", lambda e, h=h: e.max(out=bests[:, h, 0:8], in_=cf), reads=["cands"], writes=["bA"])
                P.op("dve", lambda e, h=h: e.max_index(out=bposu[:, 0:8], in_max=bests[:, h, 0:8], in_values=cf), reads=["cands", "bA"], writes=["bpA"])
                P.op("dve", lambda e, h=h: e.match_replace(out=zc[:], in_to_replace=bests[:, h, 0:8], in_values=cf, imm_value=-1e30),
                     reads=["cands", "bA"], writes=["zc"])
                P.op("dve", lambda e, h=h: e.max(out=bests[:, h, 8:16], in_=zc[:]), reads=["zc"], writes=["bB"])
                P.op("dve", lambda e, h=h: e.max_index(out=bposu[:, 8:16], in_max=bests[:, h, 8:16], in_values=zc[:]), reads=["zc", "bB"], writes=["bpB"])
                P.op("dve", lambda e: e.tensor_copy(out=bposf[:], in_=bposu[:]), reads=["bpA", "bpB"], writes=["bposf"])
                for k0 in (0, 8):
                    def idx2(e, h=h, k0=k0):
                        for k in range(k0, k0 + 8):
                            r = e.scalar_tensor_tensor(out=scr2[:, (k % 8) * 256:(k % 8 + 1) * 256], in0=iot256[:], scalar=bposf[:, k:k + 1], in1=cif,
                                                       op0=ALU.is_equal, op1=ALU.mult, accum_out=besti[:, h, k:k + 1])
                        return r
                    P.op("dve", idx2, reads=["candi", "bposf", "iot256"], writes=SCRK + [f"besti{h}"])
            bik = [f"besti{h}" for h in range(8)]
            P.op("dve", lambda e: e.tensor_copy(out=bidx[:], in_=besti[:].rearrange("p h k -> p (h k)")), reads=bik, writes=[f"bidx{i % 3}"])
            P.op("dve", lambda e: e.tensor_tensor(out=gate[:], in0=bests[:], in1=bcast(bests[:, :, 0:1], [128, 8, 16]), op=ALU.subtract),
                 reads=["bA", "bB"], writes=[f"gate{p}a"])
            P.op("act", lambda e: e.activation(out=gate[:], in_=gate[:], func=AF.Exp), reads=[f"gate{p}a"], writes=[f"gate{p}b"])
            P.op("dve", lambda e: e.tensor_reduce(out=gsm[:, 0:8], in_=gate[:], axis=AX.X, op=ALU.add), reads=[f"gate{p}b"], writes=["gsm0"])
            P.op("dve", lambda e: e.reciprocal(out=gsm[:, 8:16], in_=gsm[:, 0:8]), reads=["gsm0"], writes=["gsm8"])
            P.op("dve", lambda e: e.tensor_tensor(out=gate[:], in0=gate[:], in1=bcast(gsm[:, 8:16].unsqueeze(2), [128, 8, 16]), op=ALU.mult),
                 reads=[f"gate{p}b", "gsm8"], writes=[f"gate{p}"])

        def u_step(i, hk):
            p = i % 2
            hb = H2B[p]; bidx = bidx3[i % 3]; pre = pre2[p]
            b_ = gcount["n"] % NG
            gcount["n"] += 1
            q_ = gcount["pr"] % 2
            gcount["pr"] += 1
            P.dma("pool", lambda e: e.indirect_dma_start(
                out=gb[b_][:], out_offset=None, in_=u_bf, in_offset=bass.IndirectOffsetOnAxis(ap=bidx[:, hk:hk + 1], axis=0)),
                reads=[f"bidx{i % 3}", "utab"], writes=[f"gb{b_}"])
            P.op("dve", lambda e: e.tensor_tensor(out=prod[q_][:], in0=gb[b_][:], in1=hb[:], op=ALU.mult),
                 reads=[f"gb{b_}", f"H2B{p}"], writes=[f"prod{q_}"])
            P.op("act", lambda e: e.activation(out=prod[q_][:], in_=prod[q_][:], func=AF.Copy, accum_out=pre[:, hk:hk + 1]),
                 reads=[f"prod{q_}"], writes=[f"prod{q_}", f"pre{p}"])

        def u_end(i):
            p = i % 2
            pre = pre2[p]; actv = actv2[p]; gate = gate2[p]
            P.op("act", lambda e: e.activation(out=actv[:], in_=pre[:], func=AF.Gelu), reads=[f"pre{p}"], writes=[f"actv{p}a"])
            P.op("dve", lambda e: e.tensor_tensor(out=actv[:], in0=actv[:], in1=gate[:].rearrange("p h k -> p (h k)"), op=ALU.mult),
                 reads=[f"actv{p}a", f"gate{p}"], writes=[f"actv{p}"])

        def v_step(i, hk):
            p = i % 2
            bidx = bidx3[i % 3]; actv = actv2[p]
            b_ = gcount["n"] % NG
            gcount["n"] += 1
            dq = hk % 2
            P.dma("pool", lambda e: e.indirect_dma_start(
                out=gb[b_][:], out_offset=None, in_=v_bf, in_offset=bass.IndirectOffsetOnAxis(ap=bidx[:, hk:hk + 1], axis=0)),
                reads=[f"bidx{i % 3}", "vtab"], writes=[f"gb{b_}"])
            P.op("act", lambda e: e.activation(out=dg[dq][:], in_=ident_b[:], func=AF.Copy, scale=actv[:, hk:hk + 1]),
                 reads=[f"actv{p}", "ident_b"], writes=[f"dg{dq}"])

            def mmV(e):
                for nb in range(4):
                    r = e.matmul(pf[nb][:, :], lhsT=dg[dq][:], rhs=gb[b_][:, nb * 512:(nb + 1) * 512], start=(hk == 0), stop=(hk == 127))
                return r
            P.op("pe", mmV, reads=[f"dg{dq}", f"gb{b_}"], writes=["pf0", "pf1", "pf2", "pf3"])

        def final(i):
            for nb in range(4):
                cs_ = slice(nb * 512, (nb + 1) * 512)
                P.op("dve", lambda e, nb=nb, cs_=cs_: e.tensor_tensor(out=acc[:, cs_], in0=pf[nb][:, :], in1=g2row[:, cs_], op=ALU.mult),
                     reads=[f"pf{nb}", "g2row"], writes=["acc"])
            for hf in range(2):
                P.dma("sp", lambda e, hf=hf: e.dma_start(out=xr[:], in_=x1_d[i * 128:(i + 1) * 128, hf * 1024:(hf + 1) * 1024]), reads=[f"x1d{i}"], writes=["xr"])
                P.op("dve", lambda e, hf=hf: e.tensor_tensor(out=acc[:, hf * 1024:(hf + 1) * 1024], in0=acc[:, hf * 1024:(hf + 1) * 1024], in1=xr[:], op=ALU.add),
                     reads=["acc", "xr"], writes=["acc"])
            P.dma("sp", lambda e: e.dma_start(out=out_d[i * 128:(i + 1) * 128, :], in_=acc[:]), reads=["acc"], writes=[f"out{i}", "acc"],
                  semkey=("outst",))

        def capture(fn, *a):
            saved = P.ops
            P.ops = []
            fn(*a)
            got = P.ops
            P.ops = saved
            return got

        if stop_after in ("pdbg", "pdbg2"):
            return finish([])
        prep_topk(0)
        for i in range(nblk + 1):
            nxt = capture(prep_topk, i + 1) if i + 1 < nblk else []
            per = (len(nxt) + 127) // 128
            for hk in range(128):
                if i < nblk:
                    u_step(i, hk)
                if i >= 1:
                    v_step(i - 1, hk)
                P.ops.extend(nxt[hk * per:(hk + 1) * per])
            if i < nblk:
                u_end(i)
            if i >= 1:
                final(i - 1)
        return finish([f"out{i}" for i in range(nblk)])


_CACHE = {}


def _consts():
    ident = np.eye(128, dtype=np.float32)
    k = np.arange(128)
    tri = (k[None, :] >= k[:, None]).astype(np.float32)
    negmask = np.where(k[None, :] >= k[:, None], 0.0, -30000.0).astype(np.float32)
    invf = (1.0 / (10000.0 ** (np.arange(32, dtype=np.float32) * (2.0 / 64)))).astype(np.float32)
    invf = np.broadcast_to(invf[None, :], (128, 32)).copy()
    return ident, tri, negmask, invf


def kernel(**inputs):
    if "nc" not in _CACHE:
        _CACHE["nc"] = build()
    nc = _CACHE["nc"]
    ident, tri, negmask, invf = _consts()
    f = lambda a: np.ascontiguousarray(np.asarray(a))
    shared = {
        "w_ada": f(inputs["w_ada"][0]), "b_ada": f(inputs["b_ada"][0]).reshape(1, -1), "w_in": f(inputs["w_in"][0]),
        "q_a_norm": f(inputs["q_a_norm"][0]), "w_uq": f(inputs["w_uq"][0]), "kv_a_norm": f(inputs["kv_a_norm"][0]),
        "w_ukv": f(inputs["w_ukv"][0]), "q_norm": f(inputs["q_norm"][0]), "k_norm": f(inputs["k_norm"][0]),
        "attn_out_norm": f(inputs["attn_out_norm"][0]), "conv_w": f(inputs["conv_w"][0]), "conv_b": f(inputs["conv_b"][0]),
        "dt_bias": f(inputs["dt_bias"][0]), "a_log": f(inputs["a_log"][0]), "d_skip": f(inputs["d_skip"][0]),
        "ssd_norm": f(inputs["ssd_norm"][0]), "w_out": f(inputs["w_out"][0]), "w_query": f(inputs["w_query"][0]),
        "sub_keys": f(inputs["sub_keys"][0]).reshape(16, 128, 128), "u_experts": f(inputs["u_experts"][0]),
        "v_experts": f(inputs["v_experts"][0]), "ident": ident, "tri": tri, "negmask": negmask, "invfreq": invf,
    }
    x = np.asarray(inputs["x"]); c = np.asarray(inputs["c"]); pos = np.asarray(inputs["positions"])
    in_maps = []
    for b in range(8):
        m = dict(shared)
        m["x"] = f(x[b]); m["c"] = f(c[b]); m["pos"] = f(pos[b]).reshape(S, 1).astype(np.int32)
        in_maps.append(m)
    res = run_bass_kernel_spmd(nc, in_maps, core_ids=list(range(8)))
    return np.stack([np.asarray(r["out"]) for r in res.results], axis=0).astype(np.float32)
```
